# Optimizing a Trainium2 kernel written in Bass

```python
import jax, jax.numpy as jnp
from jax import lax
import numpy as np

D_MODEL = 2048
BATCH = 4
SEQ = 4096
DEPTH = 2

CTX_LEN = 256
GRID_W = 64
N_MIXERS = 2
N_ATTN = (DEPTH + 1) // 2
N_REC = DEPTH // 2
NORM_EPS = 1e-6
N_MOD = 6

HEAD_DIM = 128
N_Q_HEADS = D_MODEL // HEAD_DIM
N_KV_HEADS = N_Q_HEADS // 4
GQA_GROUP = N_Q_HEADS // N_KV_HEADS
WINDOW = 128
ATTN_BLOCK = 128
ROPE_BASE = 10000.0
Q_DIM = N_Q_HEADS * HEAD_DIM
KV_DIM = N_KV_HEADS * HEAD_DIM
QKV_DIM = Q_DIM + 2 * KV_DIM

D_RNN = D_MODEL
RNN_BLOCKS = 8
RNN_BLOCK_W = D_RNN // RNN_BLOCKS
CONV_W = 4
CONV_LEFT = 1
LRU_C = 8.0

PEER_HEADS = 8
PEER_KEY_DIM = 128
N_KEYS = 128
N_EXPERTS = N_KEYS * N_KEYS
PEER_TOPK = 16
PEER_CHUNK = 128

kernel_name = "hybrid_swa_rglru_peer_diffusion_block"


def rmsnorm(x, gain):
    x32 = x.astype(jnp.float32)
    y = x32 * lax.rsqrt(jnp.mean(x32 * x32, axis=-1, keepdims=True) + NORM_EPS)
    return y.astype(x.dtype) * gain


def modulate(h, shift, scale):
    return h * (1.0 + scale) + shift


def axial_rope(rows, dtype):
    t = jnp.arange(rows * GRID_W)
    row = (t // GRID_W).astype(jnp.float32)
    col = (t % GRID_W).astype(jnp.float32)
    half = HEAD_DIM // 2
    inv = ROPE_BASE ** (-jnp.arange(0, half, 2, dtype=jnp.float32) / half)
    ang_r = row[:, None] * inv[None, :]
    ang_c = col[:, None] * inv[None, :]
    ang = jnp.concatenate([ang_r, ang_r, ang_c, ang_c], axis=-1)
    return jnp.cos(ang).astype(dtype), jnp.sin(ang).astype(dtype)


def apply_rope(x, cos, sin):
    half = HEAD_DIM // 2
    qtr = half // 2

    def rot(z):
        return jnp.concatenate([-z[..., qtr:], z[..., :qtr]], axis=-1)

    rotated = jnp.concatenate([rot(x[..., :half]), rot(x[..., half:])], axis=-1)
    return x * cos[None, :, None, :] + rotated * sin[None, :, None, :]


def attn_mixer(hx, hc, cos, sin, w_qkv, w_o, sink, need_ctx_out):
    B, S, _ = hx.shape
    C = hc.shape[1]
    nb = S // ATTN_BLOCK
    dt = hx.dtype
    scale = HEAD_DIM ** -0.5

    def project(h):
        qkv = h @ w_qkv
        q, k, v = jnp.split(qkv, [Q_DIM, Q_DIM + KV_DIM], axis=-1)
        T = h.shape[1]
        return (q.reshape(B, T, N_Q_HEADS, HEAD_DIM), k.reshape(B, T, N_KV_HEADS, HEAD_DIM),
                v.reshape(B, T, N_KV_HEADS, HEAD_DIM))

    qx, kx, vx = project(hx)
    qc, kc, vc = project(hc)
    qx = apply_rope(qx, cos, sin).reshape(B, nb, ATTN_BLOCK, N_KV_HEADS, GQA_GROUP, HEAD_DIM)
    kx = apply_rope(kx, cos, sin)
    sink32 = sink.astype(jnp.float32).reshape(N_KV_HEADS, GQA_GROUP)

    def band(t):
        tp = jnp.pad(t, ((0, 0), (ATTN_BLOCK, ATTN_BLOCK), (0, 0), (0, 0)))
        tp = tp.reshape(B, nb + 2, ATTN_BLOCK, N_KV_HEADS, HEAD_DIM)
        return jnp.concatenate([tp[:, :-2], tp[:, 1:-1], tp[:, 2:]], axis=2)

    kb, vb = band(kx), band(vx)
    s_w = jnp.einsum('bnqhgd,bnkhd->bnhgqk', qx, kb).astype(jnp.float32) * scale
    blk = jnp.arange(nb)[:, None]
    qpos = blk * ATTN_BLOCK + jnp.arange(ATTN_BLOCK)[None, :]
    kpos = (blk - 1) * ATTN_BLOCK + jnp.arange(3 * ATTN_BLOCK)[None, :]
    valid = ((kpos[:, None, :] >= 0) & (kpos[:, None, :] < S)
             & (jnp.abs(qpos[:, :, None] - kpos[:, None, :]) <= WINDOW))
    s_w = jnp.where(valid[None, :, None, None], s_w, -jnp.inf)
    s_c = jnp.einsum('bnqhgd,bkhd->bnhgqk', qx, kc).astype(jnp.float32) * scale
    sink_b = sink32[None, None, :, :, None]
    m = jnp.maximum(jnp.maximum(s_w.max(-1), s_c.max(-1)), sink_b)
    p_w = jnp.exp(s_w - m[..., None])
    p_c = jnp.exp(s_c - m[..., None])
    denom = p_w.sum(-1) + p_c.sum(-1) + jnp.exp(sink_b - m)
    o = (jnp.einsum('bnhgqk,bnkhd->bnqhgd', p_w.astype(dt), vb)
         + jnp.einsum('bnhgqk,bkhd->bnqhgd', p_c.astype(dt), vc))
    o = (o / jnp.moveaxis(denom, -1, 2)[..., None]).astype(dt)
    out_x = o.reshape(B, S, Q_DIM) @ w_o

    out_c = None
    if need_ctx_out:
        qc = qc.reshape(B, C, N_KV_HEADS, GQA_GROUP, HEAD_DIM)
        s_cc = jnp.einsum('bqhgd,bkhd->bhgqk', qc, kc).astype(jnp.float32) * scale
        sink_col = jnp.broadcast_to(sink32[None, :, :, None, None], (B, N_KV_HEADS, GQA_GROUP, C, 1))
        p = jax.nn.softmax(jnp.concatenate([s_cc, sink_col], axis=-1), axis=-1)[..., :C]
        o_c = jnp.einsum('bhgqk,bkhd->bqhgd', p.astype(dt), vc)
        out_c = o_c.reshape(B, C, Q_DIM) @ w_o
    return out_x, out_c


def short_conv(u, w, b):
    T = u.shape[1]
    up = jnp.pad(u, ((0, 0), (CONV_LEFT, CONV_W - 1 - CONV_LEFT), (0, 0)))
    y = b
    for k in range(CONV_W):
        y = y + up[:, k:k + T] * w[k]
    return y


def block_diag(u, w, b):
    ub = u.reshape(u.shape[:-1] + (RNN_BLOCKS, RNN_BLOCK_W))
    return jnp.einsum('...ni,nio->...no', ub, w).reshape(u.shape) + b


def rglru_coeffs(u, w_a, b_a, w_x, b_x, lam):
    r = jax.nn.sigmoid(block_diag(u, w_a, b_a).astype(jnp.float32))
    i = jax.nn.sigmoid(block_diag(u, w_x, b_x).astype(jnp.float32))
    log_a = -LRU_C * r * jax.nn.softplus(-lam.astype(jnp.float32))
    a = jnp.exp(log_a)
    bx = jnp.sqrt(-jnp.expm1(2.0 * log_a)) * i * u.astype(jnp.float32)
    return a, bx


def bidir_scan(a_f, b_f, a_b, b_b, h0):
    A = jnp.stack([a_f, jnp.flip(a_b, 1)], axis=0)
    X = jnp.stack([b_f, jnp.flip(b_b, 1)], axis=0)

    def step(h, ab):
        a, bx = ab
        h = a * h + bx
        return h, h

    h_last, ys = lax.scan(step, h0, (jnp.moveaxis(A, 2, 0), jnp.moveaxis(X, 2, 0)))
    ys = jnp.moveaxis(ys, 0, 2)
    return ys[0] + jnp.flip(ys[1], 1), h_last


def rec_mixer(hx, hc, w_in, conv_w, conv_b, w_a, b_a, w_x, b_x, lam, w_out, need_ctx_out):
    B = hx.shape[0]
    dt = hx.dtype
    w_gate, w_u = w_in[:, :D_RNN], w_in[:, D_RNN:]

    def coeffs(h):
        u = short_conv(h @ w_u, conv_w, conv_b)
        fa, fb = rglru_coeffs(u, w_a[0], b_a[0], w_x[0], b_x[0], lam[0])
        ba, bb = rglru_coeffs(u, w_a[1], b_a[1], w_x[1], b_x[1], lam[1])
        return fa, fb, ba, bb

    h0 = jnp.zeros((2, B, D_RNN), jnp.float32)
    yc, hc_final = bidir_scan(*coeffs(hc), h0)
    yx, _ = bidir_scan(*coeffs(hx), hc_final)
    out_x = (yx.astype(dt) * jax.nn.gelu(hx @ w_gate)) @ w_out
    out_c = None
    if need_ctx_out:
        out_c = (yc.astype(dt) * jax.nn.gelu(hc @ w_gate)) @ w_out
    return out_x, out_c


def peer(h, w_q, keys, u_tab, v_tab):
    T, D = h.shape
    q = (h @ w_q).reshape(T, PEER_HEADS, 2, PEER_KEY_DIM)
    s = jnp.einsum('thpd,pnd->thpn', q, keys).astype(jnp.float32)
    s1, i1 = lax.top_k(s[:, :, 0], PEER_TOPK)
    s2, i2 = lax.top_k(s[:, :, 1], PEER_TOPK)
    cand = (s1[..., :, None] + s2[..., None, :]).reshape(T, PEER_HEADS, PEER_TOPK * PEER_TOPK)
    cidx = (i1[..., :, None] * N_KEYS + i2[..., None, :]).reshape(T, PEER_HEADS, PEER_TOPK * PEER_TOPK)
    top_s, sel = lax.top_k(cand, PEER_TOPK)
    idx = jnp.take_along_axis(cidx, sel, axis=-1).reshape(T, PEER_HEADS * PEER_TOPK)
    g = jax.nn.softmax(top_s, axis=-1).astype(h.dtype).reshape(T, PEER_HEADS * PEER_TOPK)
    n = T // PEER_CHUNK

    def chunk(args):
        hc, ic, gc = args
        z = jnp.einsum('tkd,td->tk', u_tab[ic], hc)
        act = jax.nn.gelu(z) * gc
        return jnp.einsum('tk,tkd->td', act, v_tab[ic])

    out = lax.map(chunk, (h.reshape(n, PEER_CHUNK, D), idx.reshape(n, PEER_CHUNK, -1),
                          g.reshape(n, PEER_CHUNK, -1)))
    return out.reshape(T, D)


def setup_inputs(seed: int = 0) -> dict:
    key = jax.random.key(seed)
    ks = jax.random.split(key, 32)
    f32 = jnp.float32

    def nrm(k, shape, s):
        return jax.random.normal(k, shape, f32) * s

    a0 = jax.random.uniform(ks[20], (N_REC, 2, D_RNN), f32, minval=0.9, maxval=0.999)
    return {
        "x": nrm(ks[0], (BATCH, SEQ, D_MODEL), 1.0),
        "c": nrm(ks[1], (BATCH, D_MODEL), 1.0),
        "ctx": nrm(ks[2], (BATCH, CTX_LEN, D_MODEL), 1.0),
        "c_ctx": nrm(ks[3], (D_MODEL,), 1.0),
        "w_mod": nrm(ks[4], (DEPTH, D_MODEL, N_MOD * D_MODEL), 0.3 * D_MODEL ** -0.5),
        "b_mod": nrm(ks[5], (DEPTH, N_MOD * D_MODEL), 0.02),
        "norm_mix": 1.0 + nrm(ks[6], (DEPTH, D_MODEL), 0.02),
        "norm_ffn": 1.0 + nrm(ks[7], (DEPTH, D_MODEL), 0.02),
        "norm_final": 1.0 + nrm(ks[8], (D_MODEL,), 0.02),
        "attn_w_qkv": nrm(ks[9], (N_ATTN, D_MODEL, QKV_DIM), D_MODEL ** -0.5),
        "attn_w_o": nrm(ks[10], (N_ATTN, Q_DIM, D_MODEL), Q_DIM ** -0.5),
        "attn_sink": nrm(ks[11], (N_ATTN, N_Q_HEADS), 0.5),
        "rec_w_in": nrm(ks[12], (N_REC, D_MODEL, 2 * D_RNN), D_MODEL ** -0.5),
        "rec_conv_w": nrm(ks[13], (N_REC, CONV_W, D_RNN), CONV_W ** -0.5),
        "rec_conv_b": nrm(ks[14], (N_REC, D_RNN), 0.02),
        "rec_w_a": nrm(ks[15], (N_REC, 2, RNN_BLOCKS, RNN_BLOCK_W, RNN_BLOCK_W), RNN_BLOCK_W ** -0.5),
        "rec_b_a": nrm(ks[16], (N_REC, 2, D_RNN), 0.02),
        "rec_w_x": nrm(ks[17], (N_REC, 2, RNN_BLOCKS, RNN_BLOCK_W, RNN_BLOCK_W), RNN_BLOCK_W ** -0.5),
        "rec_b_x": nrm(ks[18], (N_REC, 2, D_RNN), 0.02),
        "rec_lambda": jnp.log(a0) - jnp.log1p(-a0),
        "rec_w_out": nrm(ks[19], (N_REC, D_RNN, D_MODEL), D_RNN ** -0.5),
        "peer_w_q": nrm(ks[21], (DEPTH, D_MODEL, PEER_HEADS * 2 * PEER_KEY_DIM), D_MODEL ** -0.5),
        "peer_keys": nrm(ks[22], (DEPTH, 2, N_KEYS, PEER_KEY_DIM), PEER_KEY_DIM ** -0.5),
        "peer_u": nrm(ks[23], (DEPTH, N_EXPERTS, D_MODEL), D_MODEL ** -0.5),
        "peer_v": nrm(ks[24], (DEPTH, N_EXPERTS, D_MODEL), 0.5),
    }


def reference(x, c, ctx, c_ctx, w_mod, b_mod, norm_mix, norm_ffn, norm_final,
              attn_w_qkv, attn_w_o, attn_sink,
              rec_w_in, rec_conv_w, rec_conv_b, rec_w_a, rec_b_a, rec_w_x, rec_b_x, rec_lambda, rec_w_out,
              peer_w_q, peer_keys, peer_u, peer_v):
    B, S, D = x.shape
    C = ctx.shape[1]
    rows = S // GRID_W
    cos, sin = axial_rope(rows, x.dtype)
    xs, cs = x, ctx
    for i in range(DEPTH):
        last = i == DEPTH - 1
        j = i // N_MIXERS
        mod_x = (jax.nn.silu(c) @ w_mod[i] + b_mod[i]).reshape(B, N_MOD, 1, D)
        mod_c = (jax.nn.silu(c_ctx) @ w_mod[i] + b_mod[i]).reshape(N_MOD, 1, D)

        hx = modulate(rmsnorm(xs, norm_mix[i]), mod_x[:, 0], mod_x[:, 1])
        hc = modulate(rmsnorm(cs, norm_mix[i]), mod_c[0], mod_c[1])
        if i % N_MIXERS == 0:
            yx, yc = attn_mixer(hx, hc, cos, sin, attn_w_qkv[j], attn_w_o[j], attn_sink[j], not last)
        else:
            yx, yc = rec_mixer(hx, hc, rec_w_in[j], rec_conv_w[j], rec_conv_b[j], rec_w_a[j], rec_b_a[j],
                               rec_w_x[j], rec_b_x[j], rec_lambda[j], rec_w_out[j], not last)
        xs = xs + mod_x[:, 2] * yx
        if not last:
            cs = cs + mod_c[2] * yc

        hx = modulate(rmsnorm(xs, norm_ffn[i]), mod_x[:, 3], mod_x[:, 4])
        if last:
            fx = peer(hx.reshape(B * S, D), peer_w_q[i], peer_keys[i], peer_u[i], peer_v[i]).reshape(B, S, D)
            xs = xs + mod_x[:, 5] * fx
        else:
            hc = modulate(rmsnorm(cs, norm_ffn[i]), mod_c[3], mod_c[4])
            tokens = jnp.concatenate([hx.reshape(B * S, D), hc.reshape(B * C, D)], axis=0)
            f = peer(tokens, peer_w_q[i], peer_keys[i], peer_u[i], peer_v[i])
            xs = xs + mod_x[:, 5] * f[:B * S].reshape(B, S, D)
            cs = cs + mod_c[5] * f[B * S:].reshape(B, C, D)
    return rmsnorm(xs, norm_final)
```

```python
import os
import numpy as np
import ml_dtypes
from contextlib import ExitStack
import concourse.bass as bass
import concourse.mybir as mybir
from concourse.bass_utils import run_bass_kernel_spmd

F32 = mybir.dt.float32
BF16 = mybir.dt.bfloat16
U32 = mybir.dt.uint32
ALU = mybir.AluOpType
AF = mybir.ActivationFunctionType
AX = mybir.AxisListType

D = 2048
KC = 16
S = 4096
C = 256
NT = (S + C) // 128
EPS = 1e-6
COMPUTE = ("tensor", "vector", "scalar", "gpsimd")


class Prog:
    def __init__(self, nc, name="p"):
        self.nc = nc
        self.name = name
        self.ops = []
        self.last_w = {}
        self.readers = {}

    def op(self, eng, fn, r=(), w=(), dma=False, chan=None):
        i = len(self.ops)
        deps = set()
        for k in r:
            lw = self.last_w.get(k)
            if lw is not None:
                deps.add(lw)
        for k in w:
            lw = self.last_w.get(k)
            if lw is not None:
                deps.add(lw)
            rd = self.readers.get(k)
            if rd:
                deps.update(rd.values())
        for k in w:
            self.last_w[k] = i
            self.readers[k] = {}
        for k in r:
            d = self.readers.setdefault(k, {})
            d[("dma", i) if dma else eng] = i
        if dma:
            assert chan is not None
        self.ops.append(dict(eng=eng, fn=fn, deps=deps, dma=dma, chan=chan))
        return i

    def dma(self, eng, out, in_, r, w, chan, **kw):
        return self.op(eng, lambda e: e.dma_start(out=out, in_=in_, **kw), r=r, w=w, dma=True, chan=chan)

    def V(self, fn, r, w):
        return self.op("vector", fn, r, w)

    def A(self, fn, r, w):
        return self.op("scalar", fn, r, w)

    def G(self, fn, r, w):
        return self.op("gpsimd", fn, r, w)

    def T(self, fn, r, w):
        return self.op("tensor", fn, r, w)

    def emit(self):
        nc = self.nc
        ops = self.ops
        needed = set()
        for o in ops:
            for d in o["deps"]:
                od = ops[d]
                if od["dma"]:
                    continue
                if od["eng"] == "tensor" and o["eng"] == "tensor" and not o["dma"]:
                    continue
                needed.add(d)
        cnt = {e: 0 for e in COMPUTE + ("sync",)}
        chan_cnt = {}
        for i, o in enumerate(ops):
            if o["dma"]:
                c = o["chan"]
                chan_cnt[c] = chan_cnt.get(c, 0) + 16
                o["sig"] = ("c", c, chan_cnt[c])
            elif i in needed:
                cnt[o["eng"]] += 1
                o["sig"] = ("e", o["eng"], cnt[o["eng"]])
            else:
                o["sig"] = None
        self.stats = dict(cnt=dict(cnt), chans=len(chan_cnt), maxchan=max(chan_cnt.values()) if chan_cnt else 0, nops=len(ops))
        esem = {e: nc.alloc_semaphore(name=f"{self.name}_e_{e}") for e in COMPUTE if cnt[e] > 0}
        csem = {c: nc.alloc_semaphore(name=f"{self.name}_c_{c}") for c in chan_cnt}
        with ExitStack() as es:
            block = es.enter_context(nc.Block())
            by_eng = {}
            for i, o in enumerate(ops):
                by_eng.setdefault(o["eng"], []).append(i)

            def make(engname, idxs):
                def body(eng):
                    waited = {}
                    for i in idxs:
                        o = ops[i]
                        for d in sorted(o["deps"]):
                            od = ops[d]
                            sig = od["sig"]
                            if sig is None:
                                continue
                            if sig[0] == "e":
                                if od["eng"] == "tensor" and engname == "tensor" and not o["dma"]:
                                    continue
                                sem = esem[sig[1]]
                            else:
                                sem = csem[sig[1]]
                            key = (sig[0], sig[1])
                            if waited.get(key, 0) >= sig[2]:
                                continue
                            eng.wait_ge(sem, sig[2])
                            waited[key] = sig[2]
                        ins = o["fn"](eng)
                        sig = o["sig"]
                        if sig is not None:
                            if sig[0] == "e":
                                ins.then_inc(esem[sig[1]], 1)
                            else:
                                ins.then_inc(csem[sig[1]], 16)
                    last = {}
                    for i in idxs:
                        o = ops[i]
                        if o["dma"]:
                            last[o["chan"]] = o["sig"][2]
                    for c, v in last.items():
                        if waited.get(("c", c), 0) < v:
                            eng.wait_ge(csem[c], v)
                return body

            for engname, idxs in by_eng.items():
                getattr(block, engname)(make(engname, idxs))
        nc.all_engine_barrier()
        nc.clear_and_free_semaphores(list(esem.values()) + list(csem.values()))
        nc.all_engine_barrier()
        self.ops = []
        self.last_w = {}
        self.readers = {}


def _consts():
    ident = np.eye(128, dtype=np.float32)
    rotm = np.zeros((128, 128), np.float32)
    for m in range(128):
        base = 0 if m < 64 else 64
        d = m - base
        if d < 32:
            rotm[base + d + 32, m] = -1.0
        else:
            rotm[base + d - 32, m] = 1.0
    t = np.arange(S)
    row = (t // 64).astype(np.float32)
    col = (t % 64).astype(np.float32)
    inv = (np.float32(10000.0) ** (-np.arange(0, 64, 2, dtype=np.float32) / np.float32(64))).astype(np.float32)
    ang_r = row[:, None] * inv[None, :]
    ang_c = col[:, None] * inv[None, :]
    ang = np.concatenate([ang_r, ang_r, ang_c, ang_c], axis=-1).astype(np.float32)
    cosT = np.ascontiguousarray(np.cos(ang).astype(np.float32).T)
    sinT = np.ascontiguousarray(np.sin(ang).astype(np.float32).T)
    k = np.arange(128)[:, None]
    q = np.arange(128)[None, :]
    maskL = (k >= q).astype(ml_dtypes.bfloat16)
    maskR = (k <= q).astype(ml_dtypes.bfloat16)
    iota16 = np.tile(np.arange(16, dtype=np.float32)[None, :], (128, 1))
    return dict(ident=ident, rotm=rotm, cosT=cosT, sinT=sinT, maskL=maskL, maskR=maskR, iota16=iota16)


class Ctx:
    pass


def build(stop_after=99, debug=False, peer_tiles=None, start_at=0):
    nc = bass.Bass("TRN2", target_bir_lowering=False)
    K = Ctx()
    K.nc = nc
    din = lambda n, s, d=F32: nc.dram_tensor(n, list(s), d, kind="ExternalInput").ap()
    dscr = lambda n, s, d=F32: nc.dram_tensor(n, list(s), d, kind="Internal").ap()
    I = dict(
        x=din("x", [S, D]), c=din("c", [D]), ctx=din("ctx", [C, D]), c_ctx=din("c_ctx", [D]),
        w_mod=din("w_mod", [2, D, 6 * D]), b_mod=din("b_mod", [2, 6 * D]),
        norm_mix=din("norm_mix", [2, D]), norm_ffn=din("norm_ffn", [2, D]), norm_final=din("norm_final", [D]),
        attn_w_qkv=din("attn_w_qkv", [D, 3072]), attn_w_o=din("attn_w_o", [D, D]), attn_sink=din("attn_sink", [16]),
        rec_w_in=din("rec_w_in", [D, 2 * D]), rec_conv_w=din("rec_conv_w", [4, D]), rec_conv_b=din("rec_conv_b", [D]),
        rec_w_a=din("rec_w_a", [2, 8, 256, 256]), rec_b_a=din("rec_b_a", [2, D]),
        rec_w_x=din("rec_w_x", [2, 8, 256, 256]), rec_b_x=din("rec_b_x", [2, D]),
        rec_lambda=din("rec_lambda", [2, D]), rec_w_out=din("rec_w_out", [D, D]),
        peer_w_q=din("peer_w_q", [2, D, D]), peer_keys=din("peer_keys", [2, 2, 128, 128]),
        peer_u=din("peer_u", [2, 16384, D]), peer_v=din("peer_v", [2, 16384, D]),
        ident=din("ident", [128, 128]), rotm=din("rotm", [128, 128]), cosT=din("cosT", [128, S]), sinT=din("sinT", [128, S]),
        maskL=din("maskL", [128, 128], BF16), maskR=din("maskR", [128, 128], BF16), iota16=din("iota16", [128, 16]),
        myrows=din("myrows", [128, 16], U32),
    )
    K.I = I
    out = nc.dram_tensor("out", [S // 2, D], F32, kind="ExternalOutput").ap()
    K.out = out
    K.modd = dscr("modd", [2, 2, 6 * D])
    K.qd = dscr("qd", [128, 16, NT * 128], BF16)
    K.kd = dscr("kd", [128, 4, NT * 128], BF16)
    K.vd = dscr("vd", [NT * 128, 512], BF16)
    K.xs1 = dscr("xs1", [NT * 128, D])
    K.xs2 = dscr("xs2", [NT * 128, D]) if start_at < 3 else din("xs2", [NT * 128, D])
    K.xs3 = dscr("xs3", [S, D]) if start_at < 4 else din("xs3", [S, D])
    K.upre = dscr("upre", [16, 128, NT * 128])
    K.ggd = dscr("ggd", [16, 128, S])
    K.ygd = dscr("ygd", [128, 16, S], BF16)
    dbg = {}
    if debug:
        dbg["d_modd"] = nc.dram_tensor("d_modd", [2, 2, 6 * D], F32, kind="ExternalOutput").ap()
        dbg["d_xs1"] = nc.dram_tensor("d_xs1", [NT * 128, D], F32, kind="ExternalOutput").ap()
        dbg["d_xs2"] = nc.dram_tensor("d_xs2", [NT * 128, D], F32, kind="ExternalOutput").ap()
        dbg["d_xs3"] = nc.dram_tensor("d_xs3", [S, D], F32, kind="ExternalOutput").ap()
    K.dbg = dbg

    with ExitStack() as es:
        K.ps = [es.enter_context(nc.psum_tensor(f"ps{i}", [128, 512], F32)) for i in range(8)]
        K.ident = es.enter_context(nc.sbuf_tensor("identsb", [128, 128], F32))
        P = Prog(nc, "c0")
        P.dma("sync", K.ident[:], I["ident"], r=[], w=["ident"], chan="ident")
        P.emit()
        nc.all_engine_barrier()
        phase_mod(K)
        nc.all_engine_barrier()
        if debug:
            copy_dram(K, K.modd.rearrange("l v n -> (l v) n"), dbg["d_modd"].rearrange("l v n -> (l v) n"), 4, "cpm")
        if stop_after >= 1 and start_at <= 1:
            phase_attn(K)
            nc.all_engine_barrier()
            if debug:
                copy_dram(K, K.xs1, dbg["d_xs1"], NT * 128, "cpx1")
        if stop_after >= 2 and start_at <= 2:
            tl = [dict(src=("d", K.xs1[n * 128:(n + 1) * 128, :]), v=1 if n < 2 else 0, dst=K.xs2[n * 128:(n + 1) * 128, :]) for n in range(NT)]
            if peer_tiles is not None:
                tl = [tl[i] for i in peer_tiles]
            phase_peer(K, 0, tl, False, "pe0")
            nc.all_engine_barrier()
            if debug:
                copy_dram(K, K.xs2, dbg["d_xs2"], NT * 128, "cpx2")
        if stop_after >= 3 and start_at <= 3:
            phase_rec(K)
            nc.all_engine_barrier()
            if debug:
                copy_dram(K, K.xs3, dbg["d_xs3"], S, "cpx3")
        if stop_after >= 4:
            tl = [dict(src=("g", K.xs3, t), v=0, dst=K.out[t * 128:(t + 1) * 128, :]) for t in range(S // 256)]
            if peer_tiles is not None:
                tl = tl[:len(peer_tiles)]
            phase_peer(K, 1, tl, True, "pe1")
    return nc


def copy_dram(K, src, dst, rows, name):
    nc = K.nc
    with ExitStack() as es:
        t = es.enter_context(nc.sbuf_tensor(name + "_t", [128, src.shape[1]], src.dtype))
        P = Prog(nc, name)
        for r0 in range(0, rows, 128):
            n = min(128, rows - r0)
            P.dma("sync", t[0:n, :], src[r0:r0 + n, :], r=[], w=["t"], chan="ld")
            P.dma("sync", dst[r0:r0 + n, :], t[0:n, :], r=["t"], w=[], chan="st")
        P.emit()
    nc.all_engine_barrier()


def phase_mod(K):
    nc, I = K.nc, K.I
    with ExitStack() as es:
        sb = lambda n, s, d=F32: es.enter_context(nc.sbuf_tensor(n, s, d))
        cc = sb("m_cc", [128, KC, 2])
        craw = sb("m_craw", [128, 2, KC])
        wt = [sb(f"m_wt{i}", [128, KC, 512]) for i in range(2)]
        bt = sb("m_bt", [2, 512])
        ot = [sb(f"m_ot{i}", [2, 512]) for i in range(2)]
        P = Prog(nc, "mod")
        P.dma("sync", craw[:, 0, :], I["c"].rearrange("(k p) -> p k", p=128), r=[], w=["craw0"], chan="craw0", allow_slow_non_contiguous=True)
        P.dma("sync", craw[:, 1, :], I["c_ctx"].rearrange("(k p) -> p k", p=128), r=[], w=["craw1"], chan="craw1", allow_slow_non_contiguous=True)
        for v in range(2):
            P.A(lambda e, v=v: e.activation(out=cc[:, :, v], in_=craw[:, v, :], func=AF.Silu), r=[f"craw{v}"], w=["cc"])
        n = 0
        for l in range(2):
            for j in range(24):
                s = n % 2
                w_src = I["w_mod"][l].rearrange("(k p) n -> p k n", p=128)[:, :, j * 512:(j + 1) * 512]
                P.dma("sync" if s == 0 else "gpsimd", wt[s][:], w_src, r=[], w=[f"wt{s}"], chan=f"wt{s}")
                for v in range(2):
                    P.dma("sync", bt[v:v + 1, :], I["b_mod"][l:l + 1, j * 512:(j + 1) * 512], r=[], w=["bt"], chan=f"bt{v}")
                pb = K.ps[s]
                for kc in range(KC):
                    P.T(lambda e, kc=kc, s=s, pb=pb: e.matmul(pb[0:2, :], lhsT=cc[:, kc, :], rhs=wt[s][:, kc, :], start=(kc == 0), stop=(kc == KC - 1)),
                        r=["cc", f"wt{s}"], w=[f"ps{s}"])
                P.V(lambda e, s=s, pb=pb: e.tensor_tensor(out=ot[s][:], in0=pb[0:2, :], in1=bt[:], op=ALU.add), r=[f"ps{s}", "bt"], w=[f"ot{s}"])
                P.dma("sync", K.modd[l, :, j * 512:(j + 1) * 512], ot[s][:], r=[f"ot{s}"], w=[], chan=f"ot{s}")
                n += 1
        P.emit()


def load_bcast(P, eng, tile_ap, src_row_ap, key, chan):
    P.dma(eng, tile_ap, src_row_ap.partition_broadcast(128), r=[], w=[key], chan=chan)


def prep_GS(K, P, Gt, St, gain_row, scale_row, shift_row, tmp, tag):
    load_bcast(P, "sync", Gt[:], scale_row, f"G{tag}", f"G{tag}")
    load_bcast(P, "sync", tmp[:], gain_row, "gstmp", "gstmp")
    load_bcast(P, "sync", St[:], shift_row, f"S{tag}", f"S{tag}")
    P.V(lambda e: e.scalar_tensor_tensor(out=Gt[:], in0=Gt[:], scalar=1.0, in1=tmp[:], op0=ALU.add, op1=ALU.mult), r=[f"G{tag}", "gstmp"], w=[f"G{tag}"])


def norm_mod_T(K, P, src_ap, xt, h, ss, Gt, St, gtag, hT, hT_key, tok0, tpb, xslot):
    ps = K.ps
    P.dma("sync", xt[:], src_ap, r=[], w=[f"xt{xslot}"], chan=f"xt{xslot}")
    P.A(lambda e: e.activation(out=h[:], in_=xt[:], func=AF.Square, accum_out=ss[:, 0:1]), r=[f"xt{xslot}"], w=["h", "ss"])
    P.V(lambda e: e.tensor_scalar(out=ss[:, 1:2], in0=ss[:, 0:1], scalar1=1.0 / D, scalar2=EPS, op0=ALU.mult, op1=ALU.add), r=["ss"], w=["ss1"])
    P.A(lambda e: e.activation(out=ss[:, 2:3], in_=ss[:, 1:2], func=AF.Sqrt), r=["ss1"], w=["ss2"])
    P.V(lambda e: e.reciprocal(out=ss[:, 3:4], in_=ss[:, 2:3]), r=["ss2"], w=["ss3"])
    P.V(lambda e: e.scalar_tensor_tensor(out=h[:], in0=xt[:], scalar=ss[:, 3:4], in1=Gt[:], op0=ALU.mult, op1=ALU.mult),
        r=[f"xt{xslot}", "ss3", f"G{gtag}"], w=["h"])
    P.G(lambda e: e.tensor_tensor(out=h[:], in0=h[:], in1=St[:], op=ALU.add), r=["h", f"S{gtag}"], w=["h"])
    if hT is None:
        return
    for q in range(4):
        b = tpb[q % 2]
        for j in range(4):
            kc = 4 * q + j
            P.T(lambda e, kc=kc, j=j, b=b: e.transpose(out=ps[b][:, j * 128:(j + 1) * 128], in_=h[:, kc * 128:(kc + 1) * 128], identity=K.ident[:]),
                r=["h", "ident"], w=[f"ps{b}"])
        P.A(lambda e, q=q, b=b: e.activation(out=hT[:, 4 * q:4 * q + 4, tok0:tok0 + 128], in_=ps[b][:].rearrange("p (j t) -> p j t", j=4), func=AF.Copy),
            r=[f"ps{b}"], w=[hT_key])


def load_w_bf16(P, wsb, w_dram, ncols, key, col0=0):
    for kc in range(KC):
        for c0 in range(0, ncols, 1024):
            cn = min(1024, ncols - c0)
            P.dma("gpsimd", wsb[:, kc, c0:c0 + cn], w_dram[kc * 128:(kc + 1) * 128, col0 + c0:col0 + c0 + cn], r=[], w=[key], chan=f"{key}_{kc % 4}")


def phase_attn(K):
    nc, I, ps = K.nc, K.I, K.ps
    SCALE = 128 ** -0.5
    with ExitStack() as es:
        sb = lambda n, s, d=F32: es.enter_context(nc.sbuf_tensor(n, s, d))
        with ExitStack() as es1:
            sb1 = lambda n, s, d=F32: es1.enter_context(nc.sbuf_tensor(n, s, d))
            W = sb1("a_W", [128, KC, 3072], BF16)
            Gt = sb1("a_G", [128, D]); St = sb1("a_S", [128, D])
            ss = sb1("a_ss", [128, 4])
            xt = sb1("a_xt", [128, D]); h = sb1("a_h", [128, D])
            hT = sb1("a_hT", [128, KC, 256], BF16)
            rotm = sb1("a_rotm", [128, 128])
            cs = sb1("a_cos", [128, 256]); sn = sb1("a_sin", [128, 256])
            qsb = [sb1(f"a_qsb{i}", [128, 256]) for i in range(2)]
            t1 = [sb1(f"a_t1{i}", [128, 256]) for i in range(2)]
            t2 = [sb1(f"a_t2{i}", [128, 256]) for i in range(2)]
            qst = sb1("a_qst", [128, 20, 256], BF16)
            vst = sb1("a_vst", [128, 2, 512], BF16)
            P = Prog(nc, "qkv")
            load_w_bf16(P, W, I["attn_w_qkv"], 3072, "W")
            P.dma("sync", rotm[:], I["rotm"], r=[], w=["rotm"], chan="rotm")
            for blk in range(NT // 2):
                is_ctx = blk == 0
                if blk in (0, 1):
                    v = 1 if is_ctx else 0
                    prep_GS(K, P, Gt, St, I["norm_mix"][0], K.modd[0, v, D:2 * D], K.modd[0, v, 0:D], h, "a")
                for ti in range(2):
                    e_t = blk * 2 + ti
                    src = I["ctx"][e_t * 128:(e_t + 1) * 128, :] if is_ctx else I["x"][(e_t - 2) * 128:(e_t - 1) * 128, :]
                    norm_mod_T(K, P, src, xt, h, ss, Gt, St, "a", hT, "hT", ti * 128, (6, 7), 0)
                if not is_ctx:
                    l0 = (blk - 1) * 256
                    P.dma("sync", cs[:], I["cosT"][:, l0:l0 + 256], r=[], w=["cos"], chan="cos")
                    P.dma("sync", sn[:], I["sinT"][:, l0:l0 + 256], r=[], w=["sin"], chan="sin")
                for j in range(20):
                    pb = j % 2
                    for kc in range(KC):
                        P.T(lambda e, kc=kc, j=j, pb=pb: e.matmul(ps[pb][:, 0:256], lhsT=W[:, kc, j * 128:(j + 1) * 128], rhs=hT[:, kc, :], start=(kc == 0), stop=(kc == KC - 1)),
                            r=["W", "hT"], w=[f"ps{pb}"])
                    dst, dkey = qst[:, j, :], "qst"
                    if is_ctx:
                        P.A(lambda e, pb=pb, dst=dst: e.activation(out=dst, in_=ps[pb][:, 0:256], func=AF.Copy), r=[f"ps{pb}"], w=[dkey])
                    else:
                        s2 = j % 2
                        P.A(lambda e, pb=pb, s2=s2: e.activation(out=qsb[s2][:], in_=ps[pb][:, 0:256], func=AF.Copy), r=[f"ps{pb}"], w=[f"qsb{s2}"])
                        P.T(lambda e, s2=s2: e.matmul(ps[2 + s2][:, 0:256], lhsT=rotm[:], rhs=qsb[s2][:], start=True, stop=True), r=["rotm", f"qsb{s2}"], w=[f"ps{2 + s2}"])
                        P.G(lambda e, s2=s2: e.tensor_tensor(out=t1[s2][:], in0=qsb[s2][:], in1=cs[:], op=ALU.mult), r=[f"qsb{s2}", "cos"], w=[f"t1{s2}"])
                        P.V(lambda e, s2=s2: e.tensor_tensor(out=t2[s2][:], in0=ps[2 + s2][:, 0:256], in1=sn[:], op=ALU.mult), r=[f"ps{2 + s2}", "sin"], w=[f"t2{s2}"])
                        P.V(lambda e, s2=s2, dst=dst: e.tensor_tensor(out=dst, in0=t1[s2][:], in1=t2[s2][:], op=ALU.add), r=[f"t1{s2}", f"t2{s2}"], w=[dkey])
                P.dma("sync", K.qd[:, :, blk * 256:(blk + 1) * 256], qst[:, 0:16, :], r=["qst"], w=["qd"], chan="qst")
                P.dma("sync", K.kd[:, :, blk * 256:(blk + 1) * 256], qst[:, 16:20, :], r=["qst"], w=["kd"], chan="kst")
                for ti in range(2):
                    e_t = blk * 2 + ti
                    pb = 4 + ti
                    for kc in range(KC):
                        P.T(lambda e, kc=kc, ti=ti, pb=pb: e.matmul(ps[pb][:], lhsT=hT[:, kc, ti * 128:(ti + 1) * 128], rhs=W[:, kc, 2560:3072], start=(kc == 0), stop=(kc == KC - 1)),
                            r=["W", "hT"], w=[f"ps{pb}"])
                    P.A(lambda e, ti=ti, pb=pb: e.activation(out=vst[:, ti, :], in_=ps[pb][:], func=AF.Copy), r=[f"ps{pb}"], w=[f"vst{ti}"])
                    P.dma("sync", K.vd[e_t * 128:(e_t + 1) * 128, :], vst[:, ti, :], r=[f"vst{ti}"], w=["vd"], chan=f"vst{ti}")
            P.emit()
        nc.all_engine_barrier()
        with ExitStack() as es2:
            sb2 = lambda n, s, d=F32: es2.enter_context(nc.sbuf_tensor(n, s, d))
            Wo = sb2("a_Wo", [128, KC, D], BF16)
            kT = sb2("a_kT", [128, 4, NT * 128], BF16)
            vx = sb2("a_vx", [128, NT, 4, 129], BF16)
            gate = sb2("a_gate", [128, D])
            esink = sb2("a_esink", [128, 16])
            qt = [sb2(f"a_qt{i}", [128, 16, 128], BF16) for i in range(2)]
            E = [sb2(f"a_E{i}", [128, 5, 512], BF16) for i in range(2)]
            mL = sb2("a_mL", [128, 128], BF16); mR = sb2("a_mR", [128, 128], BF16)
            o = sb2("a_o", [128, D]); oT = sb2("a_oT", [128, KC, 128], BF16)
            den = sb2("a_den", [128, 8])
            xt = [sb2(f"a_xr{i}", [128, D]) for i in range(2)]
            y = sb2("a_y", [128, D])
            sraw = sb2("a_sraw", [128, 16])
            P = Prog(nc, "att")
            load_w_bf16(P, Wo, I["attn_w_o"], D, "Wo")
            P.G(lambda e: e.memset(vx[:, :, :, 128:129], 1.0), r=[], w=["vx1"])
            for hh in range(4):
                P.dma("sync", kT[:, hh, :], K.kd[:, hh, :], r=[], w=["kT"], chan=f"kTl{hh}")
            for t in range(NT):
                P.dma("sync", vx[:, t, :, 0:128], K.vd[t * 128:(t + 1) * 128, :].rearrange("p (g d) -> p g d", g=4), r=[], w=["vx"], chan=f"vxl{t % 4}")
            P.dma("sync", mL[:], I["maskL"], r=[], w=["mL"], chan="mL")
            P.dma("sync", mR[:], I["maskR"], r=[], w=["mR"], chan="mR")
            load_bcast(P, "sync", sraw[:], I["attn_sink"], "sraw", "sraw")
            P.A(lambda e: e.activation(out=esink[:], in_=sraw[:], func=AF.Exp), r=["sraw"], w=["esink"])
            for e_t in range(NT):
                is_ctx = e_t < 2
                if e_t in (0, 2):
                    v = 1 if is_ctx else 0
                    load_bcast(P, "sync", gate[:], K.modd[0, v, 2 * D:3 * D], "gate", "gate")
                qs = e_t % 2
                P.dma("sync", qt[qs][:], K.qd[:, :, e_t * 128:(e_t + 1) * 128], r=["qd"], w=[f"qt{qs}"], chan=f"qt{qs}")
                src = I["ctx"][e_t * 128:(e_t + 1) * 128, :] if is_ctx else I["x"][(e_t - 2) * 128:(e_t - 1) * 128, :]
                P.dma("sync", xt[qs][:], src, r=[], w=[f"xr{qs}"], chan=f"xr{qs}")
                if is_ctx:
                    kbs = [0, 1]
                else:
                    kbs = [0, 1] + [kb for kb in (e_t - 1, e_t, e_t + 1) if 2 <= kb < NT]
                for hh in range(4):
                    Es = E[hh % 2]
                    Ek = f"E{hh % 2}"
                    for n, kb in enumerate(kbs):
                        pb = n % 2
                        P.T(lambda e, hh=hh, kb=kb, pb=pb, qs=qs: e.matmul(ps[pb][:], lhsT=kT[:, hh, kb * 128:(kb + 1) * 128],
                                                                      rhs=qt[qs][:, 4 * hh:4 * hh + 4, :].rearrange("p g q -> p (g q)"), start=True, stop=True),
                            r=["kT", f"qt{qs}"], w=[f"ps{pb}"])
                        P.A(lambda e, n=n, pb=pb, Es=Es: e.activation(out=Es[:, n, :], in_=ps[pb][:], func=AF.Exp, scale=SCALE), r=[f"ps{pb}"], w=[f"{Ek}_{n}"])
                        if (not is_ctx) and kb >= 2 and kb != e_t:
                            m = mL if kb == e_t - 1 else mR
                            P.G(lambda e, n=n, m=m, Es=Es: e.tensor_tensor(out=Es[:, n, :].rearrange("p (g q) -> p g q", g=4), in0=Es[:, n, :].rearrange("p (g q) -> p g q", g=4),
                                                                        in1=m[:].unsqueeze(1).to_broadcast([128, 4, 128]), op=ALU.mult),
                                r=[f"{Ek}_{n}", "mL", "mR"], w=[f"{Ek}_{n}"])
                    for g in range(4):
                        pb = 2 + g // 2
                        for n, kb in enumerate(kbs):
                            P.T(lambda e, g=g, n=n, kb=kb, pb=pb, hh=hh, Es=Es: e.matmul(ps[pb][:, (g % 2) * 256:(g % 2) * 256 + 129], lhsT=Es[:, n, g * 128:(g + 1) * 128],
                                                                                     rhs=vx[:, kb, hh, :], start=(n == 0), stop=(n == len(kbs) - 1)),
                                r=[f"{Ek}_{n}", "vx", "vx1"], w=[f"ps{pb}"])
                    for bk in range(2):
                        pb = 2 + bk
                        P.V(lambda e, bk=bk, pb=pb, hh=hh: e.tensor_tensor(out=den[:, 2 * bk:2 * bk + 2], in0=ps[pb][:].rearrange("p (g c) -> p g c", g=2)[:, :, 128],
                                                                       in1=esink[:, 4 * hh + 2 * bk:4 * hh + 2 * bk + 2], op=ALU.add),
                            r=[f"ps{pb}", "esink"], w=["den"])
                    P.V(lambda e: e.reciprocal(out=den[:, 4:8], in_=den[:, 0:4]), r=["den"], w=["rden"])
                    for g in range(4):
                        pb = 2 + g // 2
                        hd = 4 * hh + g
                        P.V(lambda e, g=g, pb=pb, hd=hd: e.tensor_scalar(out=o[:, hd * 128:(hd + 1) * 128], in0=ps[pb][:, (g % 2) * 256:(g % 2) * 256 + 128],
                                                                     scalar1=den[:, 4 + g:5 + g], scalar2=None, op0=ALU.mult),
                            r=[f"ps{pb}", "rden"], w=["o"])
                for q in range(4):
                    b = 6 + q % 2
                    for j in range(4):
                        kc = 4 * q + j
                        P.T(lambda e, kc=kc, j=j, b=b: e.transpose(out=ps[b][:, j * 128:(j + 1) * 128], in_=o[:, kc * 128:(kc + 1) * 128], identity=K.ident[:]),
                            r=["o", "ident"], w=[f"ps{b}"])
                    P.A(lambda e, q=q, b=b: e.activation(out=oT[:, 4 * q:4 * q + 4, :], in_=ps[b][:].rearrange("p (j t) -> p j t", j=4), func=AF.Copy), r=[f"ps{b}"], w=["oT"])
                for nb in range(4):
                    pb = 4 + nb
                    for kc in range(KC):
                        P.T(lambda e, kc=kc, nb=nb, pb=pb: e.matmul(ps[pb][:], lhsT=oT[:, kc, :], rhs=Wo[:, kc, nb * 512:(nb + 1) * 512], start=(kc == 0), stop=(kc == KC - 1)),
                            r=["oT", "Wo"], w=[f"ps{pb}"])
                    P.V(lambda e, nb=nb, pb=pb: e.tensor_tensor(out=y[:, nb * 512:(nb + 1) * 512], in0=ps[pb][:], in1=gate[:, nb * 512:(nb + 1) * 512], op=ALU.mult),
                        r=[f"ps{pb}", "gate"], w=[f"y{nb}"])
                    P.G(lambda e, nb=nb, qs=qs: e.tensor_tensor(out=y[:, nb * 512:(nb + 1) * 512], in0=y[:, nb * 512:(nb + 1) * 512], in1=xt[qs][:, nb * 512:(nb + 1) * 512], op=ALU.add),
                        r=[f"y{nb}", f"xr{qs}"], w=[f"y{nb}"])
                P.dma("sync", K.xs1[e_t * 128:(e_t + 1) * 128, :], y[:], r=[f"y{nb}" for nb in range(4)], w=["xs1"], chan="y")
            P.emit()


def _lay(inputs):
    consts = _consts()
    f = lambda a: np.ascontiguousarray(np.asarray(a, dtype=np.float32))
    shared = dict(
        c_ctx=f(inputs["c_ctx"]), w_mod=f(inputs["w_mod"]), b_mod=f(inputs["b_mod"]),
        norm_mix=f(inputs["norm_mix"]), norm_ffn=f(inputs["norm_ffn"]), norm_final=f(inputs["norm_final"]),
        attn_w_qkv=f(inputs["attn_w_qkv"][0]), attn_w_o=f(inputs["attn_w_o"][0]), attn_sink=f(inputs["attn_sink"][0]),
        rec_w_in=f(inputs["rec_w_in"][0]), rec_conv_w=f(inputs["rec_conv_w"][0]), rec_conv_b=f(inputs["rec_conv_b"][0]),
        rec_w_a=f(inputs["rec_w_a"][0]), rec_b_a=f(inputs["rec_b_a"][0]), rec_w_x=f(inputs["rec_w_x"][0]), rec_b_x=f(inputs["rec_b_x"][0]),
        rec_lambda=f(inputs["rec_lambda"][0]), rec_w_out=f(inputs["rec_w_out"][0]),
        peer_w_q=f(inputs["peer_w_q"]), peer_keys=f(inputs["peer_keys"]), peer_u=f(inputs["peer_u"]), peer_v=f(inputs["peer_v"]),
        **consts,
    )
    return shared, f


def core_inputs(inputs, shared, f, core):
    b, hf = core // 2, core % 2
    m = dict(shared)
    m["x"] = f(inputs["x"][b]); m["c"] = f(inputs["c"][b]); m["ctx"] = f(inputs["ctx"][b])
    m["myrows"] = (hf * (S // 2) + np.arange(16, dtype=np.uint32)[None, :] * 128 + np.arange(128, dtype=np.uint32)[:, None]).astype(np.uint32)
    return m


def kernel(**inputs):
    nc = build()
    shared, f = _lay(inputs)
    in_maps = [core_inputs(inputs, shared, f, core) for core in range(8)]
    res = run_bass_kernel_spmd(nc, in_maps, core_ids=list(range(8)))
    outp = np.empty((4, S, D), np.float32)
    for core in range(8):
        b, hf = core // 2, core % 2
        outp[b, hf * (S // 2):(hf + 1) * (S // 2)] = res.results[core]["out"]
    return outp


def phase_peer(K, layer, tiles, final, name):
    nc, I, ps = K.nc, K.I, K.ps
    NB = 3 if final else 4
    utab = I["peer_u"].rearrange("l e d -> (l e) d")
    vtab = I["peer_v"].rearrange("l e d -> (l e) d")
    with ExitStack() as es:
        sb = lambda n, s, d=F32: es.enter_context(nc.sbuf_tensor(name + n, s, d))
        Wq = sb("p_Wq", [128, KC, D], BF16)
        Gt = sb("p_G", [128, D]); St = sb("p_S", [128, D]); gate = sb("p_gate", [128, D])
        xt = [sb(f"p_xt{i}", [128, D]) for i in range(2)]
        h = [sb(f"p_h{i}", [128, D]) for i in range(2)]
        hT = sb("p_hT", [128, KC, 128], BF16)
        wk = [sb(f"p_wk{i}", [128, D]) for i in range(3)]
        ring = [sb(f"p_ring{i}", [128, D]) for i in range(NB)]
        junk = sb("p_junk", [128, D], BF16)
        acc = sb("p_acc", [128, D])
        keysT = sb("p_keysT", [128, 2, 128])
        kraw = sb("p_kraw", [128, 2, 128])
        ss = sb("p_ss", [128, 4])
        s16 = sb("p_s16", [128, 16, 16]); i16 = sb("p_i16", [128, 16, 16], U32); i16f = sb("p_i16f", [128, 16, 16])
        ts = sb("p_ts", [128, 8, 16]); sel = sb("p_sel", [128, 8, 16], U32)
        au = sb("p_au", [128, 2, 128], U32); af = sb("p_af", [128, 2, 128])
        isel = sb("p_isel", [128, 2, 128])
        idxf = sb("p_idxf", [128, 128])
        idxu = [sb(f"p_idxu{i}", [128, 128], U32) for i in range(2)]
        gsm = [sb(f"p_g{i}", [128, 128]) for i in range(2)]
        sm = sb("p_sm", [128, 16])
        z = sb("p_z", [128, 128]); zt = sb("p_zt", [128, 3, 128]); actw = sb("p_act", [128, 128])
        iota = sb("p_iota", [128, 16])
        mr = sb("p_mr", [128, 16], U32)
        gfin = sb("p_gfin", [128, D]) if final else None
        P = Prog(nc, name)
        load_w_bf16(P, Wq, I["peer_w_q"][layer], D, "Wq")
        P.dma("sync", iota[:], I["iota16"], r=[], w=["iota"], chan="iota")
        P.dma("sync", mr[:], I["myrows"], r=[], w=["mr"], chan="mr")
        if final:
            load_bcast(P, "sync", gfin[:], I["norm_final"], "gfin", "gfin")
        for p in range(2):
            P.dma("sync", kraw[:, p, :], I["peer_keys"][layer, p], r=[], w=["kraw"], chan=f"kraw{p}")
            P.T(lambda e, p=p: e.transpose(out=ps[p][:, 0:128], in_=kraw[:, p, :], identity=K.ident[:]), r=["kraw", "ident"], w=[f"ps{p}"])
            P.V(lambda e, p=p: e.tensor_copy(out=keysT[:, p, :], in_=ps[p][:, 0:128]), r=[f"ps{p}"], w=["keysT"])

        P.emit()
        nc.all_engine_barrier()
        cur_v = [None]
        PP = [None]

        def front(n):
            P = PP[0]
            t = tiles[n]
            s = n % 2
            if cur_v[0] != t["v"]:
                cur_v[0] = t["v"]
                v = t["v"]
                prep_GS(K, P, Gt, St, I["norm_ffn"][layer], K.modd[layer, v, 4 * D:5 * D], K.modd[layer, v, 3 * D:4 * D], wk[0], "p")
                load_bcast(P, "sync", gate[:], K.modd[layer, v, 5 * D:6 * D], "gate", "gate")
            if t["src"][0] == "d":
                P.dma("sync", xt[s][:], t["src"][1], r=[], w=[f"xt{s}"], chan=f"xt{s}")
            else:
                col = t["src"][2]
                P.op("gpsimd", lambda e, s=s, col=col, src=t["src"][1]: e.indirect_dma_start(out=xt[s][:], out_offset=None, in_=src,
                                                                                         in_offset=bass.IndirectOffsetOnAxis(ap=mr[:, col:col + 1], axis=0)),
                     r=["mr"], w=[f"xt{s}"], dma=True, chan=f"xt{s}")
            hh = h[s]
            hk = f"h{s}"
            P.A(lambda e, s=s: e.activation(out=h[s][:], in_=xt[s][:], func=AF.Square, accum_out=ss[:, 0:1]), r=[f"xt{s}"], w=[hk, "ss"])
            P.V(lambda e: e.tensor_scalar(out=ss[:, 1:2], in0=ss[:, 0:1], scalar1=1.0 / D, scalar2=EPS, op0=ALU.mult, op1=ALU.add), r=["ss"], w=["ss1"])
            P.A(lambda e: e.activation(out=ss[:, 2:3], in_=ss[:, 1:2], func=AF.Sqrt), r=["ss1"], w=["ss2"])
            P.V(lambda e: e.reciprocal(out=ss[:, 3:4], in_=ss[:, 2:3]), r=["ss2"], w=["ss3"])
            P.V(lambda e, s=s: e.scalar_tensor_tensor(out=h[s][:], in0=xt[s][:], scalar=ss[:, 3:4], in1=Gt[:], op0=ALU.mult, op1=ALU.mult), r=[f"xt{s}", "ss3", "Gp"], w=[hk])
            P.V(lambda e, s=s: e.tensor_tensor(out=h[s][:], in0=h[s][:], in1=St[:], op=ALU.add), r=[hk, "Sp"], w=[hk])
            for q in range(4):
                b = 6 + q % 2
                for j in range(4):
                    kc = 4 * q + j
                    P.T(lambda e, kc=kc, j=j, b=b, s=s: e.transpose(out=ps[b][:, j * 128:(j + 1) * 128], in_=h[s][:, kc * 128:(kc + 1) * 128], identity=K.ident[:]),
                        r=[hk, "ident"], w=[f"ps{b}"])
                P.A(lambda e, q=q, b=b: e.activation(out=hT[:, 4 * q:4 * q + 4, :], in_=ps[b][:].rearrange("p (j t) -> p j t", j=4), func=AF.Copy), r=[f"ps{b}"], w=["hT"])
            qT = wk[0]
            for c in range(16):
                pb = c % 2
                for kc in range(KC):
                    P.T(lambda e, kc=kc, c=c, pb=pb: e.matmul(ps[pb][:, 0:128], lhsT=Wq[:, kc, c * 128:(c + 1) * 128], rhs=hT[:, kc, :], start=(kc == 0), stop=(kc == KC - 1)),
                        r=["Wq", "hT"], w=[f"ps{pb}"])
                P.A(lambda e, c=c, pb=pb: e.activation(out=qT[:, c * 128:(c + 1) * 128], in_=ps[pb][:, 0:128], func=AF.Copy), r=[f"ps{pb}"], w=["wk0"])
            Ssb = wk[1]
            for c in range(16):
                pb = 2 + c // 4
                P.T(lambda e, c=c, pb=pb: e.matmul(ps[pb][:, (c % 4) * 128:(c % 4 + 1) * 128], lhsT=qT[:, c * 128:(c + 1) * 128], rhs=keysT[:, c % 2, :], start=True, stop=True),
                    r=["wk0", "keysT"], w=[f"ps{pb}"])
            for q in range(4):
                P.A(lambda e, q=q: e.activation(out=Ssb[:, q * 512:(q + 1) * 512], in_=ps[2 + q][:], func=AF.Copy), r=[f"ps{2 + q}"], w=["wk1"])
            S2 = wk[2]
            for c in range(16):
                sv = Ssb[:, c * 128:(c + 1) * 128]
                s2v = S2[:, c * 128:(c + 1) * 128]
                P.V(lambda e, c=c, sv=sv: e.max(out=s16[:, c, 0:8], in_=sv), r=["wk1"], w=["s16"])
                P.V(lambda e, c=c, sv=sv: e.max_index(out=i16[:, c, 0:8], in_max=s16[:, c, 0:8], in_values=sv), r=["wk1", "s16"], w=["i16"])
                P.V(lambda e, c=c, sv=sv, s2v=s2v: e.match_replace(out=s2v, in_to_replace=s16[:, c, 0:8], in_values=sv, imm_value=-1e30), r=["wk1", "s16"], w=["wk2"])
                P.V(lambda e, c=c, s2v=s2v: e.max(out=s16[:, c, 8:16], in_=s2v), r=["wk2"], w=["s16"])
                P.V(lambda e, c=c, s2v=s2v: e.max_index(out=i16[:, c, 8:16], in_max=s16[:, c, 8:16], in_values=s2v), r=["wk2", "s16"], w=["i16"])
            cand = wk[0]
            c4 = cand[:].rearrange("p (h a b) -> p h a b", h=8, a=16)
            s16r = s16[:].rearrange("p (h t) k -> p h t k", t=2)
            P.V(lambda e: e.tensor_tensor(out=c4, in0=s16r[:, :, 0, :].unsqueeze(3).to_broadcast([128, 8, 16, 16]),
                                          in1=s16r[:, :, 1, :].unsqueeze(2).to_broadcast([128, 8, 16, 16]), op=ALU.add), r=["s16"], w=["wk0"])
            for hd in range(8):
                cv = cand[:, hd * 256:(hd + 1) * 256]
                c2v = S2[:, hd * 256:(hd + 1) * 256]
                P.V(lambda e, hd=hd, cv=cv: e.max(out=ts[:, hd, 0:8], in_=cv), r=["wk0"], w=["ts"])
                P.V(lambda e, hd=hd, cv=cv: e.max_index(out=sel[:, hd, 0:8], in_max=ts[:, hd, 0:8], in_values=cv), r=["wk0", "ts"], w=["sel"])
                P.V(lambda e, hd=hd, cv=cv, c2v=c2v: e.match_replace(out=c2v, in_to_replace=ts[:, hd, 0:8], in_values=cv, imm_value=-1e30), r=["wk0", "ts"], w=["wk2"])
                P.V(lambda e, hd=hd, c2v=c2v: e.max(out=ts[:, hd, 8:16], in_=c2v), r=["wk2"], w=["ts"])
                P.V(lambda e, hd=hd, c2v=c2v: e.max_index(out=sel[:, hd, 8:16], in_max=ts[:, hd, 8:16], in_values=c2v), r=["wk2", "ts"], w=["sel"])
            selv = sel[:].rearrange("p h k -> p (h k)")
            P.V(lambda e: e.tensor_scalar(out=au[:, 0, :], in0=selv, scalar1=4, scalar2=None, op0=ALU.logical_shift_right), r=["sel"], w=["au"])
            P.V(lambda e: e.tensor_scalar(out=au[:, 1, :], in0=selv, scalar1=15, scalar2=None, op0=ALU.bitwise_and), r=["sel"], w=["au"])
            P.V(lambda e: e.tensor_copy(out=af[:], in_=au[:]), r=["au"], w=["af"])
            P.V(lambda e: e.tensor_copy(out=i16f[:], in_=i16[:]), r=["i16"], w=["i16f"])
            eq = wk[1][:].rearrange("p (h s a) -> p h s a", h=8, s=16)
            i16r = i16f[:].rearrange("p (h t) k -> p h t k", t=2)
            for half in range(2):
                P.V(lambda e, half=half: e.tensor_tensor(out=eq, in0=af[:, half, :].rearrange("p (h s) -> p h s", h=8).unsqueeze(3).to_broadcast([128, 8, 16, 16]),
                                                         in1=iota[:].unsqueeze(1).unsqueeze(1).to_broadcast([128, 8, 16, 16]), op=ALU.is_equal), r=["af", "iota"], w=["wk1"])
                P.V(lambda e, half=half: e.tensor_tensor(out=eq, in0=eq, in1=i16r[:, :, half, :].unsqueeze(2).to_broadcast([128, 8, 16, 16]), op=ALU.mult), r=["wk1", "i16f"], w=["wk1"])
                P.V(lambda e, half=half: e.tensor_reduce(out=isel[:, half, :].rearrange("p (h s) -> p h s", h=8), in_=eq, axis=AX.X, op=ALU.add), r=["wk1"], w=["isel"])
            P.V(lambda e: e.scalar_tensor_tensor(out=idxf[:], in0=isel[:, 0, :], scalar=128.0, in1=isel[:, 1, :], op0=ALU.mult, op1=ALU.add), r=["isel"], w=["idxf"])
            if layer > 0:
                P.V(lambda e: e.tensor_scalar(out=idxf[:], in0=idxf[:], scalar1=float(layer * 16384), scalar2=None, op0=ALU.add), r=["idxf"], w=["idxf"])
            P.V(lambda e, s=s: e.tensor_copy(out=idxu[s][:], in_=idxf[:]), r=["idxf"], w=[f"idxu{s}"])
            g3 = gsm[s][:].rearrange("p (h k) -> p h k", h=8)
            P.V(lambda e, g3=g3: e.tensor_tensor(out=g3, in0=ts[:], in1=ts[:, :, 0:1].to_broadcast([128, 8, 16]), op=ALU.subtract), r=["ts"], w=[f"g{s}"])
            P.A(lambda e, s=s: e.activation(out=gsm[s][:], in_=gsm[s][:], func=AF.Exp), r=[f"g{s}"], w=[f"g{s}"])
            P.V(lambda e, g3=g3: e.tensor_reduce(out=sm[:, 0:8], in_=g3, axis=AX.X, op=ALU.add), r=[f"g{s}"], w=["sm"])
            P.V(lambda e: e.reciprocal(out=sm[:, 8:16], in_=sm[:, 0:8]), r=["sm"], w=["sm"])
            P.V(lambda e, g3=g3: e.tensor_tensor(out=g3, in0=g3, in1=sm[:, 8:16].unsqueeze(2).to_broadcast([128, 8, 16]), op=ALU.mult), r=[f"g{s}", "sm"], w=[f"g{s}"])

        gcount = [0]

        def gather(tab, s, k):
            P = PP[0]
            slot = gcount[0] % NB
            gcount[0] += 1
            P.op("gpsimd", lambda e, slot=slot, s=s, k=k, tab=tab: e.indirect_dma_start(out=ring[slot][:], out_offset=None, in_=tab,
                                                                                in_offset=bass.IndirectOffsetOnAxis(ap=idxu[s][:, k:k + 1], axis=0)),
                 r=[f"idxu{s}"], w=[f"ring{slot}"], dma=True, chan=f"ring{slot}")
            return slot

        def back(n):
            P = PP[0]
            t = tiles[n]
            s = n % 2
            for k in range(128):
                slot = gather(utab, s, k)
                P.V(lambda e, slot=slot, s=s, k=k: e.scalar_tensor_tensor(out=junk[:], in0=ring[slot][:], scalar=1.0, in1=h[s][:], op0=ALU.mult, op1=ALU.mult, accum_out=z[:, k:k + 1]),
                    r=[f"ring{slot}", f"h{s}"], w=["junk", "z"])
            P.V(lambda e: e.tensor_tensor(out=zt[:, 0, :], in0=z[:], in1=z[:], op=ALU.mult), r=["z"], w=["zt0"])
            P.V(lambda e: e.tensor_scalar(out=zt[:, 0, :], in0=zt[:, 0, :], scalar1=0.044715, scalar2=1.0, op0=ALU.mult, op1=ALU.add), r=["zt0"], w=["zt0"])
            P.V(lambda e: e.tensor_tensor(out=zt[:, 0, :], in0=zt[:, 0, :], in1=z[:], op=ALU.mult), r=["zt0", "z"], w=["zt0"])
            P.A(lambda e: e.activation(out=zt[:, 1, :], in_=zt[:, 0, :], func=AF.Tanh, scale=0.7978845608028654), r=["zt0"], w=["zt1"])
            P.V(lambda e: e.tensor_scalar(out=zt[:, 1, :], in0=zt[:, 1, :], scalar1=1.0, scalar2=0.5, op0=ALU.add, op1=ALU.mult), r=["zt1"], w=["zt1"])
            P.V(lambda e: e.tensor_tensor(out=zt[:, 2, :], in0=zt[:, 1, :], in1=z[:], op=ALU.mult), r=["zt1", "z"], w=["zt2"])
            P.V(lambda e, s=s: e.tensor_tensor(out=actw[:], in0=zt[:, 2, :], in1=gsm[s][:], op=ALU.mult), r=["zt2", f"g{s}"], w=["actw"])
            for k in range(128):
                slot = gather(vtab, s, k)
                if k == 0:
                    P.V(lambda e, slot=slot: e.tensor_scalar(out=acc[:], in0=ring[slot][:], scalar1=actw[:, 0:1], scalar2=None, op0=ALU.mult), r=[f"ring{slot}", "actw"], w=["acc"])
                else:
                    P.V(lambda e, slot=slot, k=k: e.scalar_tensor_tensor(out=acc[:], in0=ring[slot][:], scalar=actw[:, k:k + 1], in1=acc[:], op0=ALU.mult, op1=ALU.add),
                        r=[f"ring{slot}", "actw", "acc"], w=["acc"])
            P.V(lambda e: e.tensor_tensor(out=acc[:], in0=acc[:], in1=gate[:], op=ALU.mult), r=["acc", "gate"], w=["acc"])
            P.V(lambda e, s=s: e.tensor_tensor(out=acc[:], in0=acc[:], in1=xt[s][:], op=ALU.add), r=["acc", f"xt{s}"], w=["acc"])
            if final:
                P.A(lambda e: e.activation(out=junk[:], in_=acc[:], func=AF.Square, accum_out=ss[:, 0:1]), r=["acc"], w=["junk", "ss"])
                P.V(lambda e: e.tensor_scalar(out=ss[:, 1:2], in0=ss[:, 0:1], scalar1=1.0 / D, scalar2=EPS, op0=ALU.mult, op1=ALU.add), r=["ss"], w=["ss1"])
                P.A(lambda e: e.activation(out=ss[:, 2:3], in_=ss[:, 1:2], func=AF.Sqrt), r=["ss1"], w=["ss2"])
                P.V(lambda e: e.reciprocal(out=ss[:, 3:4], in_=ss[:, 2:3]), r=["ss2"], w=["ss3"])
                P.V(lambda e: e.scalar_tensor_tensor(out=acc[:], in0=acc[:], scalar=ss[:, 3:4], in1=gfin[:], op0=ALU.mult, op1=ALU.mult), r=["acc", "ss3", "gfin"], w=["acc"])
            P.dma("sync", t["dst"], acc[:], r=["acc"], w=[], chan="st")

        GSZ = 6
        for g0 in range(0, len(tiles), GSZ):
            PP[0] = Prog(nc, f"{name}g{g0}")
            for n in range(g0, min(g0 + GSZ, len(tiles))):
                front(n)
                back(n)
            PP[0].emit()
            K.peer_stats = PP[0].stats
            nc.all_engine_barrier()


def phase_rec(K):
    nc, I, ps = K.nc, K.I, K.ps
    NTOK = NT * 128
    for pas in ("u", "g"):
        with ExitStack() as es:
            sb = lambda n, s, d=F32: es.enter_context(nc.sbuf_tensor(n, s, d))
            W = sb("r_W" + pas, [128, KC, D], BF16)
            Gt = sb("r_G" + pas, [128, D]); St = sb("r_S" + pas, [128, D])
            ss = sb("r_ss" + pas, [128, 4])
            xt = sb("r_xt" + pas, [128, D]); h = sb("r_h" + pas, [128, D])
            hT = sb("r_hT" + pas, [128, KC, 256], BF16)
            ost = sb("r_ost" + pas, [128, 16, 256])
            P = Prog(nc, "rp" + pas)
            load_w_bf16(P, W, I["rec_w_in"], D, "W", col0=(D if pas == "u" else 0))
            for blk in (range(NT // 2) if pas == "u" else range(1, NT // 2)):
                is_ctx = blk == 0
                if blk in (0, 1):
                    v = 1 if is_ctx else 0
                    prep_GS(K, P, Gt, St, I["norm_mix"][1], K.modd[1, v, D:2 * D], K.modd[1, v, 0:D], h, "a")
                for ti in range(2):
                    e_t = blk * 2 + ti
                    norm_mod_T(K, P, K.xs2[e_t * 128:(e_t + 1) * 128, :], xt, h, ss, Gt, St, "a", hT, "hT", ti * 128, (6, 7), 0)
                for j in range(16):
                    pb = j % 2
                    for kc in range(KC):
                        P.T(lambda e, kc=kc, j=j, pb=pb: e.matmul(ps[pb][:, 0:256], lhsT=W[:, kc, j * 128:(j + 1) * 128], rhs=hT[:, kc, :], start=(kc == 0), stop=(kc == KC - 1)),
                            r=["W", "hT"], w=[f"ps{pb}"])
                    fn = AF.Copy if pas == "u" else AF.Gelu_apprx_tanh
                    P.A(lambda e, j=j, pb=pb, fn=fn: e.activation(out=ost[:, j, :], in_=ps[pb][:, 0:256], func=fn), r=[f"ps{pb}"], w=["ost"])
                if pas == "u":
                    P.dma("sync", K.upre[:, :, blk * 256:(blk + 1) * 256].rearrange("c p t -> p c t"), ost[:], r=["ost"], w=["upre"], chan="ost")
                else:
                    l0 = (blk - 1) * 256
                    P.dma("sync", K.ggd[:, :, l0:l0 + 256].rearrange("c p t -> p c t"), ost[:], r=["ost"], w=["ggd"], chan="ost")
            P.emit()
        nc.all_engine_barrier()
    with ExitStack() as es:
        sb = lambda n, s, d=F32: es.enter_context(nc.sbuf_tensor(n, s, d))
        UPW = 4360
        up = sb("r_up", [128, 2, UPW])
        u = sb("r_u", [128, 2, NTOK]); ub = sb("r_ub", [128, 2, NTOK], BF16)
        A = sb("r_A", [128, NTOK]); Bt = sb("r_B", [128, NTOK])
        Y = [sb(f"r_Y{d}", [128, NTOK]) for d in range(2)]
        gg = sb("r_gg", [128, S]); ygb = sb("r_ygb", [128, S], BF16)
        wg = sb("r_wg", [128, 2, 2, 2, 256], BF16)
        cw = sb("r_cw", [128, 4, 16]); cb = sb("r_cb", [128, 16])
        ba = sb("r_ba", [128, 2, 16]); bx = sb("r_bx", [128, 2, 16]); lam = sb("r_lam", [128, 2, 16]); cl = sb("r_cl", [128, 2, 16])
        tmp = [[sb(f"r_t{i}{s}", [128, 512]) for i in range(4)] for s in range(2)]
        P = Prog(nc, "rscan")
        P.G(lambda e: e.memset(up[:], 0.0), r=[], w=["up0", "up1"])
        for k in range(4):
            P.dma("sync", cw[:, k, :], I["rec_conv_w"][k].rearrange("(c p) -> p c", p=128), r=[], w=["cw"], chan=f"cw{k}", allow_slow_non_contiguous=True)
        P.dma("sync", cb[:], I["rec_conv_b"].rearrange("(c p) -> p c", p=128), r=[], w=["cb"], chan="cb", allow_slow_non_contiguous=True)
        for d in range(2):
            P.dma("sync", ba[:, d, :], I["rec_b_a"][d].rearrange("(c p) -> p c", p=128), r=[], w=["ba"], chan=f"ba{d}", allow_slow_non_contiguous=True)
            P.dma("sync", bx[:, d, :], I["rec_b_x"][d].rearrange("(c p) -> p c", p=128), r=[], w=["bx"], chan=f"bx{d}", allow_slow_non_contiguous=True)
            P.dma("sync", lam[:, d, :], I["rec_lambda"][d].rearrange("(c p) -> p c", p=128), r=[], w=["lam"], chan=f"lam{d}", allow_slow_non_contiguous=True)
        P.A(lambda e: e.activation(out=cl[:], in_=lam[:], func=AF.Exp, scale=-1.0), r=["lam"], w=["cl"])
        P.A(lambda e: e.activation(out=cl[:], in_=cl[:], func=AF.Ln, bias=1.0), r=["cl"], w=["cl"])
        P.V(lambda e: e.tensor_scalar(out=cl[:], in0=cl[:], scalar1=-8.0, scalar2=None, op0=ALU.mult), r=["cl"], w=["cl"])
        for n in range(8):
            for c in range(2):
                ch = 2 * n + c
                P.dma("sync", up[:, c, 1:257], K.upre[ch, :, 0:256], r=[], w=[f"up{c}"], chan=f"upc{c}")
                P.dma("sync", up[:, c, 260:260 + S], K.upre[ch, :, 256:NTOK], r=[], w=[f"up{c}"], chan=f"upl{c}")
                for (o0, nn, i0) in ((0, 256, 0), (256, S, 259)):
                    useg = u[:, c, o0:o0 + nn]
                    P.V(lambda e, c=c, ch=ch, useg=useg, i0=i0, nn=nn: e.tensor_scalar(out=useg, in0=up[:, c, i0:i0 + nn], scalar1=cw[:, 0, ch:ch + 1], scalar2=cb[:, ch:ch + 1], op0=ALU.mult, op1=ALU.add),
                        r=[f"up{c}", "cw", "cb"], w=[f"u{c}"])
                    for k in range(1, 4):
                        P.V(lambda e, c=c, ch=ch, useg=useg, i0=i0, nn=nn, k=k: e.scalar_tensor_tensor(out=useg, in0=up[:, c, i0 + k:i0 + k + nn], scalar=cw[:, k, ch:ch + 1], in1=useg, op0=ALU.mult, op1=ALU.add),
                            r=[f"up{c}", "cw", f"u{c}"], w=[f"u{c}"])
                P.A(lambda e, c=c: e.activation(out=ub[:, c, :], in_=u[:, c, :], func=AF.Copy), r=[f"u{c}"], w=["ub"])
            for d in range(2):
                for ty in range(2):
                    wsrc = (I["rec_w_a"] if ty == 0 else I["rec_w_x"])[d, n].rearrange("(k p) o -> p k o", p=128)
                    P.dma("gpsimd", wg[:, d, ty, :, :], wsrc, r=[], w=["wg"], chan=f"wg{d}{ty}")
            for oc in range(2):
                ch = 2 * n + oc
                for d in range(2):
                    for tb in range(9):
                        t0 = tb * 512
                        tn = min(512, NTOK - t0)
                        sl = tb % 2
                        pa, px = 2 * sl, 2 * sl + 1
                        r_, i_, a2, sq = tmp[sl]
                        for ty, pb in ((0, pa), (1, px)):
                            for kc in range(2):
                                P.T(lambda e, ty=ty, pb=pb, kc=kc, d=d, oc=oc, t0=t0, tn=tn: e.matmul(ps[pb][:, 0:tn], lhsT=wg[:, d, ty, kc, oc * 128:(oc + 1) * 128], rhs=ub[:, kc, t0:t0 + tn], start=(kc == 0), stop=(kc == 1)),
                                    r=["wg", "ub"], w=[f"ps{pb}"])
                        P.A(lambda e, pa=pa, tn=tn, d=d, ch=ch, r_=r_: e.activation(out=r_[:, 0:tn], in_=ps[pa][:, 0:tn], func=AF.Sigmoid, bias=ba[:, d, ch:ch + 1]), r=[f"ps{pa}", "ba"], w=[f"t0{sl}"])
                        P.A(lambda e, px=px, tn=tn, d=d, ch=ch, i_=i_: e.activation(out=i_[:, 0:tn], in_=ps[px][:, 0:tn], func=AF.Sigmoid, bias=bx[:, d, ch:ch + 1]), r=[f"ps{px}", "bx"], w=[f"t1{sl}"])
                        P.A(lambda e, tn=tn, t0=t0, d=d, ch=ch, r_=r_: e.activation(out=A[:, t0:t0 + tn], in_=r_[:, 0:tn], func=AF.Exp, scale=cl[:, d, ch:ch + 1]), r=[f"t0{sl}", "cl"], w=["A"])
                        P.G(lambda e, tn=tn, t0=t0, a2=a2: e.tensor_tensor(out=a2[:, 0:tn], in0=A[:, t0:t0 + tn], in1=A[:, t0:t0 + tn], op=ALU.mult), r=["A"], w=[f"t2{sl}"])
                        P.A(lambda e, tn=tn, a2=a2, sq=sq: e.activation(out=sq[:, 0:tn], in_=a2[:, 0:tn], func=AF.Sqrt, scale=-1.0, bias=1.0), r=[f"t2{sl}"], w=[f"t3{sl}"])
                        P.V(lambda e, tn=tn, sq=sq, i_=i_: e.tensor_tensor(out=sq[:, 0:tn], in0=sq[:, 0:tn], in1=i_[:, 0:tn], op=ALU.mult), r=[f"t3{sl}", f"t1{sl}"], w=[f"t3{sl}"])
                        P.V(lambda e, tn=tn, t0=t0, sq=sq, oc=oc: e.tensor_tensor(out=Bt[:, t0:t0 + tn], in0=sq[:, 0:tn], in1=u[:, oc, t0:t0 + tn], op=ALU.mult), r=[f"t3{sl}", f"u{oc}"], w=["B"])
                    Yd = Y[d]
                    if d == 0:
                        P.V(lambda e, Yd=Yd: e.tensor_tensor_scan(out=Yd[:, 0:256], data0=A[:, 0:256], data1=Bt[:, 0:256], initial=0.0, op0=ALU.mult, op1=ALU.add), r=["A", "B"], w=[f"Y{d}c"])
                        P.V(lambda e, Yd=Yd: e.tensor_tensor_scan(out=Yd[:, 256:NTOK], data0=A[:, 256:NTOK], data1=Bt[:, 256:NTOK], initial=Yd[:, 255:256], op0=ALU.mult, op1=ALU.add), r=["A", "B", f"Y{d}c"], w=[f"Y{d}l"])
                    else:
                        P.V(lambda e, Yd=Yd: e.tensor_tensor_scan(out=Yd[:, 0:256][:, ::-1], data0=A[:, 0:256][:, ::-1], data1=Bt[:, 0:256][:, ::-1], initial=0.0, op0=ALU.mult, op1=ALU.add), r=["A", "B"], w=[f"Y{d}c"])
                        P.V(lambda e, Yd=Yd: e.tensor_tensor_scan(out=Yd[:, 256:NTOK][:, ::-1], data0=A[:, 256:NTOK][:, ::-1], data1=Bt[:, 256:NTOK][:, ::-1], initial=Yd[:, 0:1], op0=ALU.mult, op1=ALU.add), r=["A", "B", f"Y{d}c"], w=[f"Y{d}l"])
                P.dma("sync", gg[:], K.ggd[ch], r=[], w=["gg"], chan="gg")
                P.G(lambda e: e.tensor_tensor(out=Y[0][:, 256:NTOK], in0=Y[0][:, 256:NTOK], in1=Y[1][:, 256:NTOK], op=ALU.add), r=["Y0l", "Y1l"], w=["Y0l"])
                P.V(lambda e: e.tensor_tensor(out=ygb[:], in0=Y[0][:, 256:NTOK], in1=gg[:], op=ALU.mult), r=["Y0l", "gg"], w=["ygb"])
                P.dma("sync", K.ygd[:, ch, :], ygb[:], r=["ygb"], w=["ygd"], chan="ygb")
        P.emit()
    nc.all_engine_barrier()
    with ExitStack() as es:
        sb = lambda n, s, d=F32: es.enter_context(nc.sbuf_tensor(n, s, d))
        Wout = sb("r_Wout", [128, KC, D], BF16)
        gate = sb("r_gate", [128, D])
        yt = [sb(f"r_yt{i}", [128, KC, 128], BF16) for i in range(2)]
        xr = [sb(f"r_xr{i}", [128, D]) for i in range(2)]
        y = sb("r_y", [128, D])
        P = Prog(nc, "rout")
        load_w_bf16(P, Wout, I["rec_w_out"], D, "Wo")
        load_bcast(P, "sync", gate[:], K.modd[1, 0, 2 * D:3 * D], "gate", "gate")
        for t in range(S // 128):
            s = t % 2
            P.dma("sync", yt[s][:], K.ygd[:, :, t * 128:(t + 1) * 128], r=[], w=[f"yt{s}"], chan=f"yt{s}")
            P.dma("sync", xr[s][:], K.xs2[(t + 2) * 128:(t + 3) * 128, :], r=[], w=[f"xr{s}"], chan=f"xr{s}")
            for nb in range(4):
                pb = 4 + nb
                for kc in range(KC):
                    P.T(lambda e, kc=kc, nb=nb, pb=pb, s=s: e.matmul(ps[pb][:], lhsT=yt[s][:, kc, :], rhs=Wout[:, kc, nb * 512:(nb + 1) * 512], start=(kc == 0), stop=(kc == KC - 1)),
                        r=[f"yt{s}", "Wo"], w=[f"ps{pb}"])
                P.V(lambda e, nb=nb, pb=pb: e.tensor_tensor(out=y[:, nb * 512:(nb + 1) * 512], in0=ps[pb][:], in1=gate[:, nb * 512:(nb + 1) * 512], op=ALU.mult), r=[f"ps{pb}", "gate"], w=[f"y{nb}"])
                P.G(lambda e, nb=nb, s=s: e.tensor_tensor(out=y[:, nb * 512:(nb + 1) * 512], in0=y[:, nb * 512:(nb + 1) * 512], in1=xr[s][:, nb * 512:(nb + 1) * 512], op=ALU.add), r=[f"y{nb}", f"xr{s}"], w=[f"y{nb}"])
            P.dma("sync", K.xs3[t * 128:(t + 1) * 128, :], y[:], r=[f"y{nb}" for nb in range(4)], w=["xs3"], chan="y")
        P.emit()
```

```python
import os
import numpy as np
import ml_dtypes
from contextlib import ExitStack
import concourse.bass as bass
import concourse.mybir as mybir
from concourse.bass_utils import run_bass_kernel_spmd

F32 = mybir.dt.float32
BF16 = mybir.dt.bfloat16
U32 = mybir.dt.uint32
ALU = mybir.AluOpType
AF = mybir.ActivationFunctionType
AX = mybir.AxisListType

D = 2048
KC = 16
S = 4096
C = 256
NT = (S + C) // 128
EPS = 1e-6
COMPUTE = ("tensor", "vector", "scalar", "gpsimd")


class Prog:
    def __init__(self, nc, name="p"):
        self.nc = nc
        self.name = name
        self.ops = []
        self.last_w = {}
        self.readers = {}

    def op(self, eng, fn, r=(), w=(), dma=False, chan=None):
        i = len(self.ops)
        deps = set()
        for k in r:
            lw = self.last_w.get(k)
            if lw is not None:
                deps.add(lw)
        for k in w:
            lw = self.last_w.get(k)
            if lw is not None:
                deps.add(lw)
            rd = self.readers.get(k)
            if rd:
                deps.update(rd.values())
        for k in w:
            self.last_w[k] = i
            self.readers[k] = {}
        for k in r:
            d = self.readers.setdefault(k, {})
            d[("dma", i) if dma else eng] = i
        if dma:
            assert chan is not None
        self.ops.append(dict(eng=eng, fn=fn, deps=deps, dma=dma, chan=chan))
        return i

    def dma(self, eng, out, in_, r, w, chan, **kw):
        return self.op(eng, lambda e: e.dma_start(out=out, in_=in_, **kw), r=r, w=w, dma=True, chan=chan)

    def V(self, fn, r, w):
        return self.op("vector", fn, r, w)

    def A(self, fn, r, w):
        return self.op("scalar", fn, r, w)

    def G(self, fn, r, w):
        return self.op("gpsimd", fn, r, w)

    def T(self, fn, r, w):
        return self.op("tensor", fn, r, w)

    def emit(self):
        nc = self.nc
        ops = self.ops
        needed = set()
        for o in ops:
            for d in o["deps"]:
                od = ops[d]
                if od["dma"]:
                    continue
                if od["eng"] == "tensor" and o["eng"] == "tensor" and not o["dma"]:
                    continue
                needed.add(d)
        cnt = {e: 0 for e in COMPUTE + ("sync",)}
        chan_cnt = {}
        for i, o in enumerate(ops):
            if o["dma"]:
                c = o["chan"]
                chan_cnt[c] = chan_cnt.get(c, 0) + 16
                o["sig"] = ("c", c, chan_cnt[c])
            elif i in needed:
                cnt[o["eng"]] += 1
                o["sig"] = ("e", o["eng"], cnt[o["eng"]])
            else:
                o["sig"] = None
        self.stats = dict(cnt=dict(cnt), chans=len(chan_cnt), maxchan=max(chan_cnt.values()) if chan_cnt else 0, nops=len(ops))
        esem = {e: nc.alloc_semaphore(name=f"{self.name}_e_{e}") for e in COMPUTE if cnt[e] > 0}
        csem = {c: nc.alloc_semaphore(name=f"{self.name}_c_{c}") for c in chan_cnt}
        with ExitStack() as es:
            block = es.enter_context(nc.Block())
            by_eng = {}
            for i, o in enumerate(ops):
                by_eng.setdefault(o["eng"], []).append(i)

            def make(engname, idxs):
                def body(eng):
                    waited = {}
                    for i in idxs:
                        o = ops[i]
                        for d in sorted(o["deps"]):
                            od = ops[d]
                            sig = od["sig"]
                            if sig is None:
                                continue
                            if sig[0] == "e":
                                if od["eng"] == "tensor" and engname == "tensor" and not o["dma"]:
                                    continue
                                sem = esem[sig[1]]
                            else:
                                sem = csem[sig[1]]
                            key = (sig[0], sig[1])
                            if waited.get(key, 0) >= sig[2]:
                                continue
                            eng.wait_ge(sem, sig[2])
                            waited[key] = sig[2]
                        ins = o["fn"](eng)
                        sig = o["sig"]
                        if sig is not None:
                            if sig[0] == "e":
                                ins.then_inc(esem[sig[1]], 1)
                            else:
                                ins.then_inc(csem[sig[1]], 16)
                    last = {}
                    for i in idxs:
                        o = ops[i]
                        if o["dma"]:
                            last[o["chan"]] = o["sig"][2]
                    for c, v in last.items():
                        if waited.get(("c", c), 0) < v:
                            eng.wait_ge(csem[c], v)
                return body

            for engname, idxs in by_eng.items():
                getattr(block, engname)(make(engname, idxs))
        nc.all_engine_barrier()
        nc.clear_and_free_semaphores(list(esem.values()) + list(csem.values()))
        nc.all_engine_barrier()
        self.ops = []
        self.last_w = {}
        self.readers = {}


def _consts():
    ident = np.eye(128, dtype=np.float32)
    rotm = np.zeros((128, 128), np.float32)
    for m in range(128):
        base = 0 if m < 64 else 64
        d = m - base
        if d < 32:
            rotm[base + d + 32, m] = -1.0
        else:
            rotm[base + d - 32, m] = 1.0
    t = np.arange(S)
    row = (t // 64).astype(np.float32)
    col = (t % 64).astype(np.float32)
    inv = (np.float32(10000.0) ** (-np.arange(0, 64, 2, dtype=np.float32) / np.float32(64))).astype(np.float32)
    ang_r = row[:, None] * inv[None, :]
    ang_c = col[:, None] * inv[None, :]
    ang = np.concatenate([ang_r, ang_r, ang_c, ang_c], axis=-1).astype(np.float32)
    cosT = np.ascontiguousarray(np.cos(ang).astype(np.float32).T)
    sinT = np.ascontiguousarray(np.sin(ang).astype(np.float32).T)
    k = np.arange(128)[:, None]
    q = np.arange(128)[None, :]
    maskL = (k >= q).astype(ml_dtypes.bfloat16)
    maskR = (k <= q).astype(ml_dtypes.bfloat16)
    iota16 = np.tile(np.arange(16, dtype=np.float32)[None, :], (128, 1))
    return dict(ident=ident, rotm=rotm, cosT=cosT, sinT=sinT, maskL=maskL, maskR=maskR, iota16=iota16)


class Ctx:
    pass


def build(stop_after=99, debug=False, peer_tiles=None, start_at=0, pair_split=False):
    nc = bass.Bass("TRN2", target_bir_lowering=False)
    K = Ctx()
    K.nc = nc
    din = lambda n, s, d=F32: nc.dram_tensor(n, list(s), d, kind="ExternalInput").ap()
    dscr = lambda n, s, d=F32: nc.dram_tensor(n, list(s), d, kind="Internal").ap()
    I = dict(
        x=din("x", [S, D]), c=din("c", [D]), ctx=din("ctx", [C, D]), c_ctx=din("c_ctx", [D]),
        w_mod=din("w_mod", [2, D, 6 * D]), b_mod=din("b_mod", [2, 6 * D]),
        norm_mix=din("norm_mix", [2, D]), norm_ffn=din("norm_ffn", [2, D]), norm_final=din("norm_final", [D]),
        attn_w_qkv=din("attn_w_qkv", [D, 3072]), attn_w_o=din("attn_w_o", [D, D]), attn_sink=din("attn_sink", [16]),
        rec_w_in=din("rec_w_in", [D, 2 * D]), rec_conv_w=din("rec_conv_w", [4, D]), rec_conv_b=din("rec_conv_b", [D]),
        rec_w_a=din("rec_w_a", [2, 8, 256, 256]), rec_b_a=din("rec_b_a", [2, D]),
        rec_w_x=din("rec_w_x", [2, 8, 256, 256]), rec_b_x=din("rec_b_x", [2, D]),
        rec_lambda=din("rec_lambda", [2, D]), rec_w_out=din("rec_w_out", [D, D]),
        peer_w_q=din("peer_w_q", [2, D, D]), peer_keys=din("peer_keys", [2, 2, 128, 128]),
        peer_u=din("peer_u", [2, 16384, D]), peer_v=din("peer_v", [2, 16384, D]),
        ident=din("ident", [128, 128]), rotm=din("rotm", [128, 128]), cosT=din("cosT", [128, S]), sinT=din("sinT", [128, S]),
        maskL=din("maskL", [128, 128], BF16), maskR=din("maskR", [128, 128], BF16), iota16=din("iota16", [128, 16]),
        myrows=din("myrows", [128, 16], U32),
        rows0=din("rows0", [128, 17], U32),
    )
    K.I = I
    out = nc.dram_tensor("out", [S // 2, D], F32, kind="ExternalOutput").ap()
    K.out = out
    K.modd = dscr("modd", [2, 2, 6 * D])
    K.qd = dscr("qd", [128, 16, NT * 128], BF16)
    K.kd = dscr("kd", [128, 4, NT * 128], BF16)
    K.vd = dscr("vd", [NT * 128, 512], BF16)
    K.xs1 = dscr("xs1", [NT * 128, D])
    K.xs2 = dscr("xs2", [NT * 128, D]) if start_at < 3 else din("xs2", [NT * 128, D])
    K.xs3 = dscr("xs3", [S, D]) if start_at < 4 else din("xs3", [S, D])
    K.upre = dscr("upre", [16, 128, NT * 128])
    K.ggd = dscr("ggd", [16, 128, S])
    K.ygd = dscr("ygd", [128, 16, S], BF16)
    K.pair_split = pair_split
    if pair_split:
        K.xs2loc = dscr("xs2loc", [17 * 128, D])
        K.xs2all = dscr("xs2all", [2 * 17 * 128, D])

    def xs2tile(e):
        if not pair_split:
            return K.xs2[e * 128:(e + 1) * 128, :]
        if e < 2:
            r0 = (e * 17) * 128
        else:
            i = e - 2
            r0 = ((i // 16) * 17 + 1 + i % 16) * 128
        return K.xs2all[r0:r0 + 128, :]
    K.xs2tile = xs2tile
    K.uvb16 = dscr("uvb16", [2 * 16384, 2 * D], BF16)
    dbg = {}
    if debug:
        dbg["d_modd"] = nc.dram_tensor("d_modd", [2, 2, 6 * D], F32, kind="ExternalOutput").ap()
        dbg["d_xs1"] = nc.dram_tensor("d_xs1", [NT * 128, D], F32, kind="ExternalOutput").ap()
        dbg["d_xs2"] = nc.dram_tensor("d_xs2", [NT * 128, D], F32, kind="ExternalOutput").ap()
        dbg["d_xs3"] = nc.dram_tensor("d_xs3", [S, D], F32, kind="ExternalOutput").ap()
    K.dbg = dbg

    with ExitStack() as es:
        K.ps = [es.enter_context(nc.psum_tensor(f"ps{i}", [128, 512], F32)) for i in range(8)]
        K.ident = es.enter_context(nc.sbuf_tensor("identsb", [128, 128], F32))
        P = Prog(nc, "c0")
        P.dma("sync", K.ident[:], I["ident"], r=[], w=["ident"], chan="ident")
        P.emit()
        nc.all_engine_barrier()
        phase_mod(K)
        nc.all_engine_barrier()
        phase_tabs(K, [l for l in (0, 1) if (l == 0 and start_at <= 2 <= stop_after) or (l == 1 and stop_after >= 4)])
        nc.all_engine_barrier()
        if debug:
            copy_dram(K, K.modd.rearrange("l v n -> (l v) n"), dbg["d_modd"].rearrange("l v n -> (l v) n"), 4, "cpm")
        if stop_after >= 1 and start_at <= 1:
            phase_attn(K)
            nc.all_engine_barrier()
            if debug:
                copy_dram(K, K.xs1, dbg["d_xs1"], NT * 128, "cpx1")
        if stop_after >= 2 and start_at <= 2:
            if pair_split:
                tl = [dict(src=("g", K.xs1, j), v=1 if j == 0 else 0, dst=K.xs2loc[j * 128:(j + 1) * 128, :]) for j in range(17)]
                phase_peer(K, 0, tl, False, "pe0", idxname="rows0")
                nc.all_engine_barrier()
                P = Prog(nc, "cc")
                P.op("gpsimd", lambda e: e.collective_compute("AllGather", ALU.bypass, replica_groups=[[0, 1], [2, 3], [4, 5], [6, 7]],
                                                              ins=[K.xs2loc], outs=[K.xs2all]), r=[], w=[], dma=True, chan="cc")
                P.emit()
            else:
                tl = [dict(src=("d", K.xs1[n * 128:(n + 1) * 128, :]), v=1 if n < 2 else 0, dst=K.xs2[n * 128:(n + 1) * 128, :]) for n in range(NT)]
                if peer_tiles is not None:
                    tl = [tl[i] for i in peer_tiles]
                phase_peer(K, 0, tl, False, "pe0")
            nc.all_engine_barrier()
            if debug:
                copy_dram(K, K.xs2, dbg["d_xs2"], NT * 128, "cpx2")
        if stop_after >= 3 and start_at <= 3:
            phase_rec(K)
            nc.all_engine_barrier()
            if debug:
                copy_dram(K, K.xs3, dbg["d_xs3"], S, "cpx3")
        if stop_after >= 4:
            tl = [dict(src=("g", K.xs3, t), v=0, dst=K.out[t * 128:(t + 1) * 128, :]) for t in range(S // 256)]
            if peer_tiles is not None:
                tl = tl[:len(peer_tiles)]
            phase_peer(K, 1, tl, True, "pe1")
    return nc


def copy_dram(K, src, dst, rows, name):
    nc = K.nc
    with ExitStack() as es:
        t = es.enter_context(nc.sbuf_tensor(name + "_t", [128, src.shape[1]], src.dtype))
        P = Prog(nc, name)
        for r0 in range(0, rows, 128):
            n = min(128, rows - r0)
            P.dma("sync", t[0:n, :], src[r0:r0 + n, :], r=[], w=["t"], chan="ld")
            P.dma("sync", dst[r0:r0 + n, :], t[0:n, :], r=["t"], w=[], chan="st")
        P.emit()
    nc.all_engine_barrier()


def phase_mod(K):
    nc, I = K.nc, K.I
    with ExitStack() as es:
        sb = lambda n, s, d=F32: es.enter_context(nc.sbuf_tensor(n, s, d))
        cc = sb("m_cc", [128, KC, 2])
        craw = sb("m_craw", [128, 2, KC])
        wt = [sb(f"m_wt{i}", [128, KC, 512]) for i in range(2)]
        bt = sb("m_bt", [2, 512])
        ot = [sb(f"m_ot{i}", [2, 512]) for i in range(2)]
        P = Prog(nc, "mod")
        P.dma("sync", craw[:, 0, :], I["c"].rearrange("(k p) -> p k", p=128), r=[], w=["craw0"], chan="craw0", allow_slow_non_contiguous=True)
        P.dma("sync", craw[:, 1, :], I["c_ctx"].rearrange("(k p) -> p k", p=128), r=[], w=["craw1"], chan="craw1", allow_slow_non_contiguous=True)
        for v in range(2):
            P.A(lambda e, v=v: e.activation(out=cc[:, :, v], in_=craw[:, v, :], func=AF.Silu), r=[f"craw{v}"], w=["cc"])
        n = 0
        for l in range(2):
            for j in range(24):
                s = n % 2
                w_src = I["w_mod"][l].rearrange("(k p) n -> p k n", p=128)[:, :, j * 512:(j + 1) * 512]
                P.dma("sync" if s == 0 else "gpsimd", wt[s][:], w_src, r=[], w=[f"wt{s}"], chan=f"wt{s}")
                for v in range(2):
                    P.dma("sync", bt[v:v + 1, :], I["b_mod"][l:l + 1, j * 512:(j + 1) * 512], r=[], w=["bt"], chan=f"bt{v}")
                pb = K.ps[s]
                for kc in range(KC):
                    P.T(lambda e, kc=kc, s=s, pb=pb: e.matmul(pb[0:2, :], lhsT=cc[:, kc, :], rhs=wt[s][:, kc, :], start=(kc == 0), stop=(kc == KC - 1)),
                        r=["cc", f"wt{s}"], w=[f"ps{s}"])
                P.V(lambda e, s=s, pb=pb: e.tensor_tensor(out=ot[s][:], in0=pb[0:2, :], in1=bt[:], op=ALU.add), r=[f"ps{s}", "bt"], w=[f"ot{s}"])
                P.dma("sync", K.modd[l, :, j * 512:(j + 1) * 512], ot[s][:], r=[f"ot{s}"], w=[], chan=f"ot{s}")
                n += 1
        P.emit()


def load_bcast(P, eng, tile_ap, src_row_ap, key, chan):
    P.dma(eng, tile_ap, src_row_ap.partition_broadcast(128), r=[], w=[key], chan=chan)


def prep_GS(K, P, Gt, St, gain_row, scale_row, shift_row, tmp, tag):
    load_bcast(P, "sync", Gt[:], scale_row, f"G{tag}", f"G{tag}")
    load_bcast(P, "sync", tmp[:], gain_row, "gstmp", "gstmp")
    load_bcast(P, "sync", St[:], shift_row, f"S{tag}", f"S{tag}")
    P.V(lambda e: e.scalar_tensor_tensor(out=Gt[:], in0=Gt[:], scalar=1.0, in1=tmp[:], op0=ALU.add, op1=ALU.mult), r=[f"G{tag}", "gstmp"], w=[f"G{tag}"])


def norm_mod_T(K, P, src_ap, xt, h, ss, Gt, St, gtag, hT, hT_key, tok0, tpb, xslot):
    ps = K.ps
    P.dma("sync", xt[:], src_ap, r=[], w=[f"xt{xslot}"], chan=f"xt{xslot}")
    P.A(lambda e: e.activation(out=h[:], in_=xt[:], func=AF.Square, accum_out=ss[:, 0:1]), r=[f"xt{xslot}"], w=["h", "ss"])
    P.V(lambda e: e.tensor_scalar(out=ss[:, 1:2], in0=ss[:, 0:1], scalar1=1.0 / D, scalar2=EPS, op0=ALU.mult, op1=ALU.add), r=["ss"], w=["ss1"])
    P.A(lambda e: e.activation(out=ss[:, 2:3], in_=ss[:, 1:2], func=AF.Sqrt), r=["ss1"], w=["ss2"])
    P.V(lambda e: e.reciprocal(out=ss[:, 3:4], in_=ss[:, 2:3]), r=["ss2"], w=["ss3"])
    P.V(lambda e: e.scalar_tensor_tensor(out=h[:], in0=xt[:], scalar=ss[:, 3:4], in1=Gt[:], op0=ALU.mult, op1=ALU.mult),
        r=[f"xt{xslot}", "ss3", f"G{gtag}"], w=["h"])
    P.G(lambda e: e.tensor_tensor(out=h[:], in0=h[:], in1=St[:], op=ALU.add), r=["h", f"S{gtag}"], w=["h"])
    if hT is None:
        return
    for q in range(4):
        b = tpb[q % 2]
        for j in range(4):
            kc = 4 * q + j
            P.T(lambda e, kc=kc, j=j, b=b: e.transpose(out=ps[b][:, j * 128:(j + 1) * 128], in_=h[:, kc * 128:(kc + 1) * 128], identity=K.ident[:]),
                r=["h", "ident"], w=[f"ps{b}"])
        P.A(lambda e, q=q, b=b: e.activation(out=hT[:, 4 * q:4 * q + 4, tok0:tok0 + 128], in_=ps[b][:].rearrange("p (j t) -> p j t", j=4), func=AF.Copy),
            r=[f"ps{b}"], w=[hT_key])


def load_w_bf16(P, wsb, w_dram, ncols, key, col0=0):
    for kc in range(KC):
        for c0 in range(0, ncols, 1024):
            cn = min(1024, ncols - c0)
            P.dma("gpsimd", wsb[:, kc, c0:c0 + cn], w_dram[kc * 128:(kc + 1) * 128, col0 + c0:col0 + c0 + cn], r=[], w=[key], chan=f"{key}_{kc % 4}")


def phase_attn(K):
    nc, I, ps = K.nc, K.I, K.ps
    SCALE = 128 ** -0.5
    with ExitStack() as es:
        sb = lambda n, s, d=F32: es.enter_context(nc.sbuf_tensor(n, s, d))
        with ExitStack() as es1:
            sb1 = lambda n, s, d=F32: es1.enter_context(nc.sbuf_tensor(n, s, d))
            W = sb1("a_W", [128, KC, 3072], BF16)
            Gt = sb1("a_G", [128, D]); St = sb1("a_S", [128, D])
            ss = sb1("a_ss", [128, 4])
            xt = sb1("a_xt", [128, D]); h = sb1("a_h", [128, D])
            hT = sb1("a_hT", [128, KC, 256], BF16)
            rotm = sb1("a_rotm", [128, 128])
            cs = sb1("a_cos", [128, 256]); sn = sb1("a_sin", [128, 256])
            qsb = [sb1(f"a_qsb{i}", [128, 256]) for i in range(2)]
            t1 = [sb1(f"a_t1{i}", [128, 256]) for i in range(2)]
            t2 = [sb1(f"a_t2{i}", [128, 256]) for i in range(2)]
            qst = sb1("a_qst", [128, 20, 256], BF16)
            vst = sb1("a_vst", [128, 2, 512], BF16)
            P = Prog(nc, "qkv")
            load_w_bf16(P, W, I["attn_w_qkv"], 3072, "W")
            P.dma("sync", rotm[:], I["rotm"], r=[], w=["rotm"], chan="rotm")
            for blk in range(NT // 2):
                is_ctx = blk == 0
                if blk in (0, 1):
                    v = 1 if is_ctx else 0
                    prep_GS(K, P, Gt, St, I["norm_mix"][0], K.modd[0, v, D:2 * D], K.modd[0, v, 0:D], h, "a")
                for ti in range(2):
                    e_t = blk * 2 + ti
                    src = I["ctx"][e_t * 128:(e_t + 1) * 128, :] if is_ctx else I["x"][(e_t - 2) * 128:(e_t - 1) * 128, :]
                    norm_mod_T(K, P, src, xt, h, ss, Gt, St, "a", hT, "hT", ti * 128, (6, 7), 0)
                if not is_ctx:
                    l0 = (blk - 1) * 256
                    P.dma("sync", cs[:], I["cosT"][:, l0:l0 + 256], r=[], w=["cos"], chan="cos")
                    P.dma("sync", sn[:], I["sinT"][:, l0:l0 + 256], r=[], w=["sin"], chan="sin")
                for j in range(20):
                    pb = j % 2
                    for kc in range(KC):
                        P.T(lambda e, kc=kc, j=j, pb=pb: e.matmul(ps[pb][:, 0:256], lhsT=W[:, kc, j * 128:(j + 1) * 128], rhs=hT[:, kc, :], start=(kc == 0), stop=(kc == KC - 1)),
                            r=["W", "hT"], w=[f"ps{pb}"])
                    dst, dkey = qst[:, j, :], "qst"
                    if is_ctx:
                        P.A(lambda e, pb=pb, dst=dst: e.activation(out=dst, in_=ps[pb][:, 0:256], func=AF.Copy), r=[f"ps{pb}"], w=[dkey])
                    else:
                        s2 = j % 2
                        P.A(lambda e, pb=pb, s2=s2: e.activation(out=qsb[s2][:], in_=ps[pb][:, 0:256], func=AF.Copy), r=[f"ps{pb}"], w=[f"qsb{s2}"])
                        P.T(lambda e, s2=s2: e.matmul(ps[2 + s2][:, 0:256], lhsT=rotm[:], rhs=qsb[s2][:], start=True, stop=True), r=["rotm", f"qsb{s2}"], w=[f"ps{2 + s2}"])
                        P.G(lambda e, s2=s2: e.tensor_tensor(out=t1[s2][:], in0=qsb[s2][:], in1=cs[:], op=ALU.mult), r=[f"qsb{s2}", "cos"], w=[f"t1{s2}"])
                        P.V(lambda e, s2=s2: e.tensor_tensor(out=t2[s2][:], in0=ps[2 + s2][:, 0:256], in1=sn[:], op=ALU.mult), r=[f"ps{2 + s2}", "sin"], w=[f"t2{s2}"])
                        P.V(lambda e, s2=s2, dst=dst: e.tensor_tensor(out=dst, in0=t1[s2][:], in1=t2[s2][:], op=ALU.add), r=[f"t1{s2}", f"t2{s2}"], w=[dkey])
                P.dma("sync", K.qd[:, :, blk * 256:(blk + 1) * 256], qst[:, 0:16, :], r=["qst"], w=["qd"], chan="qst")
                P.dma("sync", K.kd[:, :, blk * 256:(blk + 1) * 256], qst[:, 16:20, :], r=["qst"], w=["kd"], chan="kst")
                for ti in range(2):
                    e_t = blk * 2 + ti
                    pb = 4 + ti
                    for kc in range(KC):
                        P.T(lambda e, kc=kc, ti=ti, pb=pb: e.matmul(ps[pb][:], lhsT=hT[:, kc, ti * 128:(ti + 1) * 128], rhs=W[:, kc, 2560:3072], start=(kc == 0), stop=(kc == KC - 1)),
                            r=["W", "hT"], w=[f"ps{pb}"])
                    P.A(lambda e, ti=ti, pb=pb: e.activation(out=vst[:, ti, :], in_=ps[pb][:], func=AF.Copy), r=[f"ps{pb}"], w=[f"vst{ti}"])
                    P.dma("sync", K.vd[e_t * 128:(e_t + 1) * 128, :], vst[:, ti, :], r=[f"vst{ti}"], w=["vd"], chan=f"vst{ti}")
            P.emit()
        nc.all_engine_barrier()
        with ExitStack() as es2:
            sb2 = lambda n, s, d=F32: es2.enter_context(nc.sbuf_tensor(n, s, d))
            Wo = sb2("a_Wo", [128, KC, D], BF16)
            kT = sb2("a_kT", [128, 4, NT * 128], BF16)
            vx = sb2("a_vx", [128, NT, 4, 129], BF16)
            gate = sb2("a_gate", [128, D])
            esink = sb2("a_esink", [128, 16])
            qt = [sb2(f"a_qt{i}", [128, 16, 128], BF16) for i in range(2)]
            E = [sb2(f"a_E{i}", [128, 5, 512], BF16) for i in range(2)]
            mL = sb2("a_mL", [128, 128], BF16); mR = sb2("a_mR", [128, 128], BF16)
            o = sb2("a_o", [128, D]); oT = sb2("a_oT", [128, KC, 128], BF16)
            den = sb2("a_den", [128, 8])
            xt = [sb2(f"a_xr{i}", [128, D]) for i in range(2)]
            y = sb2("a_y", [128, D])
            sraw = sb2("a_sraw", [128, 16])
            P = Prog(nc, "att")
            load_w_bf16(P, Wo, I["attn_w_o"], D, "Wo")
            P.G(lambda e: e.memset(vx[:, :, :, 128:129], 1.0), r=[], w=["vx1"])
            for hh in range(4):
                P.dma("sync", kT[:, hh, :], K.kd[:, hh, :], r=[], w=["kT"], chan=f"kTl{hh}")
            for t in range(NT):
                P.dma("sync", vx[:, t, :, 0:128], K.vd[t * 128:(t + 1) * 128, :].rearrange("p (g d) -> p g d", g=4), r=[], w=["vx"], chan=f"vxl{t % 4}")
            P.dma("sync", mL[:], I["maskL"], r=[], w=["mL"], chan="mL")
            P.dma("sync", mR[:], I["maskR"], r=[], w=["mR"], chan="mR")
            load_bcast(P, "sync", sraw[:], I["attn_sink"], "sraw", "sraw")
            P.A(lambda e: e.activation(out=esink[:], in_=sraw[:], func=AF.Exp), r=["sraw"], w=["esink"])
            for e_t in range(NT):
                is_ctx = e_t < 2
                if e_t in (0, 2):
                    v = 1 if is_ctx else 0
                    load_bcast(P, "sync", gate[:], K.modd[0, v, 2 * D:3 * D], "gate", "gate")
                qs = e_t % 2
                P.dma("sync", qt[qs][:], K.qd[:, :, e_t * 128:(e_t + 1) * 128], r=["qd"], w=[f"qt{qs}"], chan=f"qt{qs}")
                src = I["ctx"][e_t * 128:(e_t + 1) * 128, :] if is_ctx else I["x"][(e_t - 2) * 128:(e_t - 1) * 128, :]
                P.dma("sync", xt[qs][:], src, r=[], w=[f"xr{qs}"], chan=f"xr{qs}")
                if is_ctx:
                    kbs = [0, 1]
                else:
                    kbs = [0, 1] + [kb for kb in (e_t - 1, e_t, e_t + 1) if 2 <= kb < NT]
                for hh in range(4):
                    Es = E[hh % 2]
                    Ek = f"E{hh % 2}"
                    for n, kb in enumerate(kbs):
                        pb = n % 2
                        P.T(lambda e, hh=hh, kb=kb, pb=pb, qs=qs: e.matmul(ps[pb][:], lhsT=kT[:, hh, kb * 128:(kb + 1) * 128],
                                                                      rhs=qt[qs][:, 4 * hh:4 * hh + 4, :].rearrange("p g q -> p (g q)"), start=True, stop=True),
                            r=["kT", f"qt{qs}"], w=[f"ps{pb}"])
                        P.A(lambda e, n=n, pb=pb, Es=Es: e.activation(out=Es[:, n, :], in_=ps[pb][:], func=AF.Exp, scale=SCALE), r=[f"ps{pb}"], w=[f"{Ek}_{n}"])
                        if (not is_ctx) and kb >= 2 and kb != e_t:
                            m = mL if kb == e_t - 1 else mR
                            P.G(lambda e, n=n, m=m, Es=Es: e.tensor_tensor(out=Es[:, n, :].rearrange("p (g q) -> p g q", g=4), in0=Es[:, n, :].rearrange("p (g q) -> p g q", g=4),
                                                                        in1=m[:].unsqueeze(1).to_broadcast([128, 4, 128]), op=ALU.mult),
                                r=[f"{Ek}_{n}", "mL", "mR"], w=[f"{Ek}_{n}"])
                    for g in range(4):
                        pb = 2 + g // 2
                        for n, kb in enumerate(kbs):
                            P.T(lambda e, g=g, n=n, kb=kb, pb=pb, hh=hh, Es=Es: e.matmul(ps[pb][:, (g % 2) * 256:(g % 2) * 256 + 129], lhsT=Es[:, n, g * 128:(g + 1) * 128],
                                                                                     rhs=vx[:, kb, hh, :], start=(n == 0), stop=(n == len(kbs) - 1)),
                                r=[f"{Ek}_{n}", "vx", "vx1"], w=[f"ps{pb}"])
                    for bk in range(2):
                        pb = 2 + bk
                        P.V(lambda e, bk=bk, pb=pb, hh=hh: e.tensor_tensor(out=den[:, 2 * bk:2 * bk + 2], in0=ps[pb][:].rearrange("p (g c) -> p g c", g=2)[:, :, 128],
                                                                       in1=esink[:, 4 * hh + 2 * bk:4 * hh + 2 * bk + 2], op=ALU.add),
                            r=[f"ps{pb}", "esink"], w=["den"])
                    P.V(lambda e: e.reciprocal(out=den[:, 4:8], in_=den[:, 0:4]), r=["den"], w=["rden"])
                    for g in range(4):
                        pb = 2 + g // 2
                        hd = 4 * hh + g
                        P.V(lambda e, g=g, pb=pb, hd=hd: e.tensor_scalar(out=o[:, hd * 128:(hd + 1) * 128], in0=ps[pb][:, (g % 2) * 256:(g % 2) * 256 + 128],
                                                                     scalar1=den[:, 4 + g:5 + g], scalar2=None, op0=ALU.mult),
                            r=[f"ps{pb}", "rden"], w=["o"])
                for q in range(4):
                    b = 6 + q % 2
                    for j in range(4):
                        kc = 4 * q + j
                        P.T(lambda e, kc=kc, j=j, b=b: e.transpose(out=ps[b][:, j * 128:(j + 1) * 128], in_=o[:, kc * 128:(kc + 1) * 128], identity=K.ident[:]),
                            r=["o", "ident"], w=[f"ps{b}"])
                    P.A(lambda e, q=q, b=b: e.activation(out=oT[:, 4 * q:4 * q + 4, :], in_=ps[b][:].rearrange("p (j t) -> p j t", j=4), func=AF.Copy), r=[f"ps{b}"], w=["oT"])
                for nb in range(4):
                    pb = 4 + nb
                    for kc in range(KC):
                        P.T(lambda e, kc=kc, nb=nb, pb=pb: e.matmul(ps[pb][:], lhsT=oT[:, kc, :], rhs=Wo[:, kc, nb * 512:(nb + 1) * 512], start=(kc == 0), stop=(kc == KC - 1)),
                            r=["oT", "Wo"], w=[f"ps{pb}"])
                    P.V(lambda e, nb=nb, pb=pb: e.tensor_tensor(out=y[:, nb * 512:(nb + 1) * 512], in0=ps[pb][:], in1=gate[:, nb * 512:(nb + 1) * 512], op=ALU.mult),
                        r=[f"ps{pb}", "gate"], w=[f"y{nb}"])
                    P.G(lambda e, nb=nb, qs=qs: e.tensor_tensor(out=y[:, nb * 512:(nb + 1) * 512], in0=y[:, nb * 512:(nb + 1) * 512], in1=xt[qs][:, nb * 512:(nb + 1) * 512], op=ALU.add),
                        r=[f"y{nb}", f"xr{qs}"], w=[f"y{nb}"])
                P.dma("sync", K.xs1[e_t * 128:(e_t + 1) * 128, :], y[:], r=[f"y{nb}" for nb in range(4)], w=["xs1"], chan="y")
            P.emit()


def _lay(inputs):
    consts = _consts()
    f = lambda a: np.ascontiguousarray(np.asarray(a, dtype=np.float32))
    shared = dict(
        c_ctx=f(inputs["c_ctx"]), w_mod=f(inputs["w_mod"]), b_mod=f(inputs["b_mod"]),
        norm_mix=f(inputs["norm_mix"]), norm_ffn=f(inputs["norm_ffn"]), norm_final=f(inputs["norm_final"]),
        attn_w_qkv=f(inputs["attn_w_qkv"][0]), attn_w_o=f(inputs["attn_w_o"][0]), attn_sink=f(inputs["attn_sink"][0]),
        rec_w_in=f(inputs["rec_w_in"][0]), rec_conv_w=f(inputs["rec_conv_w"][0]), rec_conv_b=f(inputs["rec_conv_b"][0]),
        rec_w_a=f(inputs["rec_w_a"][0]), rec_b_a=f(inputs["rec_b_a"][0]), rec_w_x=f(inputs["rec_w_x"][0]), rec_b_x=f(inputs["rec_b_x"][0]),
        rec_lambda=f(inputs["rec_lambda"][0]), rec_w_out=f(inputs["rec_w_out"][0]),
        peer_w_q=f(inputs["peer_w_q"]), peer_keys=f(inputs["peer_keys"]), peer_u=f(inputs["peer_u"]), peer_v=f(inputs["peer_v"]),
        **consts,
    )
    return shared, f


def core_inputs(inputs, shared, f, core):
    b, hf = core // 2, core % 2
    m = dict(shared)
    m["x"] = f(inputs["x"][b]); m["c"] = f(inputs["c"][b]); m["ctx"] = f(inputs["ctx"][b])
    r0 = np.zeros((128, 17), np.uint32)
    r0[:, 0] = hf * 128 + np.arange(128)
    for j in range(16):
        r0[:, 1 + j] = 256 + (hf * 16 + j) * 128 + np.arange(128)
    m["rows0"] = r0
    m["myrows"] = (hf * (S // 2) + np.arange(16, dtype=np.uint32)[None, :] * 128 + np.arange(128, dtype=np.uint32)[:, None]).astype(np.uint32)
    return m


PAIR_SPLIT = False


def kernel(**inputs):
    nc = build(pair_split=PAIR_SPLIT)
    shared, f = _lay(inputs)
    in_maps = [core_inputs(inputs, shared, f, core) for core in range(8)]
    res = run_bass_kernel_spmd(nc, in_maps, core_ids=list(range(8)))
    outp = np.empty((4, S, D), np.float32)
    for core in range(8):
        b, hf = core // 2, core % 2
        outp[b, hf * (S // 2):(hf + 1) * (S // 2)] = res.results[core]["out"]
    return outp


def phase_tabs(K, layers):
    nc, I = K.nc, K.I
    NR = 8
    with ExitStack() as es:
        tl = [es.enter_context(nc.sbuf_tensor(f"tb_t{i}", [128, 2, D], BF16)) for i in range(NR)]
        P = Prog(nc, "tabs")
        n = 0
        for (src, dst) in ((I["peer_u"].rearrange("l e d -> (l e) d"), K.uvb16[:, 0:D]), (I["peer_v"].rearrange("l e d -> (l e) d"), K.uvb16[:, D:2 * D])):
            for l in layers:
                for r0 in range(l * 16384, (l + 1) * 16384, 256):
                    sl = n % NR
                    for j in range(2):
                        P.dma("gpsimd", tl[sl][:, j, :], src[r0 + j * 128:r0 + (j + 1) * 128, :], r=[], w=[f"t{sl}"], chan=f"ld{sl}_{j}")
                    P.dma("sync" if n % 2 == 0 else "scalar", dst[r0:r0 + 256, :].rearrange("(j p) d -> p j d", p=128), tl[sl][:], r=[f"t{sl}"], w=[], chan=f"st{sl}")
                    n += 1
        P.emit()


def phase_peer(K, layer, tiles, final, name, idxname="myrows"):
    nc, I, ps = K.nc, K.I, K.ps
    NB = 5
    uvtab = K.uvb16
    with ExitStack() as es:
        sb = lambda n, s, d=F32: es.enter_context(nc.sbuf_tensor(name + n, s, d))
        Wq = sb("p_Wq", [128, KC, D], BF16)
        Gt = sb("p_G", [128, D]); St = sb("p_S", [128, D])
        variants = sorted(set(t["v"] for t in tiles))
        gates = {v: sb(f"p_gate{v}", [128, D]) for v in variants}
        xt = [sb(f"p_xt{i}", [128, D]) for i in range(2)]
        h = [sb(f"p_h{i}", [128, D]) for i in range(2)]
        hT = sb("p_hT", [128, KC, 128], BF16)
        wk = [sb(f"p_wk{i}", [128, D]) for i in range(2)]
        ring = [sb(f"p_ring{i}", [128, 2 * D], BF16) for i in range(NB)]
        junk = sb("p_junk", [128, D], BF16)
        acc = wk[1]
        keysT = sb("p_keysT", [128, 2, 128])
        kraw = sb("p_kraw", [128, 2, 128])
        ss = sb("p_ss", [128, 4]); ssb = sb("p_ssb", [128, 4])
        s16 = sb("p_s16", [128, 16, 16]); i16 = sb("p_i16", [128, 16, 16], U32); i16f = sb("p_i16f", [128, 16, 16])
        ts = sb("p_ts", [128, 8, 16]); sel = sb("p_sel", [128, 8, 16], U32)
        au = sb("p_au", [128, 2, 128], U32); af = sb("p_af", [128, 2, 128])
        isel = sb("p_isel", [128, 2, 128])
        idxf = sb("p_idxf", [128, 128])
        idxu = [sb(f"p_idxu{i}", [128, 128], U32) for i in range(2)]
        gsm = [sb(f"p_g{i}", [128, 128]) for i in range(2)]
        sm = sb("p_sm", [128, 16])
        z = sb("p_z", [128, 128]); gz = sb("p_gz", [128, 128]); av = sb("p_av", [128, 128])
        identb = sb("p_identb", [128, 128], BF16)
        dg = [sb(f"p_dg{i}", [128, 128], BF16) for i in range(4)]
        iota = sb("p_iota", [128, 16])
        mr = sb("p_mr", [128, I[idxname].shape[1]], U32)
        gfin = sb("p_gfin", [128, D]) if final else None
        P = Prog(nc, name)
        load_w_bf16(P, Wq, I["peer_w_q"][layer], D, "Wq")
        P.dma("sync", iota[:], I["iota16"], r=[], w=["iota"], chan="iota")
        P.A(lambda e: e.activation(out=identb[:], in_=K.ident[:], func=AF.Copy), r=["ident"], w=["identb"])
        P.dma("sync", mr[:], I[idxname], r=[], w=["mr"], chan="mr")
        if final:
            load_bcast(P, "sync", gfin[:], I["norm_final"], "gfin", "gfin")
        for v in variants:
            load_bcast(P, "sync", gates[v][:], K.modd[layer, v, 5 * D:6 * D], f"gate{v}", f"gate{v}")
        for p in range(2):
            P.dma("sync", kraw[:, p, :], I["peer_keys"][layer, p], r=[], w=["kraw"], chan=f"kraw{p}")
            P.T(lambda e, p=p: e.transpose(out=ps[p][:, 0:128], in_=kraw[:, p, :], identity=K.ident[:]), r=["kraw", "ident"], w=[f"ps{p}"])
            P.V(lambda e, p=p: e.tensor_copy(out=keysT[:, p, :], in_=ps[p][:, 0:128]), r=[f"ps{p}"], w=["keysT"])

        P.emit()
        nc.all_engine_barrier()
        cur_v = [None]
        PP = [None]

        def front(n):
            P = PP[0]
            t = tiles[n]
            s = n % 2
            if cur_v[0] != t["v"]:
                cur_v[0] = t["v"]
                v = t["v"]
                prep_GS(K, P, Gt, St, I["norm_ffn"][layer], K.modd[layer, v, 4 * D:5 * D], K.modd[layer, v, 3 * D:4 * D], wk[0], "p")
            if t["src"][0] == "d":
                P.dma("sync", xt[s][:], t["src"][1], r=[], w=[f"xt{s}"], chan=f"xt{s}")
            else:
                col = t["src"][2]
                P.op("gpsimd", lambda e, s=s, col=col, src=t["src"][1]: e.indirect_dma_start(out=xt[s][:], out_offset=None, in_=src,
                                                                                         in_offset=bass.IndirectOffsetOnAxis(ap=mr[:, col:col + 1], axis=0)),
                     r=["mr"], w=[f"xt{s}"], dma=True, chan=f"xt{s}")
            hh = h[s]
            hk = f"h{s}"
            P.A(lambda e, s=s: e.activation(out=h[s][:], in_=xt[s][:], func=AF.Square, accum_out=ss[:, 0:1]), r=[f"xt{s}"], w=[hk, "ss"])
            P.V(lambda e: e.tensor_scalar(out=ss[:, 1:2], in0=ss[:, 0:1], scalar1=1.0 / D, scalar2=EPS, op0=ALU.mult, op1=ALU.add), r=["ss"], w=["ss1"])
            P.A(lambda e: e.activation(out=ss[:, 2:3], in_=ss[:, 1:2], func=AF.Sqrt), r=["ss1"], w=["ss2"])
            P.V(lambda e: e.reciprocal(out=ss[:, 3:4], in_=ss[:, 2:3]), r=["ss2"], w=["ss3"])
            P.V(lambda e, s=s: e.scalar_tensor_tensor(out=h[s][:], in0=xt[s][:], scalar=ss[:, 3:4], in1=Gt[:], op0=ALU.mult, op1=ALU.mult), r=[f"xt{s}", "ss3", "Gp"], w=[hk])
            P.V(lambda e, s=s: e.tensor_tensor(out=h[s][:], in0=h[s][:], in1=St[:], op=ALU.add), r=[hk, "Sp"], w=[hk])
            for q in range(4):
                b = q % 2
                for j in range(4):
                    kc = 4 * q + j
                    P.T(lambda e, kc=kc, j=j, b=b, s=s: e.transpose(out=ps[b][:, j * 128:(j + 1) * 128], in_=h[s][:, kc * 128:(kc + 1) * 128], identity=K.ident[:]),
                        r=[hk, "ident"], w=[f"ps{b}"])
                P.A(lambda e, q=q, b=b: e.activation(out=hT[:, 4 * q:4 * q + 4, :], in_=ps[b][:].rearrange("p (j t) -> p j t", j=4), func=AF.Copy), r=[f"ps{b}"], w=["hT"])
            qT = wk[0]
            for c in range(16):
                pb = c % 2
                for kc in range(KC):
                    P.T(lambda e, kc=kc, c=c, pb=pb: e.matmul(ps[pb][:, 0:128], lhsT=Wq[:, kc, c * 128:(c + 1) * 128], rhs=hT[:, kc, :], start=(kc == 0), stop=(kc == KC - 1)),
                        r=["Wq", "hT"], w=[f"ps{pb}"])
                P.A(lambda e, c=c, pb=pb: e.activation(out=qT[:, c * 128:(c + 1) * 128], in_=ps[pb][:, 0:128], func=AF.Copy), r=[f"ps{pb}"], w=["wk0"])
            Ssb = wk[1]
            for q in range(4):
                pb = 2 + q % 2
                for c in range(4 * q, 4 * q + 4):
                    P.T(lambda e, c=c, pb=pb: e.matmul(ps[pb][:, (c % 4) * 128:(c % 4 + 1) * 128], lhsT=qT[:, c * 128:(c + 1) * 128], rhs=keysT[:, c % 2, :], start=True, stop=True),
                        r=["wk0", "keysT"], w=[f"ps{pb}"])
                P.A(lambda e, q=q, pb=pb: e.activation(out=Ssb[:, q * 512:(q + 1) * 512], in_=ps[pb][:], func=AF.Copy), r=[f"ps{pb}"], w=["wk1"])
            S2 = wk[0]
            for c in range(16):
                sv = Ssb[:, c * 128:(c + 1) * 128]
                s2v = S2[:, c * 128:(c + 1) * 128]
                P.V(lambda e, c=c, sv=sv: e.max(out=s16[:, c, 0:8], in_=sv), r=["wk1"], w=["s16"])
                P.V(lambda e, c=c, sv=sv: e.max_index(out=i16[:, c, 0:8], in_max=s16[:, c, 0:8], in_values=sv), r=["wk1", "s16"], w=["i16"])
                P.V(lambda e, c=c, sv=sv, s2v=s2v: e.match_replace(out=s2v, in_to_replace=s16[:, c, 0:8], in_values=sv, imm_value=-1e30), r=["wk1", "s16"], w=["wk0"])
                P.V(lambda e, c=c, s2v=s2v: e.max(out=s16[:, c, 8:16], in_=s2v), r=["wk0"], w=["s16"])
                P.V(lambda e, c=c, s2v=s2v: e.max_index(out=i16[:, c, 8:16], in_max=s16[:, c, 8:16], in_values=s2v), r=["wk0", "s16"], w=["i16"])
            cand = wk[1]
            c4 = cand[:].rearrange("p (h a b) -> p h a b", h=8, a=16)
            s16r = s16[:].rearrange("p (h t) k -> p h t k", t=2)
            P.V(lambda e: e.tensor_tensor(out=c4, in0=s16r[:, :, 0, :].unsqueeze(3).to_broadcast([128, 8, 16, 16]),
                                          in1=s16r[:, :, 1, :].unsqueeze(2).to_broadcast([128, 8, 16, 16]), op=ALU.add), r=["s16"], w=["wk1"])
            for hd in range(8):
                cv = cand[:, hd * 256:(hd + 1) * 256]
                c2v = S2[:, hd * 256:(hd + 1) * 256]
                P.V(lambda e, hd=hd, cv=cv: e.max(out=ts[:, hd, 0:8], in_=cv), r=["wk1"], w=["ts"])
                P.V(lambda e, hd=hd, cv=cv: e.max_index(out=sel[:, hd, 0:8], in_max=ts[:, hd, 0:8], in_values=cv), r=["wk1", "ts"], w=["sel"])
                P.V(lambda e, hd=hd, cv=cv, c2v=c2v: e.match_replace(out=c2v, in_to_replace=ts[:, hd, 0:8], in_values=cv, imm_value=-1e30), r=["wk1", "ts"], w=["wk0"])
                P.V(lambda e, hd=hd, c2v=c2v: e.max(out=ts[:, hd, 8:16], in_=c2v), r=["wk0"], w=["ts"])
                P.V(lambda e, hd=hd, c2v=c2v: e.max_index(out=sel[:, hd, 8:16], in_max=ts[:, hd, 8:16], in_values=c2v), r=["wk0", "ts"], w=["sel"])
            selv = sel[:].rearrange("p h k -> p (h k)")
            P.V(lambda e: e.tensor_scalar(out=au[:, 0, :], in0=selv, scalar1=4, scalar2=None, op0=ALU.logical_shift_right), r=["sel"], w=["au"])
            P.V(lambda e: e.tensor_scalar(out=au[:, 1, :], in0=selv, scalar1=15, scalar2=None, op0=ALU.bitwise_and), r=["sel"], w=["au"])
            P.V(lambda e: e.tensor_copy(out=af[:], in_=au[:]), r=["au"], w=["af"])
            P.V(lambda e: e.tensor_copy(out=i16f[:], in_=i16[:]), r=["i16"], w=["i16f"])
            eq = wk[1][:].rearrange("p (h s a) -> p h s a", h=8, s=16)
            i16r = i16f[:].rearrange("p (h t) k -> p h t k", t=2)
            for half in range(2):
                P.V(lambda e, half=half: e.tensor_tensor(out=eq, in0=af[:, half, :].rearrange("p (h s) -> p h s", h=8).unsqueeze(3).to_broadcast([128, 8, 16, 16]),
                                                         in1=iota[:].unsqueeze(1).unsqueeze(1).to_broadcast([128, 8, 16, 16]), op=ALU.is_equal), r=["af", "iota"], w=["wk1"])
                P.V(lambda e, half=half: e.tensor_tensor(out=eq, in0=eq, in1=i16r[:, :, half, :].unsqueeze(2).to_broadcast([128, 8, 16, 16]), op=ALU.mult), r=["wk1", "i16f"], w=["wk1"])
                P.V(lambda e, half=half: e.tensor_reduce(out=isel[:, half, :].rearrange("p (h s) -> p h s", h=8), in_=eq, axis=AX.X, op=ALU.add), r=["wk1"], w=["isel"])
            P.V(lambda e: e.scalar_tensor_tensor(out=idxf[:], in0=isel[:, 0, :], scalar=128.0, in1=isel[:, 1, :], op0=ALU.mult, op1=ALU.add), r=["isel"], w=["idxf"])
            if layer > 0:
                P.V(lambda e: e.tensor_scalar(out=idxf[:], in0=idxf[:], scalar1=float(layer * 16384), scalar2=None, op0=ALU.add), r=["idxf"], w=["idxf"])
            P.V(lambda e, s=s: e.tensor_copy(out=idxu[s][:], in_=idxf[:]), r=["idxf"], w=[f"idxu{s}"])
            g3 = gsm[s][:].rearrange("p (h k) -> p h k", h=8)
            P.V(lambda e, g3=g3: e.tensor_tensor(out=g3, in0=ts[:], in1=ts[:, :, 0:1].to_broadcast([128, 8, 16]), op=ALU.subtract), r=["ts"], w=[f"g{s}"])
            P.A(lambda e, s=s: e.activation(out=gsm[s][:], in_=gsm[s][:], func=AF.Exp), r=[f"g{s}"], w=[f"g{s}"])
            P.V(lambda e, g3=g3: e.tensor_reduce(out=sm[:, 0:8], in_=g3, axis=AX.X, op=ALU.add), r=[f"g{s}"], w=["sm"])
            P.V(lambda e: e.reciprocal(out=sm[:, 8:16], in_=sm[:, 0:8]), r=["sm"], w=["sm"])
            P.V(lambda e, g3=g3: e.tensor_tensor(out=g3, in0=g3, in1=sm[:, 8:16].unsqueeze(2).to_broadcast([128, 8, 16]), op=ALU.mult), r=[f"g{s}", "sm"], w=[f"g{s}"])

        gcount = [0]

        def gather(s, k):
            P = PP[0]
            slot = gcount[0] % NB
            gcount[0] += 1
            P.op("gpsimd", lambda e, slot=slot, s=s, k=k: e.indirect_dma_start(out=ring[slot][:], out_offset=None, in_=uvtab,
                                                                        in_offset=bass.IndirectOffsetOnAxis(ap=idxu[s][:, k:k + 1], axis=0)),
                 r=[f"idxu{s}"], w=[f"ring{slot}"], dma=True, chan=f"ring{slot}")
            return slot

        def back(n):
            P = PP[0]
            t = tiles[n]
            s = n % 2
            for k in range(128):
                slot = gather(s, k)
                P.V(lambda e, slot=slot, s=s, k=k: e.scalar_tensor_tensor(out=junk[:], in0=ring[slot][:, 0:D], scalar=1.0, in1=h[s][:], op0=ALU.mult, op1=ALU.mult, accum_out=z[:, k:k + 1]),
                    r=[f"ring{slot}", f"h{s}"], w=[f"z{k}"])
                P.A(lambda e, k=k: e.activation(out=gz[:, k:k + 1], in_=z[:, k:k + 1], func=AF.Gelu_apprx_tanh), r=[f"z{k}"], w=[f"gz{k}"])
                P.A(lambda e, k=k, s=s: e.activation(out=av[:, k:k + 1], in_=gz[:, k:k + 1], func=AF.Copy, scale=gsm[s][:, k:k + 1]), r=[f"gz{k}", f"g{s}"], w=[f"a{k}"])
                P.A(lambda e, k=k: e.activation(out=dg[k % 4][:], in_=identb[:], func=AF.Copy, scale=av[:, k:k + 1]), r=[f"a{k}", "identb"], w=[f"dg{k % 4}"])
                for nb in range(4):
                    P.T(lambda e, k=k, nb=nb, slot=slot: e.matmul(ps[4 + nb][:], lhsT=dg[k % 4][:], rhs=ring[slot][:, D + nb * 512:D + (nb + 1) * 512], start=(k == 0), stop=(k == 127)),
                        r=[f"dg{k % 4}", f"ring{slot}"], w=[f"ps{4 + nb}"])
            gate = gates[t["v"]]
            for nb in range(4):
                P.V(lambda e, nb=nb, gate=gate: e.tensor_tensor(out=acc[:, nb * 512:(nb + 1) * 512], in0=ps[4 + nb][:], in1=gate[:, nb * 512:(nb + 1) * 512], op=ALU.mult), r=[f"ps{4 + nb}"], w=["wk1"])
            P.V(lambda e, s=s: e.tensor_tensor(out=acc[:], in0=acc[:], in1=xt[s][:], op=ALU.add), r=["wk1", f"xt{s}"], w=["wk1"])
            if final:
                P.A(lambda e: e.activation(out=junk[:], in_=acc[:], func=AF.Square, accum_out=ssb[:, 0:1]), r=["wk1"], w=["junkf", "ssb"])
                P.V(lambda e: e.tensor_scalar(out=ssb[:, 1:2], in0=ssb[:, 0:1], scalar1=1.0 / D, scalar2=EPS, op0=ALU.mult, op1=ALU.add), r=["ssb"], w=["ssb1"])
                P.A(lambda e: e.activation(out=ssb[:, 2:3], in_=ssb[:, 1:2], func=AF.Sqrt), r=["ssb1"], w=["ssb2"])
                P.V(lambda e: e.reciprocal(out=ssb[:, 3:4], in_=ssb[:, 2:3]), r=["ssb2"], w=["ssb3"])
                P.V(lambda e: e.scalar_tensor_tensor(out=acc[:], in0=acc[:], scalar=ssb[:, 3:4], in1=gfin[:], op0=ALU.mult, op1=ALU.mult), r=["wk1", "ssb3", "gfin"], w=["wk1"])
            P.dma("sync", t["dst"], acc[:], r=["wk1"], w=[], chan="st")

        ngrp = -(-len(tiles) // 12)
        GSZ = -(-len(tiles) // ngrp)
        for g0 in range(0, len(tiles), GSZ):
            PP[0] = Prog(nc, f"{name}g{g0}")
            g1 = min(g0 + GSZ, len(tiles))
            front(g0)
            for n in range(g0, g1):
                if n + 1 < g1:
                    front(n + 1)
                back(n)
            PP[0].emit()
            K.peer_stats = PP[0].stats
            nc.all_engine_barrier()


def phase_rec(K):
    nc, I, ps = K.nc, K.I, K.ps
    NTOK = NT * 128
    for pas in ("u", "g"):
        with ExitStack() as es:
            sb = lambda n, s, d=F32: es.enter_context(nc.sbuf_tensor(n, s, d))
            W = sb("r_W" + pas, [128, KC, D], BF16)
            Gt = sb("r_G" + pas, [128, D]); St = sb("r_S" + pas, [128, D])
            ss = sb("r_ss" + pas, [128, 4])
            xt = sb("r_xt" + pas, [128, D]); h = sb("r_h" + pas, [128, D])
            hT = sb("r_hT" + pas, [128, KC, 256], BF16)
            ost = sb("r_ost" + pas, [128, 16, 256])
            P = Prog(nc, "rp" + pas)
            load_w_bf16(P, W, I["rec_w_in"], D, "W", col0=(D if pas == "u" else 0))
            for blk in (range(NT // 2) if pas == "u" else range(1, NT // 2)):
                is_ctx = blk == 0
                if blk in (0, 1):
                    v = 1 if is_ctx else 0
                    prep_GS(K, P, Gt, St, I["norm_mix"][1], K.modd[1, v, D:2 * D], K.modd[1, v, 0:D], h, "a")
                for ti in range(2):
                    e_t = blk * 2 + ti
                    norm_mod_T(K, P, K.xs2tile(e_t), xt, h, ss, Gt, St, "a", hT, "hT", ti * 128, (6, 7), 0)
                for j in range(16):
                    pb = j % 2
                    for kc in range(KC):
                        P.T(lambda e, kc=kc, j=j, pb=pb: e.matmul(ps[pb][:, 0:256], lhsT=W[:, kc, j * 128:(j + 1) * 128], rhs=hT[:, kc, :], start=(kc == 0), stop=(kc == KC - 1)),
                            r=["W", "hT"], w=[f"ps{pb}"])
                    fn = AF.Copy if pas == "u" else AF.Gelu_apprx_tanh
                    P.A(lambda e, j=j, pb=pb, fn=fn: e.activation(out=ost[:, j, :], in_=ps[pb][:, 0:256], func=fn), r=[f"ps{pb}"], w=["ost"])
                if pas == "u":
                    P.dma("sync", K.upre[:, :, blk * 256:(blk + 1) * 256].rearrange("c p t -> p c t"), ost[:], r=["ost"], w=["upre"], chan="ost")
                else:
                    l0 = (blk - 1) * 256
                    P.dma("sync", K.ggd[:, :, l0:l0 + 256].rearrange("c p t -> p c t"), ost[:], r=["ost"], w=["ggd"], chan="ost")
            P.emit()
        nc.all_engine_barrier()
    with ExitStack() as es:
        sb = lambda n, s, d=F32: es.enter_context(nc.sbuf_tensor(n, s, d))
        UPW = 4360
        up = sb("r_up", [128, 2, UPW])
        u = sb("r_u", [128, 2, NTOK]); ub = sb("r_ub", [128, 2, NTOK], BF16)
        A = sb("r_A", [128, NTOK]); Bt = sb("r_B", [128, NTOK])
        Y = [sb(f"r_Y{d}", [128, NTOK]) for d in range(2)]
        gg = sb("r_gg", [128, S]); ygb = sb("r_ygb", [128, S], BF16)
        wg = sb("r_wg", [128, 2, 2, 2, 256], BF16)
        cw = sb("r_cw", [128, 4, 16]); cb = sb("r_cb", [128, 16])
        ba = sb("r_ba", [128, 2, 16]); bx = sb("r_bx", [128, 2, 16]); lam = sb("r_lam", [128, 2, 16]); cl = sb("r_cl", [128, 2, 16])
        tmp = [[sb(f"r_t{i}{s}", [128, 512]) for i in range(4)] for s in range(2)]
        P = Prog(nc, "rscan")
        P.G(lambda e: e.memset(up[:], 0.0), r=[], w=["up0", "up1"])
        for k in range(4):
            P.dma("sync", cw[:, k, :], I["rec_conv_w"][k].rearrange("(c p) -> p c", p=128), r=[], w=["cw"], chan=f"cw{k}", allow_slow_non_contiguous=True)
        P.dma("sync", cb[:], I["rec_conv_b"].rearrange("(c p) -> p c", p=128), r=[], w=["cb"], chan="cb", allow_slow_non_contiguous=True)
        for d in range(2):
            P.dma("sync", ba[:, d, :], I["rec_b_a"][d].rearrange("(c p) -> p c", p=128), r=[], w=["ba"], chan=f"ba{d}", allow_slow_non_contiguous=True)
            P.dma("sync", bx[:, d, :], I["rec_b_x"][d].rearrange("(c p) -> p c", p=128), r=[], w=["bx"], chan=f"bx{d}", allow_slow_non_contiguous=True)
            P.dma("sync", lam[:, d, :], I["rec_lambda"][d].rearrange("(c p) -> p c", p=128), r=[], w=["lam"], chan=f"lam{d}", allow_slow_non_contiguous=True)
        P.A(lambda e: e.activation(out=cl[:], in_=lam[:], func=AF.Exp, scale=-1.0), r=["lam"], w=["cl"])
        P.A(lambda e: e.activation(out=cl[:], in_=cl[:], func=AF.Ln, bias=1.0), r=["cl"], w=["cl"])
        P.V(lambda e: e.tensor_scalar(out=cl[:], in0=cl[:], scalar1=-8.0, scalar2=None, op0=ALU.mult), r=["cl"], w=["cl"])
        for n in range(8):
            for c in range(2):
                ch = 2 * n + c
                P.dma("sync", up[:, c, 1:257], K.upre[ch, :, 0:256], r=[], w=[f"up{c}"], chan=f"upc{c}")
                P.dma("sync", up[:, c, 260:260 + S], K.upre[ch, :, 256:NTOK], r=[], w=[f"up{c}"], chan=f"upl{c}")
                for (o0, nn, i0) in ((0, 256, 0), (256, S, 259)):
                    useg = u[:, c, o0:o0 + nn]
                    P.V(lambda e, c=c, ch=ch, useg=useg, i0=i0, nn=nn: e.tensor_scalar(out=useg, in0=up[:, c, i0:i0 + nn], scalar1=cw[:, 0, ch:ch + 1], scalar2=cb[:, ch:ch + 1], op0=ALU.mult, op1=ALU.add),
                        r=[f"up{c}", "cw", "cb"], w=[f"u{c}"])
                    for k in range(1, 4):
                        P.V(lambda e, c=c, ch=ch, useg=useg, i0=i0, nn=nn, k=k: e.scalar_tensor_tensor(out=useg, in0=up[:, c, i0 + k:i0 + k + nn], scalar=cw[:, k, ch:ch + 1], in1=useg, op0=ALU.mult, op1=ALU.add),
                            r=[f"up{c}", "cw", f"u{c}"], w=[f"u{c}"])
                P.A(lambda e, c=c: e.activation(out=ub[:, c, :], in_=u[:, c, :], func=AF.Copy), r=[f"u{c}"], w=["ub"])
            for d in range(2):
                for ty in range(2):
                    wsrc = (I["rec_w_a"] if ty == 0 else I["rec_w_x"])[d, n].rearrange("(k p) o -> p k o", p=128)
                    P.dma("gpsimd", wg[:, d, ty, :, :], wsrc, r=[], w=["wg"], chan=f"wg{d}{ty}")
            for oc in range(2):
                ch = 2 * n + oc
                for d in range(2):
                    for tb in range(9):
                        t0 = tb * 512
                        tn = min(512, NTOK - t0)
                        sl = tb % 2
                        pa, px = 2 * sl, 2 * sl + 1
                        r_, i_, a2, sq = tmp[sl]
                        for ty, pb in ((0, pa), (1, px)):
                            for kc in range(2):
                                P.T(lambda e, ty=ty, pb=pb, kc=kc, d=d, oc=oc, t0=t0, tn=tn: e.matmul(ps[pb][:, 0:tn], lhsT=wg[:, d, ty, kc, oc * 128:(oc + 1) * 128], rhs=ub[:, kc, t0:t0 + tn], start=(kc == 0), stop=(kc == 1)),
                                    r=["wg", "ub"], w=[f"ps{pb}"])
                        P.A(lambda e, pa=pa, tn=tn, d=d, ch=ch, r_=r_: e.activation(out=r_[:, 0:tn], in_=ps[pa][:, 0:tn], func=AF.Sigmoid, bias=ba[:, d, ch:ch + 1]), r=[f"ps{pa}", "ba"], w=[f"t0{sl}"])
                        P.A(lambda e, px=px, tn=tn, d=d, ch=ch, i_=i_: e.activation(out=i_[:, 0:tn], in_=ps[px][:, 0:tn], func=AF.Sigmoid, bias=bx[:, d, ch:ch + 1]), r=[f"ps{px}", "bx"], w=[f"t1{sl}"])
                        P.A(lambda e, tn=tn, t0=t0, d=d, ch=ch, r_=r_: e.activation(out=A[:, t0:t0 + tn], in_=r_[:, 0:tn], func=AF.Exp, scale=cl[:, d, ch:ch + 1]), r=[f"t0{sl}", "cl"], w=["A"])
                        P.G(lambda e, tn=tn, t0=t0, a2=a2: e.tensor_tensor(out=a2[:, 0:tn], in0=A[:, t0:t0 + tn], in1=A[:, t0:t0 + tn], op=ALU.mult), r=["A"], w=[f"t2{sl}"])
                        P.A(lambda e, tn=tn, a2=a2, sq=sq: e.activation(out=sq[:, 0:tn], in_=a2[:, 0:tn], func=AF.Sqrt, scale=-1.0, bias=1.0), r=[f"t2{sl}"], w=[f"t3{sl}"])
                        P.V(lambda e, tn=tn, sq=sq, i_=i_: e.tensor_tensor(out=sq[:, 0:tn], in0=sq[:, 0:tn], in1=i_[:, 0:tn], op=ALU.mult), r=[f"t3{sl}", f"t1{sl}"], w=[f"t3{sl}"])
                        P.V(lambda e, tn=tn, t0=t0, sq=sq, oc=oc: e.tensor_tensor(out=Bt[:, t0:t0 + tn], in0=sq[:, 0:tn], in1=u[:, oc, t0:t0 + tn], op=ALU.mult), r=[f"t3{sl}", f"u{oc}"], w=["B"])
                    Yd = Y[d]
                    if d == 0:
                        P.V(lambda e, Yd=Yd: e.tensor_tensor_scan(out=Yd[:, 0:256], data0=A[:, 0:256], data1=Bt[:, 0:256], initial=0.0, op0=ALU.mult, op1=ALU.add), r=["A", "B"], w=[f"Y{d}c"])
                        P.V(lambda e, Yd=Yd: e.tensor_tensor_scan(out=Yd[:, 256:NTOK], data0=A[:, 256:NTOK], data1=Bt[:, 256:NTOK], initial=Yd[:, 255:256], op0=ALU.mult, op1=ALU.add), r=["A", "B", f"Y{d}c"], w=[f"Y{d}l"])
                    else:
                        P.V(lambda e, Yd=Yd: e.tensor_tensor_scan(out=Yd[:, 0:256][:, ::-1], data0=A[:, 0:256][:, ::-1], data1=Bt[:, 0:256][:, ::-1], initial=0.0, op0=ALU.mult, op1=ALU.add), r=["A", "B"], w=[f"Y{d}c"])
                        P.V(lambda e, Yd=Yd: e.tensor_tensor_scan(out=Yd[:, 256:NTOK][:, ::-1], data0=A[:, 256:NTOK][:, ::-1], data1=Bt[:, 256:NTOK][:, ::-1], initial=Yd[:, 0:1], op0=ALU.mult, op1=ALU.add), r=["A", "B", f"Y{d}c"], w=[f"Y{d}l"])
                P.dma("sync", gg[:], K.ggd[ch], r=[], w=["gg"], chan="gg")
                P.G(lambda e: e.tensor_tensor(out=Y[0][:, 256:NTOK], in0=Y[0][:, 256:NTOK], in1=Y[1][:, 256:NTOK], op=ALU.add), r=["Y0l", "Y1l"], w=["Y0l"])
                P.V(lambda e: e.tensor_tensor(out=ygb[:], in0=Y[0][:, 256:NTOK], in1=gg[:], op=ALU.mult), r=["Y0l", "gg"], w=["ygb"])
                P.dma("sync", K.ygd[:, ch, :], ygb[:], r=["ygb"], w=["ygd"], chan="ygb")
        P.emit()
    nc.all_engine_barrier()
    with ExitStack() as es:
        sb = lambda n, s, d=F32: es.enter_context(nc.sbuf_tensor(n, s, d))
        Wout = sb("r_Wout", [128, KC, D], BF16)
        gate = sb("r_gate", [128, D])
        yt = [sb(f"r_yt{i}", [128, KC, 128], BF16) for i in range(2)]
        xr = [sb(f"r_xr{i}", [128, D]) for i in range(2)]
        y = sb("r_y", [128, D])
        P = Prog(nc, "rout")
        load_w_bf16(P, Wout, I["rec_w_out"], D, "Wo")
        load_bcast(P, "sync", gate[:], K.modd[1, 0, 2 * D:3 * D], "gate", "gate")
        for t in range(S // 128):
            s = t % 2
            P.dma("sync", yt[s][:], K.ygd[:, :, t * 128:(t + 1) * 128], r=[], w=[f"yt{s}"], chan=f"yt{s}")
            P.dma("sync", xr[s][:], K.xs2tile(t + 2), r=[], w=[f"xr{s}"], chan=f"xr{s}")
            for nb in range(4):
                pb = 4 + nb
                for kc in range(KC):
                    P.T(lambda e, kc=kc, nb=nb, pb=pb, s=s: e.matmul(ps[pb][:], lhsT=yt[s][:, kc, :], rhs=Wout[:, kc, nb * 512:(nb + 1) * 512], start=(kc == 0), stop=(kc == KC - 1)),
                        r=[f"yt{s}", "Wo"], w=[f"ps{pb}"])
                P.V(lambda e, nb=nb, pb=pb: e.tensor_tensor(out=y[:, nb * 512:(nb + 1) * 512], in0=ps[pb][:], in1=gate[:, nb * 512:(nb + 1) * 512], op=ALU.mult), r=[f"ps{pb}", "gate"], w=[f"y{nb}"])
                P.G(lambda e, nb=nb, s=s: e.tensor_tensor(out=y[:, nb * 512:(nb + 1) * 512], in0=y[:, nb * 512:(nb + 1) * 512], in1=xr[s][:, nb * 512:(nb + 1) * 512], op=ALU.add), r=[f"y{nb}", f"xr{s}"], w=[f"y{nb}"])
            P.dma("sync", K.xs3[t * 128:(t + 1) * 128, :], y[:], r=[f"y{nb}" for nb in range(4)], w=["xs3"], chan="y")
        P.emit()
```

```python
import os
import numpy as np
import ml_dtypes
from contextlib import ExitStack
import concourse.bass as bass
import concourse.mybir as mybir
from concourse.bass_utils import run_bass_kernel_spmd

F32 = mybir.dt.float32
BF16 = mybir.dt.bfloat16
U32 = mybir.dt.uint32
ALU = mybir.AluOpType
AF = mybir.ActivationFunctionType
AX = mybir.AxisListType

D = 2048
KC = 16
S = 4096
C = 256
NT = (S + C) // 128
EPS = 1e-6
COMPUTE = ("tensor", "vector", "scalar", "gpsimd")


class Prog:
    def __init__(self, nc, name="p"):
        self.nc = nc
        self.name = name
        self.ops = []
        self.last_w = {}
        self.readers = {}

    def op(self, eng, fn, r=(), w=(), dma=False, chan=None):
        i = len(self.ops)
        deps = set()
        for k in r:
            lw = self.last_w.get(k)
            if lw is not None:
                deps.add(lw)
        for k in w:
            lw = self.last_w.get(k)
            if lw is not None:
                deps.add(lw)
            rd = self.readers.get(k)
            if rd:
                deps.update(rd.values())
        for k in w:
            self.last_w[k] = i
            self.readers[k] = {}
        for k in r:
            d = self.readers.setdefault(k, {})
            d[("dma", i) if dma else eng] = i
        if dma:
            assert chan is not None
        self.ops.append(dict(eng=eng, fn=fn, deps=deps, dma=dma, chan=chan))
        return i

    def dma(self, eng, out, in_, r, w, chan, **kw):
        return self.op(eng, lambda e: e.dma_start(out=out, in_=in_, **kw), r=r, w=w, dma=True, chan=chan)

    def V(self, fn, r, w):
        return self.op("vector", fn, r, w)

    def A(self, fn, r, w):
        return self.op("scalar", fn, r, w)

    def G(self, fn, r, w):
        return self.op("gpsimd", fn, r, w)

    def T(self, fn, r, w):
        return self.op("tensor", fn, r, w)

    def emit(self):
        nc = self.nc
        ops = self.ops
        needed = set()
        for o in ops:
            for d in o["deps"]:
                od = ops[d]
                if od["dma"]:
                    continue
                if od["eng"] == "tensor" and o["eng"] == "tensor" and not o["dma"]:
                    continue
                needed.add(d)
        cnt = {e: 0 for e in COMPUTE + ("sync",)}
        chan_cnt = {}
        for i, o in enumerate(ops):
            if o["dma"]:
                c = o["chan"]
                chan_cnt[c] = chan_cnt.get(c, 0) + 16
                o["sig"] = ("c", c, chan_cnt[c])
            elif i in needed:
                cnt[o["eng"]] += 1
                o["sig"] = ("e", o["eng"], cnt[o["eng"]])
            else:
                o["sig"] = None
        self.stats = dict(cnt=dict(cnt), chans=len(chan_cnt), maxchan=max(chan_cnt.values()) if chan_cnt else 0, nops=len(ops))
        esem = {e: nc.alloc_semaphore(name=f"{self.name}_e_{e}") for e in COMPUTE if cnt[e] > 0}
        csem = {c: nc.alloc_semaphore(name=f"{self.name}_c_{c}") for c in chan_cnt}
        with ExitStack() as es:
            block = es.enter_context(nc.Block())
            by_eng = {}
            for i, o in enumerate(ops):
                by_eng.setdefault(o["eng"], []).append(i)

            def make(engname, idxs):
                def body(eng):
                    waited = {}
                    for i in idxs:
                        o = ops[i]
                        for d in sorted(o["deps"]):
                            od = ops[d]
                            sig = od["sig"]
                            if sig is None:
                                continue
                            if sig[0] == "e":
                                if od["eng"] == "tensor" and engname == "tensor" and not o["dma"]:
                                    continue
                                sem = esem[sig[1]]
                            else:
                                sem = csem[sig[1]]
                            key = (sig[0], sig[1])
                            if waited.get(key, 0) >= sig[2]:
                                continue
                            eng.wait_ge(sem, sig[2])
                            waited[key] = sig[2]
                        ins = o["fn"](eng)
                        sig = o["sig"]
                        if sig is not None:
                            if sig[0] == "e":
                                ins.then_inc(esem[sig[1]], 1)
                            else:
                                ins.then_inc(csem[sig[1]], 16)
                    last = {}
                    for i in idxs:
                        o = ops[i]
                        if o["dma"]:
                            last[o["chan"]] = o["sig"][2]
                    for c, v in last.items():
                        if waited.get(("c", c), 0) < v:
                            eng.wait_ge(csem[c], v)
                return body

            for engname, idxs in by_eng.items():
                getattr(block, engname)(make(engname, idxs))
        nc.all_engine_barrier()
        nc.clear_and_free_semaphores(list(esem.values()) + list(csem.values()))
        nc.all_engine_barrier()
        self.ops = []
        self.last_w = {}
        self.readers = {}


def _consts():
    ident = np.eye(128, dtype=np.float32)
    rotm = np.zeros((128, 128), np.float32)
    for m in range(128):
        base = 0 if m < 64 else 64
        d = m - base
        if d < 32:
            rotm[base + d + 32, m] = -1.0
        else:
            rotm[base + d - 32, m] = 1.0
    t = np.arange(S)
    row = (t // 64).astype(np.float32)
    col = (t % 64).astype(np.float32)
    inv = (np.float32(10000.0) ** (-np.arange(0, 64, 2, dtype=np.float32) / np.float32(64))).astype(np.float32)
    ang_r = row[:, None] * inv[None, :]
    ang_c = col[:, None] * inv[None, :]
    ang = np.concatenate([ang_r, ang_r, ang_c, ang_c], axis=-1).astype(np.float32)
    cosT = np.ascontiguousarray(np.cos(ang).astype(np.float32).T)
    sinT = np.ascontiguousarray(np.sin(ang).astype(np.float32).T)
    k = np.arange(128)[:, None]
    q = np.arange(128)[None, :]
    maskL = (k >= q).astype(ml_dtypes.bfloat16)
    maskR = (k <= q).astype(ml_dtypes.bfloat16)
    iota16 = np.tile(np.arange(16, dtype=np.float32)[None, :], (128, 1))
    return dict(ident=ident, rotm=rotm, cosT=cosT, sinT=sinT, maskL=maskL, maskR=maskR, iota16=iota16)


class Ctx:
    pass


def build(stop_after=99, debug=False, peer_tiles=None, start_at=0, pair_split=False):
    nc = bass.Bass("TRN2", target_bir_lowering=False)
    K = Ctx()
    K.nc = nc
    K._uid = [0]

    def uid():
        K._uid[0] += 1
        return K._uid[0]
    K.uid = uid
    din = lambda n, s, d=F32: nc.dram_tensor(n, list(s), d, kind="ExternalInput").ap()
    dscr = lambda n, s, d=F32: nc.dram_tensor(n, list(s), d, kind="Internal").ap()
    I = dict(
        x=din("x", [S, D]), c=din("c", [D]), ctx=din("ctx", [C, D]), c_ctx=din("c_ctx", [D]),
        w_mod=din("w_mod", [2, D, 6 * D]), b_mod=din("b_mod", [2, 6 * D]),
        norm_mix=din("norm_mix", [2, D]), norm_ffn=din("norm_ffn", [2, D]), norm_final=din("norm_final", [D]),
        attn_w_qkv=din("attn_w_qkv", [D, 3072]), attn_w_o=din("attn_w_o", [D, D]), attn_sink=din("attn_sink", [16]),
        rec_w_in=din("rec_w_in", [D, 2 * D]), rec_conv_w=din("rec_conv_w", [4, D]), rec_conv_b=din("rec_conv_b", [D]),
        rec_w_a=din("rec_w_a", [2, 8, 256, 256]), rec_b_a=din("rec_b_a", [2, D]),
        rec_w_x=din("rec_w_x", [2, 8, 256, 256]), rec_b_x=din("rec_b_x", [2, D]),
        rec_lambda=din("rec_lambda", [2, D]), rec_w_out=din("rec_w_out", [D, D]),
        peer_w_q=din("peer_w_q", [2, D, D]), peer_keys=din("peer_keys", [2, 2, 128, 128]),
        peer_u=din("peer_u", [2, 16384, D]), peer_v=din("peer_v", [2, 16384, D]),
        ident=din("ident", [128, 128]), rotm=din("rotm", [128, 128]), cosT=din("cosT", [128, S]), sinT=din("sinT", [128, S]),
        maskL=din("maskL", [128, 128], BF16), maskR=din("maskR", [128, 128], BF16), iota16=din("iota16", [128, 16]),
        myrows=din("myrows", [128, 16], U32),
        rows0=din("rows0", [128, 17], U32),
    )
    K.I = I
    out = nc.dram_tensor("out", [S // 2, D], F32, kind="ExternalOutput").ap()
    K.out = out
    K.modd = dscr("modd", [2, 2, 6 * D])
    K.qd = dscr("qd", [128, 16, NT * 128], BF16)
    K.kd = dscr("kd", [128, 4, NT * 128], BF16)
    K.vd = dscr("vd", [NT * 128, 512], BF16)
    K.xs1 = dscr("xs1", [NT * 128, D])
    K.xs2 = dscr("xs2", [NT * 128, D]) if start_at < 3 else din("xs2", [NT * 128, D])
    K.xs3 = dscr("xs3", [S, D]) if start_at < 4 else din("xs3", [S, D])
    K.upre = dscr("upre", [16, 128, NT * 128])
    K.ggd = dscr("ggd", [16, 128, S])
    K.ygd = dscr("ygd", [128, 16, S], BF16)
    K.pair_split = pair_split
    if pair_split:
        K.xs2loc = dscr("xs2loc", [17 * 128, D])
        K.xs2all = dscr("xs2all", [2 * 17 * 128, D])

    def xs2tile(e):
        if not pair_split:
            return K.xs2[e * 128:(e + 1) * 128, :]
        if e < 2:
            r0 = (e * 17) * 128
        else:
            i = e - 2
            r0 = ((i // 16) * 17 + 1 + i % 16) * 128
        return K.xs2all[r0:r0 + 128, :]
    K.xs2tile = xs2tile
    K.uvb16 = dscr("uvb16", [2 * 16384, 2 * D], BF16)
    dbg = {}
    if debug:
        dbg["d_modd"] = nc.dram_tensor("d_modd", [2, 2, 6 * D], F32, kind="ExternalOutput").ap()
        dbg["d_xs1"] = nc.dram_tensor("d_xs1", [NT * 128, D], F32, kind="ExternalOutput").ap()
        dbg["d_xs2"] = nc.dram_tensor("d_xs2", [NT * 128, D], F32, kind="ExternalOutput").ap()
        dbg["d_xs3"] = nc.dram_tensor("d_xs3", [S, D], F32, kind="ExternalOutput").ap()
    K.dbg = dbg

    with ExitStack() as es:
        K.ps = [es.enter_context(nc.psum_tensor(f"ps{i}", [128, 512], F32)) for i in range(8)]
        K.ident = es.enter_context(nc.sbuf_tensor("identsb", [128, 128], F32))
        P = Prog(nc, "c0")
        P.dma("sync", K.ident[:], I["ident"], r=[], w=["ident"], chan="ident")
        P.emit()
        nc.all_engine_barrier()
        phase_mod(K)
        nc.all_engine_barrier()
        K.tc = [TabConv(K, 0, "a"), TabConv(K, 1, "b")]
        if debug:
            copy_dram(K, K.modd.rearrange("l v n -> (l v) n"), dbg["d_modd"].rearrange("l v n -> (l v) n"), 4, "cpm")
        if stop_after >= 1 and start_at <= 1:
            phase_attn(K)
            nc.all_engine_barrier()
            if debug:
                copy_dram(K, K.xs1, dbg["d_xs1"], NT * 128, "cpx1")
        if stop_after >= 2 and start_at <= 2:
            if pair_split:
                tl = [dict(src=("g", K.xs1, j), v=1 if j == 0 else 0, dst=K.xs2loc[j * 128:(j + 1) * 128, :]) for j in range(17)]
                phase_peer(K, 0, tl, False, "pe0", idxname="rows0")
                nc.all_engine_barrier()
                P = Prog(nc, "cc")
                P.op("gpsimd", lambda e: e.collective_compute("AllGather", ALU.bypass, replica_groups=[[0, 1], [2, 3], [4, 5], [6, 7]],
                                                              ins=[K.xs2loc], outs=[K.xs2all]), r=[], w=[], dma=True, chan="cc")
                P.emit()
            else:
                tl = [dict(src=("d", K.xs1[n * 128:(n + 1) * 128, :]), v=1 if n < 2 else 0, dst=K.xs2[n * 128:(n + 1) * 128, :]) for n in range(NT)]
                if peer_tiles is not None:
                    tl = [tl[i] for i in peer_tiles]
                phase_peer(K, 0, tl, False, "pe0")
            nc.all_engine_barrier()
            if debug:
                copy_dram(K, K.xs2, dbg["d_xs2"], NT * 128, "cpx2")
        if stop_after >= 3 and start_at <= 3:
            phase_rec(K)
            nc.all_engine_barrier()
            if debug:
                copy_dram(K, K.xs3, dbg["d_xs3"], S, "cpx3")
        if stop_after >= 4:
            tl = [dict(src=("g", K.xs3, t), v=0, dst=K.out[t * 128:(t + 1) * 128, :]) for t in range(S // 256)]
            if peer_tiles is not None:
                tl = tl[:len(peer_tiles)]
            phase_peer(K, 1, tl, True, "pe1")
    return nc


def copy_dram(K, src, dst, rows, name):
    nc = K.nc
    with ExitStack() as es:
        t = es.enter_context(nc.sbuf_tensor(name + "_t", [128, src.shape[1]], src.dtype))
        P = Prog(nc, name)
        for r0 in range(0, rows, 128):
            n = min(128, rows - r0)
            P.dma("sync", t[0:n, :], src[r0:r0 + n, :], r=[], w=["t"], chan="ld")
            P.dma("sync", dst[r0:r0 + n, :], t[0:n, :], r=["t"], w=[], chan="st")
        P.emit()
    nc.all_engine_barrier()


def phase_mod(K):
    nc, I = K.nc, K.I
    with ExitStack() as es:
        sb = lambda n, s, d=F32: es.enter_context(nc.sbuf_tensor(n, s, d))
        cc = sb("m_cc", [128, KC, 2])
        craw = sb("m_craw", [128, 2, KC])
        wt = [sb(f"m_wt{i}", [128, KC, 512]) for i in range(2)]
        bt = sb("m_bt", [2, 512])
        ot = [sb(f"m_ot{i}", [2, 512]) for i in range(2)]
        P = Prog(nc, "mod")
        P.dma("sync", craw[:, 0, :], I["c"].rearrange("(k p) -> p k", p=128), r=[], w=["craw0"], chan="craw0", allow_slow_non_contiguous=True)
        P.dma("sync", craw[:, 1, :], I["c_ctx"].rearrange("(k p) -> p k", p=128), r=[], w=["craw1"], chan="craw1", allow_slow_non_contiguous=True)
        for v in range(2):
            P.A(lambda e, v=v: e.activation(out=cc[:, :, v], in_=craw[:, v, :], func=AF.Silu), r=[f"craw{v}"], w=["cc"])
        n = 0
        for l in range(2):
            for j in range(24):
                s = n % 2
                w_src = I["w_mod"][l].rearrange("(k p) n -> p k n", p=128)[:, :, j * 512:(j + 1) * 512]
                P.dma("sync" if s == 0 else "gpsimd", wt[s][:], w_src, r=[], w=[f"wt{s}"], chan=f"wt{s}")
                for v in range(2):
                    P.dma("sync", bt[v:v + 1, :], I["b_mod"][l:l + 1, j * 512:(j + 1) * 512], r=[], w=["bt"], chan=f"bt{v}")
                pb = K.ps[s]
                for kc in range(KC):
                    P.T(lambda e, kc=kc, s=s, pb=pb: e.matmul(pb[0:2, :], lhsT=cc[:, kc, :], rhs=wt[s][:, kc, :], start=(kc == 0), stop=(kc == KC - 1)),
                        r=["cc", f"wt{s}"], w=[f"ps{s}"])
                P.V(lambda e, s=s, pb=pb: e.tensor_tensor(out=ot[s][:], in0=pb[0:2, :], in1=bt[:], op=ALU.add), r=[f"ps{s}", "bt"], w=[f"ot{s}"])
                P.dma("sync", K.modd[l, :, j * 512:(j + 1) * 512], ot[s][:], r=[f"ot{s}"], w=[], chan=f"ot{s}")
                n += 1
        P.emit()


def load_bcast(P, eng, tile_ap, src_row_ap, key, chan):
    P.dma(eng, tile_ap, src_row_ap.partition_broadcast(128), r=[], w=[key], chan=chan)


def prep_GS(K, P, Gt, St, gain_row, scale_row, shift_row, tmp, tag):
    load_bcast(P, "sync", Gt[:], scale_row, f"G{tag}", f"G{tag}")
    load_bcast(P, "sync", tmp[:], gain_row, "gstmp", "gstmp")
    load_bcast(P, "sync", St[:], shift_row, f"S{tag}", f"S{tag}")
    P.V(lambda e: e.scalar_tensor_tensor(out=Gt[:], in0=Gt[:], scalar=1.0, in1=tmp[:], op0=ALU.add, op1=ALU.mult), r=[f"G{tag}", "gstmp"], w=[f"G{tag}"])


def norm_mod_T(K, P, src_ap, xt, h, ss, Gt, St, gtag, hT, hT_key, tok0, tpb, xslot):
    ps = K.ps
    P.dma("sync", xt[:], src_ap, r=[], w=[f"xt{xslot}"], chan=f"xt{xslot}")
    P.A(lambda e: e.activation(out=h[:], in_=xt[:], func=AF.Square, accum_out=ss[:, 0:1]), r=[f"xt{xslot}"], w=["h", "ss"])
    P.V(lambda e: e.tensor_scalar(out=ss[:, 1:2], in0=ss[:, 0:1], scalar1=1.0 / D, scalar2=EPS, op0=ALU.mult, op1=ALU.add), r=["ss"], w=["ss1"])
    P.A(lambda e: e.activation(out=ss[:, 2:3], in_=ss[:, 1:2], func=AF.Sqrt), r=["ss1"], w=["ss2"])
    P.V(lambda e: e.reciprocal(out=ss[:, 3:4], in_=ss[:, 2:3]), r=["ss2"], w=["ss3"])
    P.V(lambda e: e.scalar_tensor_tensor(out=h[:], in0=xt[:], scalar=ss[:, 3:4], in1=Gt[:], op0=ALU.mult, op1=ALU.mult),
        r=[f"xt{xslot}", "ss3", f"G{gtag}"], w=["h"])
    P.V(lambda e: e.tensor_tensor(out=h[:], in0=h[:], in1=St[:], op=ALU.add), r=["h", f"S{gtag}"], w=["h"])
    if hT is None:
        return
    for q in range(4):
        b = tpb[q % 2]
        for j in range(4):
            kc = 4 * q + j
            P.T(lambda e, kc=kc, j=j, b=b: e.transpose(out=ps[b][:, j * 128:(j + 1) * 128], in_=h[:, kc * 128:(kc + 1) * 128], identity=K.ident[:]),
                r=["h", "ident"], w=[f"ps{b}"])
        P.A(lambda e, q=q, b=b: e.activation(out=hT[:, 4 * q:4 * q + 4, tok0:tok0 + 128], in_=ps[b][:].rearrange("p (j t) -> p j t", j=4), func=AF.Copy),
            r=[f"ps{b}"], w=[hT_key])


def load_w_bf16(P, wsb, w_dram, ncols, key, col0=0):
    for kc in range(KC):
        for c0 in range(0, ncols, 1024):
            cn = min(1024, ncols - c0)
            P.dma("gpsimd", wsb[:, kc, c0:c0 + cn], w_dram[kc * 128:(kc + 1) * 128, col0 + c0:col0 + c0 + cn], r=[], w=[key], chan=f"{key}_{kc % 4}")


def phase_attn(K):
    nc, I, ps = K.nc, K.I, K.ps
    SCALE = 128 ** -0.5
    with ExitStack() as es:
        sb = lambda n, s, d=F32: es.enter_context(nc.sbuf_tensor(n, s, d))
        with ExitStack() as es1:
            sb1 = lambda n, s, d=F32: es1.enter_context(nc.sbuf_tensor(n, s, d))
            W = sb1("a_W", [128, KC, 3072], BF16)
            Gt = sb1("a_G", [128, D]); St = sb1("a_S", [128, D])
            ss = sb1("a_ss", [128, 4])
            xt = sb1("a_xt", [128, D]); h = sb1("a_h", [128, D])
            hT = sb1("a_hT", [128, KC, 256], BF16)
            rotm = sb1("a_rotm", [128, 128])
            cs = sb1("a_cos", [128, 256]); sn = sb1("a_sin", [128, 256])
            qsb = [sb1(f"a_qsb{i}", [128, 256]) for i in range(2)]
            t1 = [sb1(f"a_t1{i}", [128, 256]) for i in range(2)]
            t2 = [sb1(f"a_t2{i}", [128, 256]) for i in range(2)]
            qst = sb1("a_qst", [128, 20, 256], BF16)
            vst = sb1("a_vst", [128, 2, 512], BF16)
            P = Prog(nc, "qkv")
            tc = K.tc[0]
            tc.alloc(es1, 12)
            tc.begin()
            load_w_bf16(P, W, I["attn_w_qkv"], 3072, "W")
            P.dma("sync", rotm[:], I["rotm"], r=[], w=["rotm"], chan="rotm")
            for blk in range(NT // 2):
                is_ctx = blk == 0
                if blk in (0, 1):
                    v = 1 if is_ctx else 0
                    prep_GS(K, P, Gt, St, I["norm_mix"][0], K.modd[0, v, D:2 * D], K.modd[0, v, 0:D], h, "a")
                for ti in range(2):
                    e_t = blk * 2 + ti
                    src = I["ctx"][e_t * 128:(e_t + 1) * 128, :] if is_ctx else I["x"][(e_t - 2) * 128:(e_t - 1) * 128, :]
                    norm_mod_T(K, P, src, xt, h, ss, Gt, St, "a", hT, "hT", ti * 128, (6, 7), 0)
                if not is_ctx:
                    l0 = (blk - 1) * 256
                    P.dma("sync", cs[:], I["cosT"][:, l0:l0 + 256], r=[], w=["cos"], chan="cos")
                    P.dma("sync", sn[:], I["sinT"][:, l0:l0 + 256], r=[], w=["sin"], chan="sin")
                for j in range(20):
                    pb = j % 2
                    for kc in range(KC):
                        P.T(lambda e, kc=kc, j=j, pb=pb: e.matmul(ps[pb][:, 0:256], lhsT=W[:, kc, j * 128:(j + 1) * 128], rhs=hT[:, kc, :], start=(kc == 0), stop=(kc == KC - 1)),
                            r=["W", "hT"], w=[f"ps{pb}"])
                    dst, dkey = qst[:, j, :], "qst"
                    if is_ctx:
                        P.A(lambda e, pb=pb, dst=dst: e.activation(out=dst, in_=ps[pb][:, 0:256], func=AF.Copy), r=[f"ps{pb}"], w=[dkey])
                    else:
                        s2 = j % 2
                        P.A(lambda e, pb=pb, s2=s2: e.activation(out=qsb[s2][:], in_=ps[pb][:, 0:256], func=AF.Copy), r=[f"ps{pb}"], w=[f"qsb{s2}"])
                        P.T(lambda e, s2=s2: e.matmul(ps[2 + s2][:, 0:256], lhsT=rotm[:], rhs=qsb[s2][:], start=True, stop=True), r=["rotm", f"qsb{s2}"], w=[f"ps{2 + s2}"])
                        P.V(lambda e, s2=s2: e.tensor_tensor(out=t1[s2][:], in0=qsb[s2][:], in1=cs[:], op=ALU.mult), r=[f"qsb{s2}", "cos"], w=[f"t1{s2}"])
                        P.V(lambda e, s2=s2: e.tensor_tensor(out=t2[s2][:], in0=ps[2 + s2][:, 0:256], in1=sn[:], op=ALU.mult), r=[f"ps{2 + s2}", "sin"], w=[f"t2{s2}"])
                        P.V(lambda e, s2=s2, dst=dst: e.tensor_tensor(out=dst, in0=t1[s2][:], in1=t2[s2][:], op=ALU.add), r=[f"t1{s2}", f"t2{s2}"], w=[dkey])
                tc.pump(P, 6)
                P.dma("sync", K.qd[:, :, blk * 256:(blk + 1) * 256], qst[:, 0:16, :], r=["qst"], w=["qd"], chan="qst")
                P.dma("sync", K.kd[:, :, blk * 256:(blk + 1) * 256], qst[:, 16:20, :], r=["qst"], w=["kd"], chan="kst")
                for ti in range(2):
                    e_t = blk * 2 + ti
                    pb = 4 + ti
                    for kc in range(KC):
                        P.T(lambda e, kc=kc, ti=ti, pb=pb: e.matmul(ps[pb][:], lhsT=hT[:, kc, ti * 128:(ti + 1) * 128], rhs=W[:, kc, 2560:3072], start=(kc == 0), stop=(kc == KC - 1)),
                            r=["W", "hT"], w=[f"ps{pb}"])
                    P.A(lambda e, ti=ti, pb=pb: e.activation(out=vst[:, ti, :], in_=ps[pb][:], func=AF.Copy), r=[f"ps{pb}"], w=[f"vst{ti}"])
                    P.dma("sync", K.vd[e_t * 128:(e_t + 1) * 128, :], vst[:, ti, :], r=[f"vst{ti}"], w=["vd"], chan=f"vst{ti}")
            tc.flush(P)
            P.emit()
        nc.all_engine_barrier()
        with ExitStack() as es2:
            sb2 = lambda n, s, d=F32: es2.enter_context(nc.sbuf_tensor(n, s, d))
            Wo = sb2("a_Wo", [128, KC, D], BF16)
            kT = sb2("a_kT", [128, 4, NT * 128], BF16)
            vx = sb2("a_vx", [128, NT, 4, 129], BF16)
            gate = sb2("a_gate", [128, D])
            esink = sb2("a_esink", [128, 16])
            qt = [sb2(f"a_qt{i}", [128, 16, 128], BF16) for i in range(2)]
            E = [sb2(f"a_E{i}", [128, 5, 512], BF16) for i in range(2)]
            mL = sb2("a_mL", [128, 128], BF16); mR = sb2("a_mR", [128, 128], BF16)
            o = sb2("a_o", [128, D]); oT = sb2("a_oT", [128, KC, 128], BF16)
            den = sb2("a_den", [128, 8])
            xt = [sb2(f"a_xr{i}", [128, D]) for i in range(1)]
            y = sb2("a_y", [128, D])
            sraw = sb2("a_sraw", [128, 16])
            P = Prog(nc, "att")
            tc = K.tc[0]
            tc.alloc(es2, 4)
            tc.begin()
            load_w_bf16(P, Wo, I["attn_w_o"], D, "Wo")
            P.G(lambda e: e.memset(vx[:, :, :, 128:129], 1.0), r=[], w=["vx1"])
            for hh in range(4):
                P.dma("sync", kT[:, hh, :], K.kd[:, hh, :], r=[], w=["kT"], chan=f"kTl{hh}")
            for t in range(NT):
                P.dma("sync", vx[:, t, :, 0:128], K.vd[t * 128:(t + 1) * 128, :].rearrange("p (g d) -> p g d", g=4), r=[], w=["vx"], chan=f"vxl{t % 4}")
            P.dma("sync", mL[:], I["maskL"], r=[], w=["mL"], chan="mL")
            P.dma("sync", mR[:], I["maskR"], r=[], w=["mR"], chan="mR")
            load_bcast(P, "sync", sraw[:], I["attn_sink"], "sraw", "sraw")
            P.A(lambda e: e.activation(out=esink[:], in_=sraw[:], func=AF.Exp), r=["sraw"], w=["esink"])
            for e_t in range(NT):
                is_ctx = e_t < 2
                if e_t in (0, 2):
                    v = 1 if is_ctx else 0
                    load_bcast(P, "sync", gate[:], K.modd[0, v, 2 * D:3 * D], "gate", "gate")
                qs = e_t % 2
                P.dma("sync", qt[qs][:], K.qd[:, :, e_t * 128:(e_t + 1) * 128], r=["qd"], w=[f"qt{qs}"], chan=f"qt{qs}")
                src = I["ctx"][e_t * 128:(e_t + 1) * 128, :] if is_ctx else I["x"][(e_t - 2) * 128:(e_t - 1) * 128, :]
                P.dma("sync", xt[0][:], src, r=[], w=["xr0"], chan="xr0")
                if is_ctx:
                    kbs = [0, 1]
                else:
                    kbs = [0, 1] + [kb for kb in (e_t - 1, e_t, e_t + 1) if 2 <= kb < NT]
                for hh in range(4):
                    Es = E[hh % 2]
                    Ek = f"E{hh % 2}"
                    for n, kb in enumerate(kbs):
                        pb = n % 2
                        P.T(lambda e, hh=hh, kb=kb, pb=pb, qs=qs: e.matmul(ps[pb][:], lhsT=kT[:, hh, kb * 128:(kb + 1) * 128],
                                                                      rhs=qt[qs][:, 4 * hh:4 * hh + 4, :].rearrange("p g q -> p (g q)"), start=True, stop=True),
                            r=["kT", f"qt{qs}"], w=[f"ps{pb}"])
                        P.A(lambda e, n=n, pb=pb, Es=Es: e.activation(out=Es[:, n, :], in_=ps[pb][:], func=AF.Exp, scale=SCALE), r=[f"ps{pb}"], w=[f"{Ek}_{n}"])
                        if (not is_ctx) and kb >= 2 and kb != e_t:
                            m = mL if kb == e_t - 1 else mR
                            P.V(lambda e, n=n, m=m, Es=Es: e.tensor_tensor(out=Es[:, n, :].rearrange("p (g q) -> p g q", g=4), in0=Es[:, n, :].rearrange("p (g q) -> p g q", g=4),
                                                                        in1=m[:].unsqueeze(1).to_broadcast([128, 4, 128]), op=ALU.mult),
                                r=[f"{Ek}_{n}", "mL", "mR"], w=[f"{Ek}_{n}"])
                    for g in range(4):
                        pb = 2 + g // 2
                        for n, kb in enumerate(kbs):
                            P.T(lambda e, g=g, n=n, kb=kb, pb=pb, hh=hh, Es=Es: e.matmul(ps[pb][:, (g % 2) * 256:(g % 2) * 256 + 129], lhsT=Es[:, n, g * 128:(g + 1) * 128],
                                                                                     rhs=vx[:, kb, hh, :], start=(n == 0), stop=(n == len(kbs) - 1)),
                                r=[f"{Ek}_{n}", "vx", "vx1"], w=[f"ps{pb}"])
                    for bk in range(2):
                        pb = 2 + bk
                        P.V(lambda e, bk=bk, pb=pb, hh=hh: e.tensor_tensor(out=den[:, 2 * bk:2 * bk + 2], in0=ps[pb][:].rearrange("p (g c) -> p g c", g=2)[:, :, 128],
                                                                       in1=esink[:, 4 * hh + 2 * bk:4 * hh + 2 * bk + 2], op=ALU.add),
                            r=[f"ps{pb}", "esink"], w=["den"])
                    P.V(lambda e: e.reciprocal(out=den[:, 4:8], in_=den[:, 0:4]), r=["den"], w=["rden"])
                    for g in range(4):
                        pb = 2 + g // 2
                        hd = 4 * hh + g
                        P.V(lambda e, g=g, pb=pb, hd=hd: e.tensor_scalar(out=o[:, hd * 128:(hd + 1) * 128], in0=ps[pb][:, (g % 2) * 256:(g % 2) * 256 + 128],
                                                                     scalar1=den[:, 4 + g:5 + g], scalar2=None, op0=ALU.mult),
                            r=[f"ps{pb}", "rden"], w=["o"])
                for q in range(4):
                    b = 6 + q % 2
                    for j in range(4):
                        kc = 4 * q + j
                        P.T(lambda e, kc=kc, j=j, b=b: e.transpose(out=ps[b][:, j * 128:(j + 1) * 128], in_=o[:, kc * 128:(kc + 1) * 128], identity=K.ident[:]),
                            r=["o", "ident"], w=[f"ps{b}"])
                    P.A(lambda e, q=q, b=b: e.activation(out=oT[:, 4 * q:4 * q + 4, :], in_=ps[b][:].rearrange("p (j t) -> p j t", j=4), func=AF.Copy), r=[f"ps{b}"], w=["oT"])
                for nb in range(4):
                    pb = 4 + nb
                    for kc in range(KC):
                        P.T(lambda e, kc=kc, nb=nb, pb=pb: e.matmul(ps[pb][:], lhsT=oT[:, kc, :], rhs=Wo[:, kc, nb * 512:(nb + 1) * 512], start=(kc == 0), stop=(kc == KC - 1)),
                            r=["oT", "Wo"], w=[f"ps{pb}"])
                    P.V(lambda e, nb=nb, pb=pb: e.tensor_tensor(out=y[:, nb * 512:(nb + 1) * 512], in0=ps[pb][:], in1=gate[:, nb * 512:(nb + 1) * 512], op=ALU.mult),
                        r=[f"ps{pb}", "gate"], w=[f"y{nb}"])
                    P.V(lambda e, nb=nb, qs=qs: e.tensor_tensor(out=y[:, nb * 512:(nb + 1) * 512], in0=y[:, nb * 512:(nb + 1) * 512], in1=xt[0][:, nb * 512:(nb + 1) * 512], op=ALU.add),
                        r=[f"y{nb}", "xr0"], w=[f"y{nb}"])
                P.dma("sync", K.xs1[e_t * 128:(e_t + 1) * 128, :], y[:], r=[f"y{nb}" for nb in range(4)], w=["xs1"], chan="y")
                tc.pump(P, 5)
            tc.finish(P)
            P.emit()


def _lay(inputs):
    consts = _consts()
    f = lambda a: np.ascontiguousarray(np.asarray(a, dtype=np.float32))
    shared = dict(
        c_ctx=f(inputs["c_ctx"]), w_mod=f(inputs["w_mod"]), b_mod=f(inputs["b_mod"]),
        norm_mix=f(inputs["norm_mix"]), norm_ffn=f(inputs["norm_ffn"]), norm_final=f(inputs["norm_final"]),
        attn_w_qkv=f(inputs["attn_w_qkv"][0]), attn_w_o=f(inputs["attn_w_o"][0]), attn_sink=f(inputs["attn_sink"][0]),
        rec_w_in=f(inputs["rec_w_in"][0]), rec_conv_w=f(inputs["rec_conv_w"][0]), rec_conv_b=f(inputs["rec_conv_b"][0]),
        rec_w_a=f(inputs["rec_w_a"][0]), rec_b_a=f(inputs["rec_b_a"][0]), rec_w_x=f(inputs["rec_w_x"][0]), rec_b_x=f(inputs["rec_b_x"][0]),
        rec_lambda=f(inputs["rec_lambda"][0]), rec_w_out=f(inputs["rec_w_out"][0]),
        peer_w_q=f(inputs["peer_w_q"]), peer_keys=f(inputs["peer_keys"]), peer_u=f(inputs["peer_u"]), peer_v=f(inputs["peer_v"]),
        **consts,
    )
    return shared, f


def core_inputs(inputs, shared, f, core):
    b, hf = core // 2, core % 2
    m = dict(shared)
    m["x"] = f(inputs["x"][b]); m["c"] = f(inputs["c"][b]); m["ctx"] = f(inputs["ctx"][b])
    r0 = np.zeros((128, 17), np.uint32)
    r0[:, 0] = hf * 128 + np.arange(128)
    for j in range(16):
        r0[:, 1 + j] = 256 + (hf * 16 + j) * 128 + np.arange(128)
    m["rows0"] = r0
    m["myrows"] = (hf * (S // 2) + np.arange(16, dtype=np.uint32)[None, :] * 128 + np.arange(128, dtype=np.uint32)[:, None]).astype(np.uint32)
    return m


PAIR_SPLIT = False


def kernel(**inputs):
    nc = build(pair_split=PAIR_SPLIT)
    shared, f = _lay(inputs)
    in_maps = [core_inputs(inputs, shared, f, core) for core in range(8)]
    res = run_bass_kernel_spmd(nc, in_maps, core_ids=list(range(8)))
    outp = np.empty((4, S, D), np.float32)
    for core in range(8):
        b, hf = core // 2, core % 2
        outp[b, hf * (S // 2):(hf + 1) * (S // 2)] = res.results[core]["out"]
    return outp


class TabConv:
    NR = 6
    LAG = 3

    def set_nr(self, nr):
        self.NR, self.LAG = nr, max(1, nr // 2)

    def __init__(self, K, layer, tag):
        self.K, self.layer, self.tag = K, layer, tag
        self.steps = [(tb, r0) for tb in (0, 1) for r0 in range(layer * 16384, (layer + 1) * 16384, 128)]
        self.i = 0
        self.tiles = None

    def alloc(self, es, nr=6):
        self.set_nr(nr)
        if True:
            self.tiles = [es.enter_context(self.K.nc.sbuf_tensor(f"tc{self.tag}_{self.K.uid()}_{i}", [128, D], BF16)) for i in range(self.NR)]

    def _store(self, P, j):
        tb, r0 = self.steps[j]
        sl = j % self.NR
        dst = self.K.uvb16[r0:r0 + 128, tb * D:(tb + 1) * D]
        P.dma("sync", dst, self.tiles[sl][:], r=[f"tc{sl}"], w=[], chan=f"tcs{sl}")

    def pump(self, P, n):
        I = self.K.I
        for _ in range(n):
            if self.i >= len(self.steps):
                break
            j = self.i
            tb, r0 = self.steps[j]
            src = (I["peer_u"] if tb == 0 else I["peer_v"]).rearrange("l e d -> (l e) d")[r0:r0 + 128, :]
            sl = j % self.NR
            P.dma("gpsimd", self.tiles[sl][:], src, r=[], w=[f"tc{sl}"], chan=f"tcl{sl}")
            if j - self.LAG >= self.done:
                self._store(P, j - self.LAG)
            self.i += 1

    def begin(self):
        self.done = self.i

    def flush(self, P):
        for j in range(max(self.done, self.i - self.LAG), self.i):
            self._store(P, j)
        self.done = self.i

    def finish(self, P):
        self.pump(P, len(self.steps))
        self.flush(P)


def phase_peer(K, layer, tiles, final, name, idxname="myrows"):
    nc, I, ps = K.nc, K.I, K.ps
    NB = 7
    uvtab = K.uvb16
    with ExitStack() as es:
        sb = lambda n, s, d=F32: es.enter_context(nc.sbuf_tensor(name + n, s, d))
        Wq = sb("p_Wq", [128, KC, D], BF16)
        Gt = sb("p_G", [128, D]); St = sb("p_S", [128, D])
        gate = sb("p_gate", [128, D])
        xt1 = sb("p_xt", [128, D])
        xt = [xt1, xt1]
        h = [sb(f"p_h{i}", [128, D]) for i in range(2)]
        hT = sb("p_hT", [128, KC, 128], BF16)
        wk = [sb(f"p_wk{i}", [128, D]) for i in range(2)]
        ring = [sb(f"p_ring{i}", [128, 2 * D], BF16) for i in range(NB)]
        junk = sb("p_junk", [128, D], BF16)
        acc = wk[1]
        keysT = sb("p_keysT", [128, 2, 128])
        kraw = sb("p_kraw", [128, 2, 128])
        ss = sb("p_ss", [128, 4]); ssb = sb("p_ssb", [128, 4])
        s16 = sb("p_s16", [128, 16, 16]); i16 = sb("p_i16", [128, 16, 16], U32); i16f = sb("p_i16f", [128, 16, 16])
        ts = sb("p_ts", [128, 8, 16]); sel = sb("p_sel", [128, 8, 16], U32)
        au = sb("p_au", [128, 2, 128], U32); af = sb("p_af", [128, 2, 128])
        isel = sb("p_isel", [128, 2, 128])
        idxf = sb("p_idxf", [128, 128])
        idxu = [sb(f"p_idxu{i}", [128, 128], U32) for i in range(2)]
        gsm = [sb(f"p_g{i}", [128, 128]) for i in range(2)]
        sm = sb("p_sm", [128, 16])
        z = sb("p_z", [128, 128]); gz = sb("p_gz", [128, 128]); av = sb("p_av", [128, 128])
        identb = sb("p_identb", [128, 128], BF16)
        dg = [sb(f"p_dg{i}", [128, 128], BF16) for i in range(4)]
        iota = sb("p_iota", [128, 16])
        mr = sb("p_mr", [128, I[idxname].shape[1]], U32)
        P = Prog(nc, name)
        load_w_bf16(P, Wq, I["peer_w_q"][layer], D, "Wq")
        P.dma("sync", iota[:], I["iota16"], r=[], w=["iota"], chan="iota")
        P.A(lambda e: e.activation(out=identb[:], in_=K.ident[:], func=AF.Copy), r=["ident"], w=["identb"])
        P.dma("sync", mr[:], I[idxname], r=[], w=["mr"], chan="mr")
        for p in range(2):
            P.dma("sync", kraw[:, p, :], I["peer_keys"][layer, p], r=[], w=["kraw"], chan=f"kraw{p}")
            P.T(lambda e, p=p: e.transpose(out=ps[p][:, 0:128], in_=kraw[:, p, :], identity=K.ident[:]), r=["kraw", "ident"], w=[f"ps{p}"])
            P.V(lambda e, p=p: e.tensor_copy(out=keysT[:, p, :], in_=ps[p][:, 0:128]), r=[f"ps{p}"], w=["keysT"])

        P.emit()
        nc.all_engine_barrier()
        cur_v = [None]
        PP = [None]

        def front(n):
            P = PP[0]
            t = tiles[n]
            s = n % 2
            if cur_v[0] != t["v"]:
                cur_v[0] = t["v"]
                v = t["v"]
                prep_GS(K, P, Gt, St, I["norm_ffn"][layer], K.modd[layer, v, 4 * D:5 * D], K.modd[layer, v, 3 * D:4 * D], wk[0], "p")
            if t["src"][0] == "d":
                P.dma("sync", xt[s][:], t["src"][1], r=[], w=["xt"], chan="xt")
            else:
                col = t["src"][2]
                P.op("gpsimd", lambda e, s=s, col=col, src=t["src"][1]: e.indirect_dma_start(out=xt[s][:], out_offset=None, in_=src,
                                                                                         in_offset=bass.IndirectOffsetOnAxis(ap=mr[:, col:col + 1], axis=0)),
                     r=["mr"], w=["xt"], dma=True, chan="xt")
            hh = h[s]
            hk = f"h{s}"
            P.A(lambda e, s=s: e.activation(out=h[s][:], in_=xt[s][:], func=AF.Square, accum_out=ss[:, 0:1]), r=["xt"], w=[hk, "ss"])
            P.V(lambda e: e.tensor_scalar(out=ss[:, 1:2], in0=ss[:, 0:1], scalar1=1.0 / D, scalar2=EPS, op0=ALU.mult, op1=ALU.add), r=["ss"], w=["ss1"])
            P.A(lambda e: e.activation(out=ss[:, 2:3], in_=ss[:, 1:2], func=AF.Sqrt), r=["ss1"], w=["ss2"])
            P.V(lambda e: e.reciprocal(out=ss[:, 3:4], in_=ss[:, 2:3]), r=["ss2"], w=["ss3"])
            P.V(lambda e, s=s: e.scalar_tensor_tensor(out=h[s][:], in0=xt[s][:], scalar=ss[:, 3:4], in1=Gt[:], op0=ALU.mult, op1=ALU.mult), r=["xt", "ss3", "Gp"], w=[hk])
            P.V(lambda e, s=s: e.tensor_tensor(out=h[s][:], in0=h[s][:], in1=St[:], op=ALU.add), r=[hk, "Sp"], w=[hk])
            for q in range(4):
                b = q % 2
                for j in range(4):
                    kc = 4 * q + j
                    P.T(lambda e, kc=kc, j=j, b=b, s=s: e.transpose(out=ps[b][:, j * 128:(j + 1) * 128], in_=h[s][:, kc * 128:(kc + 1) * 128], identity=K.ident[:]),
                        r=[hk, "ident"], w=[f"ps{b}"])
                P.A(lambda e, q=q, b=b: e.activation(out=hT[:, 4 * q:4 * q + 4, :], in_=ps[b][:].rearrange("p (j t) -> p j t", j=4), func=AF.Copy), r=[f"ps{b}"], w=["hT"])
            yield
            qT = wk[0]
            for c in range(16):
                pb = c % 2
                for kc in range(KC):
                    P.T(lambda e, kc=kc, c=c, pb=pb: e.matmul(ps[pb][:, 0:128], lhsT=Wq[:, kc, c * 128:(c + 1) * 128], rhs=hT[:, kc, :], start=(kc == 0), stop=(kc == KC - 1)),
                        r=["Wq", "hT"], w=[f"ps{pb}"])
                P.A(lambda e, c=c, pb=pb: e.activation(out=qT[:, c * 128:(c + 1) * 128], in_=ps[pb][:, 0:128], func=AF.Copy), r=[f"ps{pb}"], w=["wk0"])
                yield
            Ssb = wk[1]
            for q in range(4):
                pb = 2 + q % 2
                for c in range(4 * q, 4 * q + 4):
                    P.T(lambda e, c=c, pb=pb: e.matmul(ps[pb][:, (c % 4) * 128:(c % 4 + 1) * 128], lhsT=qT[:, c * 128:(c + 1) * 128], rhs=keysT[:, c % 2, :], start=True, stop=True),
                        r=["wk0", "keysT"], w=[f"ps{pb}"])
                P.A(lambda e, q=q, pb=pb: e.activation(out=Ssb[:, q * 512:(q + 1) * 512], in_=ps[pb][:], func=AF.Copy), r=[f"ps{pb}"], w=["wk1"])
            S2 = wk[0]
            for c in range(16):
                sv = Ssb[:, c * 128:(c + 1) * 128]
                s2v = S2[:, c * 128:(c + 1) * 128]
                P.V(lambda e, c=c, sv=sv: e.max(out=s16[:, c, 0:8], in_=sv), r=["wk1"], w=["s16"])
                P.V(lambda e, c=c, sv=sv: e.max_index(out=i16[:, c, 0:8], in_max=s16[:, c, 0:8], in_values=sv), r=["wk1", "s16"], w=["i16"])
                P.V(lambda e, c=c, sv=sv, s2v=s2v: e.match_replace(out=s2v, in_to_replace=s16[:, c, 0:8], in_values=sv, imm_value=-1e30), r=["wk1", "s16"], w=["wk0"])
                P.V(lambda e, c=c, s2v=s2v: e.max(out=s16[:, c, 8:16], in_=s2v), r=["wk0"], w=["s16"])
                P.V(lambda e, c=c, s2v=s2v: e.max_index(out=i16[:, c, 8:16], in_max=s16[:, c, 8:16], in_values=s2v), r=["wk0", "s16"], w=["i16"])
                yield
            cand = wk[1]
            c4 = cand[:].rearrange("p (h a b) -> p h a b", h=8, a=16)
            s16r = s16[:].rearrange("p (h t) k -> p h t k", t=2)
            P.V(lambda e: e.tensor_tensor(out=c4, in0=s16r[:, :, 0, :].unsqueeze(3).to_broadcast([128, 8, 16, 16]),
                                          in1=s16r[:, :, 1, :].unsqueeze(2).to_broadcast([128, 8, 16, 16]), op=ALU.add), r=["s16"], w=["wk1"])
            for hd in range(8):
                cv = cand[:, hd * 256:(hd + 1) * 256]
                c2v = S2[:, hd * 256:(hd + 1) * 256]
                P.V(lambda e, hd=hd, cv=cv: e.max(out=ts[:, hd, 0:8], in_=cv), r=["wk1"], w=["ts"])
                P.V(lambda e, hd=hd, cv=cv: e.max_index(out=sel[:, hd, 0:8], in_max=ts[:, hd, 0:8], in_values=cv), r=["wk1", "ts"], w=["sel"])
                P.V(lambda e, hd=hd, cv=cv, c2v=c2v: e.match_replace(out=c2v, in_to_replace=ts[:, hd, 0:8], in_values=cv, imm_value=-1e30), r=["wk1", "ts"], w=["wk0"])
                P.V(lambda e, hd=hd, c2v=c2v: e.max(out=ts[:, hd, 8:16], in_=c2v), r=["wk0"], w=["ts"])
                P.V(lambda e, hd=hd, c2v=c2v: e.max_index(out=sel[:, hd, 8:16], in_max=ts[:, hd, 8:16], in_values=c2v), r=["wk0", "ts"], w=["sel"])
                yield
            selv = sel[:].rearrange("p h k -> p (h k)")
            P.V(lambda e: e.tensor_scalar(out=au[:, 0, :], in0=selv, scalar1=4, scalar2=None, op0=ALU.logical_shift_right), r=["sel"], w=["au"])
            P.V(lambda e: e.tensor_scalar(out=au[:, 1, :], in0=selv, scalar1=15, scalar2=None, op0=ALU.bitwise_and), r=["sel"], w=["au"])
            P.V(lambda e: e.tensor_copy(out=af[:], in_=au[:]), r=["au"], w=["af"])
            P.V(lambda e: e.tensor_copy(out=i16f[:], in_=i16[:]), r=["i16"], w=["i16f"])
            eq = wk[1][:].rearrange("p (h s a) -> p h s a", h=8, s=16)
            i16r = i16f[:].rearrange("p (h t) k -> p h t k", t=2)
            for half in range(2):
                P.V(lambda e, half=half: e.tensor_tensor(out=eq, in0=af[:, half, :].rearrange("p (h s) -> p h s", h=8).unsqueeze(3).to_broadcast([128, 8, 16, 16]),
                                                         in1=iota[:].unsqueeze(1).unsqueeze(1).to_broadcast([128, 8, 16, 16]), op=ALU.is_equal), r=["af", "iota"], w=["wk1"])
                P.V(lambda e, half=half: e.tensor_tensor(out=eq, in0=eq, in1=i16r[:, :, half, :].unsqueeze(2).to_broadcast([128, 8, 16, 16]), op=ALU.mult), r=["wk1", "i16f"], w=["wk1"])
                P.V(lambda e, half=half: e.tensor_reduce(out=isel[:, half, :].rearrange("p (h s) -> p h s", h=8), in_=eq, axis=AX.X, op=ALU.add), r=["wk1"], w=["isel"])
                yield
            P.V(lambda e: e.scalar_tensor_tensor(out=idxf[:], in0=isel[:, 0, :], scalar=128.0, in1=isel[:, 1, :], op0=ALU.mult, op1=ALU.add), r=["isel"], w=["idxf"])
            if layer > 0:
                P.V(lambda e: e.tensor_scalar(out=idxf[:], in0=idxf[:], scalar1=float(layer * 16384), scalar2=None, op0=ALU.add), r=["idxf"], w=["idxf"])
            P.V(lambda e, s=s: e.tensor_copy(out=idxu[s][:], in_=idxf[:]), r=["idxf"], w=[f"idxu{s}"])
            g3 = gsm[s][:].rearrange("p (h k) -> p h k", h=8)
            P.V(lambda e, g3=g3: e.tensor_tensor(out=g3, in0=ts[:], in1=ts[:, :, 0:1].to_broadcast([128, 8, 16]), op=ALU.subtract), r=["ts"], w=[f"g{s}"])
            P.A(lambda e, s=s: e.activation(out=gsm[s][:], in_=gsm[s][:], func=AF.Exp), r=[f"g{s}"], w=[f"g{s}"])
            P.V(lambda e, g3=g3: e.tensor_reduce(out=sm[:, 0:8], in_=g3, axis=AX.X, op=ALU.add), r=[f"g{s}"], w=["sm"])
            P.V(lambda e: e.reciprocal(out=sm[:, 8:16], in_=sm[:, 0:8]), r=["sm"], w=["sm"])
            P.V(lambda e, g3=g3: e.tensor_tensor(out=g3, in0=g3, in1=sm[:, 8:16].unsqueeze(2).to_broadcast([128, 8, 16]), op=ALU.mult), r=[f"g{s}", "sm"], w=[f"g{s}"])

        gcount = [0]
        gate_v = [None]

        def gather(s, k):
            P = PP[0]
            slot = gcount[0] % NB
            gcount[0] += 1
            P.op("gpsimd", lambda e, slot=slot, s=s, k=k: e.indirect_dma_start(out=ring[slot][:], out_offset=None, in_=uvtab,
                                                                        in_offset=bass.IndirectOffsetOnAxis(ap=idxu[s][:, k:k + 1], axis=0)),
                 r=[f"idxu{s}"], w=[f"ring{slot}"], dma=True, chan=f"ring{slot}")
            return slot

        def back(n, nxt=None):
            P = PP[0]
            t = tiles[n]
            s = n % 2
            POOL_EVERY = 0
            LAGP = 2

            def consume(k, slot, via_pool):
                if via_pool:
                    P.G(lambda e, slot=slot, s=s: e.tensor_tensor(out=ring[slot][:, 0:D], in0=ring[slot][:, 0:D], in1=h[s][:], op=ALU.mult), r=[f"ring{slot}", f"h{s}"], w=[f"ring{slot}"])
                    P.A(lambda e, slot=slot, k=k: e.activation(out=ring[slot][:, 0:D], in_=ring[slot][:, 0:D], func=AF.Copy, accum_out=z[:, k:k + 1]), r=[f"ring{slot}"], w=[f"ring{slot}", f"z{k}"])
                else:
                    P.V(lambda e, slot=slot, s=s, k=k: e.scalar_tensor_tensor(out=junk[:], in0=ring[slot][:, 0:D], scalar=1.0, in1=h[s][:], op0=ALU.mult, op1=ALU.mult, accum_out=z[:, k:k + 1]),
                        r=[f"ring{slot}", f"h{s}"], w=[f"z{k}"])
                P.A(lambda e, k=k: e.activation(out=gz[:, k:k + 1], in_=z[:, k:k + 1], func=AF.Gelu_apprx_tanh), r=[f"z{k}"], w=[f"gz{k}"])
                P.A(lambda e, k=k, s=s: e.activation(out=av[:, k:k + 1], in_=gz[:, k:k + 1], func=AF.Copy, scale=gsm[s][:, k:k + 1]), r=[f"gz{k}", f"g{s}"], w=[f"a{k}"])
                P.A(lambda e, k=k: e.activation(out=dg[k % 4][:], in_=identb[:], func=AF.Copy, scale=av[:, k:k + 1]), r=[f"a{k}", "identb"], w=[f"dg{k % 4}"])
                first = not started[0]
                started[0] = True
                last = ndone[0] == 127
                ndone[0] += 1
                for nb in range(4):
                    P.T(lambda e, k=k, nb=nb, slot=slot, first=first, last=last: e.matmul(ps[4 + nb][:], lhsT=dg[k % 4][:], rhs=ring[slot][:, D + nb * 512:D + (nb + 1) * 512], start=first, stop=last),
                        r=[f"dg{k % 4}", f"ring{slot}"], w=[f"ps{4 + nb}"])

            started = [False]
            ndone = [0]
            pend = []
            for k in range(128):
                slot = gather(s, k)
                if POOL_EVERY and k % POOL_EVERY == POOL_EVERY - 1:
                    pend.append((k, slot))
                else:
                    consume(k, slot, False)
                while pend and pend[0][0] <= k - LAGP:
                    kk, sl_ = pend.pop(0)
                    consume(kk, sl_, True)
                if nxt is not None and k >= 8:
                    next(nxt, None)
            for kk, sl_ in pend:
                consume(kk, sl_, True)
            if nxt is not None:
                for _ in nxt:
                    pass
            if gate_v[0] != t["v"]:
                gate_v[0] = t["v"]
                load_bcast(P, "sync", gate[:], K.modd[layer, t["v"], 5 * D:6 * D], "gate", "gate")
            xr = wk[0]
            if t["src"][0] == "d":
                P.dma("sync", xr[:], t["src"][1], r=[], w=["wk0"], chan="xre")
            else:
                col = t["src"][2]
                P.op("gpsimd", lambda e, col=col, src=t["src"][1]: e.indirect_dma_start(out=xr[:], out_offset=None, in_=src,
                                                                                 in_offset=bass.IndirectOffsetOnAxis(ap=mr[:, col:col + 1], axis=0)),
                     r=["mr"], w=["wk0"], dma=True, chan="xre")
            for nb in range(4):
                P.V(lambda e, nb=nb: e.tensor_tensor(out=acc[:, nb * 512:(nb + 1) * 512], in0=ps[4 + nb][:], in1=gate[:, nb * 512:(nb + 1) * 512], op=ALU.mult), r=[f"ps{4 + nb}", "gate"], w=["wk1"])
            P.V(lambda e: e.tensor_tensor(out=acc[:], in0=acc[:], in1=xr[:], op=ALU.add), r=["wk1", "wk0"], w=["wk1"])
            if final:
                P.A(lambda e: e.activation(out=junk[:], in_=acc[:], func=AF.Square, accum_out=ssb[:, 0:1]), r=["wk1"], w=["junkf", "ssb"])
                P.V(lambda e: e.tensor_scalar(out=ssb[:, 1:2], in0=ssb[:, 0:1], scalar1=1.0 / D, scalar2=EPS, op0=ALU.mult, op1=ALU.add), r=["ssb"], w=["ssb1"])
                P.A(lambda e: e.activation(out=ssb[:, 2:3], in_=ssb[:, 1:2], func=AF.Sqrt), r=["ssb1"], w=["ssb2"])
                P.V(lambda e: e.reciprocal(out=ssb[:, 3:4], in_=ssb[:, 2:3]), r=["ssb2"], w=["ssb3"])
                load_bcast(P, "sync", xr[:], I["norm_final"], "wk0", "gfin")
                P.V(lambda e: e.scalar_tensor_tensor(out=acc[:], in0=acc[:], scalar=ssb[:, 3:4], in1=xr[:], op0=ALU.mult, op1=ALU.mult), r=["wk1", "ssb3", "wk0"], w=["wk1"])
            P.dma("sync", t["dst"], acc[:], r=["wk1"], w=[], chan="st")

        ngrp = -(-len(tiles) // 12)
        GSZ = -(-len(tiles) // ngrp)
        for g0 in range(0, len(tiles), GSZ):
            PP[0] = Prog(nc, f"{name}g{g0}")
            g1 = min(g0 + GSZ, len(tiles))
            for _ in front(g0):
                pass
            for n in range(g0, g1):
                back(n, front(n + 1) if n + 1 < g1 else None)
            PP[0].emit()
            K.peer_stats = PP[0].stats
            nc.all_engine_barrier()


def phase_rec(K):
    nc, I, ps = K.nc, K.I, K.ps
    NTOK = NT * 128
    for pas in ("u", "g"):
        with ExitStack() as es:
            sb = lambda n, s, d=F32: es.enter_context(nc.sbuf_tensor(n, s, d))
            W = sb("r_W" + pas, [128, KC, D], BF16)
            Gt = sb("r_G" + pas, [128, D]); St = sb("r_S" + pas, [128, D])
            ss = sb("r_ss" + pas, [128, 4])
            xt = sb("r_xt" + pas, [128, D]); h = sb("r_h" + pas, [128, D])
            hT = sb("r_hT" + pas, [128, KC, 256], BF16)
            ost = sb("r_ost" + pas, [128, 16, 256])
            P = Prog(nc, "rp" + pas)
            tc = K.tc[1]
            tc.alloc(es, 12)
            tc.begin()
            load_w_bf16(P, W, I["rec_w_in"], D, "W", col0=(D if pas == "u" else 0))
            for blk in (range(NT // 2) if pas == "u" else range(1, NT // 2)):
                is_ctx = blk == 0
                if blk in (0, 1):
                    v = 1 if is_ctx else 0
                    prep_GS(K, P, Gt, St, I["norm_mix"][1], K.modd[1, v, D:2 * D], K.modd[1, v, 0:D], h, "a")
                for ti in range(2):
                    e_t = blk * 2 + ti
                    norm_mod_T(K, P, K.xs2tile(e_t), xt, h, ss, Gt, St, "a", hT, "hT", ti * 128, (6, 7), 0)
                for j in range(16):
                    pb = j % 2
                    for kc in range(KC):
                        P.T(lambda e, kc=kc, j=j, pb=pb: e.matmul(ps[pb][:, 0:256], lhsT=W[:, kc, j * 128:(j + 1) * 128], rhs=hT[:, kc, :], start=(kc == 0), stop=(kc == KC - 1)),
                            r=["W", "hT"], w=[f"ps{pb}"])
                    fn = AF.Copy if pas == "u" else AF.Gelu_apprx_tanh
                    P.A(lambda e, j=j, pb=pb, fn=fn: e.activation(out=ost[:, j, :], in_=ps[pb][:, 0:256], func=fn), r=[f"ps{pb}"], w=["ost"])
                if pas == "u":
                    P.dma("sync", K.upre[:, :, blk * 256:(blk + 1) * 256].rearrange("c p t -> p c t"), ost[:], r=["ost"], w=["upre"], chan="ost")
                else:
                    l0 = (blk - 1) * 256
                    P.dma("sync", K.ggd[:, :, l0:l0 + 256].rearrange("c p t -> p c t"), ost[:], r=["ost"], w=["ggd"], chan="ost")
                tc.pump(P, 8)
            if pas == "g":
                tc.finish(P)
            else:
                tc.flush(P)
            P.emit()
        nc.all_engine_barrier()
    with ExitStack() as es:
        sb = lambda n, s, d=F32: es.enter_context(nc.sbuf_tensor(n, s, d))
        UPW = 4360
        up = sb("r_up", [128, 2, UPW])
        u = sb("r_u", [128, 2, NTOK]); ub = sb("r_ub", [128, 2, NTOK], BF16)
        A = sb("r_A", [128, NTOK]); Bt = sb("r_B", [128, NTOK])
        Y = [sb(f"r_Y{d}", [128, NTOK]) for d in range(2)]
        gg = sb("r_gg", [128, S]); ygb = sb("r_ygb", [128, S], BF16)
        wg = sb("r_wg", [128, 2, 2, 2, 256], BF16)
        cw = sb("r_cw", [128, 4, 16]); cb = sb("r_cb", [128, 16])
        ba = sb("r_ba", [128, 2, 16]); bx = sb("r_bx", [128, 2, 16]); lam = sb("r_lam", [128, 2, 16]); cl = sb("r_cl", [128, 2, 16])
        tmp = [[sb(f"r_t{i}{s}", [128, 512]) for i in range(4)] for s in range(2)]
        nba = sb("r_nba", [128, 2, 16]); nbx = sb("r_nbx", [128, 2, 16])
        P = Prog(nc, "rscan")
        P.G(lambda e: e.memset(up[:], 0.0), r=[], w=["up0", "up1"])
        for k in range(4):
            P.dma("sync", cw[:, k, :], I["rec_conv_w"][k].rearrange("(c p) -> p c", p=128), r=[], w=["cw"], chan=f"cw{k}", allow_slow_non_contiguous=True)
        P.dma("sync", cb[:], I["rec_conv_b"].rearrange("(c p) -> p c", p=128), r=[], w=["cb"], chan="cb", allow_slow_non_contiguous=True)
        for d in range(2):
            P.dma("sync", ba[:, d, :], I["rec_b_a"][d].rearrange("(c p) -> p c", p=128), r=[], w=["ba"], chan=f"ba{d}", allow_slow_non_contiguous=True)
            P.dma("sync", bx[:, d, :], I["rec_b_x"][d].rearrange("(c p) -> p c", p=128), r=[], w=["bx"], chan=f"bx{d}", allow_slow_non_contiguous=True)
            P.dma("sync", lam[:, d, :], I["rec_lambda"][d].rearrange("(c p) -> p c", p=128), r=[], w=["lam"], chan=f"lam{d}", allow_slow_non_contiguous=True)
        P.A(lambda e: e.activation(out=cl[:], in_=lam[:], func=AF.Exp, scale=-1.0), r=["lam"], w=["cl"])
        P.A(lambda e: e.activation(out=cl[:], in_=cl[:], func=AF.Ln, bias=1.0), r=["cl"], w=["cl"])
        P.V(lambda e: e.tensor_scalar(out=cl[:], in0=cl[:], scalar1=-8.0, scalar2=None, op0=ALU.mult), r=["cl"], w=["cl"])
        P.V(lambda e: e.tensor_scalar(out=nba[:], in0=ba[:], scalar1=-1.0, scalar2=None, op0=ALU.mult), r=["ba"], w=["nba"])
        P.V(lambda e: e.tensor_scalar(out=nbx[:], in0=bx[:], scalar1=-1.0, scalar2=None, op0=ALU.mult), r=["bx"], w=["nbx"])
        for n in range(8):
            for c in range(2):
                ch = 2 * n + c
                P.dma("sync", up[:, c, 1:257], K.upre[ch, :, 0:256], r=[], w=[f"up{c}"], chan=f"upc{c}")
                P.dma("sync", up[:, c, 260:260 + S], K.upre[ch, :, 256:NTOK], r=[], w=[f"up{c}"], chan=f"upl{c}")
                for (o0, nn, i0) in ((0, 256, 0), (256, S, 259)):
                    useg = u[:, c, o0:o0 + nn]
                    P.V(lambda e, c=c, ch=ch, useg=useg, i0=i0, nn=nn: e.tensor_scalar(out=useg, in0=up[:, c, i0:i0 + nn], scalar1=cw[:, 0, ch:ch + 1], scalar2=cb[:, ch:ch + 1], op0=ALU.mult, op1=ALU.add),
                        r=[f"up{c}", "cw", "cb"], w=[f"u{c}"])
                    for k in range(1, 4):
                        P.V(lambda e, c=c, ch=ch, useg=useg, i0=i0, nn=nn, k=k: e.scalar_tensor_tensor(out=useg, in0=up[:, c, i0 + k:i0 + k + nn], scalar=cw[:, k, ch:ch + 1], in1=useg, op0=ALU.mult, op1=ALU.add),
                            r=[f"up{c}", "cw", f"u{c}"], w=[f"u{c}"])
                P.A(lambda e, c=c: e.activation(out=ub[:, c, :], in_=u[:, c, :], func=AF.Copy), r=[f"u{c}"], w=["ub"])
            for d in range(2):
                for ty in range(2):
                    wsrc = (I["rec_w_a"] if ty == 0 else I["rec_w_x"])[d, n].rearrange("(k p) o -> p k o", p=128)
                    P.dma("gpsimd", wg[:, d, ty, :, :], wsrc, r=[], w=["wg"], chan=f"wg{d}{ty}")
            for oc in range(2):
                ch = 2 * n + oc
                for d in range(2):
                    for tb in range(9):
                        t0 = tb * 512
                        tn = min(512, NTOK - t0)
                        sl = tb % 2
                        pa, px = 2 * sl, 2 * sl + 1
                        r_, i_, a2, sq = tmp[sl]
                        for ty, pb in ((0, pa), (1, px)):
                            for kc in range(2):
                                P.T(lambda e, ty=ty, pb=pb, kc=kc, d=d, oc=oc, t0=t0, tn=tn: e.matmul(ps[pb][:, 0:tn], lhsT=wg[:, d, ty, kc, oc * 128:(oc + 1) * 128], rhs=ub[:, kc, t0:t0 + tn], start=(kc == 0), stop=(kc == 1)),
                                    r=["wg", "ub"], w=[f"ps{pb}"])
                        P.A(lambda e, pa=pa, tn=tn, d=d, ch=ch, r_=r_: e.activation(out=r_[:, 0:tn], in_=ps[pa][:, 0:tn], func=AF.Exp, scale=-1.0, bias=nba[:, d, ch:ch + 1]), r=[f"ps{pa}", "nba"], w=[f"t0{sl}"])
                        P.A(lambda e, px=px, tn=tn, d=d, ch=ch, i_=i_: e.activation(out=i_[:, 0:tn], in_=ps[px][:, 0:tn], func=AF.Exp, scale=-1.0, bias=nbx[:, d, ch:ch + 1]), r=[f"ps{px}", "nbx"], w=[f"t1{sl}"])
                        P.G(lambda e, tn=tn, r_=r_: e.tensor_scalar(out=r_[:, 0:tn], in0=r_[:, 0:tn], scalar1=1.0, scalar2=1.0, op0=ALU.add, op1=ALU.mult), r=[f"t0{sl}"], w=[f"t0{sl}"])
                        P.V(lambda e, tn=tn, r_=r_: e.reciprocal(out=r_[:, 0:tn], in_=r_[:, 0:tn]), r=[f"t0{sl}"], w=[f"t0{sl}"])
                        P.G(lambda e, tn=tn, i_=i_: e.tensor_scalar(out=i_[:, 0:tn], in0=i_[:, 0:tn], scalar1=1.0, scalar2=1.0, op0=ALU.add, op1=ALU.mult), r=[f"t1{sl}"], w=[f"t1{sl}"])
                        P.V(lambda e, tn=tn, i_=i_: e.reciprocal(out=i_[:, 0:tn], in_=i_[:, 0:tn]), r=[f"t1{sl}"], w=[f"t1{sl}"])
                        P.A(lambda e, tn=tn, t0=t0, d=d, ch=ch, r_=r_: e.activation(out=A[:, t0:t0 + tn], in_=r_[:, 0:tn], func=AF.Exp, scale=cl[:, d, ch:ch + 1]), r=[f"t0{sl}", "cl"], w=["A"])
                        P.G(lambda e, tn=tn, t0=t0, a2=a2: e.tensor_tensor(out=a2[:, 0:tn], in0=A[:, t0:t0 + tn], in1=A[:, t0:t0 + tn], op=ALU.mult), r=["A"], w=[f"t2{sl}"])
                        P.A(lambda e, tn=tn, a2=a2, sq=sq: e.activation(out=sq[:, 0:tn], in_=a2[:, 0:tn], func=AF.Ln, scale=-1.0, bias=1.0), r=[f"t2{sl}"], w=[f"t3{sl}"])
                        P.A(lambda e, tn=tn, sq=sq: e.activation(out=sq[:, 0:tn], in_=sq[:, 0:tn], func=AF.Exp, scale=0.5), r=[f"t3{sl}"], w=[f"t3{sl}"])
                        P.V(lambda e, tn=tn, sq=sq, i_=i_: e.tensor_tensor(out=sq[:, 0:tn], in0=sq[:, 0:tn], in1=i_[:, 0:tn], op=ALU.mult), r=[f"t3{sl}", f"t1{sl}"], w=[f"t3{sl}"])
                        P.V(lambda e, tn=tn, t0=t0, sq=sq, oc=oc: e.tensor_tensor(out=Bt[:, t0:t0 + tn], in0=sq[:, 0:tn], in1=u[:, oc, t0:t0 + tn], op=ALU.mult), r=[f"t3{sl}", f"u{oc}"], w=["B"])
                    Yd = Y[d]
                    if d == 0:
                        P.V(lambda e, Yd=Yd: e.tensor_tensor_scan(out=Yd[:, 0:256], data0=A[:, 0:256], data1=Bt[:, 0:256], initial=0.0, op0=ALU.mult, op1=ALU.add), r=["A", "B"], w=[f"Y{d}c"])
                        P.V(lambda e, Yd=Yd: e.tensor_tensor_scan(out=Yd[:, 256:NTOK], data0=A[:, 256:NTOK], data1=Bt[:, 256:NTOK], initial=Yd[:, 255:256], op0=ALU.mult, op1=ALU.add), r=["A", "B", f"Y{d}c"], w=[f"Y{d}l"])
                    else:
                        P.V(lambda e, Yd=Yd: e.tensor_tensor_scan(out=Yd[:, 0:256][:, ::-1], data0=A[:, 0:256][:, ::-1], data1=Bt[:, 0:256][:, ::-1], initial=0.0, op0=ALU.mult, op1=ALU.add), r=["A", "B"], w=[f"Y{d}c"])
                        P.V(lambda e, Yd=Yd: e.tensor_tensor_scan(out=Yd[:, 256:NTOK][:, ::-1], data0=A[:, 256:NTOK][:, ::-1], data1=Bt[:, 256:NTOK][:, ::-1], initial=Yd[:, 0:1], op0=ALU.mult, op1=ALU.add), r=["A", "B", f"Y{d}c"], w=[f"Y{d}l"])
                P.dma("sync", gg[:], K.ggd[ch], r=[], w=["gg"], chan="gg")
                P.G(lambda e: e.tensor_tensor(out=Y[0][:, 256:NTOK], in0=Y[0][:, 256:NTOK], in1=Y[1][:, 256:NTOK], op=ALU.add), r=["Y0l", "Y1l"], w=["Y0l"])
                P.V(lambda e: e.tensor_tensor(out=ygb[:], in0=Y[0][:, 256:NTOK], in1=gg[:], op=ALU.mult), r=["Y0l", "gg"], w=["ygb"])
                P.dma("sync", K.ygd[:, ch, :], ygb[:], r=["ygb"], w=["ygd"], chan="ygb")
        P.emit()
    nc.all_engine_barrier()
    with ExitStack() as es:
        sb = lambda n, s, d=F32: es.enter_context(nc.sbuf_tensor(n, s, d))
        Wout = sb("r_Wout", [128, KC, D], BF16)
        gate = sb("r_gate", [128, D])
        yt = [sb(f"r_yt{i}", [128, KC, 128], BF16) for i in range(2)]
        xr = [sb(f"r_xr{i}", [128, D]) for i in range(2)]
        y = sb("r_y", [128, D])
        P = Prog(nc, "rout")
        load_w_bf16(P, Wout, I["rec_w_out"], D, "Wo")
        load_bcast(P, "sync", gate[:], K.modd[1, 0, 2 * D:3 * D], "gate", "gate")
        for t in range(S // 128):
            s = t % 2
            P.dma("sync", yt[s][:], K.ygd[:, :, t * 128:(t + 1) * 128], r=[], w=[f"yt{s}"], chan=f"yt{s}")
            P.dma("sync", xr[s][:], K.xs2tile(t + 2), r=[], w=[f"xr{s}"], chan=f"xr{s}")
            for nb in range(4):
                pb = 4 + nb
                for kc in range(KC):
                    P.T(lambda e, kc=kc, nb=nb, pb=pb, s=s: e.matmul(ps[pb][:], lhsT=yt[s][:, kc, :], rhs=Wout[:, kc, nb * 512:(nb + 1) * 512], start=(kc == 0), stop=(kc == KC - 1)),
                        r=[f"yt{s}", "Wo"], w=[f"ps{pb}"])
                P.V(lambda e, nb=nb, pb=pb: e.tensor_tensor(out=y[:, nb * 512:(nb + 1) * 512], in0=ps[pb][:], in1=gate[:, nb * 512:(nb + 1) * 512], op=ALU.mult), r=[f"ps{pb}", "gate"], w=[f"y{nb}"])
                P.G(lambda e, nb=nb, s=s: e.tensor_tensor(out=y[:, nb * 512:(nb + 1) * 512], in0=y[:, nb * 512:(nb + 1) * 512], in1=xr[s][:, nb * 512:(nb + 1) * 512], op=ALU.add), r=[f"y{nb}", f"xr{s}"], w=[f"y{nb}"])
            P.dma("sync", K.xs3[t * 128:(t + 1) * 128, :], y[:], r=[f"y{nb}" for nb in range(4)], w=["xs3"], chan="y")
        P.emit()
```

```python
import os
import numpy as np
import ml_dtypes
from contextlib import ExitStack
import concourse.bass as bass
import concourse.mybir as mybir
from concourse.bass_utils import run_bass_kernel_spmd

F32 = mybir.dt.float32
BF16 = mybir.dt.bfloat16
U32 = mybir.dt.uint32
ALU = mybir.AluOpType
AF = mybir.ActivationFunctionType
AX = mybir.AxisListType

D = 2048
KC = 16
S = 4096
C = 256
NT = (S + C) // 128
EPS = 1e-6
COMPUTE = ("tensor", "vector", "scalar", "gpsimd")


class Prog:
    def __init__(self, nc, name="p"):
        self.nc = nc
        self.name = name
        self.ops = []
        self.last_w = {}
        self.readers = {}

    def op(self, eng, fn, r=(), w=(), dma=False, chan=None):
        i = len(self.ops)
        deps = set()
        for k in r:
            lw = self.last_w.get(k)
            if lw is not None:
                deps.add(lw)
        for k in w:
            lw = self.last_w.get(k)
            if lw is not None:
                deps.add(lw)
            rd = self.readers.get(k)
            if rd:
                deps.update(rd.values())
        for k in w:
            self.last_w[k] = i
            self.readers[k] = {}
        for k in r:
            d = self.readers.setdefault(k, {})
            d[("dma", i) if dma else eng] = i
        if dma:
            assert chan is not None
        self.ops.append(dict(eng=eng, fn=fn, deps=deps, dma=dma, chan=chan))
        return i

    def dma(self, eng, out, in_, r, w, chan, **kw):
        return self.op(eng, lambda e: e.dma_start(out=out, in_=in_, **kw), r=r, w=w, dma=True, chan=chan)

    def V(self, fn, r, w):
        return self.op("vector", fn, r, w)

    def A(self, fn, r, w):
        return self.op("scalar", fn, r, w)

    def G(self, fn, r, w):
        return self.op("gpsimd", fn, r, w)

    def T(self, fn, r, w):
        return self.op("tensor", fn, r, w)

    def emit(self):
        nc = self.nc
        ops = self.ops
        needed = set()
        for o in ops:
            for d in o["deps"]:
                od = ops[d]
                if od["dma"]:
                    continue
                if od["eng"] == "tensor" and o["eng"] == "tensor" and not o["dma"]:
                    continue
                needed.add(d)
        cnt = {e: 0 for e in COMPUTE + ("sync",)}
        chan_cnt = {}
        for i, o in enumerate(ops):
            if o["dma"]:
                c = o["chan"]
                chan_cnt[c] = chan_cnt.get(c, 0) + 16
                o["sig"] = ("c", c, chan_cnt[c])
            elif i in needed:
                cnt[o["eng"]] += 1
                o["sig"] = ("e", o["eng"], cnt[o["eng"]])
            else:
                o["sig"] = None
        self.stats = dict(cnt=dict(cnt), chans=len(chan_cnt), maxchan=max(chan_cnt.values()) if chan_cnt else 0, nops=len(ops))
        esem = {e: nc.alloc_semaphore(name=f"{self.name}_e_{e}") for e in COMPUTE if cnt[e] > 0}
        csem = {c: nc.alloc_semaphore(name=f"{self.name}_c_{c}") for c in chan_cnt}
        with ExitStack() as es:
            block = es.enter_context(nc.Block())
            by_eng = {}
            for i, o in enumerate(ops):
                by_eng.setdefault(o["eng"], []).append(i)

            def make(engname, idxs):
                def body(eng):
                    waited = {}
                    for i in idxs:
                        o = ops[i]
                        for d in sorted(o["deps"]):
                            od = ops[d]
                            sig = od["sig"]
                            if sig is None:
                                continue
                            if sig[0] == "e":
                                if od["eng"] == "tensor" and engname == "tensor" and not o["dma"]:
                                    continue
                                sem = esem[sig[1]]
                            else:
                                sem = csem[sig[1]]
                            key = (sig[0], sig[1])
                            if waited.get(key, 0) >= sig[2]:
                                continue
                            eng.wait_ge(sem, sig[2])
                            waited[key] = sig[2]
                        ins = o["fn"](eng)
                        sig = o["sig"]
                        if sig is not None:
                            if sig[0] == "e":
                                ins.then_inc(esem[sig[1]], 1)
                            else:
                                ins.then_inc(csem[sig[1]], 16)
                    last = {}
                    for i in idxs:
                        o = ops[i]
                        if o["dma"]:
                            last[o["chan"]] = o["sig"][2]
                    for c, v in last.items():
                        if waited.get(("c", c), 0) < v:
                            eng.wait_ge(csem[c], v)
                return body

            for engname, idxs in by_eng.items():
                getattr(block, engname)(make(engname, idxs))
        nc.all_engine_barrier()
        nc.clear_and_free_semaphores(list(esem.values()) + list(csem.values()))
        nc.all_engine_barrier()
        self.ops = []
        self.last_w = {}
        self.readers = {}


def _consts():
    ident = np.eye(128, dtype=np.float32)
    rotm = np.zeros((128, 128), np.float32)
    for m in range(128):
        base = 0 if m < 64 else 64
        d = m - base
        if d < 32:
            rotm[base + d + 32, m] = -1.0
        else:
            rotm[base + d - 32, m] = 1.0
    t = np.arange(S)
    row = (t // 64).astype(np.float32)
    col = (t % 64).astype(np.float32)
    inv = (np.float32(10000.0) ** (-np.arange(0, 64, 2, dtype=np.float32) / np.float32(64))).astype(np.float32)
    ang_r = row[:, None] * inv[None, :]
    ang_c = col[:, None] * inv[None, :]
    ang = np.concatenate([ang_r, ang_r, ang_c, ang_c], axis=-1).astype(np.float32)
    cosT = np.ascontiguousarray(np.cos(ang).astype(np.float32).T)
    sinT = np.ascontiguousarray(np.sin(ang).astype(np.float32).T)
    k = np.arange(128)[:, None]
    q = np.arange(128)[None, :]
    maskL = (k >= q).astype(ml_dtypes.bfloat16)
    maskR = (k <= q).astype(ml_dtypes.bfloat16)
    iota16 = np.tile(np.arange(16, dtype=np.float32)[None, :], (128, 1))
    return dict(ident=ident, rotm=rotm, cosT=cosT, sinT=sinT, maskL=maskL, maskR=maskR, iota16=iota16)


class Ctx:
    pass


def build(stop_after=99, debug=False, peer_tiles=None, start_at=0, pair_split=False):
    nc = bass.Bass("TRN2", target_bir_lowering=False)
    K = Ctx()
    K.nc = nc
    K._uid = [0]

    def uid():
        K._uid[0] += 1
        return K._uid[0]
    K.uid = uid
    din = lambda n, s, d=F32: nc.dram_tensor(n, list(s), d, kind="ExternalInput").ap()
    dscr = lambda n, s, d=F32: nc.dram_tensor(n, list(s), d, kind="Internal").ap()
    I = dict(
        x=din("x", [S, D]), c=din("c", [D]), ctx=din("ctx", [C, D]), c_ctx=din("c_ctx", [D]),
        w_mod=din("w_mod", [2, D, 6 * D]), b_mod=din("b_mod", [2, 6 * D]),
        norm_mix=din("norm_mix", [2, D]), norm_ffn=din("norm_ffn", [2, D]), norm_final=din("norm_final", [D]),
        attn_w_qkv=din("attn_w_qkv", [D, 3072]), attn_w_o=din("attn_w_o", [D, D]), attn_sink=din("attn_sink", [16]),
        rec_w_in=din("rec_w_in", [D, 2 * D]), rec_conv_w=din("rec_conv_w", [4, D]), rec_conv_b=din("rec_conv_b", [D]),
        rec_w_a=din("rec_w_a", [2, 8, 256, 256]), rec_b_a=din("rec_b_a", [2, D]),
        rec_w_x=din("rec_w_x", [2, 8, 256, 256]), rec_b_x=din("rec_b_x", [2, D]),
        rec_lambda=din("rec_lambda", [2, D]), rec_w_out=din("rec_w_out", [D, D]),
        peer_w_q=din("peer_w_q", [2, D, D]), peer_keys=din("peer_keys", [2, 2, 128, 128]),
        peer_u=din("peer_u", [2, 16384, D]), peer_v=din("peer_v", [2, 16384, D]),
        ident=din("ident", [128, 128]), rotm=din("rotm", [128, 128]), cosT=din("cosT", [128, S]), sinT=din("sinT", [128, S]),
        maskL=din("maskL", [128, 128], BF16), maskR=din("maskR", [128, 128], BF16), iota16=din("iota16", [128, 16]),
        myrows=din("myrows", [128, 16], U32),
        rows0=din("rows0", [128, 17], U32),
    )
    K.I = I
    out = nc.dram_tensor("out", [S // 2, D], F32, kind="ExternalOutput").ap()
    K.out = out
    K.modd = dscr("modd", [2, 2, 6 * D])
    K.qd = dscr("qd", [128, 16, NT * 128], BF16)
    K.kd = dscr("kd", [128, 4, NT * 128], BF16)
    K.vd = dscr("vd", [NT * 128, 512], BF16)
    K.xs1 = dscr("xs1", [NT * 128, D])
    K.xs2 = dscr("xs2", [NT * 128, D]) if start_at < 3 else din("xs2", [NT * 128, D])
    K.xs3 = dscr("xs3", [S, D]) if start_at < 4 else din("xs3", [S, D])
    K.upre = dscr("upre", [16, 128, NT * 128])
    K.ggd = dscr("ggd", [16, 128, S])
    K.ygd = dscr("ygd", [128, 16, S], BF16)
    K.pair_split = pair_split
    if pair_split:
        K.xs2loc = dscr("xs2loc", [17 * 128, D])
        K.xs2all = dscr("xs2all", [2 * 17 * 128, D])

    def xs2tile(e):
        if not pair_split:
            return K.xs2[e * 128:(e + 1) * 128, :]
        if e < 2:
            r0 = (e * 17) * 128
        else:
            i = e - 2
            r0 = ((i // 16) * 17 + 1 + i % 16) * 128
        return K.xs2all[r0:r0 + 128, :]
    K.xs2tile = xs2tile
    K.uvb16 = dscr("uvb16", [2 * 16384, 2 * D], BF16)
    dbg = {}
    if debug:
        dbg["d_modd"] = nc.dram_tensor("d_modd", [2, 2, 6 * D], F32, kind="ExternalOutput").ap()
        dbg["d_xs1"] = nc.dram_tensor("d_xs1", [NT * 128, D], F32, kind="ExternalOutput").ap()
        dbg["d_xs2"] = nc.dram_tensor("d_xs2", [NT * 128, D], F32, kind="ExternalOutput").ap()
        dbg["d_xs3"] = nc.dram_tensor("d_xs3", [S, D], F32, kind="ExternalOutput").ap()
    K.dbg = dbg

    with ExitStack() as es:
        K.ps = [es.enter_context(nc.psum_tensor(f"ps{i}", [128, 512], F32)) for i in range(8)]
        K.ident = es.enter_context(nc.sbuf_tensor("identsb", [128, 128], F32))
        P = Prog(nc, "c0")
        P.dma("sync", K.ident[:], I["ident"], r=[], w=["ident"], chan="ident")
        P.emit()
        nc.all_engine_barrier()
        phase_mod(K)
        nc.all_engine_barrier()
        K.tc = [TabConv(K, 0, "a"), TabConv(K, 1, "b")]
        if debug:
            copy_dram(K, K.modd.rearrange("l v n -> (l v) n"), dbg["d_modd"].rearrange("l v n -> (l v) n"), 4, "cpm")
        if stop_after >= 1 and start_at <= 1:
            phase_attn(K)
            nc.all_engine_barrier()
            if debug:
                copy_dram(K, K.xs1, dbg["d_xs1"], NT * 128, "cpx1")
        if stop_after >= 2 and start_at <= 2:
            if pair_split:
                tl = [dict(src=("g", K.xs1, j), v=1 if j == 0 else 0, dst=K.xs2loc[j * 128:(j + 1) * 128, :]) for j in range(17)]
                phase_peer(K, 0, tl, False, "pe0", idxname="rows0")
                nc.all_engine_barrier()
                P = Prog(nc, "cc")
                P.op("gpsimd", lambda e: e.collective_compute("AllGather", ALU.bypass, replica_groups=[[0, 1], [2, 3], [4, 5], [6, 7]],
                                                              ins=[K.xs2loc], outs=[K.xs2all]), r=[], w=[], dma=True, chan="cc")
                P.emit()
            else:
                tl = [dict(src=("d", K.xs1[n * 128:(n + 1) * 128, :]), v=1 if n < 2 else 0, dst=K.xs2[n * 128:(n + 1) * 128, :]) for n in range(NT)]
                if peer_tiles is not None:
                    tl = [tl[i] for i in peer_tiles]
                phase_peer(K, 0, tl, False, "pe0")
            nc.all_engine_barrier()
            if debug:
                copy_dram(K, K.xs2, dbg["d_xs2"], NT * 128, "cpx2")
        if stop_after >= 3 and start_at <= 3:
            phase_rec(K)
            nc.all_engine_barrier()
            if debug:
                copy_dram(K, K.xs3, dbg["d_xs3"], S, "cpx3")
        if stop_after >= 4:
            tl = [dict(src=("g", K.xs3, t), v=0, dst=K.out[t * 128:(t + 1) * 128, :]) for t in range(S // 256)]
            if peer_tiles is not None:
                tl = tl[:len(peer_tiles)]
            phase_peer(K, 1, tl, True, "pe1")
    return nc


def copy_dram(K, src, dst, rows, name):
    nc = K.nc
    with ExitStack() as es:
        t = es.enter_context(nc.sbuf_tensor(name + "_t", [128, src.shape[1]], src.dtype))
        P = Prog(nc, name)
        for r0 in range(0, rows, 128):
            n = min(128, rows - r0)
            P.dma("sync", t[0:n, :], src[r0:r0 + n, :], r=[], w=["t"], chan="ld")
            P.dma("sync", dst[r0:r0 + n, :], t[0:n, :], r=["t"], w=[], chan="st")
        P.emit()
    nc.all_engine_barrier()


def phase_mod(K):
    nc, I = K.nc, K.I
    with ExitStack() as es:
        sb = lambda n, s, d=F32: es.enter_context(nc.sbuf_tensor(n, s, d))
        cc = sb("m_cc", [128, KC, 2])
        craw = sb("m_craw", [128, 2, KC])
        wt = [sb(f"m_wt{i}", [128, KC, 512]) for i in range(2)]
        bt = sb("m_bt", [2, 512])
        ot = [sb(f"m_ot{i}", [2, 512]) for i in range(2)]
        P = Prog(nc, "mod")
        P.dma("sync", craw[:, 0, :], I["c"].rearrange("(k p) -> p k", p=128), r=[], w=["craw0"], chan="craw0", allow_slow_non_contiguous=True)
        P.dma("sync", craw[:, 1, :], I["c_ctx"].rearrange("(k p) -> p k", p=128), r=[], w=["craw1"], chan="craw1", allow_slow_non_contiguous=True)
        for v in range(2):
            P.A(lambda e, v=v: e.activation(out=cc[:, :, v], in_=craw[:, v, :], func=AF.Silu), r=[f"craw{v}"], w=["cc"])
        n = 0
        for l in range(2):
            for j in range(24):
                s = n % 2
                w_src = I["w_mod"][l].rearrange("(k p) n -> p k n", p=128)[:, :, j * 512:(j + 1) * 512]
                P.dma("sync" if s == 0 else "gpsimd", wt[s][:], w_src, r=[], w=[f"wt{s}"], chan=f"wt{s}")
                for v in range(2):
                    P.dma("sync", bt[v:v + 1, :], I["b_mod"][l:l + 1, j * 512:(j + 1) * 512], r=[], w=["bt"], chan=f"bt{v}")
                pb = K.ps[s]
                for kc in range(KC):
                    P.T(lambda e, kc=kc, s=s, pb=pb: e.matmul(pb[0:2, :], lhsT=cc[:, kc, :], rhs=wt[s][:, kc, :], start=(kc == 0), stop=(kc == KC - 1)),
                        r=["cc", f"wt{s}"], w=[f"ps{s}"])
                P.V(lambda e, s=s, pb=pb: e.tensor_tensor(out=ot[s][:], in0=pb[0:2, :], in1=bt[:], op=ALU.add), r=[f"ps{s}", "bt"], w=[f"ot{s}"])
                P.dma("sync", K.modd[l, :, j * 512:(j + 1) * 512], ot[s][:], r=[f"ot{s}"], w=[], chan=f"ot{s}")
                n += 1
        P.emit()


def load_bcast(P, eng, tile_ap, src_row_ap, key, chan):
    P.dma(eng, tile_ap, src_row_ap.partition_broadcast(128), r=[], w=[key], chan=chan)


def prep_GS(K, P, Gt, St, gain_row, scale_row, shift_row, tmp, tag):
    load_bcast(P, "sync", Gt[:], scale_row, f"G{tag}", f"G{tag}")
    load_bcast(P, "sync", tmp[:], gain_row, "gstmp", "gstmp")
    load_bcast(P, "sync", St[:], shift_row, f"S{tag}", f"S{tag}")
    P.V(lambda e: e.scalar_tensor_tensor(out=Gt[:], in0=Gt[:], scalar=1.0, in1=tmp[:], op0=ALU.add, op1=ALU.mult), r=[f"G{tag}", "gstmp"], w=[f"G{tag}"])


def norm_mod_T(K, P, src_ap, xt, h, ss, Gt, St, gtag, hT, hT_key, tok0, tpb, xslot):
    ps = K.ps
    P.dma("sync", xt[:], src_ap, r=[], w=[f"xt{xslot}"], chan=f"xt{xslot}")
    P.A(lambda e: e.activation(out=h[:], in_=xt[:], func=AF.Square, accum_out=ss[:, 0:1]), r=[f"xt{xslot}"], w=["h", "ss"])
    P.V(lambda e: e.tensor_scalar(out=ss[:, 1:2], in0=ss[:, 0:1], scalar1=1.0 / D, scalar2=EPS, op0=ALU.mult, op1=ALU.add), r=["ss"], w=["ss1"])
    P.A(lambda e: e.activation(out=ss[:, 2:3], in_=ss[:, 1:2], func=AF.Sqrt), r=["ss1"], w=["ss2"])
    P.V(lambda e: e.reciprocal(out=ss[:, 3:4], in_=ss[:, 2:3]), r=["ss2"], w=["ss3"])
    P.V(lambda e: e.scalar_tensor_tensor(out=h[:], in0=xt[:], scalar=ss[:, 3:4], in1=Gt[:], op0=ALU.mult, op1=ALU.mult),
        r=[f"xt{xslot}", "ss3", f"G{gtag}"], w=["h"])
    P.V(lambda e: e.tensor_tensor(out=h[:], in0=h[:], in1=St[:], op=ALU.add), r=["h", f"S{gtag}"], w=["h"])
    if hT is None:
        return
    for q in range(4):
        b = tpb[q % 2]
        for j in range(4):
            kc = 4 * q + j
            P.T(lambda e, kc=kc, j=j, b=b: e.transpose(out=ps[b][:, j * 128:(j + 1) * 128], in_=h[:, kc * 128:(kc + 1) * 128], identity=K.ident[:]),
                r=["h", "ident"], w=[f"ps{b}"])
        P.A(lambda e, q=q, b=b: e.activation(out=hT[:, 4 * q:4 * q + 4, tok0:tok0 + 128], in_=ps[b][:].rearrange("p (j t) -> p j t", j=4), func=AF.Copy),
            r=[f"ps{b}"], w=[hT_key])


def load_w_bf16(P, wsb, w_dram, ncols, key, col0=0):
    for kc in range(KC):
        for c0 in range(0, ncols, 1024):
            cn = min(1024, ncols - c0)
            P.dma("gpsimd", wsb[:, kc, c0:c0 + cn], w_dram[kc * 128:(kc + 1) * 128, col0 + c0:col0 + c0 + cn], r=[], w=[key], chan=f"{key}_{kc % 4}")


def phase_attn(K):
    nc, I, ps = K.nc, K.I, K.ps
    SCALE = 128 ** -0.5
    with ExitStack() as es:
        sb = lambda n, s, d=F32: es.enter_context(nc.sbuf_tensor(n, s, d))
        with ExitStack() as es1:
            sb1 = lambda n, s, d=F32: es1.enter_context(nc.sbuf_tensor(n, s, d))
            W = sb1("a_W", [128, KC, 3072], BF16)
            Gt = sb1("a_G", [128, D]); St = sb1("a_S", [128, D])
            ss = sb1("a_ss", [128, 4])
            xt = sb1("a_xt", [128, D]); h = sb1("a_h", [128, D])
            hT = sb1("a_hT", [128, KC, 256], BF16)
            rotm = sb1("a_rotm", [128, 128])
            cs = sb1("a_cos", [128, 256]); sn = sb1("a_sin", [128, 256])
            qsb = [sb1(f"a_qsb{i}", [128, 256]) for i in range(2)]
            t1 = [sb1(f"a_t1{i}", [128, 256]) for i in range(2)]
            t2 = [sb1(f"a_t2{i}", [128, 256]) for i in range(2)]
            qst = sb1("a_qst", [128, 20, 256], BF16)
            vst = sb1("a_vst", [128, 2, 512], BF16)
            P = Prog(nc, "qkv")
            tc = K.tc[0]
            tc.alloc(es1, 12)
            tc.begin()
            load_w_bf16(P, W, I["attn_w_qkv"], 3072, "W")
            P.dma("sync", rotm[:], I["rotm"], r=[], w=["rotm"], chan="rotm")
            for blk in range(NT // 2):
                is_ctx = blk == 0
                if blk in (0, 1):
                    v = 1 if is_ctx else 0
                    prep_GS(K, P, Gt, St, I["norm_mix"][0], K.modd[0, v, D:2 * D], K.modd[0, v, 0:D], h, "a")
                for ti in range(2):
                    e_t = blk * 2 + ti
                    src = I["ctx"][e_t * 128:(e_t + 1) * 128, :] if is_ctx else I["x"][(e_t - 2) * 128:(e_t - 1) * 128, :]
                    norm_mod_T(K, P, src, xt, h, ss, Gt, St, "a", hT, "hT", ti * 128, (6, 7), 0)
                if not is_ctx:
                    l0 = (blk - 1) * 256
                    P.dma("sync", cs[:], I["cosT"][:, l0:l0 + 256], r=[], w=["cos"], chan="cos")
                    P.dma("sync", sn[:], I["sinT"][:, l0:l0 + 256], r=[], w=["sin"], chan="sin")
                for j in range(20):
                    pb = j % 2
                    for kc in range(KC):
                        P.T(lambda e, kc=kc, j=j, pb=pb: e.matmul(ps[pb][:, 0:256], lhsT=W[:, kc, j * 128:(j + 1) * 128], rhs=hT[:, kc, :], start=(kc == 0), stop=(kc == KC - 1)),
                            r=["W", "hT"], w=[f"ps{pb}"])
                    dst, dkey = qst[:, j, :], "qst"
                    if is_ctx:
                        P.A(lambda e, pb=pb, dst=dst: e.activation(out=dst, in_=ps[pb][:, 0:256], func=AF.Copy), r=[f"ps{pb}"], w=[dkey])
                    else:
                        s2 = j % 2
                        P.A(lambda e, pb=pb, s2=s2: e.activation(out=qsb[s2][:], in_=ps[pb][:, 0:256], func=AF.Copy), r=[f"ps{pb}"], w=[f"qsb{s2}"])
                        P.T(lambda e, s2=s2: e.matmul(ps[2 + s2][:, 0:256], lhsT=rotm[:], rhs=qsb[s2][:], start=True, stop=True), r=["rotm", f"qsb{s2}"], w=[f"ps{2 + s2}"])
                        P.V(lambda e, s2=s2: e.tensor_tensor(out=t1[s2][:], in0=qsb[s2][:], in1=cs[:], op=ALU.mult), r=[f"qsb{s2}", "cos"], w=[f"t1{s2}"])
                        P.V(lambda e, s2=s2: e.tensor_tensor(out=t2[s2][:], in0=ps[2 + s2][:, 0:256], in1=sn[:], op=ALU.mult), r=[f"ps{2 + s2}", "sin"], w=[f"t2{s2}"])
                        P.V(lambda e, s2=s2, dst=dst: e.tensor_tensor(out=dst, in0=t1[s2][:], in1=t2[s2][:], op=ALU.add), r=[f"t1{s2}", f"t2{s2}"], w=[dkey])
                tc.pump(P, 6)
                P.dma("sync", K.qd[:, :, blk * 256:(blk + 1) * 256], qst[:, 0:16, :], r=["qst"], w=["qd"], chan="qst")
                P.dma("sync", K.kd[:, :, blk * 256:(blk + 1) * 256], qst[:, 16:20, :], r=["qst"], w=["kd"], chan="kst")
                for ti in range(2):
                    e_t = blk * 2 + ti
                    pb = 4 + ti
                    for kc in range(KC):
                        P.T(lambda e, kc=kc, ti=ti, pb=pb: e.matmul(ps[pb][:], lhsT=hT[:, kc, ti * 128:(ti + 1) * 128], rhs=W[:, kc, 2560:3072], start=(kc == 0), stop=(kc == KC - 1)),
                            r=["W", "hT"], w=[f"ps{pb}"])
                    P.A(lambda e, ti=ti, pb=pb: e.activation(out=vst[:, ti, :], in_=ps[pb][:], func=AF.Copy), r=[f"ps{pb}"], w=[f"vst{ti}"])
                    P.dma("sync", K.vd[e_t * 128:(e_t + 1) * 128, :], vst[:, ti, :], r=[f"vst{ti}"], w=["vd"], chan=f"vst{ti}")
            tc.flush(P)
            P.emit()
        nc.all_engine_barrier()
        with ExitStack() as es2:
            sb2 = lambda n, s, d=F32: es2.enter_context(nc.sbuf_tensor(n, s, d))
            Wo = sb2("a_Wo", [128, KC, D], BF16)
            kT = sb2("a_kT", [128, 4, NT * 128], BF16)
            vx = sb2("a_vx", [128, NT, 4, 129], BF16)
            gate = sb2("a_gate", [128, D])
            esink = sb2("a_esink", [128, 16])
            qt = [sb2(f"a_qt{i}", [128, 16, 128], BF16) for i in range(2)]
            E = [sb2(f"a_E{i}", [128, 5, 512], BF16) for i in range(2)]
            mL = sb2("a_mL", [128, 128], BF16); mR = sb2("a_mR", [128, 128], BF16)
            o = sb2("a_o", [128, D]); oT = sb2("a_oT", [128, KC, 128], BF16)
            den = sb2("a_den", [128, 8])
            xt = [sb2(f"a_xr{i}", [128, D]) for i in range(1)]
            y = sb2("a_y", [128, D])
            sraw = sb2("a_sraw", [128, 16])
            P = Prog(nc, "att")
            tc = K.tc[0]
            tc.alloc(es2, 4)
            tc.begin()
            load_w_bf16(P, Wo, I["attn_w_o"], D, "Wo")
            P.G(lambda e: e.memset(vx[:, :, :, 128:129], 1.0), r=[], w=["vx1"])
            for hh in range(4):
                P.dma("sync", kT[:, hh, :], K.kd[:, hh, :], r=[], w=["kT"], chan=f"kTl{hh}")
            for t in range(NT):
                P.dma("sync", vx[:, t, :, 0:128], K.vd[t * 128:(t + 1) * 128, :].rearrange("p (g d) -> p g d", g=4), r=[], w=["vx"], chan=f"vxl{t % 4}")
            P.dma("sync", mL[:], I["maskL"], r=[], w=["mL"], chan="mL")
            P.dma("sync", mR[:], I["maskR"], r=[], w=["mR"], chan="mR")
            load_bcast(P, "sync", sraw[:], I["attn_sink"], "sraw", "sraw")
            P.A(lambda e: e.activation(out=esink[:], in_=sraw[:], func=AF.Exp), r=["sraw"], w=["esink"])
            for e_t in range(NT):
                is_ctx = e_t < 2
                if e_t in (0, 2):
                    v = 1 if is_ctx else 0
                    load_bcast(P, "sync", gate[:], K.modd[0, v, 2 * D:3 * D], "gate", "gate")
                qs = e_t % 2
                P.dma("sync", qt[qs][:], K.qd[:, :, e_t * 128:(e_t + 1) * 128], r=["qd"], w=[f"qt{qs}"], chan=f"qt{qs}")
                src = I["ctx"][e_t * 128:(e_t + 1) * 128, :] if is_ctx else I["x"][(e_t - 2) * 128:(e_t - 1) * 128, :]
                P.dma("sync", xt[0][:], src, r=[], w=["xr0"], chan="xr0")
                if is_ctx:
                    kbs = [0, 1]
                else:
                    kbs = [0, 1] + [kb for kb in (e_t - 1, e_t, e_t + 1) if 2 <= kb < NT]
                for hh in range(4):
                    Es = E[hh % 2]
                    Ek = f"E{hh % 2}"
                    for n, kb in enumerate(kbs):
                        pb = n % 2
                        P.T(lambda e, hh=hh, kb=kb, pb=pb, qs=qs: e.matmul(ps[pb][:], lhsT=kT[:, hh, kb * 128:(kb + 1) * 128],
                                                                      rhs=qt[qs][:, 4 * hh:4 * hh + 4, :].rearrange("p g q -> p (g q)"), start=True, stop=True),
                            r=["kT", f"qt{qs}"], w=[f"ps{pb}"])
                        P.A(lambda e, n=n, pb=pb, Es=Es: e.activation(out=Es[:, n, :], in_=ps[pb][:], func=AF.Exp, scale=SCALE), r=[f"ps{pb}"], w=[f"{Ek}_{n}"])
                        if (not is_ctx) and kb >= 2 and kb != e_t:
                            m = mL if kb == e_t - 1 else mR
                            P.V(lambda e, n=n, m=m, Es=Es: e.tensor_tensor(out=Es[:, n, :].rearrange("p (g q) -> p g q", g=4), in0=Es[:, n, :].rearrange("p (g q) -> p g q", g=4),
                                                                        in1=m[:].unsqueeze(1).to_broadcast([128, 4, 128]), op=ALU.mult),
                                r=[f"{Ek}_{n}", "mL", "mR"], w=[f"{Ek}_{n}"])
                    for g in range(4):
                        pb = 2 + g // 2
                        for n, kb in enumerate(kbs):
                            P.T(lambda e, g=g, n=n, kb=kb, pb=pb, hh=hh, Es=Es: e.matmul(ps[pb][:, (g % 2) * 256:(g % 2) * 256 + 129], lhsT=Es[:, n, g * 128:(g + 1) * 128],
                                                                                     rhs=vx[:, kb, hh, :], start=(n == 0), stop=(n == len(kbs) - 1)),
                                r=[f"{Ek}_{n}", "vx", "vx1"], w=[f"ps{pb}"])
                    for bk in range(2):
                        pb = 2 + bk
                        P.V(lambda e, bk=bk, pb=pb, hh=hh: e.tensor_tensor(out=den[:, 2 * bk:2 * bk + 2], in0=ps[pb][:].rearrange("p (g c) -> p g c", g=2)[:, :, 128],
                                                                       in1=esink[:, 4 * hh + 2 * bk:4 * hh + 2 * bk + 2], op=ALU.add),
                            r=[f"ps{pb}", "esink"], w=["den"])
                    P.V(lambda e: e.reciprocal(out=den[:, 4:8], in_=den[:, 0:4]), r=["den"], w=["rden"])
                    for g in range(4):
                        pb = 2 + g // 2
                        hd = 4 * hh + g
                        P.V(lambda e, g=g, pb=pb, hd=hd: e.tensor_scalar(out=o[:, hd * 128:(hd + 1) * 128], in0=ps[pb][:, (g % 2) * 256:(g % 2) * 256 + 128],
                                                                     scalar1=den[:, 4 + g:5 + g], scalar2=None, op0=ALU.mult),
                            r=[f"ps{pb}", "rden"], w=["o"])
                for q in range(4):
                    b = 6 + q % 2
                    for j in range(4):
                        kc = 4 * q + j
                        P.T(lambda e, kc=kc, j=j, b=b: e.transpose(out=ps[b][:, j * 128:(j + 1) * 128], in_=o[:, kc * 128:(kc + 1) * 128], identity=K.ident[:]),
                            r=["o", "ident"], w=[f"ps{b}"])
                    P.A(lambda e, q=q, b=b: e.activation(out=oT[:, 4 * q:4 * q + 4, :], in_=ps[b][:].rearrange("p (j t) -> p j t", j=4), func=AF.Copy), r=[f"ps{b}"], w=["oT"])
                for nb in range(4):
                    pb = 4 + nb
                    for kc in range(KC):
                        P.T(lambda e, kc=kc, nb=nb, pb=pb: e.matmul(ps[pb][:], lhsT=oT[:, kc, :], rhs=Wo[:, kc, nb * 512:(nb + 1) * 512], start=(kc == 0), stop=(kc == KC - 1)),
                            r=["oT", "Wo"], w=[f"ps{pb}"])
                    P.V(lambda e, nb=nb, pb=pb: e.tensor_tensor(out=y[:, nb * 512:(nb + 1) * 512], in0=ps[pb][:], in1=gate[:, nb * 512:(nb + 1) * 512], op=ALU.mult),
                        r=[f"ps{pb}", "gate"], w=[f"y{nb}"])
                    P.V(lambda e, nb=nb, qs=qs: e.tensor_tensor(out=y[:, nb * 512:(nb + 1) * 512], in0=y[:, nb * 512:(nb + 1) * 512], in1=xt[0][:, nb * 512:(nb + 1) * 512], op=ALU.add),
                        r=[f"y{nb}", "xr0"], w=[f"y{nb}"])
                P.dma("sync", K.xs1[e_t * 128:(e_t + 1) * 128, :], y[:], r=[f"y{nb}" for nb in range(4)], w=["xs1"], chan="y")
                tc.pump(P, 5)
            tc.finish(P)
            P.emit()


def _lay(inputs):
    consts = _consts()
    f = lambda a: np.ascontiguousarray(np.asarray(a, dtype=np.float32))
    shared = dict(
        c_ctx=f(inputs["c_ctx"]), w_mod=f(inputs["w_mod"]), b_mod=f(inputs["b_mod"]),
        norm_mix=f(inputs["norm_mix"]), norm_ffn=f(inputs["norm_ffn"]), norm_final=f(inputs["norm_final"]),
        attn_w_qkv=f(inputs["attn_w_qkv"][0]), attn_w_o=f(inputs["attn_w_o"][0]), attn_sink=f(inputs["attn_sink"][0]),
        rec_w_in=f(inputs["rec_w_in"][0]), rec_conv_w=f(inputs["rec_conv_w"][0]), rec_conv_b=f(inputs["rec_conv_b"][0]),
        rec_w_a=f(inputs["rec_w_a"][0]), rec_b_a=f(inputs["rec_b_a"][0]), rec_w_x=f(inputs["rec_w_x"][0]), rec_b_x=f(inputs["rec_b_x"][0]),
        rec_lambda=f(inputs["rec_lambda"][0]), rec_w_out=f(inputs["rec_w_out"][0]),
        peer_w_q=f(inputs["peer_w_q"]), peer_keys=f(inputs["peer_keys"]), peer_u=f(inputs["peer_u"]), peer_v=f(inputs["peer_v"]),
        **consts,
    )
    return shared, f


def core_inputs(inputs, shared, f, core):
    b, hf = core // 2, core % 2
    m = dict(shared)
    m["x"] = f(inputs["x"][b]); m["c"] = f(inputs["c"][b]); m["ctx"] = f(inputs["ctx"][b])
    r0 = np.zeros((128, 17), np.uint32)
    r0[:, 0] = hf * 128 + np.arange(128)
    for j in range(16):
        r0[:, 1 + j] = 256 + (hf * 16 + j) * 128 + np.arange(128)
    m["rows0"] = r0
    m["myrows"] = (hf * (S // 2) + np.arange(16, dtype=np.uint32)[None, :] * 128 + np.arange(128, dtype=np.uint32)[:, None]).astype(np.uint32)
    return m


PAIR_SPLIT = False


def kernel(**inputs):
    nc = build(pair_split=PAIR_SPLIT)
    shared, f = _lay(inputs)
    in_maps = [core_inputs(inputs, shared, f, core) for core in range(8)]
    res = run_bass_kernel_spmd(nc, in_maps, core_ids=list(range(8)))
    outp = np.empty((4, S, D), np.float32)
    for core in range(8):
        b, hf = core // 2, core % 2
        outp[b, hf * (S // 2):(hf + 1) * (S // 2)] = res.results[core]["out"]
    return outp


class TabConv:
    NR = 6
    LAG = 3

    def set_nr(self, nr):
        self.NR, self.LAG = nr, max(1, nr // 2)

    def __init__(self, K, layer, tag):
        self.K, self.layer, self.tag = K, layer, tag
        self.steps = [(tb, r0) for tb in (0, 1) for r0 in range(layer * 16384, (layer + 1) * 16384, 128)]
        self.i = 0
        self.tiles = None

    def alloc(self, es, nr=6):
        self.set_nr(nr)
        if True:
            self.tiles = [es.enter_context(self.K.nc.sbuf_tensor(f"tc{self.tag}_{self.K.uid()}_{i}", [128, D], BF16)) for i in range(self.NR)]

    def _store(self, P, j):
        tb, r0 = self.steps[j]
        sl = j % self.NR
        dst = self.K.uvb16[r0:r0 + 128, tb * D:(tb + 1) * D]
        P.dma("sync", dst, self.tiles[sl][:], r=[f"tc{sl}"], w=[], chan=f"tcs{sl}")

    def pump(self, P, n):
        I = self.K.I
        for _ in range(n):
            if self.i >= len(self.steps):
                break
            j = self.i
            tb, r0 = self.steps[j]
            src = (I["peer_u"] if tb == 0 else I["peer_v"]).rearrange("l e d -> (l e) d")[r0:r0 + 128, :]
            sl = j % self.NR
            P.dma("gpsimd", self.tiles[sl][:], src, r=[], w=[f"tc{sl}"], chan=f"tcl{sl}")
            if j - self.LAG >= self.done:
                self._store(P, j - self.LAG)
            self.i += 1

    def begin(self):
        self.done = self.i

    def flush(self, P):
        for j in range(max(self.done, self.i - self.LAG), self.i):
            self._store(P, j)
        self.done = self.i

    def finish(self, P):
        self.pump(P, len(self.steps))
        self.flush(P)


def phase_peer(K, layer, tiles, final, name, idxname="myrows"):
    nc, I, ps = K.nc, K.I, K.ps
    NB = 7
    uvtab = K.uvb16
    with ExitStack() as es:
        sb = lambda n, s, d=F32: es.enter_context(nc.sbuf_tensor(name + n, s, d))
        Wq = sb("p_Wq", [128, KC, D], BF16)
        Gt = sb("p_G", [128, D]); St = sb("p_S", [128, D])
        gate = sb("p_gate", [128, D])
        xt1 = sb("p_xt", [128, D])
        xt = [xt1, xt1]
        h = [sb(f"p_h{i}", [128, D]) for i in range(2)]
        hT = sb("p_hT", [128, KC, 128], BF16)
        wk = [sb(f"p_wk{i}", [128, D]) for i in range(2)]
        ring = [sb(f"p_ring{i}", [128, 2 * D], BF16) for i in range(NB)]
        junk = sb("p_junk", [128, D], BF16)
        acc = wk[1]
        keysT = sb("p_keysT", [128, 2, 128])
        kraw = sb("p_kraw", [128, 2, 128])
        ss = sb("p_ss", [128, 4]); ssb = sb("p_ssb", [128, 4])
        s16 = sb("p_s16", [128, 16, 16]); i16 = sb("p_i16", [128, 16, 16], U32); i16f = sb("p_i16f", [128, 16, 16])
        ts = sb("p_ts", [128, 8, 16]); sel = sb("p_sel", [128, 8, 16], U32)
        au = sb("p_au", [128, 2, 128], U32); af = sb("p_af", [128, 2, 128])
        isel = sb("p_isel", [128, 2, 128])
        idxf = sb("p_idxf", [128, 128])
        idxu = [sb(f"p_idxu{i}", [128, 128], U32) for i in range(2)]
        gsm = [sb(f"p_g{i}", [128, 128]) for i in range(2)]
        sm = sb("p_sm", [128, 16])
        z = sb("p_z", [128, 128]); gz = sb("p_gz", [128, 128]); av = sb("p_av", [128, 128])
        identb = sb("p_identb", [128, 128], BF16)
        dg = [sb(f"p_dg{i}", [128, 128], BF16) for i in range(4)]
        iota = sb("p_iota", [128, 16])
        mr = sb("p_mr", [128, I[idxname].shape[1]], U32)
        P = Prog(nc, name)
        load_w_bf16(P, Wq, I["peer_w_q"][layer], D, "Wq")
        P.dma("sync", iota[:], I["iota16"], r=[], w=["iota"], chan="iota")
        P.A(lambda e: e.activation(out=identb[:], in_=K.ident[:], func=AF.Copy), r=["ident"], w=["identb"])
        P.dma("sync", mr[:], I[idxname], r=[], w=["mr"], chan="mr")
        for p in range(2):
            P.dma("sync", kraw[:, p, :], I["peer_keys"][layer, p], r=[], w=["kraw"], chan=f"kraw{p}")
            P.T(lambda e, p=p: e.transpose(out=ps[p][:, 0:128], in_=kraw[:, p, :], identity=K.ident[:]), r=["kraw", "ident"], w=[f"ps{p}"])
            P.V(lambda e, p=p: e.tensor_copy(out=keysT[:, p, :], in_=ps[p][:, 0:128]), r=[f"ps{p}"], w=["keysT"])

        P.emit()
        nc.all_engine_barrier()
        cur_v = [None]
        PP = [None]

        def front(n):
            P = PP[0]
            t = tiles[n]
            s = n % 2
            if cur_v[0] != t["v"]:
                cur_v[0] = t["v"]
                v = t["v"]
                prep_GS(K, P, Gt, St, I["norm_ffn"][layer], K.modd[layer, v, 4 * D:5 * D], K.modd[layer, v, 3 * D:4 * D], wk[0], "p")
            if t["src"][0] == "d":
                P.dma("sync", xt[s][:], t["src"][1], r=[], w=["xt"], chan="xt")
            else:
                col = t["src"][2]
                P.op("gpsimd", lambda e, s=s, col=col, src=t["src"][1]: e.indirect_dma_start(out=xt[s][:], out_offset=None, in_=src,
                                                                                         in_offset=bass.IndirectOffsetOnAxis(ap=mr[:, col:col + 1], axis=0)),
                     r=["mr"], w=["xt"], dma=True, chan="xt")
            hh = h[s]
            hk = f"h{s}"
            P.A(lambda e, s=s: e.activation(out=h[s][:], in_=xt[s][:], func=AF.Square, accum_out=ss[:, 0:1]), r=["xt"], w=[hk, "ss"])
            P.V(lambda e: e.tensor_scalar(out=ss[:, 1:2], in0=ss[:, 0:1], scalar1=1.0 / D, scalar2=EPS, op0=ALU.mult, op1=ALU.add), r=["ss"], w=["ss1"])
            P.A(lambda e: e.activation(out=ss[:, 2:3], in_=ss[:, 1:2], func=AF.Sqrt), r=["ss1"], w=["ss2"])
            P.V(lambda e: e.reciprocal(out=ss[:, 3:4], in_=ss[:, 2:3]), r=["ss2"], w=["ss3"])
            P.V(lambda e, s=s: e.scalar_tensor_tensor(out=h[s][:], in0=xt[s][:], scalar=ss[:, 3:4], in1=Gt[:], op0=ALU.mult, op1=ALU.mult), r=["xt", "ss3", "Gp"], w=[hk])
            P.V(lambda e, s=s: e.tensor_tensor(out=h[s][:], in0=h[s][:], in1=St[:], op=ALU.add), r=[hk, "Sp"], w=[hk])
            for q in range(4):
                b = q % 2
                for j in range(4):
                    kc = 4 * q + j
                    P.T(lambda e, kc=kc, j=j, b=b, s=s: e.transpose(out=ps[b][:, j * 128:(j + 1) * 128], in_=h[s][:, kc * 128:(kc + 1) * 128], identity=K.ident[:]),
                        r=[hk, "ident"], w=[f"ps{b}"])
                P.A(lambda e, q=q, b=b: e.activation(out=hT[:, 4 * q:4 * q + 4, :], in_=ps[b][:].rearrange("p (j t) -> p j t", j=4), func=AF.Copy), r=[f"ps{b}"], w=["hT"])
            yield
            qT = wk[0]
            for c in range(16):
                pb = c % 2
                for kc in range(KC):
                    P.T(lambda e, kc=kc, c=c, pb=pb: e.matmul(ps[pb][:, 0:128], lhsT=Wq[:, kc, c * 128:(c + 1) * 128], rhs=hT[:, kc, :], start=(kc == 0), stop=(kc == KC - 1)),
                        r=["Wq", "hT"], w=[f"ps{pb}"])
                P.A(lambda e, c=c, pb=pb: e.activation(out=qT[:, c * 128:(c + 1) * 128], in_=ps[pb][:, 0:128], func=AF.Copy), r=[f"ps{pb}"], w=["wk0"])
                yield
            Ssb = wk[1]
            for q in range(4):
                pb = 2 + q % 2
                for c in range(4 * q, 4 * q + 4):
                    P.T(lambda e, c=c, pb=pb: e.matmul(ps[pb][:, (c % 4) * 128:(c % 4 + 1) * 128], lhsT=qT[:, c * 128:(c + 1) * 128], rhs=keysT[:, c % 2, :], start=True, stop=True),
                        r=["wk0", "keysT"], w=[f"ps{pb}"])
                P.A(lambda e, q=q, pb=pb: e.activation(out=Ssb[:, q * 512:(q + 1) * 512], in_=ps[pb][:], func=AF.Copy), r=[f"ps{pb}"], w=["wk1"])
            S2 = wk[0]
            for c in range(16):
                sv = Ssb[:, c * 128:(c + 1) * 128]
                s2v = S2[:, c * 128:(c + 1) * 128]
                P.V(lambda e, c=c, sv=sv: e.max(out=s16[:, c, 0:8], in_=sv), r=["wk1"], w=["s16"])
                P.V(lambda e, c=c, sv=sv: e.max_index(out=i16[:, c, 0:8], in_max=s16[:, c, 0:8], in_values=sv), r=["wk1", "s16"], w=["i16"])
                P.V(lambda e, c=c, sv=sv, s2v=s2v: e.match_replace(out=s2v, in_to_replace=s16[:, c, 0:8], in_values=sv, imm_value=-1e30), r=["wk1", "s16"], w=["wk0"])
                P.V(lambda e, c=c, s2v=s2v: e.max(out=s16[:, c, 8:16], in_=s2v), r=["wk0"], w=["s16"])
                P.V(lambda e, c=c, s2v=s2v: e.max_index(out=i16[:, c, 8:16], in_max=s16[:, c, 8:16], in_values=s2v), r=["wk0", "s16"], w=["i16"])
                yield
            cand = wk[1]
            c4 = cand[:].rearrange("p (h a b) -> p h a b", h=8, a=16)
            s16r = s16[:].rearrange("p (h t) k -> p h t k", t=2)
            P.V(lambda e: e.tensor_tensor(out=c4, in0=s16r[:, :, 0, :].unsqueeze(3).to_broadcast([128, 8, 16, 16]),
                                          in1=s16r[:, :, 1, :].unsqueeze(2).to_broadcast([128, 8, 16, 16]), op=ALU.add), r=["s16"], w=["wk1"])
            for hd in range(8):
                cv = cand[:, hd * 256:(hd + 1) * 256]
                c2v = S2[:, hd * 256:(hd + 1) * 256]
                P.V(lambda e, hd=hd, cv=cv: e.max(out=ts[:, hd, 0:8], in_=cv), r=["wk1"], w=["ts"])
                P.V(lambda e, hd=hd, cv=cv: e.max_index(out=sel[:, hd, 0:8], in_max=ts[:, hd, 0:8], in_values=cv), r=["wk1", "ts"], w=["sel"])
                P.V(lambda e, hd=hd, cv=cv, c2v=c2v: e.match_replace(out=c2v, in_to_replace=ts[:, hd, 0:8], in_values=cv, imm_value=-1e30), r=["wk1", "ts"], w=["wk0"])
                P.V(lambda e, hd=hd, c2v=c2v: e.max(out=ts[:, hd, 8:16], in_=c2v), r=["wk0"], w=["ts"])
                P.V(lambda e, hd=hd, c2v=c2v: e.max_index(out=sel[:, hd, 8:16], in_max=ts[:, hd, 8:16], in_values=c2v), r=["wk0", "ts"], w=["sel"])
                yield
            selv = sel[:].rearrange("p h k -> p (h k)")
            P.V(lambda e: e.tensor_scalar(out=au[:, 0, :], in0=selv, scalar1=4, scalar2=None, op0=ALU.logical_shift_right), r=["sel"], w=["au"])
            P.V(lambda e: e.tensor_scalar(out=au[:, 1, :], in0=selv, scalar1=15, scalar2=None, op0=ALU.bitwise_and), r=["sel"], w=["au"])
            P.V(lambda e: e.tensor_copy(out=af[:], in_=au[:]), r=["au"], w=["af"])
            P.V(lambda e: e.tensor_copy(out=i16f[:], in_=i16[:]), r=["i16"], w=["i16f"])
            eq = wk[1][:].rearrange("p (h s a) -> p h s a", h=8, s=16)
            i16r = i16f[:].rearrange("p (h t) k -> p h t k", t=2)
            for half in range(2):
                P.V(lambda e, half=half: e.tensor_tensor(out=eq, in0=af[:, half, :].rearrange("p (h s) -> p h s", h=8).unsqueeze(3).to_broadcast([128, 8, 16, 16]),
                                                         in1=iota[:].unsqueeze(1).unsqueeze(1).to_broadcast([128, 8, 16, 16]), op=ALU.is_equal), r=["af", "iota"], w=["wk1"])
                P.V(lambda e, half=half: e.tensor_tensor(out=eq, in0=eq, in1=i16r[:, :, half, :].unsqueeze(2).to_broadcast([128, 8, 16, 16]), op=ALU.mult), r=["wk1", "i16f"], w=["wk1"])
                P.V(lambda e, half=half: e.tensor_reduce(out=isel[:, half, :].rearrange("p (h s) -> p h s", h=8), in_=eq, axis=AX.X, op=ALU.add), r=["wk1"], w=["isel"])
                yield
            P.V(lambda e: e.scalar_tensor_tensor(out=idxf[:], in0=isel[:, 0, :], scalar=128.0, in1=isel[:, 1, :], op0=ALU.mult, op1=ALU.add), r=["isel"], w=["idxf"])
            if layer > 0:
                P.V(lambda e: e.tensor_scalar(out=idxf[:], in0=idxf[:], scalar1=float(layer * 16384), scalar2=None, op0=ALU.add), r=["idxf"], w=["idxf"])
            P.V(lambda e, s=s: e.tensor_copy(out=idxu[s][:], in_=idxf[:]), r=["idxf"], w=[f"idxu{s}"])
            g3 = gsm[s][:].rearrange("p (h k) -> p h k", h=8)
            P.V(lambda e, g3=g3: e.tensor_tensor(out=g3, in0=ts[:], in1=ts[:, :, 0:1].to_broadcast([128, 8, 16]), op=ALU.subtract), r=["ts"], w=[f"g{s}"])
            P.A(lambda e, s=s: e.activation(out=gsm[s][:], in_=gsm[s][:], func=AF.Exp), r=[f"g{s}"], w=[f"g{s}"])
            P.V(lambda e, g3=g3: e.tensor_reduce(out=sm[:, 0:8], in_=g3, axis=AX.X, op=ALU.add), r=[f"g{s}"], w=["sm"])
            P.V(lambda e: e.reciprocal(out=sm[:, 8:16], in_=sm[:, 0:8]), r=["sm"], w=["sm"])
            P.V(lambda e, g3=g3: e.tensor_tensor(out=g3, in0=g3, in1=sm[:, 8:16].unsqueeze(2).to_broadcast([128, 8, 16]), op=ALU.mult), r=[f"g{s}", "sm"], w=[f"g{s}"])

        gcount = [0]
        gate_v = [None]

        def gather(s, k):
            P = PP[0]
            slot = gcount[0] % NB
            gcount[0] += 1
            P.op("gpsimd", lambda e, slot=slot, s=s, k=k: e.indirect_dma_start(out=ring[slot][:], out_offset=None, in_=uvtab,
                                                                        in_offset=bass.IndirectOffsetOnAxis(ap=idxu[s][:, k:k + 1], axis=0)),
                 r=[f"idxu{s}"], w=[f"ring{slot}"], dma=True, chan=f"ring{slot}")
            return slot

        def back(n, nxt=None):
            P = PP[0]
            t = tiles[n]
            s = n % 2
            POOL_EVERY = 0
            LAGP = 2

            def consume(k, slot, via_pool):
                if via_pool:
                    P.G(lambda e, slot=slot, s=s: e.tensor_tensor(out=ring[slot][:, 0:D], in0=ring[slot][:, 0:D], in1=h[s][:], op=ALU.mult), r=[f"ring{slot}", f"h{s}"], w=[f"ring{slot}"])
                    P.A(lambda e, slot=slot, k=k: e.activation(out=ring[slot][:, 0:D], in_=ring[slot][:, 0:D], func=AF.Copy, accum_out=z[:, k:k + 1]), r=[f"ring{slot}"], w=[f"ring{slot}", f"z{k}"])
                else:
                    P.V(lambda e, slot=slot, s=s, k=k: e.scalar_tensor_tensor(out=junk[:], in0=ring[slot][:, 0:D], scalar=1.0, in1=h[s][:], op0=ALU.mult, op1=ALU.mult, accum_out=z[:, k:k + 1]),
                        r=[f"ring{slot}", f"h{s}"], w=[f"z{k}"])
                P.A(lambda e, k=k: e.activation(out=gz[:, k:k + 1], in_=z[:, k:k + 1], func=AF.Gelu_apprx_tanh), r=[f"z{k}"], w=[f"gz{k}"])
                P.A(lambda e, k=k, s=s: e.activation(out=av[:, k:k + 1], in_=gz[:, k:k + 1], func=AF.Copy, scale=gsm[s][:, k:k + 1]), r=[f"gz{k}", f"g{s}"], w=[f"a{k}"])
                P.A(lambda e, k=k: e.activation(out=dg[k % 4][:], in_=identb[:], func=AF.Copy, scale=av[:, k:k + 1]), r=[f"a{k}", "identb"], w=[f"dg{k % 4}"])
                first = not started[0]
                started[0] = True
                last = ndone[0] == 127
                ndone[0] += 1
                for nb in range(4):
                    P.T(lambda e, k=k, nb=nb, slot=slot, first=first, last=last: e.matmul(ps[4 + nb][:], lhsT=dg[k % 4][:], rhs=ring[slot][:, D + nb * 512:D + (nb + 1) * 512], start=first, stop=last),
                        r=[f"dg{k % 4}", f"ring{slot}"], w=[f"ps{4 + nb}"])

            started = [False]
            ndone = [0]
            pend = []
            for k in range(128):
                slot = gather(s, k)
                if POOL_EVERY and k % POOL_EVERY == POOL_EVERY - 1:
                    pend.append((k, slot))
                else:
                    consume(k, slot, False)
                while pend and pend[0][0] <= k - LAGP:
                    kk, sl_ = pend.pop(0)
                    consume(kk, sl_, True)
                if nxt is not None and k >= 8:
                    next(nxt, None)
            for kk, sl_ in pend:
                consume(kk, sl_, True)
            if nxt is not None:
                for _ in nxt:
                    pass
            if gate_v[0] != t["v"]:
                gate_v[0] = t["v"]
                load_bcast(P, "sync", gate[:], K.modd[layer, t["v"], 5 * D:6 * D], "gate", "gate")
            xr = wk[0]
            if t["src"][0] == "d":
                P.dma("sync", xr[:], t["src"][1], r=[], w=["wk0"], chan="xre")
            else:
                col = t["src"][2]
                P.op("gpsimd", lambda e, col=col, src=t["src"][1]: e.indirect_dma_start(out=xr[:], out_offset=None, in_=src,
                                                                                 in_offset=bass.IndirectOffsetOnAxis(ap=mr[:, col:col + 1], axis=0)),
                     r=["mr"], w=["wk0"], dma=True, chan="xre")
            for nb in range(4):
                P.V(lambda e, nb=nb: e.tensor_tensor(out=acc[:, nb * 512:(nb + 1) * 512], in0=ps[4 + nb][:], in1=gate[:, nb * 512:(nb + 1) * 512], op=ALU.mult), r=[f"ps{4 + nb}", "gate"], w=["wk1"])
            P.V(lambda e: e.tensor_tensor(out=acc[:], in0=acc[:], in1=xr[:], op=ALU.add), r=["wk1", "wk0"], w=["wk1"])
            if final:
                P.A(lambda e: e.activation(out=junk[:], in_=acc[:], func=AF.Square, accum_out=ssb[:, 0:1]), r=["wk1"], w=["junkf", "ssb"])
                P.V(lambda e: e.tensor_scalar(out=ssb[:, 1:2], in0=ssb[:, 0:1], scalar1=1.0 / D, scalar2=EPS, op0=ALU.mult, op1=ALU.add), r=["ssb"], w=["ssb1"])
                P.A(lambda e: e.activation(out=ssb[:, 2:3], in_=ssb[:, 1:2], func=AF.Sqrt), r=["ssb1"], w=["ssb2"])
                P.V(lambda e: e.reciprocal(out=ssb[:, 3:4], in_=ssb[:, 2:3]), r=["ssb2"], w=["ssb3"])
                load_bcast(P, "sync", xr[:], I["norm_final"], "wk0", "gfin")
                P.V(lambda e: e.scalar_tensor_tensor(out=acc[:], in0=acc[:], scalar=ssb[:, 3:4], in1=xr[:], op0=ALU.mult, op1=ALU.mult), r=["wk1", "ssb3", "wk0"], w=["wk1"])
            P.dma("sync", t["dst"], acc[:], r=["wk1"], w=[], chan="st")

        ngrp = -(-len(tiles) // 12)
        GSZ = -(-len(tiles) // ngrp)
        for g0 in range(0, len(tiles), GSZ):
            PP[0] = Prog(nc, f"{name}g{g0}")
            g1 = min(g0 + GSZ, len(tiles))
            for _ in front(g0):
                pass
            for n in range(g0, g1):
                back(n, front(n + 1) if n + 1 < g1 else None)
            PP[0].emit()
            K.peer_stats = PP[0].stats
            nc.all_engine_barrier()


def phase_rec(K):
    nc, I, ps = K.nc, K.I, K.ps
    NTOK = NT * 128
    for pas in ("u", "g"):
        with ExitStack() as es:
            sb = lambda n, s, d=F32: es.enter_context(nc.sbuf_tensor(n, s, d))
            W = sb("r_W" + pas, [128, KC, D], BF16)
            Gt = sb("r_G" + pas, [128, D]); St = sb("r_S" + pas, [128, D])
            ss = sb("r_ss" + pas, [128, 4])
            xt = sb("r_xt" + pas, [128, D]); h = sb("r_h" + pas, [128, D])
            hT = sb("r_hT" + pas, [128, KC, 256], BF16)
            ost = sb("r_ost" + pas, [128, 16, 256])
            P = Prog(nc, "rp" + pas)
            tc = K.tc[1]
            tc.alloc(es, 12)
            tc.begin()
            load_w_bf16(P, W, I["rec_w_in"], D, "W", col0=(D if pas == "u" else 0))
            for blk in (range(NT // 2) if pas == "u" else range(1, NT // 2)):
                is_ctx = blk == 0
                if blk in (0, 1):
                    v = 1 if is_ctx else 0
                    prep_GS(K, P, Gt, St, I["norm_mix"][1], K.modd[1, v, D:2 * D], K.modd[1, v, 0:D], h, "a")
                for ti in range(2):
                    e_t = blk * 2 + ti
                    norm_mod_T(K, P, K.xs2tile(e_t), xt, h, ss, Gt, St, "a", hT, "hT", ti * 128, (6, 7), 0)
                for j in range(16):
                    pb = j % 2
                    for kc in range(KC):
                        P.T(lambda e, kc=kc, j=j, pb=pb: e.matmul(ps[pb][:, 0:256], lhsT=W[:, kc, j * 128:(j + 1) * 128], rhs=hT[:, kc, :], start=(kc == 0), stop=(kc == KC - 1)),
                            r=["W", "hT"], w=[f"ps{pb}"])
                    fn = AF.Copy if pas == "u" else AF.Gelu_apprx_tanh
                    P.A(lambda e, j=j, pb=pb, fn=fn: e.activation(out=ost[:, j, :], in_=ps[pb][:, 0:256], func=fn), r=[f"ps{pb}"], w=["ost"])
                if pas == "u":
                    P.dma("sync", K.upre[:, :, blk * 256:(blk + 1) * 256].rearrange("c p t -> p c t"), ost[:], r=["ost"], w=["upre"], chan="ost")
                else:
                    l0 = (blk - 1) * 256
                    P.dma("sync", K.ggd[:, :, l0:l0 + 256].rearrange("c p t -> p c t"), ost[:], r=["ost"], w=["ggd"], chan="ost")
                tc.pump(P, 8)
            if pas == "g":
                tc.finish(P)
            else:
                tc.flush(P)
            P.emit()
        nc.all_engine_barrier()
    with ExitStack() as es:
        sb = lambda n, s, d=F32: es.enter_context(nc.sbuf_tensor(n, s, d))
        UPW = 4360
        up = sb("r_up", [128, 2, UPW])
        u = sb("r_u", [128, 2, NTOK]); ub = sb("r_ub", [128, 2, NTOK], BF16)
        A = sb("r_A", [128, NTOK]); Bt = sb("r_B", [128, NTOK])
        Y = [sb(f"r_Y{d}", [128, NTOK]) for d in range(2)]
        gg = sb("r_gg", [128, S]); ygb = sb("r_ygb", [128, S], BF16)
        wg = sb("r_wg", [128, 2, 2, 2, 256], BF16)
        cw = sb("r_cw", [128, 4, 16]); cb = sb("r_cb", [128, 16])
        ba = sb("r_ba", [128, 2, 16]); bx = sb("r_bx", [128, 2, 16]); lam = sb("r_lam", [128, 2, 16]); cl = sb("r_cl", [128, 2, 16])
        tmp = [[sb(f"r_t{i}{s}", [128, 512]) for i in range(4)] for s in range(2)]
        nba = sb("r_nba", [128, 2, 16]); nbx = sb("r_nbx", [128, 2, 16])
        P = Prog(nc, "rscan")
        P.G(lambda e: e.memset(up[:], 0.0), r=[], w=["up0", "up1"])
        for k in range(4):
            P.dma("sync", cw[:, k, :], I["rec_conv_w"][k].rearrange("(c p) -> p c", p=128), r=[], w=["cw"], chan=f"cw{k}", allow_slow_non_contiguous=True)
        P.dma("sync", cb[:], I["rec_conv_b"].rearrange("(c p) -> p c", p=128), r=[], w=["cb"], chan="cb", allow_slow_non_contiguous=True)
        for d in range(2):
            P.dma("sync", ba[:, d, :], I["rec_b_a"][d].rearrange("(c p) -> p c", p=128), r=[], w=["ba"], chan=f"ba{d}", allow_slow_non_contiguous=True)
            P.dma("sync", bx[:, d, :], I["rec_b_x"][d].rearrange("(c p) -> p c", p=128), r=[], w=["bx"], chan=f"bx{d}", allow_slow_non_contiguous=True)
            P.dma("sync", lam[:, d, :], I["rec_lambda"][d].rearrange("(c p) -> p c", p=128), r=[], w=["lam"], chan=f"lam{d}", allow_slow_non_contiguous=True)
        P.A(lambda e: e.activation(out=cl[:], in_=lam[:], func=AF.Exp, scale=-1.0), r=["lam"], w=["cl"])
        P.A(lambda e: e.activation(out=cl[:], in_=cl[:], func=AF.Ln, bias=1.0), r=["cl"], w=["cl"])
        P.V(lambda e: e.tensor_scalar(out=cl[:], in0=cl[:], scalar1=-8.0, scalar2=None, op0=ALU.mult), r=["cl"], w=["cl"])
        P.V(lambda e: e.tensor_scalar(out=nba[:], in0=ba[:], scalar1=-1.0, scalar2=None, op0=ALU.mult), r=["ba"], w=["nba"])
        P.V(lambda e: e.tensor_scalar(out=nbx[:], in0=bx[:], scalar1=-1.0, scalar2=None, op0=ALU.mult), r=["bx"], w=["nbx"])
        for n in range(8):
            for c in range(2):
                ch = 2 * n + c
                P.dma("sync", up[:, c, 1:257], K.upre[ch, :, 0:256], r=[], w=[f"up{c}"], chan=f"upc{c}")
                P.dma("sync", up[:, c, 260:260 + S], K.upre[ch, :, 256:NTOK], r=[], w=[f"up{c}"], chan=f"upl{c}")
                for (o0, nn, i0) in ((0, 256, 0), (256, S, 259)):
                    useg = u[:, c, o0:o0 + nn]
                    P.V(lambda e, c=c, ch=ch, useg=useg, i0=i0, nn=nn: e.tensor_scalar(out=useg, in0=up[:, c, i0:i0 + nn], scalar1=cw[:, 0, ch:ch + 1], scalar2=cb[:, ch:ch + 1], op0=ALU.mult, op1=ALU.add),
                        r=[f"up{c}", "cw", "cb"], w=[f"u{c}"])
                    for k in range(1, 4):
                        P.V(lambda e, c=c, ch=ch, useg=useg, i0=i0, nn=nn, k=k: e.scalar_tensor_tensor(out=useg, in0=up[:, c, i0 + k:i0 + k + nn], scalar=cw[:, k, ch:ch + 1], in1=useg, op0=ALU.mult, op1=ALU.add),
                            r=[f"up{c}", "cw", f"u{c}"], w=[f"u{c}"])
                P.A(lambda e, c=c: e.activation(out=ub[:, c, :], in_=u[:, c, :], func=AF.Copy), r=[f"u{c}"], w=["ub"])
            for d in range(2):
                for ty in range(2):
                    wsrc = (I["rec_w_a"] if ty == 0 else I["rec_w_x"])[d, n].rearrange("(k p) o -> p k o", p=128)
                    P.dma("gpsimd", wg[:, d, ty, :, :], wsrc, r=[], w=["wg"], chan=f"wg{d}{ty}")
            for oc in range(2):
                ch = 2 * n + oc
                for d in range(2):
                    for tb in range(9):
                        t0 = tb * 512
                        tn = min(512, NTOK - t0)
                        sl = tb % 2
                        pa, px = 2 * sl, 2 * sl + 1
                        r_, i_, a2, sq = tmp[sl]
                        for ty, pb in ((0, pa), (1, px)):
                            for kc in range(2):
                                P.T(lambda e, ty=ty, pb=pb, kc=kc, d=d, oc=oc, t0=t0, tn=tn: e.matmul(ps[pb][:, 0:tn], lhsT=wg[:, d, ty, kc, oc * 128:(oc + 1) * 128], rhs=ub[:, kc, t0:t0 + tn], start=(kc == 0), stop=(kc == 1)),
                                    r=["wg", "ub"], w=[f"ps{pb}"])
                        P.A(lambda e, pa=pa, tn=tn, d=d, ch=ch, r_=r_: e.activation(out=r_[:, 0:tn], in_=ps[pa][:, 0:tn], func=AF.Exp, scale=-1.0, bias=nba[:, d, ch:ch + 1]), r=[f"ps{pa}", "nba"], w=[f"t0{sl}"])
                        P.A(lambda e, px=px, tn=tn, d=d, ch=ch, i_=i_: e.activation(out=i_[:, 0:tn], in_=ps[px][:, 0:tn], func=AF.Exp, scale=-1.0, bias=nbx[:, d, ch:ch + 1]), r=[f"ps{px}", "nbx"], w=[f"t1{sl}"])
                        P.A(lambda e, tn=tn, r_=r_: e.activation(out=r_[:, 0:tn], in_=r_[:, 0:tn], func=AF.Ln, bias=1.0), r=[f"t0{sl}"], w=[f"t0{sl}"])
                        P.A(lambda e, tn=tn, r_=r_: e.activation(out=r_[:, 0:tn], in_=r_[:, 0:tn], func=AF.Exp, scale=-1.0), r=[f"t0{sl}"], w=[f"t0{sl}"])
                        P.A(lambda e, tn=tn, i_=i_: e.activation(out=i_[:, 0:tn], in_=i_[:, 0:tn], func=AF.Ln, bias=1.0), r=[f"t1{sl}"], w=[f"t1{sl}"])
                        P.A(lambda e, tn=tn, i_=i_: e.activation(out=i_[:, 0:tn], in_=i_[:, 0:tn], func=AF.Exp, scale=-1.0), r=[f"t1{sl}"], w=[f"t1{sl}"])
                        P.A(lambda e, tn=tn, t0=t0, d=d, ch=ch, r_=r_: e.activation(out=A[:, t0:t0 + tn], in_=r_[:, 0:tn], func=AF.Exp, scale=cl[:, d, ch:ch + 1]), r=[f"t0{sl}", "cl"], w=["A"])
                        P.G(lambda e, tn=tn, t0=t0, a2=a2: e.tensor_tensor(out=a2[:, 0:tn], in0=A[:, t0:t0 + tn], in1=A[:, t0:t0 + tn], op=ALU.mult), r=["A"], w=[f"t2{sl}"])
                        P.A(lambda e, tn=tn, a2=a2, sq=sq: e.activation(out=sq[:, 0:tn], in_=a2[:, 0:tn], func=AF.Ln, scale=-1.0, bias=1.0), r=[f"t2{sl}"], w=[f"t3{sl}"])
                        P.A(lambda e, tn=tn, sq=sq: e.activation(out=sq[:, 0:tn], in_=sq[:, 0:tn], func=AF.Exp, scale=0.5), r=[f"t3{sl}"], w=[f"t3{sl}"])
                        P.V(lambda e, tn=tn, sq=sq, i_=i_: e.tensor_tensor(out=sq[:, 0:tn], in0=sq[:, 0:tn], in1=i_[:, 0:tn], op=ALU.mult), r=[f"t3{sl}", f"t1{sl}"], w=[f"t3{sl}"])
                        P.V(lambda e, tn=tn, t0=t0, sq=sq, oc=oc: e.tensor_tensor(out=Bt[:, t0:t0 + tn], in0=sq[:, 0:tn], in1=u[:, oc, t0:t0 + tn], op=ALU.mult), r=[f"t3{sl}", f"u{oc}"], w=["B"])
                    Yd = Y[d]
                    if d == 0:
                        P.V(lambda e, Yd=Yd: e.tensor_tensor_scan(out=Yd[:, 0:256], data0=A[:, 0:256], data1=Bt[:, 0:256], initial=0.0, op0=ALU.mult, op1=ALU.add), r=["A", "B"], w=[f"Y{d}c"])
                        P.V(lambda e, Yd=Yd: e.tensor_tensor_scan(out=Yd[:, 256:NTOK], data0=A[:, 256:NTOK], data1=Bt[:, 256:NTOK], initial=Yd[:, 255:256], op0=ALU.mult, op1=ALU.add), r=["A", "B", f"Y{d}c"], w=[f"Y{d}l"])
                    else:
                        P.V(lambda e, Yd=Yd: e.tensor_tensor_scan(out=Yd[:, 0:256][:, ::-1], data0=A[:, 0:256][:, ::-1], data1=Bt[:, 0:256][:, ::-1], initial=0.0, op0=ALU.mult, op1=ALU.add), r=["A", "B"], w=[f"Y{d}c"])
                        P.V(lambda e, Yd=Yd: e.tensor_tensor_scan(out=Yd[:, 256:NTOK][:, ::-1], data0=A[:, 256:NTOK][:, ::-1], data1=Bt[:, 256:NTOK][:, ::-1], initial=Yd[:, 0:1], op0=ALU.mult, op1=ALU.add), r=["A", "B", f"Y{d}c"], w=[f"Y{d}l"])
                P.dma("sync", gg[:], K.ggd[ch], r=[], w=["gg"], chan="gg")
                P.G(lambda e: e.tensor_tensor(out=Y[0][:, 256:NTOK], in0=Y[0][:, 256:NTOK], in1=Y[1][:, 256:NTOK], op=ALU.add), r=["Y0l", "Y1l"], w=["Y0l"])
                P.V(lambda e: e.tensor_tensor(out=ygb[:], in0=Y[0][:, 256:NTOK], in1=gg[:], op=ALU.mult), r=["Y0l", "gg"], w=["ygb"])
                P.dma("sync", K.ygd[:, ch, :], ygb[:], r=["ygb"], w=["ygd"], chan="ygb")
        P.emit()
    nc.all_engine_barrier()
    with ExitStack() as es:
        sb = lambda n, s, d=F32: es.enter_context(nc.sbuf_tensor(n, s, d))
        Wout = sb("r_Wout", [128, KC, D], BF16)
        gate = sb("r_gate", [128, D])
        yt = [sb(f"r_yt{i}", [128, KC, 128], BF16) for i in range(2)]
        xr = [sb(f"r_xr{i}", [128, D]) for i in range(2)]
        y = sb("r_y", [128, D])
        P = Prog(nc, "rout")
        load_w_bf16(P, Wout, I["rec_w_out"], D, "Wo")
        load_bcast(P, "sync", gate[:], K.modd[1, 0, 2 * D:3 * D], "gate", "gate")
        for t in range(S // 128):
            s = t % 2
            P.dma("sync", yt[s][:], K.ygd[:, :, t * 128:(t + 1) * 128], r=[], w=[f"yt{s}"], chan=f"yt{s}")
            P.dma("sync", xr[s][:], K.xs2tile(t + 2), r=[], w=[f"xr{s}"], chan=f"xr{s}")
            for nb in range(4):
                pb = 4 + nb
                for kc in range(KC):
                    P.T(lambda e, kc=kc, nb=nb, pb=pb, s=s: e.matmul(ps[pb][:], lhsT=yt[s][:, kc, :], rhs=Wout[:, kc, nb * 512:(nb + 1) * 512], start=(kc == 0), stop=(kc == KC - 1)),
                        r=[f"yt{s}", "Wo"], w=[f"ps{pb}"])
                P.V(lambda e, nb=nb, pb=pb: e.tensor_tensor(out=y[:, nb * 512:(nb + 1) * 512], in0=ps[pb][:], in1=gate[:, nb * 512:(nb + 1) * 512], op=ALU.mult), r=[f"ps{pb}", "gate"], w=[f"y{nb}"])
                P.G(lambda e, nb=nb, s=s: e.tensor_tensor(out=y[:, nb * 512:(nb + 1) * 512], in0=y[:, nb * 512:(nb + 1) * 512], in1=xr[s][:, nb * 512:(nb + 1) * 512], op=ALU.add), r=[f"y{nb}", f"xr{s}"], w=[f"y{nb}"])
            P.dma("sync", K.xs3[t * 128:(t + 1) * 128, :], y[:], r=[f"y{nb}" for nb in range(4)], w=["xs3"], chan="y")
        P.emit()
```

```python
import os
import numpy as np
import ml_dtypes
from contextlib import ExitStack
import concourse.bass as bass
import concourse.mybir as mybir
from concourse.bass_utils import run_bass_kernel_spmd

F32 = mybir.dt.float32
BF16 = mybir.dt.bfloat16
U32 = mybir.dt.uint32
ALU = mybir.AluOpType
AF = mybir.ActivationFunctionType
AX = mybir.AxisListType

D = 2048
KC = 16
S = 4096
C = 256
NT = (S + C) // 128
EPS = 1e-6
COMPUTE = ("tensor", "vector", "scalar", "gpsimd")


class Prog:
    def __init__(self, nc, name="p"):
        self.nc = nc
        self.name = name
        self.ops = []
        self.last_w = {}
        self.readers = {}

    def op(self, eng, fn, r=(), w=(), dma=False, chan=None):
        i = len(self.ops)
        deps = set()
        for k in r:
            lw = self.last_w.get(k)
            if lw is not None:
                deps.add(lw)
        for k in w:
            lw = self.last_w.get(k)
            if lw is not None:
                deps.add(lw)
            rd = self.readers.get(k)
            if rd:
                deps.update(rd.values())
        for k in w:
            self.last_w[k] = i
            self.readers[k] = {}
        for k in r:
            d = self.readers.setdefault(k, {})
            d[("dma", i) if dma else eng] = i
        if dma:
            assert chan is not None
        self.ops.append(dict(eng=eng, fn=fn, deps=deps, dma=dma, chan=chan))
        return i

    def dma(self, eng, out, in_, r, w, chan, **kw):
        return self.op(eng, lambda e: e.dma_start(out=out, in_=in_, **kw), r=r, w=w, dma=True, chan=chan)

    def V(self, fn, r, w):
        return self.op("vector", fn, r, w)

    def A(self, fn, r, w):
        return self.op("scalar", fn, r, w)

    def G(self, fn, r, w):
        return self.op("gpsimd", fn, r, w)

    def T(self, fn, r, w):
        return self.op("tensor", fn, r, w)

    def emit(self):
        nc = self.nc
        ops = self.ops
        needed = set()
        for o in ops:
            for d in o["deps"]:
                od = ops[d]
                if od["dma"]:
                    continue
                if od["eng"] == "tensor" and o["eng"] == "tensor" and not o["dma"]:
                    continue
                needed.add(d)
        cnt = {e: 0 for e in COMPUTE + ("sync",)}
        chan_cnt = {}
        for i, o in enumerate(ops):
            if o["dma"]:
                c = o["chan"]
                chan_cnt[c] = chan_cnt.get(c, 0) + 16
                o["sig"] = ("c", c, chan_cnt[c])
            elif i in needed:
                cnt[o["eng"]] += 1
                o["sig"] = ("e", o["eng"], cnt[o["eng"]])
            else:
                o["sig"] = None
        self.stats = dict(cnt=dict(cnt), chans=len(chan_cnt), maxchan=max(chan_cnt.values()) if chan_cnt else 0, nops=len(ops))
        esem = {e: nc.alloc_semaphore(name=f"{self.name}_e_{e}") for e in COMPUTE if cnt[e] > 0}
        csem = {c: nc.alloc_semaphore(name=f"{self.name}_c_{c}") for c in chan_cnt}
        with ExitStack() as es:
            block = es.enter_context(nc.Block())
            by_eng = {}
            for i, o in enumerate(ops):
                by_eng.setdefault(o["eng"], []).append(i)

            def make(engname, idxs):
                def body(eng):
                    waited = {}
                    for i in idxs:
                        o = ops[i]
                        for d in sorted(o["deps"]):
                            od = ops[d]
                            sig = od["sig"]
                            if sig is None:
                                continue
                            if sig[0] == "e":
                                if od["eng"] == "tensor" and engname == "tensor" and not o["dma"]:
                                    continue
                                sem = esem[sig[1]]
                            else:
                                sem = csem[sig[1]]
                            key = (sig[0], sig[1])
                            if waited.get(key, 0) >= sig[2]:
                                continue
                            eng.wait_ge(sem, sig[2])
                            waited[key] = sig[2]
                        ins = o["fn"](eng)
                        sig = o["sig"]
                        if sig is not None:
                            if sig[0] == "e":
                                ins.then_inc(esem[sig[1]], 1)
                            else:
                                ins.then_inc(csem[sig[1]], 16)
                    last = {}
                    for i in idxs:
                        o = ops[i]
                        if o["dma"]:
                            last[o["chan"]] = o["sig"][2]
                    for c, v in last.items():
                        if waited.get(("c", c), 0) < v:
                            eng.wait_ge(csem[c], v)
                return body

            for engname, idxs in by_eng.items():
                getattr(block, engname)(make(engname, idxs))
        nc.all_engine_barrier()
        nc.clear_and_free_semaphores(list(esem.values()) + list(csem.values()))
        nc.all_engine_barrier()
        self.ops = []
        self.last_w = {}
        self.readers = {}


def _consts():
    ident = np.eye(128, dtype=np.float32)
    rotm = np.zeros((128, 128), np.float32)
    for m in range(128):
        base = 0 if m < 64 else 64
        d = m - base
        if d < 32:
            rotm[base + d + 32, m] = -1.0
        else:
            rotm[base + d - 32, m] = 1.0
    t = np.arange(S)
    row = (t // 64).astype(np.float32)
    col = (t % 64).astype(np.float32)
    inv = (np.float32(10000.0) ** (-np.arange(0, 64, 2, dtype=np.float32) / np.float32(64))).astype(np.float32)
    ang_r = row[:, None] * inv[None, :]
    ang_c = col[:, None] * inv[None, :]
    ang = np.concatenate([ang_r, ang_r, ang_c, ang_c], axis=-1).astype(np.float32)
    cosT = np.ascontiguousarray(np.cos(ang).astype(np.float32).T)
    sinT = np.ascontiguousarray(np.sin(ang).astype(np.float32).T)
    k = np.arange(128)[:, None]
    q = np.arange(128)[None, :]
    maskL = (k >= q).astype(ml_dtypes.bfloat16)
    maskR = (k <= q).astype(ml_dtypes.bfloat16)
    iota16 = np.tile(np.arange(16, dtype=np.float32)[None, :], (128, 1))
    return dict(ident=ident, rotm=rotm, cosT=cosT, sinT=sinT, maskL=maskL, maskR=maskR, iota16=iota16)


class Ctx:
    pass


def build(stop_after=99, debug=False, peer_tiles=None, start_at=0, pair_split=False):
    nc = bass.Bass("TRN2", target_bir_lowering=False)
    K = Ctx()
    K.nc = nc
    K._uid = [0]

    def uid():
        K._uid[0] += 1
        return K._uid[0]
    K.uid = uid
    din = lambda n, s, d=F32: nc.dram_tensor(n, list(s), d, kind="ExternalInput").ap()
    dscr = lambda n, s, d=F32: nc.dram_tensor(n, list(s), d, kind="Internal").ap()
    I = dict(
        x=din("x", [S, D]), c=din("c", [D]), ctx=din("ctx", [C, D]), c_ctx=din("c_ctx", [D]),
        w_mod=din("w_mod", [2, D, 6 * D]), b_mod=din("b_mod", [2, 6 * D]),
        norm_mix=din("norm_mix", [2, D]), norm_ffn=din("norm_ffn", [2, D]), norm_final=din("norm_final", [D]),
        attn_w_qkv=din("attn_w_qkv", [D, 3072]), attn_w_o=din("attn_w_o", [D, D]), attn_sink=din("attn_sink", [16]),
        rec_w_in=din("rec_w_in", [D, 2 * D]), rec_conv_w=din("rec_conv_w", [4, D]), rec_conv_b=din("rec_conv_b", [D]),
        rec_w_a=din("rec_w_a", [2, 8, 256, 256]), rec_b_a=din("rec_b_a", [2, D]),
        rec_w_x=din("rec_w_x", [2, 8, 256, 256]), rec_b_x=din("rec_b_x", [2, D]),
        rec_lambda=din("rec_lambda", [2, D]), rec_w_out=din("rec_w_out", [D, D]),
        peer_w_q=din("peer_w_q", [2, D, D]), peer_keys=din("peer_keys", [2, 2, 128, 128]),
        peer_u=din("peer_u", [2, 16384, D]), peer_v=din("peer_v", [2, 16384, D]),
        ident=din("ident", [128, 128]), rotm=din("rotm", [128, 128]), cosT=din("cosT", [128, S]), sinT=din("sinT", [128, S]),
        maskL=din("maskL", [128, 128], BF16), maskR=din("maskR", [128, 128], BF16), iota16=din("iota16", [128, 16]),
        myrows=din("myrows", [128, 16], U32),
        rows0=din("rows0", [128, 17], U32),
    )
    K.I = I
    out = nc.dram_tensor("out", [S // 2, D], F32, kind="ExternalOutput").ap()
    K.out = out
    K.modd = dscr("modd", [2, 2, 6 * D])
    K.qd = dscr("qd", [128, 16, NT * 128], BF16)
    K.kd = dscr("kd", [128, 4, NT * 128], BF16)
    K.vd = dscr("vd", [NT * 128, 512], BF16)
    K.xs1 = dscr("xs1", [NT * 128, D])
    K.xs2 = dscr("xs2", [NT * 128, D]) if start_at < 3 else din("xs2", [NT * 128, D])
    K.xs3 = dscr("xs3", [S, D]) if start_at < 4 else din("xs3", [S, D])
    K.upre = dscr("upre", [16, 128, NT * 128])
    K.ggd = dscr("ggd", [16, 128, S])
    K.ygd = dscr("ygd", [128, 16, S], BF16)
    K.pair_split = pair_split
    if pair_split:
        K.xs2loc = dscr("xs2loc", [17 * 128, D])
        K.xs2all = dscr("xs2all", [2 * 17 * 128, D])

    def xs2tile(e):
        if not pair_split:
            return K.xs2[e * 128:(e + 1) * 128, :]
        if e < 2:
            r0 = (e * 17) * 128
        else:
            i = e - 2
            r0 = ((i // 16) * 17 + 1 + i % 16) * 128
        return K.xs2all[r0:r0 + 128, :]
    K.xs2tile = xs2tile
    K.uvb16 = dscr("uvb16", [2 * 16384, 2 * D], BF16)
    dbg = {}
    if debug:
        dbg["d_modd"] = nc.dram_tensor("d_modd", [2, 2, 6 * D], F32, kind="ExternalOutput").ap()
        dbg["d_xs1"] = nc.dram_tensor("d_xs1", [NT * 128, D], F32, kind="ExternalOutput").ap()
        dbg["d_xs2"] = nc.dram_tensor("d_xs2", [NT * 128, D], F32, kind="ExternalOutput").ap()
        dbg["d_xs3"] = nc.dram_tensor("d_xs3", [S, D], F32, kind="ExternalOutput").ap()
    K.dbg = dbg

    with ExitStack() as es:
        K.ps = [es.enter_context(nc.psum_tensor(f"ps{i}", [128, 512], F32)) for i in range(8)]
        K.ident = es.enter_context(nc.sbuf_tensor("identsb", [128, 128], F32))
        P = Prog(nc, "c0")
        P.dma("sync", K.ident[:], I["ident"], r=[], w=["ident"], chan="ident")
        P.emit()
        nc.all_engine_barrier()
        phase_mod(K)
        nc.all_engine_barrier()
        K.tc = [TabConv(K, 0, "a"), TabConv(K, 1, "b")]
        if debug:
            copy_dram(K, K.modd.rearrange("l v n -> (l v) n"), dbg["d_modd"].rearrange("l v n -> (l v) n"), 4, "cpm")
        if stop_after >= 1 and start_at <= 1:
            phase_attn(K)
            nc.all_engine_barrier()
            if debug:
                copy_dram(K, K.xs1, dbg["d_xs1"], NT * 128, "cpx1")
        if stop_after >= 2 and start_at <= 2:
            if pair_split:
                tl = [dict(src=("g", K.xs1, j), v=1 if j == 0 else 0, dst=K.xs2loc[j * 128:(j + 1) * 128, :]) for j in range(17)]
                phase_peer(K, 0, tl, False, "pe0", idxname="rows0")
                nc.all_engine_barrier()
                P = Prog(nc, "cc")
                P.op("gpsimd", lambda e: e.collective_compute("AllGather", ALU.bypass, replica_groups=[[0, 1], [2, 3], [4, 5], [6, 7]],
                                                              ins=[K.xs2loc], outs=[K.xs2all]), r=[], w=[], dma=True, chan="cc")
                P.emit()
            else:
                tl = [dict(src=("d", K.xs1[n * 128:(n + 1) * 128, :]), v=1 if n < 2 else 0, dst=K.xs2[n * 128:(n + 1) * 128, :]) for n in range(NT)]
                if peer_tiles is not None:
                    tl = [tl[i] for i in peer_tiles]
                phase_peer(K, 0, tl, False, "pe0")
            nc.all_engine_barrier()
            if debug:
                copy_dram(K, K.xs2, dbg["d_xs2"], NT * 128, "cpx2")
        if stop_after >= 3 and start_at <= 3:
            phase_rec(K)
            nc.all_engine_barrier()
            if debug:
                copy_dram(K, K.xs3, dbg["d_xs3"], S, "cpx3")
        if stop_after >= 4:
            tl = [dict(src=("g", K.xs3, t), v=0, dst=K.out[t * 128:(t + 1) * 128, :]) for t in range(S // 256)]
            if peer_tiles is not None:
                tl = tl[:len(peer_tiles)]
            phase_peer(K, 1, tl, True, "pe1")
    return nc


def copy_dram(K, src, dst, rows, name):
    nc = K.nc
    with ExitStack() as es:
        t = es.enter_context(nc.sbuf_tensor(name + "_t", [128, src.shape[1]], src.dtype))
        P = Prog(nc, name)
        for r0 in range(0, rows, 128):
            n = min(128, rows - r0)
            P.dma("sync", t[0:n, :], src[r0:r0 + n, :], r=[], w=["t"], chan="ld")
            P.dma("sync", dst[r0:r0 + n, :], t[0:n, :], r=["t"], w=[], chan="st")
        P.emit()
    nc.all_engine_barrier()


def phase_mod(K):
    nc, I = K.nc, K.I
    with ExitStack() as es:
        sb = lambda n, s, d=F32: es.enter_context(nc.sbuf_tensor(n, s, d))
        cc = sb("m_cc", [128, KC, 2])
        craw = sb("m_craw", [128, 2, KC])
        wt = [sb(f"m_wt{i}", [128, KC, 512]) for i in range(2)]
        bt = sb("m_bt", [2, 512])
        ot = [sb(f"m_ot{i}", [2, 512]) for i in range(2)]
        P = Prog(nc, "mod")
        P.dma("sync", craw[:, 0, :], I["c"].rearrange("(k p) -> p k", p=128), r=[], w=["craw0"], chan="craw0", allow_slow_non_contiguous=True)
        P.dma("sync", craw[:, 1, :], I["c_ctx"].rearrange("(k p) -> p k", p=128), r=[], w=["craw1"], chan="craw1", allow_slow_non_contiguous=True)
        for v in range(2):
            P.A(lambda e, v=v: e.activation(out=cc[:, :, v], in_=craw[:, v, :], func=AF.Silu), r=[f"craw{v}"], w=["cc"])
        n = 0
        for l in range(2):
            for j in range(24):
                s = n % 2
                w_src = I["w_mod"][l].rearrange("(k p) n -> p k n", p=128)[:, :, j * 512:(j + 1) * 512]
                P.dma("sync" if s == 0 else "gpsimd", wt[s][:], w_src, r=[], w=[f"wt{s}"], chan=f"wt{s}")
                for v in range(2):
                    P.dma("sync", bt[v:v + 1, :], I["b_mod"][l:l + 1, j * 512:(j + 1) * 512], r=[], w=["bt"], chan=f"bt{v}")
                pb = K.ps[s]
                for kc in range(KC):
                    P.T(lambda e, kc=kc, s=s, pb=pb: e.matmul(pb[0:2, :], lhsT=cc[:, kc, :], rhs=wt[s][:, kc, :], start=(kc == 0), stop=(kc == KC - 1)),
                        r=["cc", f"wt{s}"], w=[f"ps{s}"])
                P.V(lambda e, s=s, pb=pb: e.tensor_tensor(out=ot[s][:], in0=pb[0:2, :], in1=bt[:], op=ALU.add), r=[f"ps{s}", "bt"], w=[f"ot{s}"])
                P.dma("sync", K.modd[l, :, j * 512:(j + 1) * 512], ot[s][:], r=[f"ot{s}"], w=[], chan=f"ot{s}")
                n += 1
        P.emit()


def load_bcast(P, eng, tile_ap, src_row_ap, key, chan):
    P.dma(eng, tile_ap, src_row_ap.partition_broadcast(128), r=[], w=[key], chan=chan)


def prep_GS(K, P, Gt, St, gain_row, scale_row, shift_row, tmp, tag):
    load_bcast(P, "sync", Gt[:], scale_row, f"G{tag}", f"G{tag}")
    load_bcast(P, "sync", tmp[:], gain_row, "gstmp", "gstmp")
    load_bcast(P, "sync", St[:], shift_row, f"S{tag}", f"S{tag}")
    P.V(lambda e: e.scalar_tensor_tensor(out=Gt[:], in0=Gt[:], scalar=1.0, in1=tmp[:], op0=ALU.add, op1=ALU.mult), r=[f"G{tag}", "gstmp"], w=[f"G{tag}"])


def norm_mod_T(K, P, src_ap, xt, h, ss, Gt, St, gtag, hT, hT_key, tok0, tpb, xslot):
    ps = K.ps
    P.dma("sync", xt[:], src_ap, r=[], w=[f"xt{xslot}"], chan=f"xt{xslot}")
    P.A(lambda e: e.activation(out=h[:], in_=xt[:], func=AF.Square, accum_out=ss[:, 0:1]), r=[f"xt{xslot}"], w=["h", "ss"])
    P.V(lambda e: e.tensor_scalar(out=ss[:, 1:2], in0=ss[:, 0:1], scalar1=1.0 / D, scalar2=EPS, op0=ALU.mult, op1=ALU.add), r=["ss"], w=["ss1"])
    P.A(lambda e: e.activation(out=ss[:, 2:3], in_=ss[:, 1:2], func=AF.Sqrt), r=["ss1"], w=["ss2"])
    P.V(lambda e: e.reciprocal(out=ss[:, 3:4], in_=ss[:, 2:3]), r=["ss2"], w=["ss3"])
    P.V(lambda e: e.scalar_tensor_tensor(out=h[:], in0=xt[:], scalar=ss[:, 3:4], in1=Gt[:], op0=ALU.mult, op1=ALU.mult),
        r=[f"xt{xslot}", "ss3", f"G{gtag}"], w=["h"])
    P.V(lambda e: e.tensor_tensor(out=h[:], in0=h[:], in1=St[:], op=ALU.add), r=["h", f"S{gtag}"], w=["h"])
    if hT is None:
        return
    for q in range(4):
        b = tpb[q % 2]
        for j in range(4):
            kc = 4 * q + j
            P.T(lambda e, kc=kc, j=j, b=b: e.transpose(out=ps[b][:, j * 128:(j + 1) * 128], in_=h[:, kc * 128:(kc + 1) * 128], identity=K.ident[:]),
                r=["h", "ident"], w=[f"ps{b}"])
        P.A(lambda e, q=q, b=b: e.activation(out=hT[:, 4 * q:4 * q + 4, tok0:tok0 + 128], in_=ps[b][:].rearrange("p (j t) -> p j t", j=4), func=AF.Copy),
            r=[f"ps{b}"], w=[hT_key])


def load_w_bf16(P, wsb, w_dram, ncols, key, col0=0):
    for kc in range(KC):
        for c0 in range(0, ncols, 1024):
            cn = min(1024, ncols - c0)
            P.dma("gpsimd", wsb[:, kc, c0:c0 + cn], w_dram[kc * 128:(kc + 1) * 128, col0 + c0:col0 + c0 + cn], r=[], w=[key], chan=f"{key}_{kc % 4}")


def phase_attn(K):
    nc, I, ps = K.nc, K.I, K.ps
    SCALE = 128 ** -0.5
    with ExitStack() as es:
        sb = lambda n, s, d=F32: es.enter_context(nc.sbuf_tensor(n, s, d))
        with ExitStack() as es1:
            sb1 = lambda n, s, d=F32: es1.enter_context(nc.sbuf_tensor(n, s, d))
            W = sb1("a_W", [128, KC, 3072], BF16)
            Gt = sb1("a_G", [128, D]); St = sb1("a_S", [128, D])
            ss = sb1("a_ss", [128, 4])
            xt = sb1("a_xt", [128, D]); h = sb1("a_h", [128, D])
            hT = sb1("a_hT", [128, KC, 256], BF16)
            rotm = sb1("a_rotm", [128, 128])
            cs = sb1("a_cos", [128, 256]); sn = sb1("a_sin", [128, 256])
            qsb = [sb1(f"a_qsb{i}", [128, 256]) for i in range(2)]
            t1 = [sb1(f"a_t1{i}", [128, 256]) for i in range(2)]
            t2 = [sb1(f"a_t2{i}", [128, 256]) for i in range(2)]
            qst = sb1("a_qst", [128, 20, 256], BF16)
            vst = sb1("a_vst", [128, 2, 512], BF16)
            P = Prog(nc, "qkv")
            tc = K.tc[0]
            tc.alloc(es1, 12)
            tc.begin()
            load_w_bf16(P, W, I["attn_w_qkv"], 3072, "W")
            P.dma("sync", rotm[:], I["rotm"], r=[], w=["rotm"], chan="rotm")
            for blk in range(NT // 2):
                is_ctx = blk == 0
                if blk in (0, 1):
                    v = 1 if is_ctx else 0
                    prep_GS(K, P, Gt, St, I["norm_mix"][0], K.modd[0, v, D:2 * D], K.modd[0, v, 0:D], h, "a")
                for ti in range(2):
                    e_t = blk * 2 + ti
                    src = I["ctx"][e_t * 128:(e_t + 1) * 128, :] if is_ctx else I["x"][(e_t - 2) * 128:(e_t - 1) * 128, :]
                    norm_mod_T(K, P, src, xt, h, ss, Gt, St, "a", hT, "hT", ti * 128, (6, 7), 0)
                if not is_ctx:
                    l0 = (blk - 1) * 256
                    P.dma("sync", cs[:], I["cosT"][:, l0:l0 + 256], r=[], w=["cos"], chan="cos")
                    P.dma("sync", sn[:], I["sinT"][:, l0:l0 + 256], r=[], w=["sin"], chan="sin")
                for j in range(20):
                    pb = j % 2
                    for kc in range(KC):
                        P.T(lambda e, kc=kc, j=j, pb=pb: e.matmul(ps[pb][:, 0:256], lhsT=W[:, kc, j * 128:(j + 1) * 128], rhs=hT[:, kc, :], start=(kc == 0), stop=(kc == KC - 1)),
                            r=["W", "hT"], w=[f"ps{pb}"])
                    dst, dkey = qst[:, j, :], "qst"
                    if is_ctx:
                        P.A(lambda e, pb=pb, dst=dst: e.activation(out=dst, in_=ps[pb][:, 0:256], func=AF.Copy), r=[f"ps{pb}"], w=[dkey])
                    else:
                        s2 = j % 2
                        P.A(lambda e, pb=pb, s2=s2: e.activation(out=qsb[s2][:], in_=ps[pb][:, 0:256], func=AF.Copy), r=[f"ps{pb}"], w=[f"qsb{s2}"])
                        P.T(lambda e, s2=s2: e.matmul(ps[2 + s2][:, 0:256], lhsT=rotm[:], rhs=qsb[s2][:], start=True, stop=True), r=["rotm", f"qsb{s2}"], w=[f"ps{2 + s2}"])
                        P.V(lambda e, s2=s2: e.tensor_tensor(out=t1[s2][:], in0=qsb[s2][:], in1=cs[:], op=ALU.mult), r=[f"qsb{s2}", "cos"], w=[f"t1{s2}"])
                        P.V(lambda e, s2=s2: e.tensor_tensor(out=t2[s2][:], in0=ps[2 + s2][:, 0:256], in1=sn[:], op=ALU.mult), r=[f"ps{2 + s2}", "sin"], w=[f"t2{s2}"])
                        P.V(lambda e, s2=s2, dst=dst: e.tensor_tensor(out=dst, in0=t1[s2][:], in1=t2[s2][:], op=ALU.add), r=[f"t1{s2}", f"t2{s2}"], w=[dkey])
                tc.pump(P, 6)
                P.dma("sync", K.qd[:, :, blk * 256:(blk + 1) * 256], qst[:, 0:16, :], r=["qst"], w=["qd"], chan="qst")
                P.dma("sync", K.kd[:, :, blk * 256:(blk + 1) * 256], qst[:, 16:20, :], r=["qst"], w=["kd"], chan="kst")
                for ti in range(2):
                    e_t = blk * 2 + ti
                    pb = 4 + ti
                    for kc in range(KC):
                        P.T(lambda e, kc=kc, ti=ti, pb=pb: e.matmul(ps[pb][:], lhsT=hT[:, kc, ti * 128:(ti + 1) * 128], rhs=W[:, kc, 2560:3072], start=(kc == 0), stop=(kc == KC - 1)),
                            r=["W", "hT"], w=[f"ps{pb}"])
                    P.A(lambda e, ti=ti, pb=pb: e.activation(out=vst[:, ti, :], in_=ps[pb][:], func=AF.Copy), r=[f"ps{pb}"], w=[f"vst{ti}"])
                    P.dma("sync", K.vd[e_t * 128:(e_t + 1) * 128, :], vst[:, ti, :], r=[f"vst{ti}"], w=["vd"], chan=f"vst{ti}")
            tc.flush(P)
            P.emit()
        nc.all_engine_barrier()
        with ExitStack() as es2:
            sb2 = lambda n, s, d=F32: es2.enter_context(nc.sbuf_tensor(n, s, d))
            Wo = sb2("a_Wo", [128, KC, D], BF16)
            kT = sb2("a_kT", [128, 4, NT * 128], BF16)
            vx = sb2("a_vx", [128, NT, 4, 129], BF16)
            gate = sb2("a_gate", [128, D])
            esink = sb2("a_esink", [128, 16])
            qt = [sb2(f"a_qt{i}", [128, 16, 128], BF16) for i in range(2)]
            E = [sb2(f"a_E{i}", [128, 5, 512], BF16) for i in range(2)]
            mL = sb2("a_mL", [128, 128], BF16); mR = sb2("a_mR", [128, 128], BF16)
            o = sb2("a_o", [128, D]); oT = sb2("a_oT", [128, KC, 128], BF16)
            den = sb2("a_den", [128, 8])
            xt = [sb2(f"a_xr{i}", [128, D]) for i in range(1)]
            y = sb2("a_y", [128, D])
            sraw = sb2("a_sraw", [128, 16])
            P = Prog(nc, "att")
            tc = K.tc[0]
            tc.alloc(es2, 4)
            tc.begin()
            load_w_bf16(P, Wo, I["attn_w_o"], D, "Wo")
            P.G(lambda e: e.memset(vx[:, :, :, 128:129], 1.0), r=[], w=["vx1"])
            for hh in range(4):
                P.dma("sync", kT[:, hh, :], K.kd[:, hh, :], r=[], w=["kT"], chan=f"kTl{hh}")
            for t in range(NT):
                P.dma("sync", vx[:, t, :, 0:128], K.vd[t * 128:(t + 1) * 128, :].rearrange("p (g d) -> p g d", g=4), r=[], w=["vx"], chan=f"vxl{t % 4}")
            P.dma("sync", mL[:], I["maskL"], r=[], w=["mL"], chan="mL")
            P.dma("sync", mR[:], I["maskR"], r=[], w=["mR"], chan="mR")
            load_bcast(P, "sync", sraw[:], I["attn_sink"], "sraw", "sraw")
            P.A(lambda e: e.activation(out=esink[:], in_=sraw[:], func=AF.Exp), r=["sraw"], w=["esink"])
            for e_t in range(NT):
                is_ctx = e_t < 2
                if e_t in (0, 2):
                    v = 1 if is_ctx else 0
                    load_bcast(P, "sync", gate[:], K.modd[0, v, 2 * D:3 * D], "gate", "gate")
                qs = e_t % 2
                P.dma("sync", qt[qs][:], K.qd[:, :, e_t * 128:(e_t + 1) * 128], r=["qd"], w=[f"qt{qs}"], chan=f"qt{qs}")
                src = I["ctx"][e_t * 128:(e_t + 1) * 128, :] if is_ctx else I["x"][(e_t - 2) * 128:(e_t - 1) * 128, :]
                P.dma("sync", xt[0][:], src, r=[], w=["xr0"], chan="xr0")
                if is_ctx:
                    kbs = [0, 1]
                else:
                    kbs = [0, 1] + [kb for kb in (e_t - 1, e_t, e_t + 1) if 2 <= kb < NT]
                for hh in range(4):
                    Es = E[hh % 2]
                    Ek = f"E{hh % 2}"
                    for n, kb in enumerate(kbs):
                        pb = n % 2
                        P.T(lambda e, hh=hh, kb=kb, pb=pb, qs=qs: e.matmul(ps[pb][:], lhsT=kT[:, hh, kb * 128:(kb + 1) * 128],
                                                                      rhs=qt[qs][:, 4 * hh:4 * hh + 4, :].rearrange("p g q -> p (g q)"), start=True, stop=True),
                            r=["kT", f"qt{qs}"], w=[f"ps{pb}"])
                        P.A(lambda e, n=n, pb=pb, Es=Es: e.activation(out=Es[:, n, :], in_=ps[pb][:], func=AF.Exp, scale=SCALE), r=[f"ps{pb}"], w=[f"{Ek}_{n}"])
                        if (not is_ctx) and kb >= 2 and kb != e_t:
                            m = mL if kb == e_t - 1 else mR
                            P.V(lambda e, n=n, m=m, Es=Es: e.tensor_tensor(out=Es[:, n, :].rearrange("p (g q) -> p g q", g=4), in0=Es[:, n, :].rearrange("p (g q) -> p g q", g=4),
                                                                        in1=m[:].unsqueeze(1).to_broadcast([128, 4, 128]), op=ALU.mult),
                                r=[f"{Ek}_{n}", "mL", "mR"], w=[f"{Ek}_{n}"])
                    for g in range(4):
                        pb = 2 + g // 2
                        for n, kb in enumerate(kbs):
                            P.T(lambda e, g=g, n=n, kb=kb, pb=pb, hh=hh, Es=Es: e.matmul(ps[pb][:, (g % 2) * 256:(g % 2) * 256 + 129], lhsT=Es[:, n, g * 128:(g + 1) * 128],
                                                                                     rhs=vx[:, kb, hh, :], start=(n == 0), stop=(n == len(kbs) - 1)),
                                r=[f"{Ek}_{n}", "vx", "vx1"], w=[f"ps{pb}"])
                    for bk in range(2):
                        pb = 2 + bk
                        P.V(lambda e, bk=bk, pb=pb, hh=hh: e.tensor_tensor(out=den[:, 2 * bk:2 * bk + 2], in0=ps[pb][:].rearrange("p (g c) -> p g c", g=2)[:, :, 128],
                                                                       in1=esink[:, 4 * hh + 2 * bk:4 * hh + 2 * bk + 2], op=ALU.add),
                            r=[f"ps{pb}", "esink"], w=["den"])
                    P.V(lambda e: e.reciprocal(out=den[:, 4:8], in_=den[:, 0:4]), r=["den"], w=["rden"])
                    for g in range(4):
                        pb = 2 + g // 2
                        hd = 4 * hh + g
                        P.V(lambda e, g=g, pb=pb, hd=hd: e.tensor_scalar(out=o[:, hd * 128:(hd + 1) * 128], in0=ps[pb][:, (g % 2) * 256:(g % 2) * 256 + 128],
                                                                     scalar1=den[:, 4 + g:5 + g], scalar2=None, op0=ALU.mult),
                            r=[f"ps{pb}", "rden"], w=["o"])
                for q in range(4):
                    b = 6 + q % 2
                    for j in range(4):
                        kc = 4 * q + j
                        P.T(lambda e, kc=kc, j=j, b=b: e.transpose(out=ps[b][:, j * 128:(j + 1) * 128], in_=o[:, kc * 128:(kc + 1) * 128], identity=K.ident[:]),
                            r=["o", "ident"], w=[f"ps{b}"])
                    P.A(lambda e, q=q, b=b: e.activation(out=oT[:, 4 * q:4 * q + 4, :], in_=ps[b][:].rearrange("p (j t) -> p j t", j=4), func=AF.Copy), r=[f"ps{b}"], w=["oT"])
                for nb in range(4):
                    pb = 4 + nb
                    for kc in range(KC):
                        P.T(lambda e, kc=kc, nb=nb, pb=pb: e.matmul(ps[pb][:], lhsT=oT[:, kc, :], rhs=Wo[:, kc, nb * 512:(nb + 1) * 512], start=(kc == 0), stop=(kc == KC - 1)),
                            r=["oT", "Wo"], w=[f"ps{pb}"])
                    P.V(lambda e, nb=nb, pb=pb: e.tensor_tensor(out=y[:, nb * 512:(nb + 1) * 512], in0=ps[pb][:], in1=gate[:, nb * 512:(nb + 1) * 512], op=ALU.mult),
                        r=[f"ps{pb}", "gate"], w=[f"y{nb}"])
                    P.V(lambda e, nb=nb, qs=qs: e.tensor_tensor(out=y[:, nb * 512:(nb + 1) * 512], in0=y[:, nb * 512:(nb + 1) * 512], in1=xt[0][:, nb * 512:(nb + 1) * 512], op=ALU.add),
                        r=[f"y{nb}", "xr0"], w=[f"y{nb}"])
                P.dma("sync", K.xs1[e_t * 128:(e_t + 1) * 128, :], y[:], r=[f"y{nb}" for nb in range(4)], w=["xs1"], chan="y")
                tc.pump(P, 5)
            tc.finish(P)
            P.emit()


def _lay(inputs):
    consts = _consts()
    f = lambda a: np.ascontiguousarray(np.asarray(a, dtype=np.float32))
    shared = dict(
        c_ctx=f(inputs["c_ctx"]), w_mod=f(inputs["w_mod"]), b_mod=f(inputs["b_mod"]),
        norm_mix=f(inputs["norm_mix"]), norm_ffn=f(inputs["norm_ffn"]), norm_final=f(inputs["norm_final"]),
        attn_w_qkv=f(inputs["attn_w_qkv"][0]), attn_w_o=f(inputs["attn_w_o"][0]), attn_sink=f(inputs["attn_sink"][0]),
        rec_w_in=f(inputs["rec_w_in"][0]), rec_conv_w=f(inputs["rec_conv_w"][0]), rec_conv_b=f(inputs["rec_conv_b"][0]),
        rec_w_a=f(inputs["rec_w_a"][0]), rec_b_a=f(inputs["rec_b_a"][0]), rec_w_x=f(inputs["rec_w_x"][0]), rec_b_x=f(inputs["rec_b_x"][0]),
        rec_lambda=f(inputs["rec_lambda"][0]), rec_w_out=f(inputs["rec_w_out"][0]),
        peer_w_q=f(inputs["peer_w_q"]), peer_keys=f(inputs["peer_keys"]), peer_u=f(inputs["peer_u"]), peer_v=f(inputs["peer_v"]),
        **consts,
    )
    return shared, f


def core_inputs(inputs, shared, f, core):
    b, hf = core // 2, core % 2
    m = dict(shared)
    m["x"] = f(inputs["x"][b]); m["c"] = f(inputs["c"][b]); m["ctx"] = f(inputs["ctx"][b])
    r0 = np.zeros((128, 17), np.uint32)
    r0[:, 0] = hf * 128 + np.arange(128)
    for j in range(16):
        r0[:, 1 + j] = 256 + (hf * 16 + j) * 128 + np.arange(128)
    m["rows0"] = r0
    m["myrows"] = (hf * (S // 2) + np.arange(16, dtype=np.uint32)[None, :] * 128 + np.arange(128, dtype=np.uint32)[:, None]).astype(np.uint32)
    return m


PAIR_SPLIT = False


def kernel(**inputs):
    nc = build(pair_split=PAIR_SPLIT)
    shared, f = _lay(inputs)
    in_maps = [core_inputs(inputs, shared, f, core) for core in range(8)]
    res = run_bass_kernel_spmd(nc, in_maps, core_ids=list(range(8)))
    outp = np.empty((4, S, D), np.float32)
    for core in range(8):
        b, hf = core // 2, core % 2
        outp[b, hf * (S // 2):(hf + 1) * (S // 2)] = res.results[core]["out"]
    return outp


class TabConv:
    NR = 6
    LAG = 3

    def set_nr(self, nr):
        self.NR, self.LAG = nr, max(1, nr // 2)

    def __init__(self, K, layer, tag):
        self.K, self.layer, self.tag = K, layer, tag
        self.steps = [(tb, r0) for tb in (0, 1) for r0 in range(layer * 16384, (layer + 1) * 16384, 128)]
        self.i = 0
        self.tiles = None

    def alloc(self, es, nr=6):
        self.set_nr(nr)
        if True:
            self.tiles = [es.enter_context(self.K.nc.sbuf_tensor(f"tc{self.tag}_{self.K.uid()}_{i}", [128, D], BF16)) for i in range(self.NR)]

    def _store(self, P, j):
        tb, r0 = self.steps[j]
        sl = j % self.NR
        dst = self.K.uvb16[r0:r0 + 128, tb * D:(tb + 1) * D]
        P.dma("sync", dst, self.tiles[sl][:], r=[f"tc{sl}"], w=[], chan=f"tcs{sl}")

    def pump(self, P, n):
        I = self.K.I
        for _ in range(n):
            if self.i >= len(self.steps):
                break
            j = self.i
            tb, r0 = self.steps[j]
            src = (I["peer_u"] if tb == 0 else I["peer_v"]).rearrange("l e d -> (l e) d")[r0:r0 + 128, :]
            sl = j % self.NR
            P.dma("gpsimd", self.tiles[sl][:], src, r=[], w=[f"tc{sl}"], chan=f"tcl{sl}")
            if j - self.LAG >= self.done:
                self._store(P, j - self.LAG)
            self.i += 1

    def begin(self):
        self.done = self.i

    def flush(self, P):
        for j in range(max(self.done, self.i - self.LAG), self.i):
            self._store(P, j)
        self.done = self.i

    def finish(self, P):
        self.pump(P, len(self.steps))
        self.flush(P)


def phase_peer(K, layer, tiles, final, name, idxname="myrows"):
    nc, I, ps = K.nc, K.I, K.ps
    NB = 7
    uvtab = K.uvb16
    with ExitStack() as es:
        sb = lambda n, s, d=F32: es.enter_context(nc.sbuf_tensor(name + n, s, d))
        Wq = sb("p_Wq", [128, KC, D], BF16)
        Gt = sb("p_G", [128, D]); St = sb("p_S", [128, D])
        gate = sb("p_gate", [128, D])
        xt1 = sb("p_xt", [128, D])
        xt = [xt1, xt1]
        h = [sb(f"p_h{i}", [128, D]) for i in range(2)]
        hT = sb("p_hT", [128, KC, 128], BF16)
        wk = [sb(f"p_wk{i}", [128, D]) for i in range(2)]
        ring = [sb(f"p_ring{i}", [128, 2 * D], BF16) for i in range(NB)]
        junk = sb("p_junk", [128, D], BF16)
        acc = wk[1]
        keysT = sb("p_keysT", [128, 2, 128])
        kraw = sb("p_kraw", [128, 2, 128])
        ss = sb("p_ss", [128, 4]); ssb = sb("p_ssb", [128, 4])
        s16 = sb("p_s16", [128, 16, 16]); i16 = sb("p_i16", [128, 16, 16], U32); i16f = sb("p_i16f", [128, 16, 16])
        ts = sb("p_ts", [128, 8, 16]); sel = sb("p_sel", [128, 8, 16], U32)
        au = sb("p_au", [128, 2, 128], U32); af = sb("p_af", [128, 2, 128])
        isel = sb("p_isel", [128, 2, 128])
        idxf = sb("p_idxf", [128, 128])
        idxu = [sb(f"p_idxu{i}", [128, 128], U32) for i in range(2)]
        gsm = [sb(f"p_g{i}", [128, 128]) for i in range(2)]
        sm = sb("p_sm", [128, 16])
        z = sb("p_z", [128, 128]); gz = sb("p_gz", [128, 128]); av = sb("p_av", [128, 128])
        identb = sb("p_identb", [128, 128], BF16)
        dg = [sb(f"p_dg{i}", [128, 128], BF16) for i in range(4)]
        iota = sb("p_iota", [128, 16])
        mr = sb("p_mr", [128, I[idxname].shape[1]], U32)
        P = Prog(nc, name)
        load_w_bf16(P, Wq, I["peer_w_q"][layer], D, "Wq")
        P.dma("sync", iota[:], I["iota16"], r=[], w=["iota"], chan="iota")
        P.A(lambda e: e.activation(out=identb[:], in_=K.ident[:], func=AF.Copy), r=["ident"], w=["identb"])
        P.dma("sync", mr[:], I[idxname], r=[], w=["mr"], chan="mr")
        for p in range(2):
            P.dma("sync", kraw[:, p, :], I["peer_keys"][layer, p], r=[], w=["kraw"], chan=f"kraw{p}")
            P.T(lambda e, p=p: e.transpose(out=ps[p][:, 0:128], in_=kraw[:, p, :], identity=K.ident[:]), r=["kraw", "ident"], w=[f"ps{p}"])
            P.V(lambda e, p=p: e.tensor_copy(out=keysT[:, p, :], in_=ps[p][:, 0:128]), r=[f"ps{p}"], w=["keysT"])

        P.emit()
        nc.all_engine_barrier()
        cur_v = [None]
        PP = [None]

        def front(n):
            P = PP[0]
            t = tiles[n]
            s = n % 2
            if cur_v[0] != t["v"]:
                cur_v[0] = t["v"]
                v = t["v"]
                prep_GS(K, P, Gt, St, I["norm_ffn"][layer], K.modd[layer, v, 4 * D:5 * D], K.modd[layer, v, 3 * D:4 * D], wk[0], "p")
            if t["src"][0] == "d":
                P.dma("sync", xt[s][:], t["src"][1], r=[], w=["xt"], chan="xt")
            else:
                col = t["src"][2]
                P.op("gpsimd", lambda e, s=s, col=col, src=t["src"][1]: e.indirect_dma_start(out=xt[s][:], out_offset=None, in_=src,
                                                                                         in_offset=bass.IndirectOffsetOnAxis(ap=mr[:, col:col + 1], axis=0)),
                     r=["mr"], w=["xt"], dma=True, chan="xt")
            hh = h[s]
            hk = f"h{s}"
            P.A(lambda e, s=s: e.activation(out=h[s][:], in_=xt[s][:], func=AF.Square, accum_out=ss[:, 0:1]), r=["xt"], w=[hk, "ss"])
            P.V(lambda e: e.tensor_scalar(out=ss[:, 1:2], in0=ss[:, 0:1], scalar1=1.0 / D, scalar2=EPS, op0=ALU.mult, op1=ALU.add), r=["ss"], w=["ss1"])
            P.A(lambda e: e.activation(out=ss[:, 2:3], in_=ss[:, 1:2], func=AF.Sqrt), r=["ss1"], w=["ss2"])
            P.V(lambda e: e.reciprocal(out=ss[:, 3:4], in_=ss[:, 2:3]), r=["ss2"], w=["ss3"])
            P.V(lambda e, s=s: e.scalar_tensor_tensor(out=h[s][:], in0=xt[s][:], scalar=ss[:, 3:4], in1=Gt[:], op0=ALU.mult, op1=ALU.mult), r=["xt", "ss3", "Gp"], w=[hk])
            P.V(lambda e, s=s: e.tensor_tensor(out=h[s][:], in0=h[s][:], in1=St[:], op=ALU.add), r=[hk, "Sp"], w=[hk])
            for q in range(4):
                b = q % 2
                for j in range(4):
                    kc = 4 * q + j
                    P.T(lambda e, kc=kc, j=j, b=b, s=s: e.transpose(out=ps[b][:, j * 128:(j + 1) * 128], in_=h[s][:, kc * 128:(kc + 1) * 128], identity=K.ident[:]),
                        r=[hk, "ident"], w=[f"ps{b}"])
                P.A(lambda e, q=q, b=b: e.activation(out=hT[:, 4 * q:4 * q + 4, :], in_=ps[b][:].rearrange("p (j t) -> p j t", j=4), func=AF.Copy), r=[f"ps{b}"], w=["hT"])
            yield
            qT = wk[0]
            for c in range(16):
                pb = c % 2
                for kc in range(KC):
                    P.T(lambda e, kc=kc, c=c, pb=pb: e.matmul(ps[pb][:, 0:128], lhsT=Wq[:, kc, c * 128:(c + 1) * 128], rhs=hT[:, kc, :], start=(kc == 0), stop=(kc == KC - 1)),
                        r=["Wq", "hT"], w=[f"ps{pb}"])
                P.A(lambda e, c=c, pb=pb: e.activation(out=qT[:, c * 128:(c + 1) * 128], in_=ps[pb][:, 0:128], func=AF.Copy), r=[f"ps{pb}"], w=["wk0"])
                yield
            Ssb = wk[1]
            for q in range(4):
                pb = 2 + q % 2
                for c in range(4 * q, 4 * q + 4):
                    P.T(lambda e, c=c, pb=pb: e.matmul(ps[pb][:, (c % 4) * 128:(c % 4 + 1) * 128], lhsT=qT[:, c * 128:(c + 1) * 128], rhs=keysT[:, c % 2, :], start=True, stop=True),
                        r=["wk0", "keysT"], w=[f"ps{pb}"])
                P.A(lambda e, q=q, pb=pb: e.activation(out=Ssb[:, q * 512:(q + 1) * 512], in_=ps[pb][:], func=AF.Copy), r=[f"ps{pb}"], w=["wk1"])
            S2 = wk[0]
            for c in range(16):
                sv = Ssb[:, c * 128:(c + 1) * 128]
                s2v = S2[:, c * 128:(c + 1) * 128]
                P.V(lambda e, c=c, sv=sv: e.max(out=s16[:, c, 0:8], in_=sv), r=["wk1"], w=["s16"])
                P.V(lambda e, c=c, sv=sv: e.max_index(out=i16[:, c, 0:8], in_max=s16[:, c, 0:8], in_values=sv), r=["wk1", "s16"], w=["i16"])
                P.V(lambda e, c=c, sv=sv, s2v=s2v: e.match_replace(out=s2v, in_to_replace=s16[:, c, 0:8], in_values=sv, imm_value=-1e30), r=["wk1", "s16"], w=["wk0"])
                P.V(lambda e, c=c, s2v=s2v: e.max(out=s16[:, c, 8:16], in_=s2v), r=["wk0"], w=["s16"])
                P.V(lambda e, c=c, s2v=s2v: e.max_index(out=i16[:, c, 8:16], in_max=s16[:, c, 8:16], in_values=s2v), r=["wk0", "s16"], w=["i16"])
                yield
            cand = wk[1]
            c4 = cand[:].rearrange("p (h a b) -> p h a b", h=8, a=16)
            s16r = s16[:].rearrange("p (h t) k -> p h t k", t=2)
            P.V(lambda e: e.tensor_tensor(out=c4, in0=s16r[:, :, 0, :].unsqueeze(3).to_broadcast([128, 8, 16, 16]),
                                          in1=s16r[:, :, 1, :].unsqueeze(2).to_broadcast([128, 8, 16, 16]), op=ALU.add), r=["s16"], w=["wk1"])
            for hd in range(8):
                cv = cand[:, hd * 256:(hd + 1) * 256]
                c2v = S2[:, hd * 256:(hd + 1) * 256]
                P.V(lambda e, hd=hd, cv=cv: e.max(out=ts[:, hd, 0:8], in_=cv), r=["wk1"], w=["ts"])
                P.V(lambda e, hd=hd, cv=cv: e.max_index(out=sel[:, hd, 0:8], in_max=ts[:, hd, 0:8], in_values=cv), r=["wk1", "ts"], w=["sel"])
                P.V(lambda e, hd=hd, cv=cv, c2v=c2v: e.match_replace(out=c2v, in_to_replace=ts[:, hd, 0:8], in_values=cv, imm_value=-1e30), r=["wk1", "ts"], w=["wk0"])
                P.V(lambda e, hd=hd, c2v=c2v: e.max(out=ts[:, hd, 8:16], in_=c2v), r=["wk0"], w=["ts"])
                P.V(lambda e, hd=hd, c2v=c2v: e.max_index(out=sel[:, hd, 8:16], in_max=ts[:, hd, 8:16], in_values=c2v), r=["wk0", "ts"], w=["sel"])
                yield
            selv = sel[:].rearrange("p h k -> p (h k)")
            P.V(lambda e: e.tensor_scalar(out=au[:, 0, :], in0=selv, scalar1=4, scalar2=None, op0=ALU.logical_shift_right), r=["sel"], w=["au"])
            P.V(lambda e: e.tensor_scalar(out=au[:, 1, :], in0=selv, scalar1=15, scalar2=None, op0=ALU.bitwise_and), r=["sel"], w=["au"])
            P.V(lambda e: e.tensor_copy(out=af[:], in_=au[:]), r=["au"], w=["af"])
            P.V(lambda e: e.tensor_copy(out=i16f[:], in_=i16[:]), r=["i16"], w=["i16f"])
            eq = wk[1][:].rearrange("p (h s a) -> p h s a", h=8, s=16)
            i16r = i16f[:].rearrange("p (h t) k -> p h t k", t=2)
            for half in range(2):
                P.V(lambda e, half=half: e.tensor_tensor(out=eq, in0=af[:, half, :].rearrange("p (h s) -> p h s", h=8).unsqueeze(3).to_broadcast([128, 8, 16, 16]),
                                                         in1=iota[:].unsqueeze(1).unsqueeze(1).to_broadcast([128, 8, 16, 16]), op=ALU.is_equal), r=["af", "iota"], w=["wk1"])
                P.V(lambda e, half=half: e.tensor_tensor(out=eq, in0=eq, in1=i16r[:, :, half, :].unsqueeze(2).to_broadcast([128, 8, 16, 16]), op=ALU.mult), r=["wk1", "i16f"], w=["wk1"])
                P.V(lambda e, half=half: e.tensor_reduce(out=isel[:, half, :].rearrange("p (h s) -> p h s", h=8), in_=eq, axis=AX.X, op=ALU.add), r=["wk1"], w=["isel"])
                yield
            P.V(lambda e: e.scalar_tensor_tensor(out=idxf[:], in0=isel[:, 0, :], scalar=128.0, in1=isel[:, 1, :], op0=ALU.mult, op1=ALU.add), r=["isel"], w=["idxf"])
            if layer > 0:
                P.V(lambda e: e.tensor_scalar(out=idxf[:], in0=idxf[:], scalar1=float(layer * 16384), scalar2=None, op0=ALU.add), r=["idxf"], w=["idxf"])
            P.V(lambda e, s=s: e.tensor_copy(out=idxu[s][:], in_=idxf[:]), r=["idxf"], w=[f"idxu{s}"])
            g3 = gsm[s][:].rearrange("p (h k) -> p h k", h=8)
            P.V(lambda e, g3=g3: e.tensor_tensor(out=g3, in0=ts[:], in1=ts[:, :, 0:1].to_broadcast([128, 8, 16]), op=ALU.subtract), r=["ts"], w=[f"g{s}"])
            P.A(lambda e, s=s: e.activation(out=gsm[s][:], in_=gsm[s][:], func=AF.Exp), r=[f"g{s}"], w=[f"g{s}"])
            P.V(lambda e, g3=g3: e.tensor_reduce(out=sm[:, 0:8], in_=g3, axis=AX.X, op=ALU.add), r=[f"g{s}"], w=["sm"])
            P.V(lambda e: e.reciprocal(out=sm[:, 8:16], in_=sm[:, 0:8]), r=["sm"], w=["sm"])
            P.V(lambda e, g3=g3: e.tensor_tensor(out=g3, in0=g3, in1=sm[:, 8:16].unsqueeze(2).to_broadcast([128, 8, 16]), op=ALU.mult), r=[f"g{s}", "sm"], w=[f"g{s}"])

        gcount = [0]
        gate_v = [None]

        def gather(s, k):
            P = PP[0]
            slot = gcount[0] % NB
            gcount[0] += 1
            P.op("gpsimd", lambda e, slot=slot, s=s, k=k: e.indirect_dma_start(out=ring[slot][:], out_offset=None, in_=uvtab,
                                                                        in_offset=bass.IndirectOffsetOnAxis(ap=idxu[s][:, k:k + 1], axis=0)),
                 r=[f"idxu{s}"], w=[f"ring{slot}"], dma=True, chan=f"ring{slot}")
            return slot

        def back(n, nxt=None):
            P = PP[0]
            t = tiles[n]
            s = n % 2
            POOL_EVERY = 0
            LAGP = 2

            def consume(k, slot, via_pool):
                if via_pool:
                    P.G(lambda e, slot=slot, s=s: e.tensor_tensor(out=ring[slot][:, 0:D], in0=ring[slot][:, 0:D], in1=h[s][:], op=ALU.mult), r=[f"ring{slot}", f"h{s}"], w=[f"ring{slot}"])
                    P.A(lambda e, slot=slot, k=k: e.activation(out=ring[slot][:, 0:D], in_=ring[slot][:, 0:D], func=AF.Copy, accum_out=z[:, k:k + 1]), r=[f"ring{slot}"], w=[f"ring{slot}", f"z{k}"])
                else:
                    P.V(lambda e, slot=slot, s=s, k=k: e.scalar_tensor_tensor(out=junk[:], in0=ring[slot][:, 0:D], scalar=1.0, in1=h[s][:], op0=ALU.mult, op1=ALU.mult, accum_out=z[:, k:k + 1]),
                        r=[f"ring{slot}", f"h{s}"], w=[f"z{k}"])
                P.A(lambda e, k=k: e.activation(out=gz[:, k:k + 1], in_=z[:, k:k + 1], func=AF.Gelu_apprx_tanh), r=[f"z{k}"], w=[f"gz{k}"])
                P.A(lambda e, k=k, s=s: e.activation(out=av[:, k:k + 1], in_=gz[:, k:k + 1], func=AF.Copy, scale=gsm[s][:, k:k + 1]), r=[f"gz{k}", f"g{s}"], w=[f"a{k}"])
                P.A(lambda e, k=k: e.activation(out=dg[k % 4][:], in_=identb[:], func=AF.Copy, scale=av[:, k:k + 1]), r=[f"a{k}", "identb"], w=[f"dg{k % 4}"])
                first = not started[0]
                started[0] = True
                last = ndone[0] == 127
                ndone[0] += 1
                for nb in range(4):
                    P.T(lambda e, k=k, nb=nb, slot=slot, first=first, last=last: e.matmul(ps[4 + nb][:], lhsT=dg[k % 4][:], rhs=ring[slot][:, D + nb * 512:D + (nb + 1) * 512], start=first, stop=last),
                        r=[f"dg{k % 4}", f"ring{slot}"], w=[f"ps{4 + nb}"])

            started = [False]
            ndone = [0]
            pend = []
            for k in range(128):
                slot = gather(s, k)
                if POOL_EVERY and k % POOL_EVERY == POOL_EVERY - 1:
                    pend.append((k, slot))
                else:
                    consume(k, slot, False)
                while pend and pend[0][0] <= k - LAGP:
                    kk, sl_ = pend.pop(0)
                    consume(kk, sl_, True)
                if nxt is not None and k >= 8:
                    next(nxt, None)
            for kk, sl_ in pend:
                consume(kk, sl_, True)
            if nxt is not None:
                for _ in nxt:
                    pass
            if gate_v[0] != t["v"]:
                gate_v[0] = t["v"]
                load_bcast(P, "sync", gate[:], K.modd[layer, t["v"], 5 * D:6 * D], "gate", "gate")
            xr = wk[0]
            if t["src"][0] == "d":
                P.dma("sync", xr[:], t["src"][1], r=[], w=["wk0"], chan="xre")
            else:
                col = t["src"][2]
                P.op("gpsimd", lambda e, col=col, src=t["src"][1]: e.indirect_dma_start(out=xr[:], out_offset=None, in_=src,
                                                                                 in_offset=bass.IndirectOffsetOnAxis(ap=mr[:, col:col + 1], axis=0)),
                     r=["mr"], w=["wk0"], dma=True, chan="xre")
            for nb in range(4):
                P.V(lambda e, nb=nb: e.tensor_tensor(out=acc[:, nb * 512:(nb + 1) * 512], in0=ps[4 + nb][:], in1=gate[:, nb * 512:(nb + 1) * 512], op=ALU.mult), r=[f"ps{4 + nb}", "gate"], w=["wk1"])
            P.V(lambda e: e.tensor_tensor(out=acc[:], in0=acc[:], in1=xr[:], op=ALU.add), r=["wk1", "wk0"], w=["wk1"])
            if final:
                P.A(lambda e: e.activation(out=junk[:], in_=acc[:], func=AF.Square, accum_out=ssb[:, 0:1]), r=["wk1"], w=["junkf", "ssb"])
                P.V(lambda e: e.tensor_scalar(out=ssb[:, 1:2], in0=ssb[:, 0:1], scalar1=1.0 / D, scalar2=EPS, op0=ALU.mult, op1=ALU.add), r=["ssb"], w=["ssb1"])
                P.A(lambda e: e.activation(out=ssb[:, 2:3], in_=ssb[:, 1:2], func=AF.Sqrt), r=["ssb1"], w=["ssb2"])
                P.V(lambda e: e.reciprocal(out=ssb[:, 3:4], in_=ssb[:, 2:3]), r=["ssb2"], w=["ssb3"])
                load_bcast(P, "sync", xr[:], I["norm_final"], "wk0", "gfin")
                P.V(lambda e: e.scalar_tensor_tensor(out=acc[:], in0=acc[:], scalar=ssb[:, 3:4], in1=xr[:], op0=ALU.mult, op1=ALU.mult), r=["wk1", "ssb3", "wk0"], w=["wk1"])
            P.dma("sync", t["dst"], acc[:], r=["wk1"], w=[], chan="st")

        ngrp = -(-len(tiles) // 17)
        GSZ = -(-len(tiles) // ngrp)
        for g0 in range(0, len(tiles), GSZ):
            PP[0] = Prog(nc, f"{name}g{g0}")
            g1 = min(g0 + GSZ, len(tiles))
            for _ in front(g0):
                pass
            for n in range(g0, g1):
                back(n, front(n + 1) if n + 1 < g1 else None)
            PP[0].emit()
            K.peer_stats = PP[0].stats
            nc.all_engine_barrier()


def phase_rec(K):
    nc, I, ps = K.nc, K.I, K.ps
    NTOK = NT * 128
    for pas in ("u", "g"):
        with ExitStack() as es:
            sb = lambda n, s, d=F32: es.enter_context(nc.sbuf_tensor(n, s, d))
            W = sb("r_W" + pas, [128, KC, D], BF16)
            Gt = sb("r_G" + pas, [128, D]); St = sb("r_S" + pas, [128, D])
            ss = sb("r_ss" + pas, [128, 4])
            xt = sb("r_xt" + pas, [128, D]); h = sb("r_h" + pas, [128, D])
            hT = sb("r_hT" + pas, [128, KC, 256], BF16)
            ost = sb("r_ost" + pas, [128, 16, 256])
            P = Prog(nc, "rp" + pas)
            tc = K.tc[1]
            tc.alloc(es, 12)
            tc.begin()
            load_w_bf16(P, W, I["rec_w_in"], D, "W", col0=(D if pas == "u" else 0))
            for blk in (range(NT // 2) if pas == "u" else range(1, NT // 2)):
                is_ctx = blk == 0
                if blk in (0, 1):
                    v = 1 if is_ctx else 0
                    prep_GS(K, P, Gt, St, I["norm_mix"][1], K.modd[1, v, D:2 * D], K.modd[1, v, 0:D], h, "a")
                for ti in range(2):
                    e_t = blk * 2 + ti
                    norm_mod_T(K, P, K.xs2tile(e_t), xt, h, ss, Gt, St, "a", hT, "hT", ti * 128, (6, 7), 0)
                for j in range(16):
                    pb = j % 2
                    for kc in range(KC):
                        P.T(lambda e, kc=kc, j=j, pb=pb: e.matmul(ps[pb][:, 0:256], lhsT=W[:, kc, j * 128:(j + 1) * 128], rhs=hT[:, kc, :], start=(kc == 0), stop=(kc == KC - 1)),
                            r=["W", "hT"], w=[f"ps{pb}"])
                    fn = AF.Copy if pas == "u" else AF.Gelu_apprx_tanh
                    P.A(lambda e, j=j, pb=pb, fn=fn: e.activation(out=ost[:, j, :], in_=ps[pb][:, 0:256], func=fn), r=[f"ps{pb}"], w=["ost"])
                if pas == "u":
                    P.dma("sync", K.upre[:, :, blk * 256:(blk + 1) * 256].rearrange("c p t -> p c t"), ost[:], r=["ost"], w=["upre"], chan="ost")
                else:
                    l0 = (blk - 1) * 256
                    P.dma("sync", K.ggd[:, :, l0:l0 + 256].rearrange("c p t -> p c t"), ost[:], r=["ost"], w=["ggd"], chan="ost")
                tc.pump(P, 8)
            if pas == "g":
                tc.finish(P)
            else:
                tc.flush(P)
            P.emit()
        nc.all_engine_barrier()
    with ExitStack() as es:
        sb = lambda n, s, d=F32: es.enter_context(nc.sbuf_tensor(n, s, d))
        UPW = 4360
        up = sb("r_up", [128, 2, UPW])
        u = sb("r_u", [128, 2, NTOK]); ub = sb("r_ub", [128, 2, NTOK], BF16)
        A = sb("r_A", [128, NTOK]); Bt = sb("r_B", [128, NTOK])
        Y = [sb(f"r_Y{d}", [128, NTOK]) for d in range(2)]
        gg = sb("r_gg", [128, S]); ygb = sb("r_ygb", [128, S], BF16)
        wg = sb("r_wg", [128, 2, 2, 2, 256], BF16)
        cw = sb("r_cw", [128, 4, 16]); cb = sb("r_cb", [128, 16])
        ba = sb("r_ba", [128, 2, 16]); bx = sb("r_bx", [128, 2, 16]); lam = sb("r_lam", [128, 2, 16]); cl = sb("r_cl", [128, 2, 16])
        tmp = [[sb(f"r_t{i}{s}", [128, 512]) for i in range(4)] for s in range(2)]
        nba = sb("r_nba", [128, 2, 16]); nbx = sb("r_nbx", [128, 2, 16])
        P = Prog(nc, "rscan")
        P.G(lambda e: e.memset(up[:], 0.0), r=[], w=["up0", "up1"])
        for k in range(4):
            P.dma("sync", cw[:, k, :], I["rec_conv_w"][k].rearrange("(c p) -> p c", p=128), r=[], w=["cw"], chan=f"cw{k}", allow_slow_non_contiguous=True)
        P.dma("sync", cb[:], I["rec_conv_b"].rearrange("(c p) -> p c", p=128), r=[], w=["cb"], chan="cb", allow_slow_non_contiguous=True)
        for d in range(2):
            P.dma("sync", ba[:, d, :], I["rec_b_a"][d].rearrange("(c p) -> p c", p=128), r=[], w=["ba"], chan=f"ba{d}", allow_slow_non_contiguous=True)
            P.dma("sync", bx[:, d, :], I["rec_b_x"][d].rearrange("(c p) -> p c", p=128), r=[], w=["bx"], chan=f"bx{d}", allow_slow_non_contiguous=True)
            P.dma("sync", lam[:, d, :], I["rec_lambda"][d].rearrange("(c p) -> p c", p=128), r=[], w=["lam"], chan=f"lam{d}", allow_slow_non_contiguous=True)
        P.A(lambda e: e.activation(out=cl[:], in_=lam[:], func=AF.Exp, scale=-1.0), r=["lam"], w=["cl"])
        P.A(lambda e: e.activation(out=cl[:], in_=cl[:], func=AF.Ln, bias=1.0), r=["cl"], w=["cl"])
        P.V(lambda e: e.tensor_scalar(out=cl[:], in0=cl[:], scalar1=-8.0, scalar2=None, op0=ALU.mult), r=["cl"], w=["cl"])
        P.V(lambda e: e.tensor_scalar(out=nba[:], in0=ba[:], scalar1=-1.0, scalar2=None, op0=ALU.mult), r=["ba"], w=["nba"])
        P.V(lambda e: e.tensor_scalar(out=nbx[:], in0=bx[:], scalar1=-1.0, scalar2=None, op0=ALU.mult), r=["bx"], w=["nbx"])
        for n in range(8):
            for c in range(2):
                ch = 2 * n + c
                P.dma("sync", up[:, c, 1:257], K.upre[ch, :, 0:256], r=[], w=[f"up{c}"], chan=f"upc{c}")
                P.dma("sync", up[:, c, 260:260 + S], K.upre[ch, :, 256:NTOK], r=[], w=[f"up{c}"], chan=f"upl{c}")
                for (o0, nn, i0) in ((0, 256, 0), (256, S, 259)):
                    useg = u[:, c, o0:o0 + nn]
                    P.V(lambda e, c=c, ch=ch, useg=useg, i0=i0, nn=nn: e.tensor_scalar(out=useg, in0=up[:, c, i0:i0 + nn], scalar1=cw[:, 0, ch:ch + 1], scalar2=cb[:, ch:ch + 1], op0=ALU.mult, op1=ALU.add),
                        r=[f"up{c}", "cw", "cb"], w=[f"u{c}"])
                    for k in range(1, 4):
                        P.V(lambda e, c=c, ch=ch, useg=useg, i0=i0, nn=nn, k=k: e.scalar_tensor_tensor(out=useg, in0=up[:, c, i0 + k:i0 + k + nn], scalar=cw[:, k, ch:ch + 1], in1=useg, op0=ALU.mult, op1=ALU.add),
                            r=[f"up{c}", "cw", f"u{c}"], w=[f"u{c}"])
                P.A(lambda e, c=c: e.activation(out=ub[:, c, :], in_=u[:, c, :], func=AF.Copy), r=[f"u{c}"], w=["ub"])
            for d in range(2):
                for ty in range(2):
                    wsrc = (I["rec_w_a"] if ty == 0 else I["rec_w_x"])[d, n].rearrange("(k p) o -> p k o", p=128)
                    P.dma("gpsimd", wg[:, d, ty, :, :], wsrc, r=[], w=["wg"], chan=f"wg{d}{ty}")
            for oc in range(2):
                ch = 2 * n + oc
                for d in range(2):
                    for tb in range(9):
                        t0 = tb * 512
                        tn = min(512, NTOK - t0)
                        sl = tb % 2
                        pa, px = 2 * sl, 2 * sl + 1
                        r_, i_, a2, sq = tmp[sl]
                        for ty, pb in ((0, pa), (1, px)):
                            for kc in range(2):
                                P.T(lambda e, ty=ty, pb=pb, kc=kc, d=d, oc=oc, t0=t0, tn=tn: e.matmul(ps[pb][:, 0:tn], lhsT=wg[:, d, ty, kc, oc * 128:(oc + 1) * 128], rhs=ub[:, kc, t0:t0 + tn], start=(kc == 0), stop=(kc == 1)),
                                    r=["wg", "ub"], w=[f"ps{pb}"])
                        P.A(lambda e, pa=pa, tn=tn, d=d, ch=ch, r_=r_: e.activation(out=r_[:, 0:tn], in_=ps[pa][:, 0:tn], func=AF.Exp, scale=-1.0, bias=nba[:, d, ch:ch + 1]), r=[f"ps{pa}", "nba"], w=[f"t0{sl}"])
                        P.A(lambda e, px=px, tn=tn, d=d, ch=ch, i_=i_: e.activation(out=i_[:, 0:tn], in_=ps[px][:, 0:tn], func=AF.Exp, scale=-1.0, bias=nbx[:, d, ch:ch + 1]), r=[f"ps{px}", "nbx"], w=[f"t1{sl}"])
                        P.A(lambda e, tn=tn, r_=r_: e.activation(out=r_[:, 0:tn], in_=r_[:, 0:tn], func=AF.Ln, bias=1.0), r=[f"t0{sl}"], w=[f"t0{sl}"])
                        P.A(lambda e, tn=tn, r_=r_: e.activation(out=r_[:, 0:tn], in_=r_[:, 0:tn], func=AF.Exp, scale=-1.0), r=[f"t0{sl}"], w=[f"t0{sl}"])
                        P.A(lambda e, tn=tn, i_=i_: e.activation(out=i_[:, 0:tn], in_=i_[:, 0:tn], func=AF.Ln, bias=1.0), r=[f"t1{sl}"], w=[f"t1{sl}"])
                        P.A(lambda e, tn=tn, i_=i_: e.activation(out=i_[:, 0:tn], in_=i_[:, 0:tn], func=AF.Exp, scale=-1.0), r=[f"t1{sl}"], w=[f"t1{sl}"])
                        P.A(lambda e, tn=tn, t0=t0, d=d, ch=ch, r_=r_: e.activation(out=A[:, t0:t0 + tn], in_=r_[:, 0:tn], func=AF.Exp, scale=cl[:, d, ch:ch + 1]), r=[f"t0{sl}", "cl"], w=["A"])
                        P.G(lambda e, tn=tn, t0=t0, a2=a2: e.tensor_tensor(out=a2[:, 0:tn], in0=A[:, t0:t0 + tn], in1=A[:, t0:t0 + tn], op=ALU.mult), r=["A"], w=[f"t2{sl}"])
                        P.A(lambda e, tn=tn, a2=a2, sq=sq: e.activation(out=sq[:, 0:tn], in_=a2[:, 0:tn], func=AF.Ln, scale=-1.0, bias=1.0), r=[f"t2{sl}"], w=[f"t3{sl}"])
                        P.A(lambda e, tn=tn, sq=sq: e.activation(out=sq[:, 0:tn], in_=sq[:, 0:tn], func=AF.Exp, scale=0.5), r=[f"t3{sl}"], w=[f"t3{sl}"])
                        P.V(lambda e, tn=tn, sq=sq, i_=i_: e.tensor_tensor(out=sq[:, 0:tn], in0=sq[:, 0:tn], in1=i_[:, 0:tn], op=ALU.mult), r=[f"t3{sl}", f"t1{sl}"], w=[f"t3{sl}"])
                        P.V(lambda e, tn=tn, t0=t0, sq=sq, oc=oc: e.tensor_tensor(out=Bt[:, t0:t0 + tn], in0=sq[:, 0:tn], in1=u[:, oc, t0:t0 + tn], op=ALU.mult), r=[f"t3{sl}", f"u{oc}"], w=["B"])
                    Yd = Y[d]
                    if d == 0:
                        P.V(lambda e, Yd=Yd: e.tensor_tensor_scan(out=Yd[:, 0:256], data0=A[:, 0:256], data1=Bt[:, 0:256], initial=0.0, op0=ALU.mult, op1=ALU.add), r=["A", "B"], w=[f"Y{d}c"])
                        P.V(lambda e, Yd=Yd: e.tensor_tensor_scan(out=Yd[:, 256:NTOK], data0=A[:, 256:NTOK], data1=Bt[:, 256:NTOK], initial=Yd[:, 255:256], op0=ALU.mult, op1=ALU.add), r=["A", "B", f"Y{d}c"], w=[f"Y{d}l"])
                    else:
                        P.V(lambda e, Yd=Yd: e.tensor_tensor_scan(out=Yd[:, 0:256][:, ::-1], data0=A[:, 0:256][:, ::-1], data1=Bt[:, 0:256][:, ::-1], initial=0.0, op0=ALU.mult, op1=ALU.add), r=["A", "B"], w=[f"Y{d}c"])
                        P.V(lambda e, Yd=Yd: e.tensor_tensor_scan(out=Yd[:, 256:NTOK][:, ::-1], data0=A[:, 256:NTOK][:, ::-1], data1=Bt[:, 256:NTOK][:, ::-1], initial=Yd[:, 0:1], op0=ALU.mult, op1=ALU.add), r=["A", "B", f"Y{d}c"], w=[f"Y{d}l"])
                P.dma("sync", gg[:], K.ggd[ch], r=[], w=["gg"], chan="gg")
                P.G(lambda e: e.tensor_tensor(out=Y[0][:, 256:NTOK], in0=Y[0][:, 256:NTOK], in1=Y[1][:, 256:NTOK], op=ALU.add), r=["Y0l", "Y1l"], w=["Y0l"])
                P.V(lambda e: e.tensor_tensor(out=ygb[:], in0=Y[0][:, 256:NTOK], in1=gg[:], op=ALU.mult), r=["Y0l", "gg"], w=["ygb"])
                P.dma("sync", K.ygd[:, ch, :], ygb[:], r=["ygb"], w=["ygd"], chan="ygb")
        P.emit()
    nc.all_engine_barrier()
    with ExitStack() as es:
        sb = lambda n, s, d=F32: es.enter_context(nc.sbuf_tensor(n, s, d))
        Wout = sb("r_Wout", [128, KC, D], BF16)
        gate = sb("r_gate", [128, D])
        yt = [sb(f"r_yt{i}", [128, KC, 128], BF16) for i in range(2)]
        xr = [sb(f"r_xr{i}", [128, D]) for i in range(2)]
        y = sb("r_y", [128, D])
        P = Prog(nc, "rout")
        load_w_bf16(P, Wout, I["rec_w_out"], D, "Wo")
        load_bcast(P, "sync", gate[:], K.modd[1, 0, 2 * D:3 * D], "gate", "gate")
        for t in range(S // 128):
            s = t % 2
            P.dma("sync", yt[s][:], K.ygd[:, :, t * 128:(t + 1) * 128], r=[], w=[f"yt{s}"], chan=f"yt{s}")
            P.dma("sync", xr[s][:], K.xs2tile(t + 2), r=[], w=[f"xr{s}"], chan=f"xr{s}")
            for nb in range(4):
                pb = 4 + nb
                for kc in range(KC):
                    P.T(lambda e, kc=kc, nb=nb, pb=pb, s=s: e.matmul(ps[pb][:], lhsT=yt[s][:, kc, :], rhs=Wout[:, kc, nb * 512:(nb + 1) * 512], start=(kc == 0), stop=(kc == KC - 1)),
                        r=[f"yt{s}", "Wo"], w=[f"ps{pb}"])
                P.V(lambda e, nb=nb, pb=pb: e.tensor_tensor(out=y[:, nb * 512:(nb + 1) * 512], in0=ps[pb][:], in1=gate[:, nb * 512:(nb + 1) * 512], op=ALU.mult), r=[f"ps{pb}", "gate"], w=[f"y{nb}"])
                P.G(lambda e, nb=nb, s=s: e.tensor_tensor(out=y[:, nb * 512:(nb + 1) * 512], in0=y[:, nb * 512:(nb + 1) * 512], in1=xr[s][:, nb * 512:(nb + 1) * 512], op=ALU.add), r=[f"y{nb}", f"xr{s}"], w=[f"y{nb}"])
            P.dma("sync", K.xs3[t * 128:(t + 1) * 128, :], y[:], r=[f"y{nb}" for nb in range(4)], w=["xs3"], chan="y")
        P.emit()
```

```python
import os
import numpy as np
import ml_dtypes
from contextlib import ExitStack
import concourse.bass as bass
import concourse.mybir as mybir
from concourse.bass_utils import run_bass_kernel_spmd

F32 = mybir.dt.float32
BF16 = mybir.dt.bfloat16
U32 = mybir.dt.uint32
ALU = mybir.AluOpType
AF = mybir.ActivationFunctionType
AX = mybir.AxisListType

D = 2048
KC = 16
S = 4096
C = 256
NT = (S + C) // 128
EPS = 1e-6
COMPUTE = ("tensor", "vector", "scalar", "gpsimd")


class Prog:
    def __init__(self, nc, name="p"):
        self.nc = nc
        self.name = name
        self.ops = []
        self.last_w = {}
        self.readers = {}

    def op(self, eng, fn, r=(), w=(), dma=False, chan=None):
        i = len(self.ops)
        deps = set()
        for k in r:
            lw = self.last_w.get(k)
            if lw is not None:
                deps.add(lw)
        for k in w:
            lw = self.last_w.get(k)
            if lw is not None:
                deps.add(lw)
            rd = self.readers.get(k)
            if rd:
                deps.update(rd.values())
        for k in w:
            self.last_w[k] = i
            self.readers[k] = {}
        for k in r:
            d = self.readers.setdefault(k, {})
            d[("dma", i) if dma else eng] = i
        if dma:
            assert chan is not None
        self.ops.append(dict(eng=eng, fn=fn, deps=deps, dma=dma, chan=chan))
        return i

    def dma(self, eng, out, in_, r, w, chan, **kw):
        return self.op(eng, lambda e: e.dma_start(out=out, in_=in_, **kw), r=r, w=w, dma=True, chan=chan)

    def V(self, fn, r, w):
        return self.op("vector", fn, r, w)

    def A(self, fn, r, w):
        return self.op("scalar", fn, r, w)

    def G(self, fn, r, w):
        return self.op("gpsimd", fn, r, w)

    def T(self, fn, r, w):
        return self.op("tensor", fn, r, w)

    def emit(self):
        nc = self.nc
        ops = self.ops
        needed = set()
        for o in ops:
            for d in o["deps"]:
                od = ops[d]
                if od["dma"]:
                    continue
                if od["eng"] == "tensor" and o["eng"] == "tensor" and not o["dma"]:
                    continue
                needed.add(d)
        cnt = {e: 0 for e in COMPUTE + ("sync",)}
        chan_cnt = {}
        for i, o in enumerate(ops):
            if o["dma"]:
                c = o["chan"]
                chan_cnt[c] = chan_cnt.get(c, 0) + 16
                o["sig"] = ("c", c, chan_cnt[c])
            elif i in needed:
                cnt[o["eng"]] += 1
                o["sig"] = ("e", o["eng"], cnt[o["eng"]])
            else:
                o["sig"] = None
        self.stats = dict(cnt=dict(cnt), chans=len(chan_cnt), maxchan=max(chan_cnt.values()) if chan_cnt else 0, nops=len(ops))
        esem = {e: nc.alloc_semaphore(name=f"{self.name}_e_{e}") for e in COMPUTE if cnt[e] > 0}
        csem = {c: nc.alloc_semaphore(name=f"{self.name}_c_{c}") for c in chan_cnt}
        with ExitStack() as es:
            block = es.enter_context(nc.Block())
            by_eng = {}
            for i, o in enumerate(ops):
                by_eng.setdefault(o["eng"], []).append(i)

            def make(engname, idxs):
                def body(eng):
                    waited = {}
                    for i in idxs:
                        o = ops[i]
                        for d in sorted(o["deps"]):
                            od = ops[d]
                            sig = od["sig"]
                            if sig is None:
                                continue
                            if sig[0] == "e":
                                if od["eng"] == "tensor" and engname == "tensor" and not o["dma"]:
                                    continue
                                sem = esem[sig[1]]
                            else:
                                sem = csem[sig[1]]
                            key = (sig[0], sig[1])
                            if waited.get(key, 0) >= sig[2]:
                                continue
                            eng.wait_ge(sem, sig[2])
                            waited[key] = sig[2]
                        ins = o["fn"](eng)
                        sig = o["sig"]
                        if sig is not None:
                            if sig[0] == "e":
                                ins.then_inc(esem[sig[1]], 1)
                            else:
                                ins.then_inc(csem[sig[1]], 16)
                    last = {}
                    for i in idxs:
                        o = ops[i]
                        if o["dma"]:
                            last[o["chan"]] = o["sig"][2]
                    for c, v in last.items():
                        if waited.get(("c", c), 0) < v:
                            eng.wait_ge(csem[c], v)
                return body

            for engname, idxs in by_eng.items():
                getattr(block, engname)(make(engname, idxs))
        nc.all_engine_barrier()
        nc.clear_and_free_semaphores(list(esem.values()) + list(csem.values()))
        nc.all_engine_barrier()
        self.ops = []
        self.last_w = {}
        self.readers = {}


def _consts():
    ident = np.eye(128, dtype=np.float32)
    rotm = np.zeros((128, 128), np.float32)
    for m in range(128):
        base = 0 if m < 64 else 64
        d = m - base
        if d < 32:
            rotm[base + d + 32, m] = -1.0
        else:
            rotm[base + d - 32, m] = 1.0
    t = np.arange(S)
    row = (t // 64).astype(np.float32)
    col = (t % 64).astype(np.float32)
    inv = (np.float32(10000.0) ** (-np.arange(0, 64, 2, dtype=np.float32) / np.float32(64))).astype(np.float32)
    ang_r = row[:, None] * inv[None, :]
    ang_c = col[:, None] * inv[None, :]
    ang = np.concatenate([ang_r, ang_r, ang_c, ang_c], axis=-1).astype(np.float32)
    cosT = np.ascontiguousarray(np.cos(ang).astype(np.float32).T)
    sinT = np.ascontiguousarray(np.sin(ang).astype(np.float32).T)
    k = np.arange(128)[:, None]
    q = np.arange(128)[None, :]
    maskL = (k >= q).astype(ml_dtypes.bfloat16)
    maskR = (k <= q).astype(ml_dtypes.bfloat16)
    iota16 = np.tile(np.arange(16, dtype=np.float32)[None, :], (128, 1))
    return dict(ident=ident, rotm=rotm, cosT=cosT, sinT=sinT, maskL=maskL, maskR=maskR, iota16=iota16)


class Ctx:
    pass


def build(stop_after=99, debug=False, peer_tiles=None, start_at=0, pair_split=False):
    nc = bass.Bass("TRN2", target_bir_lowering=False)
    K = Ctx()
    K.nc = nc
    K._uid = [0]

    def uid():
        K._uid[0] += 1
        return K._uid[0]
    K.uid = uid
    din = lambda n, s, d=F32: nc.dram_tensor(n, list(s), d, kind="ExternalInput").ap()
    dscr = lambda n, s, d=F32: nc.dram_tensor(n, list(s), d, kind="Internal").ap()
    I = dict(
        x=din("x", [S, D]), c=din("c", [D]), ctx=din("ctx", [C, D]), c_ctx=din("c_ctx", [D]),
        w_mod=din("w_mod", [2, D, 6 * D]), b_mod=din("b_mod", [2, 6 * D]),
        norm_mix=din("norm_mix", [2, D]), norm_ffn=din("norm_ffn", [2, D]), norm_final=din("norm_final", [D]),
        attn_w_qkv=din("attn_w_qkv", [D, 3072]), attn_w_o=din("attn_w_o", [D, D]), attn_sink=din("attn_sink", [16]),
        rec_w_in=din("rec_w_in", [D, 2 * D]), rec_conv_w=din("rec_conv_w", [4, D]), rec_conv_b=din("rec_conv_b", [D]),
        rec_w_a=din("rec_w_a", [2, 8, 256, 256]), rec_b_a=din("rec_b_a", [2, D]),
        rec_w_x=din("rec_w_x", [2, 8, 256, 256]), rec_b_x=din("rec_b_x", [2, D]),
        rec_lambda=din("rec_lambda", [2, D]), rec_w_out=din("rec_w_out", [D, D]),
        peer_w_q=din("peer_w_q", [2, D, D]), peer_keys=din("peer_keys", [2, 2, 128, 128]),
        peer_u=din("peer_u", [2, 16384, D]), peer_v=din("peer_v", [2, 16384, D]),
        ident=din("ident", [128, 128]), rotm=din("rotm", [128, 128]), cosT=din("cosT", [128, S]), sinT=din("sinT", [128, S]),
        maskL=din("maskL", [128, 128], BF16), maskR=din("maskR", [128, 128], BF16), iota16=din("iota16", [128, 16]),
        myrows=din("myrows", [128, 16], U32),
        rows0=din("rows0", [128, 17], U32),
    )
    K.I = I
    out = nc.dram_tensor("out", [S // 2, D], F32, kind="ExternalOutput").ap()
    K.out = out
    K.modd = dscr("modd", [2, 2, 6 * D])
    K.qd = dscr("qd", [128, 16, NT * 128], BF16)
    K.kd = dscr("kd", [128, 4, NT * 128], BF16)
    K.vd = dscr("vd", [NT * 128, 512], BF16)
    K.xs1 = dscr("xs1", [NT * 128, D])
    K.xs2 = dscr("xs2", [NT * 128, D]) if start_at < 3 else din("xs2", [NT * 128, D])
    K.xs3 = dscr("xs3", [S, D]) if start_at < 4 else din("xs3", [S, D])
    K.upre = dscr("upre", [16, 128, NT * 128])
    K.ggd = dscr("ggd", [16, 128, S])
    K.ygd = dscr("ygd", [128, 16, S], BF16)
    K.pair_split = pair_split
    if pair_split:
        K.xs2loc = dscr("xs2loc", [17 * 128, D])
        K.xs2all = dscr("xs2all", [2 * 17 * 128, D])

    def xs2tile(e):
        if not pair_split:
            return K.xs2[e * 128:(e + 1) * 128, :]
        if e < 2:
            r0 = (e * 17) * 128
        else:
            i = e - 2
            r0 = ((i // 16) * 17 + 1 + i % 16) * 128
        return K.xs2all[r0:r0 + 128, :]
    K.xs2tile = xs2tile
    K.uvb16 = dscr("uvb16", [2 * 16384, 2 * D], BF16)
    dbg = {}
    if debug:
        dbg["d_modd"] = nc.dram_tensor("d_modd", [2, 2, 6 * D], F32, kind="ExternalOutput").ap()
        dbg["d_xs1"] = nc.dram_tensor("d_xs1", [NT * 128, D], F32, kind="ExternalOutput").ap()
        dbg["d_xs2"] = nc.dram_tensor("d_xs2", [NT * 128, D], F32, kind="ExternalOutput").ap()
        dbg["d_xs3"] = nc.dram_tensor("d_xs3", [S, D], F32, kind="ExternalOutput").ap()
    K.dbg = dbg

    with ExitStack() as es:
        K.ps = [es.enter_context(nc.psum_tensor(f"ps{i}", [128, 512], F32)) for i in range(8)]
        K.ident = es.enter_context(nc.sbuf_tensor("identsb", [128, 128], F32))
        P = Prog(nc, "c0")
        P.dma("sync", K.ident[:], I["ident"], r=[], w=["ident"], chan="ident")
        P.emit()
        nc.all_engine_barrier()
        phase_mod(K)
        nc.all_engine_barrier()
        K.tc = [TabConv(K, 0, "a"), TabConv(K, 1, "b")]
        if debug:
            copy_dram(K, K.modd.rearrange("l v n -> (l v) n"), dbg["d_modd"].rearrange("l v n -> (l v) n"), 4, "cpm")
        if stop_after >= 1 and start_at <= 1:
            phase_attn(K)
            nc.all_engine_barrier()
            if debug:
                copy_dram(K, K.xs1, dbg["d_xs1"], NT * 128, "cpx1")
        if stop_after >= 2 and start_at <= 2:
            if pair_split:
                tl = [dict(src=("g", K.xs1, j), v=1 if j == 0 else 0, dst=K.xs2loc[j * 128:(j + 1) * 128, :]) for j in range(17)]
                phase_peer(K, 0, tl, False, "pe0", idxname="rows0")
                nc.all_engine_barrier()
                P = Prog(nc, "cc")
                P.op("gpsimd", lambda e: e.collective_compute("AllGather", ALU.bypass, replica_groups=[[0, 1], [2, 3], [4, 5], [6, 7]],
                                                              ins=[K.xs2loc], outs=[K.xs2all]), r=[], w=[], dma=True, chan="cc")
                P.emit()
            else:
                tl = [dict(src=("d", K.xs1[n * 128:(n + 1) * 128, :]), v=1 if n < 2 else 0, dst=K.xs2[n * 128:(n + 1) * 128, :]) for n in range(NT)]
                if peer_tiles is not None:
                    tl = [tl[i] for i in peer_tiles]
                phase_peer(K, 0, tl, False, "pe0")
            nc.all_engine_barrier()
            if debug:
                copy_dram(K, K.xs2, dbg["d_xs2"], NT * 128, "cpx2")
        if stop_after >= 3 and start_at <= 3:
            phase_rec(K)
            nc.all_engine_barrier()
            if debug:
                copy_dram(K, K.xs3, dbg["d_xs3"], S, "cpx3")
        if stop_after >= 4:
            tl = [dict(src=("g", K.xs3, t), v=0, dst=K.out[t * 128:(t + 1) * 128, :]) for t in range(S // 256)]
            if peer_tiles is not None:
                tl = tl[:len(peer_tiles)]
            phase_peer(K, 1, tl, True, "pe1")
    return nc


def copy_dram(K, src, dst, rows, name):
    nc = K.nc
    with ExitStack() as es:
        t = es.enter_context(nc.sbuf_tensor(name + "_t", [128, src.shape[1]], src.dtype))
        P = Prog(nc, name)
        for r0 in range(0, rows, 128):
            n = min(128, rows - r0)
            P.dma("sync", t[0:n, :], src[r0:r0 + n, :], r=[], w=["t"], chan="ld")
            P.dma("sync", dst[r0:r0 + n, :], t[0:n, :], r=["t"], w=[], chan="st")
        P.emit()
    nc.all_engine_barrier()


def phase_mod(K):
    nc, I = K.nc, K.I
    with ExitStack() as es:
        sb = lambda n, s, d=F32: es.enter_context(nc.sbuf_tensor(n, s, d))
        cc = sb("m_cc", [128, KC, 2])
        craw = sb("m_craw", [128, 2, KC])
        wt = [sb(f"m_wt{i}", [128, KC, 512]) for i in range(2)]
        bt = sb("m_bt", [2, 512])
        ot = [sb(f"m_ot{i}", [2, 512]) for i in range(2)]
        P = Prog(nc, "mod")
        P.dma("sync", craw[:, 0, :], I["c"].rearrange("(k p) -> p k", p=128), r=[], w=["craw0"], chan="craw0", allow_slow_non_contiguous=True)
        P.dma("sync", craw[:, 1, :], I["c_ctx"].rearrange("(k p) -> p k", p=128), r=[], w=["craw1"], chan="craw1", allow_slow_non_contiguous=True)
        for v in range(2):
            P.A(lambda e, v=v: e.activation(out=cc[:, :, v], in_=craw[:, v, :], func=AF.Silu), r=[f"craw{v}"], w=["cc"])
        n = 0
        for l in range(2):
            for j in range(24):
                s = n % 2
                w_src = I["w_mod"][l].rearrange("(k p) n -> p k n", p=128)[:, :, j * 512:(j + 1) * 512]
                P.dma("sync" if s == 0 else "gpsimd", wt[s][:], w_src, r=[], w=[f"wt{s}"], chan=f"wt{s}")
                for v in range(2):
                    P.dma("sync", bt[v:v + 1, :], I["b_mod"][l:l + 1, j * 512:(j + 1) * 512], r=[], w=["bt"], chan=f"bt{v}")
                pb = K.ps[s]
                for kc in range(KC):
                    P.T(lambda e, kc=kc, s=s, pb=pb: e.matmul(pb[0:2, :], lhsT=cc[:, kc, :], rhs=wt[s][:, kc, :], start=(kc == 0), stop=(kc == KC - 1)),
                        r=["cc", f"wt{s}"], w=[f"ps{s}"])
                P.V(lambda e, s=s, pb=pb: e.tensor_tensor(out=ot[s][:], in0=pb[0:2, :], in1=bt[:], op=ALU.add), r=[f"ps{s}", "bt"], w=[f"ot{s}"])
                P.dma("sync", K.modd[l, :, j * 512:(j + 1) * 512], ot[s][:], r=[f"ot{s}"], w=[], chan=f"ot{s}")
                n += 1
        P.emit()


def load_bcast(P, eng, tile_ap, src_row_ap, key, chan):
    P.dma(eng, tile_ap, src_row_ap.partition_broadcast(128), r=[], w=[key], chan=chan)


def prep_GS(K, P, Gt, St, gain_row, scale_row, shift_row, tmp, tag):
    load_bcast(P, "sync", Gt[:], scale_row, f"G{tag}", f"G{tag}")
    load_bcast(P, "sync", tmp[:], gain_row, "gstmp", "gstmp")
    load_bcast(P, "sync", St[:], shift_row, f"S{tag}", f"S{tag}")
    P.V(lambda e: e.scalar_tensor_tensor(out=Gt[:], in0=Gt[:], scalar=1.0, in1=tmp[:], op0=ALU.add, op1=ALU.mult), r=[f"G{tag}", "gstmp"], w=[f"G{tag}"])


def norm_mod_T(K, P, src_ap, xt, h, ss, Gt, St, gtag, hT, hT_key, tok0, tpb, xslot):
    ps = K.ps
    P.dma("sync", xt[:], src_ap, r=[], w=[f"xt{xslot}"], chan=f"xt{xslot}")
    P.A(lambda e: e.activation(out=h[:], in_=xt[:], func=AF.Square, accum_out=ss[:, 0:1]), r=[f"xt{xslot}"], w=["h", "ss"])
    P.V(lambda e: e.tensor_scalar(out=ss[:, 1:2], in0=ss[:, 0:1], scalar1=1.0 / D, scalar2=EPS, op0=ALU.mult, op1=ALU.add), r=["ss"], w=["ss1"])
    P.A(lambda e: e.activation(out=ss[:, 2:3], in_=ss[:, 1:2], func=AF.Sqrt), r=["ss1"], w=["ss2"])
    P.V(lambda e: e.reciprocal(out=ss[:, 3:4], in_=ss[:, 2:3]), r=["ss2"], w=["ss3"])
    P.V(lambda e: e.scalar_tensor_tensor(out=h[:], in0=xt[:], scalar=ss[:, 3:4], in1=Gt[:], op0=ALU.mult, op1=ALU.mult),
        r=[f"xt{xslot}", "ss3", f"G{gtag}"], w=["h"])
    P.V(lambda e: e.tensor_tensor(out=h[:], in0=h[:], in1=St[:], op=ALU.add), r=["h", f"S{gtag}"], w=["h"])
    if hT is None:
        return
    for q in range(4):
        b = tpb[q % 2]
        for j in range(4):
            kc = 4 * q + j
            P.T(lambda e, kc=kc, j=j, b=b: e.transpose(out=ps[b][:, j * 128:(j + 1) * 128], in_=h[:, kc * 128:(kc + 1) * 128], identity=K.ident[:]),
                r=["h", "ident"], w=[f"ps{b}"])
        P.A(lambda e, q=q, b=b: e.activation(out=hT[:, 4 * q:4 * q + 4, tok0:tok0 + 128], in_=ps[b][:].rearrange("p (j t) -> p j t", j=4), func=AF.Copy),
            r=[f"ps{b}"], w=[hT_key])


def load_w_bf16(P, wsb, w_dram, ncols, key, col0=0):
    for kc in range(KC):
        for c0 in range(0, ncols, 1024):
            cn = min(1024, ncols - c0)
            P.dma("gpsimd", wsb[:, kc, c0:c0 + cn], w_dram[kc * 128:(kc + 1) * 128, col0 + c0:col0 + c0 + cn], r=[], w=[key], chan=f"{key}_{kc % 4}")


def phase_attn(K):
    nc, I, ps = K.nc, K.I, K.ps
    SCALE = 128 ** -0.5
    with ExitStack() as es:
        sb = lambda n, s, d=F32: es.enter_context(nc.sbuf_tensor(n, s, d))
        with ExitStack() as es1:
            sb1 = lambda n, s, d=F32: es1.enter_context(nc.sbuf_tensor(n, s, d))
            W = sb1("a_W", [128, KC, 3072], BF16)
            Gt = sb1("a_G", [128, D]); St = sb1("a_S", [128, D])
            ss = sb1("a_ss", [128, 4])
            xt = sb1("a_xt", [128, D]); h = sb1("a_h", [128, D])
            hT = sb1("a_hT", [128, KC, 256], BF16)
            rotm = sb1("a_rotm", [128, 128])
            cs = sb1("a_cos", [128, 256]); sn = sb1("a_sin", [128, 256])
            qsb = [sb1(f"a_qsb{i}", [128, 256]) for i in range(2)]
            t1 = [sb1(f"a_t1{i}", [128, 256]) for i in range(2)]
            t2 = [sb1(f"a_t2{i}", [128, 256]) for i in range(2)]
            qst = sb1("a_qst", [128, 20, 256], BF16)
            vst = sb1("a_vst", [128, 2, 512], BF16)
            P = Prog(nc, "qkv")
            tc = K.tc[0]
            tc.alloc(es1, 12)
            tc.begin()
            load_w_bf16(P, W, I["attn_w_qkv"], 3072, "W")
            P.dma("sync", rotm[:], I["rotm"], r=[], w=["rotm"], chan="rotm")
            for blk in range(NT // 2):
                is_ctx = blk == 0
                if blk in (0, 1):
                    v = 1 if is_ctx else 0
                    prep_GS(K, P, Gt, St, I["norm_mix"][0], K.modd[0, v, D:2 * D], K.modd[0, v, 0:D], h, "a")
                for ti in range(2):
                    e_t = blk * 2 + ti
                    src = I["ctx"][e_t * 128:(e_t + 1) * 128, :] if is_ctx else I["x"][(e_t - 2) * 128:(e_t - 1) * 128, :]
                    norm_mod_T(K, P, src, xt, h, ss, Gt, St, "a", hT, "hT", ti * 128, (6, 7), 0)
                if not is_ctx:
                    l0 = (blk - 1) * 256
                    P.dma("sync", cs[:], I["cosT"][:, l0:l0 + 256], r=[], w=["cos"], chan="cos")
                    P.dma("sync", sn[:], I["sinT"][:, l0:l0 + 256], r=[], w=["sin"], chan="sin")
                for j in range(20):
                    pb = j % 2
                    for kc in range(KC):
                        P.T(lambda e, kc=kc, j=j, pb=pb: e.matmul(ps[pb][:, 0:256], lhsT=W[:, kc, j * 128:(j + 1) * 128], rhs=hT[:, kc, :], start=(kc == 0), stop=(kc == KC - 1)),
                            r=["W", "hT"], w=[f"ps{pb}"])
                    dst, dkey = qst[:, j, :], "qst"
                    if is_ctx:
                        P.A(lambda e, pb=pb, dst=dst: e.activation(out=dst, in_=ps[pb][:, 0:256], func=AF.Copy), r=[f"ps{pb}"], w=[dkey])
                    else:
                        s2 = j % 2
                        P.A(lambda e, pb=pb, s2=s2: e.activation(out=qsb[s2][:], in_=ps[pb][:, 0:256], func=AF.Copy), r=[f"ps{pb}"], w=[f"qsb{s2}"])
                        P.T(lambda e, s2=s2: e.matmul(ps[2 + s2][:, 0:256], lhsT=rotm[:], rhs=qsb[s2][:], start=True, stop=True), r=["rotm", f"qsb{s2}"], w=[f"ps{2 + s2}"])
                        P.V(lambda e, s2=s2: e.tensor_tensor(out=t1[s2][:], in0=qsb[s2][:], in1=cs[:], op=ALU.mult), r=[f"qsb{s2}", "cos"], w=[f"t1{s2}"])
                        P.V(lambda e, s2=s2: e.tensor_tensor(out=t2[s2][:], in0=ps[2 + s2][:, 0:256], in1=sn[:], op=ALU.mult), r=[f"ps{2 + s2}", "sin"], w=[f"t2{s2}"])
                        P.V(lambda e, s2=s2, dst=dst: e.tensor_tensor(out=dst, in0=t1[s2][:], in1=t2[s2][:], op=ALU.add), r=[f"t1{s2}", f"t2{s2}"], w=[dkey])
                tc.pump(P, 6)
                P.dma("sync", K.qd[:, :, blk * 256:(blk + 1) * 256], qst[:, 0:16, :], r=["qst"], w=["qd"], chan="qst")
                P.dma("sync", K.kd[:, :, blk * 256:(blk + 1) * 256], qst[:, 16:20, :], r=["qst"], w=["kd"], chan="kst")
                for ti in range(2):
                    e_t = blk * 2 + ti
                    pb = 4 + ti
                    for kc in range(KC):
                        P.T(lambda e, kc=kc, ti=ti, pb=pb: e.matmul(ps[pb][:], lhsT=hT[:, kc, ti * 128:(ti + 1) * 128], rhs=W[:, kc, 2560:3072], start=(kc == 0), stop=(kc == KC - 1)),
                            r=["W", "hT"], w=[f"ps{pb}"])
                    P.A(lambda e, ti=ti, pb=pb: e.activation(out=vst[:, ti, :], in_=ps[pb][:], func=AF.Copy), r=[f"ps{pb}"], w=[f"vst{ti}"])
                    P.dma("sync", K.vd[e_t * 128:(e_t + 1) * 128, :], vst[:, ti, :], r=[f"vst{ti}"], w=["vd"], chan=f"vst{ti}")
            tc.flush(P)
            P.emit()
        nc.all_engine_barrier()
        with ExitStack() as es2:
            sb2 = lambda n, s, d=F32: es2.enter_context(nc.sbuf_tensor(n, s, d))
            Wo = sb2("a_Wo", [128, KC, D], BF16)
            kT = sb2("a_kT", [128, 4, NT * 128], BF16)
            vx = sb2("a_vx", [128, NT, 4, 129], BF16)
            gate = sb2("a_gate", [128, D])
            esink = sb2("a_esink", [128, 16])
            qt = [sb2(f"a_qt{i}", [128, 16, 128], BF16) for i in range(2)]
            E = [sb2(f"a_E{i}", [128, 5, 512], BF16) for i in range(2)]
            mL = sb2("a_mL", [128, 128], BF16); mR = sb2("a_mR", [128, 128], BF16)
            o = sb2("a_o", [128, D]); oT = sb2("a_oT", [128, KC, 128], BF16)
            den = sb2("a_den", [128, 8])
            xt = [sb2(f"a_xr{i}", [128, D]) for i in range(1)]
            y = sb2("a_y", [128, D])
            sraw = sb2("a_sraw", [128, 16])
            P = Prog(nc, "att")
            tc = K.tc[0]
            tc.alloc(es2, 4)
            tc.begin()
            load_w_bf16(P, Wo, I["attn_w_o"], D, "Wo")
            P.G(lambda e: e.memset(vx[:, :, :, 128:129], 1.0), r=[], w=["vx1"])
            for hh in range(4):
                P.dma("sync", kT[:, hh, :], K.kd[:, hh, :], r=[], w=["kT"], chan=f"kTl{hh}")
            for t in range(NT):
                P.dma("sync", vx[:, t, :, 0:128], K.vd[t * 128:(t + 1) * 128, :].rearrange("p (g d) -> p g d", g=4), r=[], w=["vx"], chan=f"vxl{t % 4}")
            P.dma("sync", mL[:], I["maskL"], r=[], w=["mL"], chan="mL")
            P.dma("sync", mR[:], I["maskR"], r=[], w=["mR"], chan="mR")
            load_bcast(P, "sync", sraw[:], I["attn_sink"], "sraw", "sraw")
            P.A(lambda e: e.activation(out=esink[:], in_=sraw[:], func=AF.Exp), r=["sraw"], w=["esink"])
            for e_t in range(NT):
                is_ctx = e_t < 2
                if e_t in (0, 2):
                    v = 1 if is_ctx else 0
                    load_bcast(P, "sync", gate[:], K.modd[0, v, 2 * D:3 * D], "gate", "gate")
                qs = e_t % 2
                P.dma("sync", qt[qs][:], K.qd[:, :, e_t * 128:(e_t + 1) * 128], r=["qd"], w=[f"qt{qs}"], chan=f"qt{qs}")
                src = I["ctx"][e_t * 128:(e_t + 1) * 128, :] if is_ctx else I["x"][(e_t - 2) * 128:(e_t - 1) * 128, :]
                P.dma("sync", xt[0][:], src, r=[], w=["xr0"], chan="xr0")
                if is_ctx:
                    kbs = [0, 1]
                else:
                    kbs = [0, 1] + [kb for kb in (e_t - 1, e_t, e_t + 1) if 2 <= kb < NT]
                for hh in range(4):
                    Es = E[hh % 2]
                    Ek = f"E{hh % 2}"
                    for n, kb in enumerate(kbs):
                        pb = n % 2
                        P.T(lambda e, hh=hh, kb=kb, pb=pb, qs=qs: e.matmul(ps[pb][:], lhsT=kT[:, hh, kb * 128:(kb + 1) * 128],
                                                                      rhs=qt[qs][:, 4 * hh:4 * hh + 4, :].rearrange("p g q -> p (g q)"), start=True, stop=True),
                            r=["kT", f"qt{qs}"], w=[f"ps{pb}"])
                        P.A(lambda e, n=n, pb=pb, Es=Es: e.activation(out=Es[:, n, :], in_=ps[pb][:], func=AF.Exp, scale=SCALE), r=[f"ps{pb}"], w=[f"{Ek}_{n}"])
                        if (not is_ctx) and kb >= 2 and kb != e_t:
                            m = mL if kb == e_t - 1 else mR
                            P.V(lambda e, n=n, m=m, Es=Es: e.tensor_tensor(out=Es[:, n, :].rearrange("p (g q) -> p g q", g=4), in0=Es[:, n, :].rearrange("p (g q) -> p g q", g=4),
                                                                        in1=m[:].unsqueeze(1).to_broadcast([128, 4, 128]), op=ALU.mult),
                                r=[f"{Ek}_{n}", "mL", "mR"], w=[f"{Ek}_{n}"])
                    for g in range(4):
                        pb = 2 + g // 2
                        for n, kb in enumerate(kbs):
                            P.T(lambda e, g=g, n=n, kb=kb, pb=pb, hh=hh, Es=Es: e.matmul(ps[pb][:, (g % 2) * 256:(g % 2) * 256 + 129], lhsT=Es[:, n, g * 128:(g + 1) * 128],
                                                                                     rhs=vx[:, kb, hh, :], start=(n == 0), stop=(n == len(kbs) - 1)),
                                r=[f"{Ek}_{n}", "vx", "vx1"], w=[f"ps{pb}"])
                    for bk in range(2):
                        pb = 2 + bk
                        P.V(lambda e, bk=bk, pb=pb, hh=hh: e.tensor_tensor(out=den[:, 2 * bk:2 * bk + 2], in0=ps[pb][:].rearrange("p (g c) -> p g c", g=2)[:, :, 128],
                                                                       in1=esink[:, 4 * hh + 2 * bk:4 * hh + 2 * bk + 2], op=ALU.add),
                            r=[f"ps{pb}", "esink"], w=["den"])
                    P.V(lambda e: e.reciprocal(out=den[:, 4:8], in_=den[:, 0:4]), r=["den"], w=["rden"])
                    for g in range(4):
                        pb = 2 + g // 2
                        hd = 4 * hh + g
                        P.V(lambda e, g=g, pb=pb, hd=hd: e.tensor_scalar(out=o[:, hd * 128:(hd + 1) * 128], in0=ps[pb][:, (g % 2) * 256:(g % 2) * 256 + 128],
                                                                     scalar1=den[:, 4 + g:5 + g], scalar2=None, op0=ALU.mult),
                            r=[f"ps{pb}", "rden"], w=["o"])
                for q in range(4):
                    b = 6 + q % 2
                    for j in range(4):
                        kc = 4 * q + j
                        P.T(lambda e, kc=kc, j=j, b=b: e.transpose(out=ps[b][:, j * 128:(j + 1) * 128], in_=o[:, kc * 128:(kc + 1) * 128], identity=K.ident[:]),
                            r=["o", "ident"], w=[f"ps{b}"])
                    P.A(lambda e, q=q, b=b: e.activation(out=oT[:, 4 * q:4 * q + 4, :], in_=ps[b][:].rearrange("p (j t) -> p j t", j=4), func=AF.Copy), r=[f"ps{b}"], w=["oT"])
                for nb in range(4):
                    pb = 4 + nb
                    for kc in range(KC):
                        P.T(lambda e, kc=kc, nb=nb, pb=pb: e.matmul(ps[pb][:], lhsT=oT[:, kc, :], rhs=Wo[:, kc, nb * 512:(nb + 1) * 512], start=(kc == 0), stop=(kc == KC - 1)),
                            r=["oT", "Wo"], w=[f"ps{pb}"])
                    P.V(lambda e, nb=nb, pb=pb: e.tensor_tensor(out=y[:, nb * 512:(nb + 1) * 512], in0=ps[pb][:], in1=gate[:, nb * 512:(nb + 1) * 512], op=ALU.mult),
                        r=[f"ps{pb}", "gate"], w=[f"y{nb}"])
                    P.V(lambda e, nb=nb, qs=qs: e.tensor_tensor(out=y[:, nb * 512:(nb + 1) * 512], in0=y[:, nb * 512:(nb + 1) * 512], in1=xt[0][:, nb * 512:(nb + 1) * 512], op=ALU.add),
                        r=[f"y{nb}", "xr0"], w=[f"y{nb}"])
                P.dma("sync", K.xs1[e_t * 128:(e_t + 1) * 128, :], y[:], r=[f"y{nb}" for nb in range(4)], w=["xs1"], chan="y")
                tc.pump(P, 5)
            tc.finish(P)
            P.emit()


def _lay(inputs):
    consts = _consts()
    f = lambda a: np.ascontiguousarray(np.asarray(a, dtype=np.float32))
    shared = dict(
        c_ctx=f(inputs["c_ctx"]), w_mod=f(inputs["w_mod"]), b_mod=f(inputs["b_mod"]),
        norm_mix=f(inputs["norm_mix"]), norm_ffn=f(inputs["norm_ffn"]), norm_final=f(inputs["norm_final"]),
        attn_w_qkv=f(inputs["attn_w_qkv"][0]), attn_w_o=f(inputs["attn_w_o"][0]), attn_sink=f(inputs["attn_sink"][0]),
        rec_w_in=f(inputs["rec_w_in"][0]), rec_conv_w=f(inputs["rec_conv_w"][0]), rec_conv_b=f(inputs["rec_conv_b"][0]),
        rec_w_a=f(inputs["rec_w_a"][0]), rec_b_a=f(inputs["rec_b_a"][0]), rec_w_x=f(inputs["rec_w_x"][0]), rec_b_x=f(inputs["rec_b_x"][0]),
        rec_lambda=f(inputs["rec_lambda"][0]), rec_w_out=f(inputs["rec_w_out"][0]),
        peer_w_q=f(inputs["peer_w_q"]), peer_keys=f(inputs["peer_keys"]), peer_u=f(inputs["peer_u"]), peer_v=f(inputs["peer_v"]),
        **consts,
    )
    return shared, f


def core_inputs(inputs, shared, f, core):
    b, hf = core // 2, core % 2
    m = dict(shared)
    m["x"] = f(inputs["x"][b]); m["c"] = f(inputs["c"][b]); m["ctx"] = f(inputs["ctx"][b])
    r0 = np.zeros((128, 17), np.uint32)
    r0[:, 0] = hf * 128 + np.arange(128)
    for j in range(16):
        r0[:, 1 + j] = 256 + (hf * 16 + j) * 128 + np.arange(128)
    m["rows0"] = r0
    m["myrows"] = (hf * (S // 2) + np.arange(16, dtype=np.uint32)[None, :] * 128 + np.arange(128, dtype=np.uint32)[:, None]).astype(np.uint32)
    return m


PAIR_SPLIT = False


def kernel(**inputs):
    nc = build(pair_split=PAIR_SPLIT)
    shared, f = _lay(inputs)
    in_maps = [core_inputs(inputs, shared, f, core) for core in range(8)]
    res = run_bass_kernel_spmd(nc, in_maps, core_ids=list(range(8)))
    outp = np.empty((4, S, D), np.float32)
    for core in range(8):
        b, hf = core // 2, core % 2
        outp[b, hf * (S // 2):(hf + 1) * (S // 2)] = res.results[core]["out"]
    return outp


class TabConv:
    NR = 6
    LAG = 3

    def set_nr(self, nr):
        self.NR, self.LAG = nr, max(1, nr // 2)

    def __init__(self, K, layer, tag):
        self.K, self.layer, self.tag = K, layer, tag
        self.steps = [(tb, r0) for tb in (0, 1) for r0 in range(layer * 16384, (layer + 1) * 16384, 128)]
        self.i = 0
        self.tiles = None

    def alloc(self, es, nr=6):
        self.set_nr(nr)
        if True:
            self.tiles = [es.enter_context(self.K.nc.sbuf_tensor(f"tc{self.tag}_{self.K.uid()}_{i}", [128, D], BF16)) for i in range(self.NR)]

    def _store(self, P, j):
        tb, r0 = self.steps[j]
        sl = j % self.NR
        dst = self.K.uvb16[r0:r0 + 128, tb * D:(tb + 1) * D]
        P.dma("sync", dst, self.tiles[sl][:], r=[f"tc{sl}"], w=[], chan=f"tcs{sl}")

    def pump(self, P, n):
        I = self.K.I
        for _ in range(n):
            if self.i >= len(self.steps):
                break
            j = self.i
            tb, r0 = self.steps[j]
            src = (I["peer_u"] if tb == 0 else I["peer_v"]).rearrange("l e d -> (l e) d")[r0:r0 + 128, :]
            sl = j % self.NR
            P.dma("gpsimd", self.tiles[sl][:], src, r=[], w=[f"tc{sl}"], chan=f"tcl{sl}")
            if j - self.LAG >= self.done:
                self._store(P, j - self.LAG)
            self.i += 1

    def begin(self):
        self.done = self.i

    def flush(self, P):
        for j in range(max(self.done, self.i - self.LAG), self.i):
            self._store(P, j)
        self.done = self.i

    def finish(self, P):
        self.pump(P, len(self.steps))
        self.flush(P)


def phase_peer(K, layer, tiles, final, name, idxname="myrows"):
    nc, I, ps = K.nc, K.I, K.ps
    NB = 7
    uvtab = K.uvb16
    with ExitStack() as es:
        sb = lambda n, s, d=F32: es.enter_context(nc.sbuf_tensor(name + n, s, d))
        Wq = sb("p_Wq", [128, KC, D], BF16)
        Gt = sb("p_G", [128, D]); St = sb("p_S", [128, D])
        gate = sb("p_gate", [128, D])
        xt1 = sb("p_xt", [128, D])
        xt = [xt1, xt1]
        h = [sb(f"p_h{i}", [128, D]) for i in range(2)]
        hT = sb("p_hT", [128, KC, 128], BF16)
        wk = [sb(f"p_wk{i}", [128, D]) for i in range(2)]
        ring = [sb(f"p_ring{i}", [128, 2 * D], BF16) for i in range(NB)]
        junk = sb("p_junk", [128, D], BF16)
        acc = wk[1]
        keysT = sb("p_keysT", [128, 2, 128])
        kraw = sb("p_kraw", [128, 2, 128])
        ss = sb("p_ss", [128, 4]); ssb = sb("p_ssb", [128, 4])
        s16 = sb("p_s16", [128, 16, 16]); i16 = sb("p_i16", [128, 16, 16], U32); i16f = sb("p_i16f", [128, 16, 16])
        ts = sb("p_ts", [128, 8, 16]); sel = sb("p_sel", [128, 8, 16], U32)
        au = sb("p_au", [128, 2, 128], U32); af = sb("p_af", [128, 2, 128])
        isel = sb("p_isel", [128, 2, 128])
        idxf = sb("p_idxf", [128, 128])
        idxu = [sb(f"p_idxu{i}", [128, 128], U32) for i in range(2)]
        gsm = [sb(f"p_g{i}", [128, 128]) for i in range(2)]
        sm = sb("p_sm", [128, 16])
        z = sb("p_z", [128, 128]); gz = sb("p_gz", [128, 128]); av = sb("p_av", [128, 128])
        identb = sb("p_identb", [128, 128], BF16)
        dg = [sb(f"p_dg{i}", [128, 128], BF16) for i in range(4)]
        iota = sb("p_iota", [128, 16])
        mr = sb("p_mr", [128, I[idxname].shape[1]], U32)
        P = Prog(nc, name)
        load_w_bf16(P, Wq, I["peer_w_q"][layer], D, "Wq")
        P.dma("sync", iota[:], I["iota16"], r=[], w=["iota"], chan="iota")
        P.A(lambda e: e.activation(out=identb[:], in_=K.ident[:], func=AF.Copy), r=["ident"], w=["identb"])
        P.dma("sync", mr[:], I[idxname], r=[], w=["mr"], chan="mr")
        for p in range(2):
            P.dma("sync", kraw[:, p, :], I["peer_keys"][layer, p], r=[], w=["kraw"], chan=f"kraw{p}")
            P.T(lambda e, p=p: e.transpose(out=ps[p][:, 0:128], in_=kraw[:, p, :], identity=K.ident[:]), r=["kraw", "ident"], w=[f"ps{p}"])
            P.V(lambda e, p=p: e.tensor_copy(out=keysT[:, p, :], in_=ps[p][:, 0:128]), r=[f"ps{p}"], w=["keysT"])

        P.emit()
        nc.all_engine_barrier()
        cur_v = [None]
        PP = [None]

        def front(n):
            P = PP[0]
            t = tiles[n]
            s = n % 2
            if cur_v[0] != t["v"]:
                cur_v[0] = t["v"]
                v = t["v"]
                prep_GS(K, P, Gt, St, I["norm_ffn"][layer], K.modd[layer, v, 4 * D:5 * D], K.modd[layer, v, 3 * D:4 * D], wk[0], "p")
            if t["src"][0] == "d":
                P.dma("sync", xt[s][:], t["src"][1], r=[], w=["xt"], chan="xt")
            else:
                col = t["src"][2]
                P.op("gpsimd", lambda e, s=s, col=col, src=t["src"][1]: e.indirect_dma_start(out=xt[s][:], out_offset=None, in_=src,
                                                                                         in_offset=bass.IndirectOffsetOnAxis(ap=mr[:, col:col + 1], axis=0)),
                     r=["mr"], w=["xt"], dma=True, chan="xt")
            hh = h[s]
            hk = f"h{s}"
            P.A(lambda e, s=s: e.activation(out=h[s][:], in_=xt[s][:], func=AF.Square, accum_out=ss[:, 0:1]), r=["xt"], w=[hk, "ss"])
            P.V(lambda e: e.tensor_scalar(out=ss[:, 1:2], in0=ss[:, 0:1], scalar1=1.0 / D, scalar2=EPS, op0=ALU.mult, op1=ALU.add), r=["ss"], w=["ss1"])
            P.A(lambda e: e.activation(out=ss[:, 2:3], in_=ss[:, 1:2], func=AF.Sqrt), r=["ss1"], w=["ss2"])
            P.V(lambda e: e.reciprocal(out=ss[:, 3:4], in_=ss[:, 2:3]), r=["ss2"], w=["ss3"])
            P.V(lambda e, s=s: e.scalar_tensor_tensor(out=h[s][:], in0=xt[s][:], scalar=ss[:, 3:4], in1=Gt[:], op0=ALU.mult, op1=ALU.mult), r=["xt", "ss3", "Gp"], w=[hk])
            P.V(lambda e, s=s: e.tensor_tensor(out=h[s][:], in0=h[s][:], in1=St[:], op=ALU.add), r=[hk, "Sp"], w=[hk])
            for q in range(4):
                b = q % 2
                for j in range(4):
                    kc = 4 * q + j
                    P.T(lambda e, kc=kc, j=j, b=b, s=s: e.transpose(out=ps[b][:, j * 128:(j + 1) * 128], in_=h[s][:, kc * 128:(kc + 1) * 128], identity=K.ident[:]),
                        r=[hk, "ident"], w=[f"ps{b}"])
                P.A(lambda e, q=q, b=b: e.activation(out=hT[:, 4 * q:4 * q + 4, :], in_=ps[b][:].rearrange("p (j t) -> p j t", j=4), func=AF.Copy), r=[f"ps{b}"], w=["hT"])
            yield
            qT = wk[0]
            for c in range(16):
                pb = c % 2
                for kc in range(KC):
                    P.T(lambda e, kc=kc, c=c, pb=pb: e.matmul(ps[pb][:, 0:128], lhsT=Wq[:, kc, c * 128:(c + 1) * 128], rhs=hT[:, kc, :], start=(kc == 0), stop=(kc == KC - 1)),
                        r=["Wq", "hT"], w=[f"ps{pb}"])
                P.A(lambda e, c=c, pb=pb: e.activation(out=qT[:, c * 128:(c + 1) * 128], in_=ps[pb][:, 0:128], func=AF.Copy), r=[f"ps{pb}"], w=["wk0"])
                yield
            Ssb = wk[1]
            for q in range(4):
                pb = 2 + q % 2
                for c in range(4 * q, 4 * q + 4):
                    P.T(lambda e, c=c, pb=pb: e.matmul(ps[pb][:, (c % 4) * 128:(c % 4 + 1) * 128], lhsT=qT[:, c * 128:(c + 1) * 128], rhs=keysT[:, c % 2, :], start=True, stop=True),
                        r=["wk0", "keysT"], w=[f"ps{pb}"])
                P.A(lambda e, q=q, pb=pb: e.activation(out=Ssb[:, q * 512:(q + 1) * 512], in_=ps[pb][:], func=AF.Copy), r=[f"ps{pb}"], w=["wk1"])
            S2 = wk[0]
            for c in range(16):
                sv = Ssb[:, c * 128:(c + 1) * 128]
                s2v = S2[:, c * 128:(c + 1) * 128]
                P.V(lambda e, c=c, sv=sv: e.max(out=s16[:, c, 0:8], in_=sv), r=["wk1"], w=["s16"])
                P.V(lambda e, c=c, sv=sv: e.max_index(out=i16[:, c, 0:8], in_max=s16[:, c, 0:8], in_values=sv), r=["wk1", "s16"], w=["i16"])
                P.V(lambda e, c=c, sv=sv, s2v=s2v: e.match_replace(out=s2v, in_to_replace=s16[:, c, 0:8], in_values=sv, imm_value=-1e30), r=["wk1", "s16"], w=["wk0"])
                P.V(lambda e, c=c, s2v=s2v: e.max(out=s16[:, c, 8:16], in_=s2v), r=["wk0"], w=["s16"])
                P.V(lambda e, c=c, s2v=s2v: e.max_index(out=i16[:, c, 8:16], in_max=s16[:, c, 8:16], in_values=s2v), r=["wk0", "s16"], w=["i16"])
                yield
            cand = wk[1]
            c4 = cand[:].rearrange("p (h a b) -> p h a b", h=8, a=16)
            s16r = s16[:].rearrange("p (h t) k -> p h t k", t=2)
            P.V(lambda e: e.tensor_tensor(out=c4, in0=s16r[:, :, 0, :].unsqueeze(3).to_broadcast([128, 8, 16, 16]),
                                          in1=s16r[:, :, 1, :].unsqueeze(2).to_broadcast([128, 8, 16, 16]), op=ALU.add), r=["s16"], w=["wk1"])
            for hd in range(8):
                cv = cand[:, hd * 256:(hd + 1) * 256]
                c2v = S2[:, hd * 256:(hd + 1) * 256]
                P.V(lambda e, hd=hd, cv=cv: e.max(out=ts[:, hd, 0:8], in_=cv), r=["wk1"], w=["ts"])
                P.V(lambda e, hd=hd, cv=cv: e.max_index(out=sel[:, hd, 0:8], in_max=ts[:, hd, 0:8], in_values=cv), r=["wk1", "ts"], w=["sel"])
                P.V(lambda e, hd=hd, cv=cv, c2v=c2v: e.match_replace(out=c2v, in_to_replace=ts[:, hd, 0:8], in_values=cv, imm_value=-1e30), r=["wk1", "ts"], w=["wk0"])
                P.V(lambda e, hd=hd, c2v=c2v: e.max(out=ts[:, hd, 8:16], in_=c2v), r=["wk0"], w=["ts"])
                P.V(lambda e, hd=hd, c2v=c2v: e.max_index(out=sel[:, hd, 8:16], in_max=ts[:, hd, 8:16], in_values=c2v), r=["wk0", "ts"], w=["sel"])
                yield
            selv = sel[:].rearrange("p h k -> p (h k)")
            P.V(lambda e: e.tensor_scalar(out=au[:, 0, :], in0=selv, scalar1=4, scalar2=None, op0=ALU.logical_shift_right), r=["sel"], w=["au"])
            P.V(lambda e: e.tensor_scalar(out=au[:, 1, :], in0=selv, scalar1=15, scalar2=None, op0=ALU.bitwise_and), r=["sel"], w=["au"])
            P.V(lambda e: e.tensor_copy(out=af[:], in_=au[:]), r=["au"], w=["af"])
            P.V(lambda e: e.tensor_copy(out=i16f[:], in_=i16[:]), r=["i16"], w=["i16f"])
            eq = wk[1][:].rearrange("p (h s a) -> p h s a", h=8, s=16)
            i16r = i16f[:].rearrange("p (h t) k -> p h t k", t=2)
            for half in range(2):
                P.V(lambda e, half=half: e.tensor_tensor(out=eq, in0=af[:, half, :].rearrange("p (h s) -> p h s", h=8).unsqueeze(3).to_broadcast([128, 8, 16, 16]),
                                                         in1=iota[:].unsqueeze(1).unsqueeze(1).to_broadcast([128, 8, 16, 16]), op=ALU.is_equal), r=["af", "iota"], w=["wk1"])
                P.V(lambda e, half=half: e.tensor_tensor(out=eq, in0=eq, in1=i16r[:, :, half, :].unsqueeze(2).to_broadcast([128, 8, 16, 16]), op=ALU.mult), r=["wk1", "i16f"], w=["wk1"])
                P.V(lambda e, half=half: e.tensor_reduce(out=isel[:, half, :].rearrange("p (h s) -> p h s", h=8), in_=eq, axis=AX.X, op=ALU.add), r=["wk1"], w=["isel"])
                yield
            P.V(lambda e: e.scalar_tensor_tensor(out=idxf[:], in0=isel[:, 0, :], scalar=128.0, in1=isel[:, 1, :], op0=ALU.mult, op1=ALU.add), r=["isel"], w=["idxf"])
            if layer > 0:
                P.V(lambda e: e.tensor_scalar(out=idxf[:], in0=idxf[:], scalar1=float(layer * 16384), scalar2=None, op0=ALU.add), r=["idxf"], w=["idxf"])
            P.V(lambda e, s=s: e.tensor_copy(out=idxu[s][:], in_=idxf[:]), r=["idxf"], w=[f"idxu{s}"])
            g3 = gsm[s][:].rearrange("p (h k) -> p h k", h=8)
            P.V(lambda e, g3=g3: e.tensor_tensor(out=g3, in0=ts[:], in1=ts[:, :, 0:1].to_broadcast([128, 8, 16]), op=ALU.subtract), r=["ts"], w=[f"g{s}"])
            P.A(lambda e, s=s: e.activation(out=gsm[s][:], in_=gsm[s][:], func=AF.Exp), r=[f"g{s}"], w=[f"g{s}"])
            P.V(lambda e, g3=g3: e.tensor_reduce(out=sm[:, 0:8], in_=g3, axis=AX.X, op=ALU.add), r=[f"g{s}"], w=["sm"])
            P.V(lambda e: e.reciprocal(out=sm[:, 8:16], in_=sm[:, 0:8]), r=["sm"], w=["sm"])
            P.V(lambda e, g3=g3: e.tensor_tensor(out=g3, in0=g3, in1=sm[:, 8:16].unsqueeze(2).to_broadcast([128, 8, 16]), op=ALU.mult), r=[f"g{s}", "sm"], w=[f"g{s}"])

        gcount = [0]
        gate_v = [None]

        def gather(s, k):
            P = PP[0]
            slot = gcount[0] % NB
            gcount[0] += 1
            P.op("gpsimd", lambda e, slot=slot, s=s, k=k: e.indirect_dma_start(out=ring[slot][:], out_offset=None, in_=uvtab,
                                                                        in_offset=bass.IndirectOffsetOnAxis(ap=idxu[s][:, k:k + 1], axis=0)),
                 r=[f"idxu{s}"], w=[f"ring{slot}"], dma=True, chan=f"ring{slot}")
            return slot

        def back(n, nxt=None):
            P = PP[0]
            t = tiles[n]
            s = n % 2
            POOL_EVERY = 0
            LAGP = 2

            def consume(k, slot, via_pool):
                if via_pool:
                    P.G(lambda e, slot=slot, s=s: e.tensor_tensor(out=ring[slot][:, 0:D], in0=ring[slot][:, 0:D], in1=h[s][:], op=ALU.mult), r=[f"ring{slot}", f"h{s}"], w=[f"ring{slot}"])
                    P.A(lambda e, slot=slot, k=k: e.activation(out=ring[slot][:, 0:D], in_=ring[slot][:, 0:D], func=AF.Copy, accum_out=z[:, k:k + 1]), r=[f"ring{slot}"], w=[f"ring{slot}", f"z{k}"])
                else:
                    P.V(lambda e, slot=slot, s=s, k=k: e.scalar_tensor_tensor(out=junk[:], in0=ring[slot][:, 0:D], scalar=1.0, in1=h[s][:], op0=ALU.mult, op1=ALU.mult, accum_out=z[:, k:k + 1]),
                        r=[f"ring{slot}", f"h{s}"], w=[f"z{k}"])
                P.A(lambda e, k=k: e.activation(out=gz[:, k:k + 1], in_=z[:, k:k + 1], func=AF.Gelu_apprx_tanh), r=[f"z{k}"], w=[f"gz{k}"])
                P.A(lambda e, k=k, s=s: e.activation(out=av[:, k:k + 1], in_=gz[:, k:k + 1], func=AF.Copy, scale=gsm[s][:, k:k + 1]), r=[f"gz{k}", f"g{s}"], w=[f"a{k}"])
                P.A(lambda e, k=k: e.activation(out=dg[k % 4][:], in_=identb[:], func=AF.Copy, scale=av[:, k:k + 1]), r=[f"a{k}", "identb"], w=[f"dg{k % 4}"])
                first = not started[0]
                started[0] = True
                last = ndone[0] == 127
                ndone[0] += 1
                for nb in range(4):
                    P.T(lambda e, k=k, nb=nb, slot=slot, first=first, last=last: e.matmul(ps[4 + nb][:], lhsT=dg[k % 4][:], rhs=ring[slot][:, D + nb * 512:D + (nb + 1) * 512], start=first, stop=last),
                        r=[f"dg{k % 4}", f"ring{slot}"], w=[f"ps{4 + nb}"])

            started = [False]
            ndone = [0]
            pend = []
            for k in range(128):
                slot = gather(s, k)
                if POOL_EVERY and k % POOL_EVERY == POOL_EVERY - 1:
                    pend.append((k, slot))
                else:
                    consume(k, slot, False)
                while pend and pend[0][0] <= k - LAGP:
                    kk, sl_ = pend.pop(0)
                    consume(kk, sl_, True)
                if nxt is not None and k >= 8 and k % 2 == 0:
                    next(nxt, None)
            for kk, sl_ in pend:
                consume(kk, sl_, True)
            if nxt is not None:
                for _ in nxt:
                    pass
            if gate_v[0] != t["v"]:
                gate_v[0] = t["v"]
                load_bcast(P, "sync", gate[:], K.modd[layer, t["v"], 5 * D:6 * D], "gate", "gate")
            xr = wk[0]
            if t["src"][0] == "d":
                P.dma("sync", xr[:], t["src"][1], r=[], w=["wk0"], chan="xre")
            else:
                col = t["src"][2]
                P.op("gpsimd", lambda e, col=col, src=t["src"][1]: e.indirect_dma_start(out=xr[:], out_offset=None, in_=src,
                                                                                 in_offset=bass.IndirectOffsetOnAxis(ap=mr[:, col:col + 1], axis=0)),
                     r=["mr"], w=["wk0"], dma=True, chan="xre")
            for nb in range(4):
                P.V(lambda e, nb=nb: e.tensor_tensor(out=acc[:, nb * 512:(nb + 1) * 512], in0=ps[4 + nb][:], in1=gate[:, nb * 512:(nb + 1) * 512], op=ALU.mult), r=[f"ps{4 + nb}", "gate"], w=["wk1"])
            P.V(lambda e: e.tensor_tensor(out=acc[:], in0=acc[:], in1=xr[:], op=ALU.add), r=["wk1", "wk0"], w=["wk1"])
            if final:
                P.A(lambda e: e.activation(out=junk[:], in_=acc[:], func=AF.Square, accum_out=ssb[:, 0:1]), r=["wk1"], w=["junkf", "ssb"])
                P.V(lambda e: e.tensor_scalar(out=ssb[:, 1:2], in0=ssb[:, 0:1], scalar1=1.0 / D, scalar2=EPS, op0=ALU.mult, op1=ALU.add), r=["ssb"], w=["ssb1"])
                P.A(lambda e: e.activation(out=ssb[:, 2:3], in_=ssb[:, 1:2], func=AF.Sqrt), r=["ssb1"], w=["ssb2"])
                P.V(lambda e: e.reciprocal(out=ssb[:, 3:4], in_=ssb[:, 2:3]), r=["ssb2"], w=["ssb3"])
                load_bcast(P, "sync", xr[:], I["norm_final"], "wk0", "gfin")
                P.V(lambda e: e.scalar_tensor_tensor(out=acc[:], in0=acc[:], scalar=ssb[:, 3:4], in1=xr[:], op0=ALU.mult, op1=ALU.mult), r=["wk1", "ssb3", "wk0"], w=["wk1"])
            P.dma("sync", t["dst"], acc[:], r=["wk1"], w=[], chan="st")

        ngrp = -(-len(tiles) // 12)
        GSZ = -(-len(tiles) // ngrp)
        for g0 in range(0, len(tiles), GSZ):
            PP[0] = Prog(nc, f"{name}g{g0}")
            g1 = min(g0 + GSZ, len(tiles))
            for _ in front(g0):
                pass
            for n in range(g0, g1):
                back(n, front(n + 1) if n + 1 < g1 else None)
            PP[0].emit()
            K.peer_stats = PP[0].stats
            nc.all_engine_barrier()


def phase_rec(K):
    nc, I, ps = K.nc, K.I, K.ps
    NTOK = NT * 128
    for pas in ("u", "g"):
        with ExitStack() as es:
            sb = lambda n, s, d=F32: es.enter_context(nc.sbuf_tensor(n, s, d))
            W = sb("r_W" + pas, [128, KC, D], BF16)
            Gt = sb("r_G" + pas, [128, D]); St = sb("r_S" + pas, [128, D])
            ss = sb("r_ss" + pas, [128, 4])
            xt = sb("r_xt" + pas, [128, D]); h = sb("r_h" + pas, [128, D])
            hT = sb("r_hT" + pas, [128, KC, 256], BF16)
            ost = sb("r_ost" + pas, [128, 16, 256])
            P = Prog(nc, "rp" + pas)
            tc = K.tc[1]
            tc.alloc(es, 12)
            tc.begin()
            load_w_bf16(P, W, I["rec_w_in"], D, "W", col0=(D if pas == "u" else 0))
            for blk in (range(NT // 2) if pas == "u" else range(1, NT // 2)):
                is_ctx = blk == 0
                if blk in (0, 1):
                    v = 1 if is_ctx else 0
                    prep_GS(K, P, Gt, St, I["norm_mix"][1], K.modd[1, v, D:2 * D], K.modd[1, v, 0:D], h, "a")
                for ti in range(2):
                    e_t = blk * 2 + ti
                    norm_mod_T(K, P, K.xs2tile(e_t), xt, h, ss, Gt, St, "a", hT, "hT", ti * 128, (6, 7), 0)
                for j in range(16):
                    pb = j % 2
                    for kc in range(KC):
                        P.T(lambda e, kc=kc, j=j, pb=pb: e.matmul(ps[pb][:, 0:256], lhsT=W[:, kc, j * 128:(j + 1) * 128], rhs=hT[:, kc, :], start=(kc == 0), stop=(kc == KC - 1)),
                            r=["W", "hT"], w=[f"ps{pb}"])
                    fn = AF.Copy if pas == "u" else AF.Gelu_apprx_tanh
                    P.A(lambda e, j=j, pb=pb, fn=fn: e.activation(out=ost[:, j, :], in_=ps[pb][:, 0:256], func=fn), r=[f"ps{pb}"], w=["ost"])
                if pas == "u":
                    P.dma("sync", K.upre[:, :, blk * 256:(blk + 1) * 256].rearrange("c p t -> p c t"), ost[:], r=["ost"], w=["upre"], chan="ost")
                else:
                    l0 = (blk - 1) * 256
                    P.dma("sync", K.ggd[:, :, l0:l0 + 256].rearrange("c p t -> p c t"), ost[:], r=["ost"], w=["ggd"], chan="ost")
                tc.pump(P, 8)
            if pas == "g":
                tc.finish(P)
            else:
                tc.flush(P)
            P.emit()
        nc.all_engine_barrier()
    with ExitStack() as es:
        sb = lambda n, s, d=F32: es.enter_context(nc.sbuf_tensor(n, s, d))
        UPW = 4360
        up = sb("r_up", [128, 2, UPW])
        u = sb("r_u", [128, 2, NTOK]); ub = sb("r_ub", [128, 2, NTOK], BF16)
        A = sb("r_A", [128, NTOK]); Bt = sb("r_B", [128, NTOK])
        Y = [sb(f"r_Y{d}", [128, NTOK]) for d in range(2)]
        gg = sb("r_gg", [128, S]); ygb = sb("r_ygb", [128, S], BF16)
        wg = sb("r_wg", [128, 2, 2, 2, 256], BF16)
        cw = sb("r_cw", [128, 4, 16]); cb = sb("r_cb", [128, 16])
        ba = sb("r_ba", [128, 2, 16]); bx = sb("r_bx", [128, 2, 16]); lam = sb("r_lam", [128, 2, 16]); cl = sb("r_cl", [128, 2, 16])
        tmp = [[sb(f"r_t{i}{s}", [128, 512]) for i in range(4)] for s in range(2)]
        nba = sb("r_nba", [128, 2, 16]); nbx = sb("r_nbx", [128, 2, 16])
        P = Prog(nc, "rscan")
        P.G(lambda e: e.memset(up[:], 0.0), r=[], w=["up0", "up1"])
        for k in range(4):
            P.dma("sync", cw[:, k, :], I["rec_conv_w"][k].rearrange("(c p) -> p c", p=128), r=[], w=["cw"], chan=f"cw{k}", allow_slow_non_contiguous=True)
        P.dma("sync", cb[:], I["rec_conv_b"].rearrange("(c p) -> p c", p=128), r=[], w=["cb"], chan="cb", allow_slow_non_contiguous=True)
        for d in range(2):
            P.dma("sync", ba[:, d, :], I["rec_b_a"][d].rearrange("(c p) -> p c", p=128), r=[], w=["ba"], chan=f"ba{d}", allow_slow_non_contiguous=True)
            P.dma("sync", bx[:, d, :], I["rec_b_x"][d].rearrange("(c p) -> p c", p=128), r=[], w=["bx"], chan=f"bx{d}", allow_slow_non_contiguous=True)
            P.dma("sync", lam[:, d, :], I["rec_lambda"][d].rearrange("(c p) -> p c", p=128), r=[], w=["lam"], chan=f"lam{d}", allow_slow_non_contiguous=True)
        P.A(lambda e: e.activation(out=cl[:], in_=lam[:], func=AF.Exp, scale=-1.0), r=["lam"], w=["cl"])
        P.A(lambda e: e.activation(out=cl[:], in_=cl[:], func=AF.Ln, bias=1.0), r=["cl"], w=["cl"])
        P.V(lambda e: e.tensor_scalar(out=cl[:], in0=cl[:], scalar1=-8.0, scalar2=None, op0=ALU.mult), r=["cl"], w=["cl"])
        P.V(lambda e: e.tensor_scalar(out=nba[:], in0=ba[:], scalar1=-1.0, scalar2=None, op0=ALU.mult), r=["ba"], w=["nba"])
        P.V(lambda e: e.tensor_scalar(out=nbx[:], in0=bx[:], scalar1=-1.0, scalar2=None, op0=ALU.mult), r=["bx"], w=["nbx"])
        for n in range(8):
            for c in range(2):
                ch = 2 * n + c
                P.dma("sync", up[:, c, 1:257], K.upre[ch, :, 0:256], r=[], w=[f"up{c}"], chan=f"upc{c}")
                P.dma("sync", up[:, c, 260:260 + S], K.upre[ch, :, 256:NTOK], r=[], w=[f"up{c}"], chan=f"upl{c}")
                for (o0, nn, i0) in ((0, 256, 0), (256, S, 259)):
                    useg = u[:, c, o0:o0 + nn]
                    P.V(lambda e, c=c, ch=ch, useg=useg, i0=i0, nn=nn: e.tensor_scalar(out=useg, in0=up[:, c, i0:i0 + nn], scalar1=cw[:, 0, ch:ch + 1], scalar2=cb[:, ch:ch + 1], op0=ALU.mult, op1=ALU.add),
                        r=[f"up{c}", "cw", "cb"], w=[f"u{c}"])
                    for k in range(1, 4):
                        P.V(lambda e, c=c, ch=ch, useg=useg, i0=i0, nn=nn, k=k: e.scalar_tensor_tensor(out=useg, in0=up[:, c, i0 + k:i0 + k + nn], scalar=cw[:, k, ch:ch + 1], in1=useg, op0=ALU.mult, op1=ALU.add),
                            r=[f"up{c}", "cw", f"u{c}"], w=[f"u{c}"])
                P.A(lambda e, c=c: e.activation(out=ub[:, c, :], in_=u[:, c, :], func=AF.Copy), r=[f"u{c}"], w=["ub"])
            for d in range(2):
                for ty in range(2):
                    wsrc = (I["rec_w_a"] if ty == 0 else I["rec_w_x"])[d, n].rearrange("(k p) o -> p k o", p=128)
                    P.dma("gpsimd", wg[:, d, ty, :, :], wsrc, r=[], w=["wg"], chan=f"wg{d}{ty}")
            for oc in range(2):
                ch = 2 * n + oc
                for d in range(2):
                    for tb in range(9):
                        t0 = tb * 512
                        tn = min(512, NTOK - t0)
                        sl = tb % 2
                        pa, px = 2 * sl, 2 * sl + 1
                        r_, i_, a2, sq = tmp[sl]
                        for ty, pb in ((0, pa), (1, px)):
                            for kc in range(2):
                                P.T(lambda e, ty=ty, pb=pb, kc=kc, d=d, oc=oc, t0=t0, tn=tn: e.matmul(ps[pb][:, 0:tn], lhsT=wg[:, d, ty, kc, oc * 128:(oc + 1) * 128], rhs=ub[:, kc, t0:t0 + tn], start=(kc == 0), stop=(kc == 1)),
                                    r=["wg", "ub"], w=[f"ps{pb}"])
                        P.A(lambda e, pa=pa, tn=tn, d=d, ch=ch, r_=r_: e.activation(out=r_[:, 0:tn], in_=ps[pa][:, 0:tn], func=AF.Exp, scale=-1.0, bias=nba[:, d, ch:ch + 1]), r=[f"ps{pa}", "nba"], w=[f"t0{sl}"])
                        P.A(lambda e, px=px, tn=tn, d=d, ch=ch, i_=i_: e.activation(out=i_[:, 0:tn], in_=ps[px][:, 0:tn], func=AF.Exp, scale=-1.0, bias=nbx[:, d, ch:ch + 1]), r=[f"ps{px}", "nbx"], w=[f"t1{sl}"])
                        P.A(lambda e, tn=tn, r_=r_: e.activation(out=r_[:, 0:tn], in_=r_[:, 0:tn], func=AF.Ln, bias=1.0), r=[f"t0{sl}"], w=[f"t0{sl}"])
                        P.A(lambda e, tn=tn, r_=r_: e.activation(out=r_[:, 0:tn], in_=r_[:, 0:tn], func=AF.Exp, scale=-1.0), r=[f"t0{sl}"], w=[f"t0{sl}"])
                        P.A(lambda e, tn=tn, i_=i_: e.activation(out=i_[:, 0:tn], in_=i_[:, 0:tn], func=AF.Ln, bias=1.0), r=[f"t1{sl}"], w=[f"t1{sl}"])
                        P.A(lambda e, tn=tn, i_=i_: e.activation(out=i_[:, 0:tn], in_=i_[:, 0:tn], func=AF.Exp, scale=-1.0), r=[f"t1{sl}"], w=[f"t1{sl}"])
                        P.A(lambda e, tn=tn, t0=t0, d=d, ch=ch, r_=r_: e.activation(out=A[:, t0:t0 + tn], in_=r_[:, 0:tn], func=AF.Exp, scale=cl[:, d, ch:ch + 1]), r=[f"t0{sl}", "cl"], w=["A"])
                        P.G(lambda e, tn=tn, t0=t0, a2=a2: e.tensor_tensor(out=a2[:, 0:tn], in0=A[:, t0:t0 + tn], in1=A[:, t0:t0 + tn], op=ALU.mult), r=["A"], w=[f"t2{sl}"])
                        P.A(lambda e, tn=tn, a2=a2, sq=sq: e.activation(out=sq[:, 0:tn], in_=a2[:, 0:tn], func=AF.Ln, scale=-1.0, bias=1.0), r=[f"t2{sl}"], w=[f"t3{sl}"])
                        P.A(lambda e, tn=tn, sq=sq: e.activation(out=sq[:, 0:tn], in_=sq[:, 0:tn], func=AF.Exp, scale=0.5), r=[f"t3{sl}"], w=[f"t3{sl}"])
                        P.V(lambda e, tn=tn, sq=sq, i_=i_: e.tensor_tensor(out=sq[:, 0:tn], in0=sq[:, 0:tn], in1=i_[:, 0:tn], op=ALU.mult), r=[f"t3{sl}", f"t1{sl}"], w=[f"t3{sl}"])
                        P.V(lambda e, tn=tn, t0=t0, sq=sq, oc=oc: e.tensor_tensor(out=Bt[:, t0:t0 + tn], in0=sq[:, 0:tn], in1=u[:, oc, t0:t0 + tn], op=ALU.mult), r=[f"t3{sl}", f"u{oc}"], w=["B"])
                    Yd = Y[d]
                    if d == 0:
                        P.V(lambda e, Yd=Yd: e.tensor_tensor_scan(out=Yd[:, 0:256], data0=A[:, 0:256], data1=Bt[:, 0:256], initial=0.0, op0=ALU.mult, op1=ALU.add), r=["A", "B"], w=[f"Y{d}c"])
                        P.V(lambda e, Yd=Yd: e.tensor_tensor_scan(out=Yd[:, 256:NTOK], data0=A[:, 256:NTOK], data1=Bt[:, 256:NTOK], initial=Yd[:, 255:256], op0=ALU.mult, op1=ALU.add), r=["A", "B", f"Y{d}c"], w=[f"Y{d}l"])
                    else:
                        P.V(lambda e, Yd=Yd: e.tensor_tensor_scan(out=Yd[:, 0:256][:, ::-1], data0=A[:, 0:256][:, ::-1], data1=Bt[:, 0:256][:, ::-1], initial=0.0, op0=ALU.mult, op1=ALU.add), r=["A", "B"], w=[f"Y{d}c"])
                        P.V(lambda e, Yd=Yd: e.tensor_tensor_scan(out=Yd[:, 256:NTOK][:, ::-1], data0=A[:, 256:NTOK][:, ::-1], data1=Bt[:, 256:NTOK][:, ::-1], initial=Yd[:, 0:1], op0=ALU.mult, op1=ALU.add), r=["A", "B", f"Y{d}c"], w=[f"Y{d}l"])
                P.dma("sync", gg[:], K.ggd[ch], r=[], w=["gg"], chan="gg")
                P.G(lambda e: e.tensor_tensor(out=Y[0][:, 256:NTOK], in0=Y[0][:, 256:NTOK], in1=Y[1][:, 256:NTOK], op=ALU.add), r=["Y0l", "Y1l"], w=["Y0l"])
                P.V(lambda e: e.tensor_tensor(out=ygb[:], in0=Y[0][:, 256:NTOK], in1=gg[:], op=ALU.mult), r=["Y0l", "gg"], w=["ygb"])
                P.dma("sync", K.ygd[:, ch, :], ygb[:], r=["ygb"], w=["ygd"], chan="ygb")
        P.emit()
    nc.all_engine_barrier()
    with ExitStack() as es:
        sb = lambda n, s, d=F32: es.enter_context(nc.sbuf_tensor(n, s, d))
        Wout = sb("r_Wout", [128, KC, D], BF16)
        gate = sb("r_gate", [128, D])
        yt = [sb(f"r_yt{i}", [128, KC, 128], BF16) for i in range(2)]
        xr = [sb(f"r_xr{i}", [128, D]) for i in range(2)]
        y = sb("r_y", [128, D])
        P = Prog(nc, "rout")
        load_w_bf16(P, Wout, I["rec_w_out"], D, "Wo")
        load_bcast(P, "sync", gate[:], K.modd[1, 0, 2 * D:3 * D], "gate", "gate")
        for t in range(S // 128):
            s = t % 2
            P.dma("sync", yt[s][:], K.ygd[:, :, t * 128:(t + 1) * 128], r=[], w=[f"yt{s}"], chan=f"yt{s}")
            P.dma("sync", xr[s][:], K.xs2tile(t + 2), r=[], w=[f"xr{s}"], chan=f"xr{s}")
            for nb in range(4):
                pb = 4 + nb
                for kc in range(KC):
                    P.T(lambda e, kc=kc, nb=nb, pb=pb, s=s: e.matmul(ps[pb][:], lhsT=yt[s][:, kc, :], rhs=Wout[:, kc, nb * 512:(nb + 1) * 512], start=(kc == 0), stop=(kc == KC - 1)),
                        r=[f"yt{s}", "Wo"], w=[f"ps{pb}"])
                P.V(lambda e, nb=nb, pb=pb: e.tensor_tensor(out=y[:, nb * 512:(nb + 1) * 512], in0=ps[pb][:], in1=gate[:, nb * 512:(nb + 1) * 512], op=ALU.mult), r=[f"ps{pb}", "gate"], w=[f"y{nb}"])
                P.G(lambda e, nb=nb, s=s: e.tensor_tensor(out=y[:, nb * 512:(nb + 1) * 512], in0=y[:, nb * 512:(nb + 1) * 512], in1=xr[s][:, nb * 512:(nb + 1) * 512], op=ALU.add), r=[f"y{nb}", f"xr{s}"], w=[f"y{nb}"])
            P.dma("sync", K.xs3[t * 128:(t + 1) * 128, :], y[:], r=[f"y{nb}" for nb in range(4)], w=["xs3"], chan="y")
        P.emit()
```
